# Optimizing a Trainium2 kernel written in Bass

```python
import jax, jax.numpy as jnp
from jax import lax
import numpy as np

D_MODEL = 1024
BATCH = 4
SEQ = 8192
DEPTH = 1

N_HEADS = 8
N_KV_HEADS = 2
HEAD_DIM = 64
ATTN_WIDTH = N_HEADS * HEAD_DIM
CONV_CH = D_MODEL - ATTN_WIDTH
N_IDX_HEADS = 8
IDX_DIM = 64
TOPK_MAX = 256
CONV_K = 31
D_FF = 4 * D_MODEL
ROPE_THETA = 500000.0
Q_BLOCK = 128
EPS = 1e-6
SPLITS = (ATTN_WIDTH, N_KV_HEADS * HEAD_DIM, N_KV_HEADS * HEAD_DIM,
          N_IDX_HEADS * IDX_DIM, IDX_DIM, N_IDX_HEADS, 2 * CONV_CH)
D_IN = ATTN_WIDTH + 4 * N_KV_HEADS * HEAD_DIM // 2 + N_IDX_HEADS * IDX_DIM + IDX_DIM + N_IDX_HEADS + 2 * CONV_CH

kernel_name = "hybrid_dsa_conformer_adaln_layer"


def _rmsnorm(x, g):
    xf = x.astype(jnp.float32)
    y = xf * lax.rsqrt(jnp.mean(xf * xf, axis=-1, keepdims=True) + EPS)
    return (y * g.astype(jnp.float32)).astype(x.dtype)


def _layernorm(x, g, b):
    xf = x.astype(jnp.float32)
    mu = jnp.mean(xf, axis=-1, keepdims=True)
    var = jnp.mean(jnp.square(xf - mu), axis=-1, keepdims=True)
    y = (xf - mu) * lax.rsqrt(var + EPS)
    return (y * g.astype(jnp.float32) + b.astype(jnp.float32)).astype(x.dtype)


def _partial_rope(x, positions):
    d = x.shape[-1]
    rot = d // 4
    half = rot // 2
    inv_freq = jnp.power(ROPE_THETA, -jnp.arange(half, dtype=jnp.float32) * 2.0 / rot)
    ang = positions.astype(jnp.float32)[:, :, None] * inv_freq
    cos = jnp.cos(ang)[:, :, None, :]
    sin = jnp.sin(ang)[:, :, None, :]
    xf = x.astype(jnp.float32)
    x1, x2, rest = xf[..., :half], xf[..., half:rot], xf[..., rot:]
    out = jnp.concatenate([x1 * cos - x2 * sin, x2 * cos + x1 * sin, rest], axis=-1)
    return out.astype(x.dtype)


def _dsa_attention(q, k, v, q_idx, k_idx, w_idx):
    B, S = q.shape[0], q.shape[1]
    nb = S // Q_BLOCK
    topk = min(TOPK_MAX, S // 4)
    groups = N_HEADS // N_KV_HEADS
    key_pos = jnp.arange(S, dtype=jnp.int32)

    def to_blocks(a):
        a = a.reshape((B, nb, Q_BLOCK) + a.shape[2:])
        return jnp.moveaxis(a, 1, 0)

    xs = (to_blocks(q), to_blocks(q_idx), to_blocks(w_idx),
          key_pos.reshape(nb, Q_BLOCK))

    def block_fn(blk):
        qb, qib, wb, tb = blk
        logits = jnp.einsum('bthd,bsd->bths', qib.astype(jnp.float32),
                            k_idx.astype(jnp.float32)) * (IDX_DIM ** -0.5)
        score = jnp.einsum('bth,bths->bts', wb.astype(jnp.float32), jax.nn.relu(logits))
        causal = key_pos[None, :] <= tb[:, None]
        score = jnp.where(causal[None], score, -jnp.inf)
        _, sel = lax.top_k(score, topk)
        sel_ok = sel <= tb[None, :, None]
        k_sel = jax.vmap(lambda kb, ib: kb[ib])(k, sel)
        v_sel = jax.vmap(lambda vb, ib: vb[ib])(v, sel)
        qg = qb.reshape(B, Q_BLOCK, N_KV_HEADS, groups, HEAD_DIM)
        s = jnp.einsum('btkgd,btnkd->btkgn', qg, k_sel).astype(jnp.float32) * (HEAD_DIM ** -0.5)
        s = jnp.where(sel_ok[:, :, None, None, :], s, -jnp.inf)
        p = jax.nn.softmax(s, axis=-1).astype(v.dtype)
        o = jnp.einsum('btkgn,btnkd->btkgd', p, v_sel)
        return o.reshape(B, Q_BLOCK, ATTN_WIDTH)

    out = lax.map(block_fn, xs)
    return jnp.moveaxis(out, 0, 1).reshape(B, S, ATTN_WIDTH)


def _conformer_conv(u, conv_w, conv_b, norm_g, norm_b):
    a, g = jnp.split(u, 2, axis=-1)
    glu = a * jax.nn.sigmoid(g)
    y = lax.conv_general_dilated(
        glu, conv_w[:, None, :], window_strides=(1,),
        padding=[(CONV_K - 1, 0)],
        dimension_numbers=('NWC', 'WIO', 'NWC'),
        feature_group_count=CONV_CH) + conv_b
    y = _layernorm(y, norm_g, norm_b)
    return jax.nn.silu(y)


def setup_inputs(seed: int = 0) -> dict:
    key = jax.random.key(seed)
    ks = jax.random.split(key, 20)
    f32 = jnp.float32
    nrm = lambda k, shp, s: jax.random.normal(k, shp, f32) * s
    x = jax.random.normal(ks[0], (BATCH, SEQ, D_MODEL), f32)
    c = jax.random.normal(ks[1], (BATCH, D_MODEL), f32)
    offset = jax.random.randint(ks[2], (BATCH, 1), 0, 1024, dtype=jnp.int32)
    positions = offset + jnp.arange(SEQ, dtype=jnp.int32)[None, :]
    return {
        "x": x,
        "c": c,
        "positions": positions,
        "w_ada": nrm(ks[3], (DEPTH, D_MODEL, 6 * D_MODEL), 0.5 * D_MODEL ** -0.5),
        "b_ada": nrm(ks[4], (DEPTH, 6 * D_MODEL), 0.01),
        "g_mix": 1.0 + nrm(ks[5], (DEPTH, D_MODEL), 0.02),
        "w_in": nrm(ks[6], (DEPTH, D_MODEL, D_IN), D_MODEL ** -0.5),
        "conv_w": nrm(ks[7], (DEPTH, CONV_K, CONV_CH), CONV_K ** -0.5),
        "conv_b": nrm(ks[8], (DEPTH, CONV_CH), 0.01),
        "conv_norm_g": 1.0 + nrm(ks[9], (DEPTH, CONV_CH), 0.02),
        "conv_norm_b": nrm(ks[10], (DEPTH, CONV_CH), 0.01),
        "w_out": nrm(ks[11], (DEPTH, D_MODEL, D_MODEL), D_MODEL ** -0.5),
        "g_mlp": 1.0 + nrm(ks[12], (DEPTH, D_MODEL), 0.02),
        "w_up": nrm(ks[13], (DEPTH, D_MODEL, D_FF), D_MODEL ** -0.5),
        "w_down": nrm(ks[14], (DEPTH, D_FF, D_MODEL), D_FF ** -0.5),
        "g_final": 1.0 + nrm(ks[15], (D_MODEL,), 0.02),
    }


def reference(x, c, positions, w_ada, b_ada, g_mix, w_in, conv_w, conv_b,
              conv_norm_g, conv_norm_b, w_out, g_mlp, w_up, w_down, g_final):
    B, S, _ = x.shape
    split_at = [int(i) for i in np.cumsum(SPLITS)[:-1]]
    cond = jax.nn.silu(c)
    for i in range(DEPTH):
        mod = jnp.einsum('bd,de->be', cond, w_ada[i]) + b_ada[i]
        sh1, sc1, gt1, sh2, sc2, gt2 = jnp.split(mod, 6, axis=-1)

        h = _rmsnorm(x, g_mix[i]) * (1.0 + sc1[:, None, :]) + sh1[:, None, :]
        proj = jnp.einsum('bsd,de->bse', h, w_in[i])
        q, k, v, qi, ki, wi, cu = jnp.split(proj, split_at, axis=-1)
        q = _partial_rope(q.reshape(B, S, N_HEADS, HEAD_DIM), positions)
        k = _partial_rope(k.reshape(B, S, N_KV_HEADS, HEAD_DIM), positions)
        v = v.reshape(B, S, N_KV_HEADS, HEAD_DIM)
        qi = _partial_rope(qi.reshape(B, S, N_IDX_HEADS, IDX_DIM), positions)
        ki = _partial_rope(ki.reshape(B, S, 1, IDX_DIM), positions)[:, :, 0, :]
        wi = wi * (N_IDX_HEADS ** -0.5)
        attn_out = _dsa_attention(q, k, v, qi, ki, wi)
        conv_out = _conformer_conv(cu, conv_w[i], conv_b[i],
                                   conv_norm_g[i], conv_norm_b[i])
        mixed = jnp.concatenate([attn_out, conv_out], axis=-1)
        x = x + gt1[:, None, :] * jnp.einsum('bse,ed->bsd', mixed, w_out[i])

        h = _rmsnorm(x, g_mlp[i]) * (1.0 + sc2[:, None, :]) + sh2[:, None, :]
        u = jnp.square(jax.nn.relu(jnp.einsum('bsd,df->bsf', h, w_up[i])))
        x = x + gt2[:, None, :] * jnp.einsum('bsf,fd->bsd', u, w_down[i])
    return _rmsnorm(x, g_final)
```

```python
import numpy as np
import concourse.bass as bass
import concourse.mybir as mybir
from concourse.bass_utils import run_bass_kernel_spmd

F32 = mybir.dt.float32
BF16 = mybir.dt.bfloat16
I32 = mybir.dt.int32
U8 = mybir.dt.uint8
ALU = mybir.AluOpType
AF = mybir.ActivationFunctionType
AX = mybir.AxisListType

D = 1024
S = 8192
NT = 66
NEG = -30000.0
EPS = 1e-6
NBIS = 10
BR = 6.0
JA = 4608
OWN = ([0, 3, 4, 7, 8, 11, 12, 15], [1, 2, 5, 6, 9, 10, 13, 14])
DEBUG = False


class T:
    __slots__ = ("w", "r")

    def __init__(self):
        self.w = {}
        self.r = {}


class Eng:
    def __init__(self, obj, sem, key):
        self.obj = obj
        self.sem = sem
        self.key = key
        self.cnt = 0
        self.seen = {}


class K:
    def __init__(self, nc, sems):
        self.nc = nc
        it = iter(sems)
        self.pe = Eng(nc.tensor, next(it), "pe")
        self.act = Eng(nc.scalar, next(it), "act")
        self.dve = Eng(nc.vector, next(it), "dve")
        self.pool = Eng(nc.gpsimd, next(it), "pool")
        self.sp = Eng(nc.sync, next(it), "sp")
        self.engs = [self.pe, self.act, self.dve, self.pool, self.sp]
        self.dsems = {"sp": [[s, 0] for s in [next(it) for _ in range(8)]],
                      "pool": [[s, 0] for s in [next(it) for _ in range(8)]]}
        self.dptr = {"sp": 0, "pool": 0}

    def _waits(self, eng, rd, wr):
        need = {}

        def add(d, skip_self):
            for k, (s, v) in d.items():
                if skip_self and k == eng.key:
                    continue
                if k not in need or need[k][1] < v:
                    need[k] = (s, v)
        for t in rd:
            add(t.w, False)
        skip = (eng.key == "pe")
        for t in wr:
            add(t.w, skip)
            add(t.r, skip)
        for k, (s, v) in need.items():
            if eng.seen.get(k, 0) < v:
                eng.obj.wait_ge(s, v)
                eng.seen[k] = v

    def op(self, eng, fn, rd=(), wr=()):
        self._waits(eng, rd, wr)
        inst = fn(eng.obj)
        eng.cnt += 1
        inst.then_inc(eng.sem, 1)
        tok = (eng.sem, eng.cnt)
        for t in rd:
            t.r[eng.key] = tok
        for t in wr:
            t.w = {eng.key: tok}
            t.r = {}

    def dma(self, eng, out, in_, rd=(), wr=()):
        ring = self.dsems[eng.key]
        i = self.dptr[eng.key]
        self.dptr[eng.key] = (i + 1) % len(ring)
        sem, val = ring[i]
        key = "d%s%d" % (eng.key, i)
        self._waits(eng, rd, wr)
        if val > 0 and eng.seen.get(key, 0) < val:
            eng.obj.wait_ge(sem, val)
            eng.seen[key] = val
        eng.obj.dma_start(out=out, in_=in_).then_inc(sem, 16)
        ring[i][1] = val + 16
        tok = (sem, val + 16)
        for t in rd:
            t.r[key] = tok
        for t in wr:
            t.w = {key: tok}
            t.r = {}

    def barrier(self):
        for e in self.engs:
            for f in self.engs:
                if f is not e and f.cnt > 0 and e.seen.get(f.key, 0) < f.cnt:
                    e.obj.wait_ge(f.sem, f.cnt)
                    e.seen[f.key] = f.cnt
            for qk, ring in self.dsems.items():
                for i, (s, v) in enumerate(ring):
                    key = "d%s%d" % (qk, i)
                    if v > 0 and e.seen.get(key, 0) < v:
                        e.obj.wait_ge(s, v)
                        e.seen[key] = v


class _Stop(Exception):
    pass


def build_program(stage=None, dumps=(), nchunks=8, nt1=64):
    nc = bass.Bass("TRN2", target_bir_lowering=False)
    dt = nc.dram_tensor
    xp = dt("xp", [NT * 128, D], F32, kind="ExternalInput").ap()
    posp = dt("posp", [128, NT], I32, kind="ExternalInput").ap()
    oflag = dt("oflag", [128, 8], F32, kind="ExternalInput").ap()
    hmask = dt("hmask", [128, 256], F32, kind="ExternalInput").ap()
    invf = dt("invf", [128, 8], F32, kind="ExternalInput").ap()
    cT = dt("cT", [128, 8], F32, kind="ExternalInput").ap()
    w_ada = dt("w_ada", [D, 6 * D], F32, kind="ExternalInput").ap()
    badac = dt("badac", [128, 48], F32, kind="ExternalInput").ap()
    badar = dt("badar", [1, 6 * D], F32, kind="ExternalInput").ap()
    gmixc = dt("gmixc", [128, 8], F32, kind="ExternalInput").ap()
    gmlpc = dt("gmlpc", [128, 8], F32, kind="ExternalInput").ap()
    w_in = dt("w_in", [D, 2376], F32, kind="ExternalInput").ap()
    convw = dt("convw", [128, 4, 31], F32, kind="ExternalInput").ap()
    convb = dt("convb", [128, 4], F32, kind="ExternalInput").ap()
    cng = dt("cng", [128, 4], F32, kind="ExternalInput").ap()
    cnb = dt("cnb", [128, 4], F32, kind="ExternalInput").ap()
    w_out = dt("w_out", [D, D], F32, kind="ExternalInput").ap()
    w_up = dt("w_up", [D, 4 * D], F32, kind="ExternalInput").ap()
    w_down = dt("w_down", [4 * D, D], F32, kind="ExternalInput").ap()
    gfb = dt("gfb", [128, D], F32, kind="ExternalInput").ap()
    out = dt("out", [4096, D], F32, kind="ExternalOutput").ap()
    x1s = dt("x1s", [4096, D], F32).ap()
    g1s = dt("g1s", [128, D], F32).ap()
    g2s = dt("g2s", [128, D], F32).ap()
    if "x1s" in dumps:
        x1s = dt("dbg_x1s", [4096, D], F32, kind="ExternalOutput").ap()

    def wview(w, c0, c1):
        return w[:, c0:c1].rearrange("(k p) e -> p k e", p=128)

    import contextlib
    dump_aps = {}

    def dump(name, ap, tiles, kbref):
        if name not in dumps:
            return
        shp = [int(v) for v in ap.shape]
        d_ap = dt("dbg_" + name, shp, ap.dtype, kind="ExternalOutput").ap()
        kbref.dma(kbref.sp, d_ap, ap, rd=tiles)

    def stop_if(st, kbref):
        if stage == st:
            kbref.barrier()
            raise _Stop()

    def _body():
        with contextlib.ExitStack() as es:
            sems = [es.enter_context(nc.semaphore("s%d" % i)) for i in range(21)]
            kb = K(nc, sems)
            pe, act, dve, pool, sp = kb.pe, kb.act, kb.dve, kb.pool, kb.sp
            op, dma = kb.op, kb.dma

            def sb(name, shape, dtype=F32):
                return es2.enter_context(nc.sbuf_tensor(name, shape, dtype))

            ps = es.enter_context(nc.psum_tensor("ps", [128, 8, 512], F32))
            PB = [T() for _ in range(8)]

            def psb16(b):
                return ps[:, b, :].bitcast(BF16)

            es2 = es
            ident = sb("ident", [128, 128], BF16); t_ident = T()
            ident4 = sb("ident4", [128, 4, 128], BF16)
            identf = sb("identf", [128, 128], F32)
            trim = sb("trim", [128, 128], F32)
            onesm = sb("onesm", [128, 128], BF16)
            onesr = sb("onesr", [1, 128], F32)
            cosT = sb("cosT", [128, NT, 8], F32)
            sinT = sb("sinT", [128, NT, 8], F32); t_cs = T()
            modc = sb("modc", [128, 48], F32); t_modc = T()
            ab = sb("ab", [128, 4, 8], F32); t_ab = T()
            t_G1 = T(); t_G2 = T()
            oflg = sb("oflg", [128, 8], F32); t_small = T()
            cw = sb("cw", [128, 4, 31], F32)
            cb = sb("cb", [128, 4], F32)
            cg = sb("cg", [128, 4], F32)
            cbn = sb("cbn", [128, 4], F32)
            wst = {"slots": None, "tiles": None, "ptr": 0}

            def walloc(tag):
                wst["slots"] = [sb("wslot%s%d" % (tag, i), [128, 8, 512], BF16) for i in range(2)]
                wst["tiles"] = [T() for _ in range(2)]
                wst["ptr"] = 0

            def wload(src_ap):
                i = wst["ptr"]
                wst["ptr"] = (i + 1) % 2
                dma(pool, wst["slots"][i][:], src_ap, wr=[wst["tiles"][i]])
                return wst["slots"][i], wst["tiles"][i]

            op(pool, lambda e: e.memset(identf[:], 0.0), wr=[t_ident])
            op(pool, lambda e: e.affine_select(out=identf[:], in_=identf[:], pattern=[[-1, 128]],
                                               compare_op=ALU.not_equal, fill=1.0, base=0,
                                               channel_multiplier=1), rd=[t_ident], wr=[t_ident])
            op(pool, lambda e: e.tensor_copy(out=ident[:], in_=identf[:]), rd=[t_ident], wr=[t_ident])
            op(pool, lambda e: e.tensor_copy(out=ident4[:], in_=identf[:].unsqueeze(1).to_broadcast([128, 4, 128])),
               rd=[t_ident], wr=[t_ident])
            op(pool, lambda e: e.memset(trim[:], 0.0), wr=[t_ident])
            op(pool, lambda e: e.affine_select(out=trim[:], in_=trim[:], pattern=[[-1, 128]],
                                               compare_op=ALU.is_ge, fill=NEG, base=0,
                                               channel_multiplier=1), rd=[t_ident], wr=[t_ident])
            op(pool, lambda e: e.memset(onesm[:], 1.0 / 512.0), wr=[t_ident])
            op(pool, lambda e: e.memset(onesr[:], 1.0), wr=[t_ident])
            dma(sp, oflg[:], oflag, wr=[t_small])
            dma(sp, cw[:], convw, wr=[t_small])
            dma(sp, cb[:], convb, wr=[t_small])
            dma(sp, cg[:], cng, wr=[t_small])
            dma(sp, cbn[:], cnb, wr=[t_small])

            with contextlib.ExitStack() as es2:
                posi = sb("posi", [128, NT], I32)
                posf = sb("posf", [128, NT], F32)
                ivf = sb("ivf", [128, 8], F32)
                ang = sb("ang", [128, NT, 8], F32)
                tq = sb("tq", [128, NT, 8], F32)
                kq = sb("kq", [128, NT, 8], I32)
                kf = sb("kf", [128, NT, 8], F32)
                red = sb("red", [128, NT, 8], F32)
                t_p0 = T()
                cTs = sb("cTs", [128, 8], F32)
                cond = sb("cond", [128, 8], BF16); t_cond = T()
                badc = sb("badc", [128, 48], F32)
                gmc = sb("gmc", [128, 2, 8], F32)
                rowb = sb("rowb", [1, 2048], F32)
                rows = sb("rows", [1, 512], F32); t_rows = T()
                Gtmp = sb("Gtmp", [128, 512], F32); t_Gtmp = T()
                walloc("a")
                dma(sp, posi[:], posp, wr=[t_p0])
                dma(sp, ivf[:], invf, wr=[t_p0])
                dma(sp, cTs[:], cT, wr=[t_cond])
                dma(sp, badc[:], badac, wr=[t_cond])
                dma(sp, gmc[:, 0, :], gmixc, wr=[t_cond])
                dma(sp, gmc[:, 1, :], gmlpc, wr=[t_cond])
                dma(sp, rowb[:, 0:1024], badar[:, 2048:3072], wr=[t_cond])
                dma(sp, rowb[:, 1024:2048], badar[:, 5120:6144], wr=[t_cond])
                rw = dict(rd=[t_p0], wr=[t_p0])
                op(dve, lambda e: e.tensor_copy(out=posf[:], in_=posi[:]), **rw)
                op(dve, lambda e: e.tensor_tensor(out=ang[:], in0=posf[:].unsqueeze(2).to_broadcast([128, NT, 8]),
                                                  in1=ivf[:].unsqueeze(1).to_broadcast([128, NT, 8]), op=ALU.mult), **rw)
                TWO_PI = 2.0 * np.pi
                C1 = 6.28125
                C2 = TWO_PI - C1

                def reduce_to(dst, shift):
                    op(dve, lambda e: e.tensor_scalar(out=tq[:], in0=ang[:], scalar1=shift, scalar2=1.0 / TWO_PI,
                                                      op0=ALU.add, op1=ALU.mult), **rw)
                    op(dve, lambda e: e.tensor_copy(out=kq[:], in_=tq[:]), **rw)
                    op(dve, lambda e: e.tensor_copy(out=kf[:], in_=kq[:]), **rw)
                    op(dve, lambda e: e.scalar_tensor_tensor(out=red[:], in0=kf[:], scalar=-C1, in1=ang[:],
                                                             op0=ALU.mult, op1=ALU.add), **rw)
                    op(dve, lambda e: e.scalar_tensor_tensor(out=red[:], in0=kf[:], scalar=-C2, in1=red[:],
                                                             op0=ALU.mult, op1=ALU.add), **rw)
                    op(dve, lambda e: e.tensor_scalar(out=red[:], in0=red[:], scalar1=shift, scalar2=None,
                                                      op0=ALU.add), **rw)
                    op(dve, lambda e: e.tensor_scalar(out=tq[:], in0=red[:], scalar1=np.pi, scalar2=-TWO_PI,
                                                      op0=ALU.is_gt, op1=ALU.mult), **rw)
                    op(dve, lambda e: e.tensor_tensor(out=red[:], in0=red[:], in1=tq[:], op=ALU.add), **rw)
                    op(dve, lambda e: e.tensor_scalar(out=tq[:], in0=red[:], scalar1=-np.pi, scalar2=TWO_PI,
                                                      op0=ALU.is_lt, op1=ALU.mult), **rw)
                    op(dve, lambda e: e.tensor_tensor(out=red[:], in0=red[:], in1=tq[:], op=ALU.add), **rw)
                    op(dve, lambda e: e.tensor_scalar(out=red[:], in0=red[:], scalar1=-3.1415925, scalar2=3.1415925,
                                                      op0=ALU.max, op1=ALU.min), **rw)
                    op(act, lambda e: e.activation(out=dst[:], in_=red[:], func=AF.Sin), rd=[t_p0], wr=[t_cs])

                reduce_to(sinT, 0.0)
                reduce_to(cosT, np.pi / 2.0)

                op(act, lambda e: e.activation(out=cond[:], in_=cTs[:], func=AF.Silu), rd=[t_cond], wr=[t_cond])
                for cc in range(12):
                    ws, tw = wload(wview(w_ada, cc * 512, (cc + 1) * 512))
                    if cc in (4, 5, 10, 11):
                        for k in range(8):
                            op(pe, lambda e, k=k: e.matmul(ps[0:1, 1, :], lhsT=cond[:, k:k + 1], rhs=ws[:, k, :],
                                                           start=(k == 0), stop=(k == 7)),
                               rd=[t_cond, tw], wr=[PB[1]])
                        ro = (cc - 4) * 512 if cc < 6 else 1024 + (cc - 10) * 512
                        op(dve, lambda e, ro=ro: e.tensor_tensor(out=rows[:], in0=ps[0:1, 1, :], in1=rowb[:, ro:ro + 512],
                                                                 op=ALU.add), rd=[PB[1], t_cond], wr=[t_rows])
                        op(pe, lambda e: e.matmul(ps[:, 2, :], lhsT=onesr[:], rhs=rows[:], start=True, stop=True),
                           rd=[t_rows, t_ident], wr=[PB[2]])
                        Gs, tG = (g1s, t_G1) if cc < 6 else (g2s, t_G2)
                        go = (cc - 4) * 512 if cc < 6 else (cc - 10) * 512
                        op(act, lambda e: e.activation(out=Gtmp[:], in_=ps[:, 2, :], func=AF.Copy),
                           rd=[PB[2]], wr=[t_Gtmp])
                        dma(sp, Gs[:, go:go + 512], Gtmp[:], rd=[t_Gtmp], wr=[tG])
                    else:
                        for el in range(4):
                            et = cc * 4 + el
                            for k in range(8):
                                op(pe, lambda e, k=k, el=el, et=et: e.matmul(
                                    ps[:, 0, et:et + 1], lhsT=ws[:, k, el * 128:(el + 1) * 128], rhs=cond[:, k:k + 1],
                                    start=(k == 0), stop=(k == 7), skip_group_check=True),
                                   rd=[t_cond, tw], wr=[PB[0]])
                op(dve, lambda e: e.memset(modc[:], 0.0), wr=[t_modc])
                for lo_, hi_ in ((0, 16), (24, 40)):
                    op(dve, lambda e: e.tensor_tensor(out=modc[:, lo_:hi_], in0=ps[:, 0, lo_:hi_], in1=badc[:, lo_:hi_], op=ALU.add),
                       rd=[PB[0], t_cond], wr=[t_modc])
                op(dve, lambda e: e.scalar_tensor_tensor(out=ab[:, 0, :], in0=modc[:, 8:16], scalar=1.0, in1=gmc[:, 0, :],
                                                         op0=ALU.add, op1=ALU.mult), rd=[t_modc, t_cond], wr=[t_ab])
                op(dve, lambda e: e.tensor_copy(out=ab[:, 1, :], in_=modc[:, 0:8]), rd=[t_modc], wr=[t_ab])
                op(dve, lambda e: e.scalar_tensor_tensor(out=ab[:, 2, :], in0=modc[:, 32:40], scalar=1.0, in1=gmc[:, 1, :],
                                                         op0=ALU.add, op1=ALU.mult), rd=[t_modc, t_cond], wr=[t_ab])
                op(dve, lambda e: e.tensor_copy(out=ab[:, 3, :], in_=modc[:, 24:32]), rd=[t_modc], wr=[t_ab])
                dump("cosT", cosT[:], [t_cs], kb)
                dump("sinT", sinT[:], [t_cs], kb)
                dump("ab", ab[:], [t_ab], kb)
                dump("modc", modc[:], [t_modc], kb)
                kb.barrier()
                stop_if("p0", kb)

            def norm_tile(row0, xt, t_xt, xn, t_xn, sq, t_sq, st, t_st, src=None):
                dma(sp, xt[:], (xp if src is None else src)[row0:row0 + 128, :], wr=[t_xt])
                op(act, lambda e: e.activation(out=xn[:], in_=xt[:], func=AF.Square, accum_out=st[:, 0:1]),
                   rd=[t_xt], wr=[t_xn, t_st])
                op(act, lambda e: e.activation(out=st[:, 1:2], in_=st[:, 0:1], func=AF.Sqrt, bias=EPS, scale=1.0 / D),
                   rd=[t_st], wr=[t_st])
                op(dve, lambda e: e.reciprocal(out=st[:, 2:3], in_=st[:, 1:2]), rd=[t_st], wr=[t_st])
                op(act, lambda e: e.activation(out=xn[:], in_=xt[:], func=AF.Copy, scale=st[:, 2:3]),
                   rd=[t_xt, t_st], wr=[t_xn])

            def transpose_mod(xn, t_xn, bank, hT_dst, t_hT, abi):
                pv = psb16(bank)
                for k in range(8):
                    op(pe, lambda e, k=k: e.transpose(out=pv[:, k * 128:(k + 1) * 128], in_=xn[:, k * 128:(k + 1) * 128],
                                                      identity=ident[:]), rd=[t_xn, t_ident], wr=[PB[bank]])
                pv3 = pv.rearrange("p (k t) -> p k t", k=8)
                op(dve, lambda e: e.tensor_tensor(out=hT_dst, in0=pv3,
                                                  in1=ab[:, abi, :].unsqueeze(2).to_broadcast([128, 8, 128]), op=ALU.mult),
                   rd=[PB[bank], t_ab], wr=[t_hT])
                op(pool, lambda e: e.tensor_tensor(out=hT_dst, in0=hT_dst,
                                                   in1=ab[:, abi + 1, :].unsqueeze(2).to_broadcast([128, 8, 128]), op=ALU.add),
                   rd=[t_hT, t_ab], wr=[t_hT])

            with contextlib.ExitStack() as es2:
                kT = sb("kT", [128, S], BF16); t_kT = T()
                kiT = sb("kiT", [128, S], BF16); t_kiT = T()
                Vaug = sb("Vaug", [128, 64, 2, 65], BF16); t_V = T()
                W1 = sb("W1", [128, 8, 328], BF16); t_W1 = T()
                xts = [sb("xt%d" % i, [128, D], F32) for i in range(2)]; t_xts = [T(), T()]
                xns = [sb("xn%d" % i, [128, D], BF16) for i in range(2)]; t_xns = [T(), T()]
                sqj = None; t_sqj = None
                walloc("b")
                G1 = sb("G1", [128, D], F32)
                dma(sp, G1[:], g1s, rd=[t_G1], wr=[t_G1])
                sts = [sb("st%d" % i, [128, 4], F32) for i in range(2)]; t_sts = [T(), T()]
                hTc = sb("hTc", [128, 8, 512], BF16); t_hTc = [T() for _ in range(4)]
                rtmp = sb("rtmp", [128, 4, 16, 8], F32); t_rtmp = T()
                krot = [sb("krot%d" % i, [128, 256], BF16) for i in range(2)]; t_krot = [T(), T()]

                op(pool, lambda e: e.memset(Vaug[:], 1.0), wr=[t_V])
                dma(pool, W1[:, :, 0:128], wview(w_in, 512, 640), wr=[t_W1])
                dma(pool, W1[:, :, 128:192], wview(w_in, 1280, 1344), wr=[t_W1])
                dma(pool, W1[:, :, 192:320], wview(w_in, 640, 768), wr=[t_W1])
                dma(pool, W1[:, :, 320:328], wview(w_in, 1344, 1352), wr=[t_W1])

                def rope(src3, dst3, nh, ti, tsrc, tdst):
                    cs = cosT[:, ti, :].unsqueeze(1).to_broadcast([128, nh, 8])
                    sn = sinT[:, ti, :].unsqueeze(1).to_broadcast([128, nh, 8])
                    x1, x2 = src3[:, :, 0:8], src3[:, :, 8:16]
                    t1, t2, t3, t4 = (rtmp[:, i, 0:nh, :] for i in range(4))
                    op(dve, lambda e: e.tensor_tensor(out=t1, in0=x1, in1=cs, op=ALU.mult), rd=[tsrc, t_cs], wr=[t_rtmp])
                    op(dve, lambda e: e.tensor_tensor(out=t2, in0=x2, in1=sn, op=ALU.mult), rd=[tsrc, t_cs], wr=[t_rtmp])
                    op(dve, lambda e: e.tensor_tensor(out=t3, in0=x2, in1=cs, op=ALU.mult), rd=[tsrc, t_cs], wr=[t_rtmp])
                    op(dve, lambda e: e.tensor_tensor(out=t4, in0=x1, in1=sn, op=ALU.mult), rd=[tsrc, t_cs], wr=[t_rtmp])
                    op(dve, lambda e: e.tensor_tensor(out=dst3[:, :, 0:8], in0=t1, in1=t2, op=ALU.subtract),
                       rd=[t_rtmp], wr=[tdst])
                    op(dve, lambda e: e.tensor_tensor(out=dst3[:, :, 8:16], in0=t3, in1=t4, op=ALU.add),
                       rd=[t_rtmp], wr=[tdst])
                    op(act, lambda e: e.activation(out=dst3[:, :, 16:64], in_=src3[:, :, 16:64], func=AF.Copy),
                       rd=[tsrc], wr=[tdst])

                def rope4(src4, dst4, ti, tsrc, tdst):
                    cs = cosT[:, ti, :].unsqueeze(1).unsqueeze(1).to_broadcast([128, 2, 4, 8])
                    sn = sinT[:, ti, :].unsqueeze(1).unsqueeze(1).to_broadcast([128, 2, 4, 8])
                    x1, x2 = src4[:, :, :, 0:8], src4[:, :, :, 8:16]
                    t1, t2, t3, t4 = (rtmp[:, i, 0:8, :].rearrange("p (g b) d -> p g b d", g=2) for i in range(4))
                    op(dve, lambda e: e.tensor_tensor(out=t1, in0=x1, in1=cs, op=ALU.mult), rd=[tsrc, t_cs], wr=[t_rtmp])
                    op(dve, lambda e: e.tensor_tensor(out=t2, in0=x2, in1=sn, op=ALU.mult), rd=[tsrc, t_cs], wr=[t_rtmp])
                    op(dve, lambda e: e.tensor_tensor(out=t3, in0=x2, in1=cs, op=ALU.mult), rd=[tsrc, t_cs], wr=[t_rtmp])
                    op(dve, lambda e: e.tensor_tensor(out=t4, in0=x1, in1=sn, op=ALU.mult), rd=[tsrc, t_cs], wr=[t_rtmp])
                    op(dve, lambda e: e.tensor_tensor(out=dst4[:, :, :, 0:8], in0=t1, in1=t2, op=ALU.subtract),
                       rd=[t_rtmp], wr=[tdst])
                    op(dve, lambda e: e.tensor_tensor(out=dst4[:, :, :, 8:16], in0=t3, in1=t4, op=ALU.add),
                       rd=[t_rtmp], wr=[tdst])
                    op(act, lambda e: e.activation(out=dst4[:, :, :, 16:64], in_=src4[:, :, :, 16:64], func=AF.Copy),
                       rd=[tsrc], wr=[tdst])

                def ph1_S1(ti):
                    s2 = ti % 2
                    norm_tile(ti * 128, xts[s2], t_xts[s2], xns[s2], t_xns[s2], sqj, t_sqj, sts[s2], t_sts[s2])
                    hs = ti % 4
                    hdst = hTc[:, :, hs * 128:(hs + 1) * 128]
                    transpose_mod(xns[s2], t_xns[s2], 6 + s2, hdst, t_hTc[hs], 0)

                def ph1_S2(ti):
                    s2 = ti % 2
                    hs = ti % 4
                    bk = s2
                    for k in range(8):
                        op(pe, lambda e, k=k: e.matmul(ps[:, bk, 0:320], lhsT=hTc[:, k, hs * 128:(hs + 1) * 128],
                                                       rhs=W1[:, k, 0:320], start=(k == 0), stop=(k == 7)),
                           rd=[t_hTc[hs], t_W1], wr=[PB[bk]])
                    kr = krot[s2]
                    rope(ps[:, bk, 0:192].rearrange("p (h d) -> p h d", d=64),
                         kr[:, 0:192].rearrange("p (h d) -> p h d", d=64), 3, ti, PB[bk], t_krot[s2])
                    op(pool, lambda e: e.tensor_copy(out=kr[:, 192:256], in_=kr[:, 128:192]), rd=[t_krot[s2]], wr=[t_krot[s2]])
                    op(act, lambda e: e.activation(out=Vaug[:, ti, :, 0:64],
                                                   in_=ps[:, bk, 192:320].rearrange("p (g d) -> p g d", d=64), func=AF.Copy),
                       rd=[PB[bk]], wr=[t_V])
                    tb = 4 + s2
                    pv = psb16(tb)
                    op(pe, lambda e: e.transpose(out=pv[:, 0:128], in_=kr[:, 0:128], identity=ident[:]),
                       rd=[t_krot[s2], t_ident], wr=[PB[tb]])
                    op(pe, lambda e: e.transpose(out=pv[:, 128:256], in_=kr[:, 128:256], identity=ident[:]),
                       rd=[t_krot[s2], t_ident], wr=[PB[tb]])
                    op(act, lambda e: e.activation(out=kT[:, ti * 128:(ti + 1) * 128], in_=pv[:, 0:128], func=AF.Copy),
                       rd=[PB[tb]], wr=[t_kT])
                    op(dve, lambda e: e.tensor_copy(out=kiT[:, ti * 128:(ti + 1) * 128], in_=pv[:, 128:256]),
                       rd=[PB[tb]], wr=[t_kiT])


                ph1_S1(0)
                for ti in range(nt1):
                    if ti + 1 < nt1:
                        ph1_S1(ti + 1)
                    ph1_S2(ti)
                dump("kT", kT[:], [t_kT], kb)
                dump("kiT", kiT[:], [t_kiT], kb)
                dump("Vaug", Vaug[:], [t_V], kb)
                stop_if("p1", kb)
                SC = sb("SC", [128, S], F32); t_SC = T()
                junk = hTc[:].rearrange("p k t -> p (k t)").bitcast(U8)
                RbA = sb("RbA", [128, 8, 512], BF16)
                Rb = [RbA[:, i, :] for i in range(8)]; t_Rb = [T() for _ in range(8)]
                Dg = sb("Dg", [128, 8, 128], BF16); t_Dg = T()
                qT = sb("qT", [128, 2, 4, 512], BF16); t_qT = T()
                qiT = sb("qiT", [128, 4, 2, 512], BF16); t_qiT = T()
                op(pool, lambda e: e.memset(qT[:], 0.0), wr=[t_qT])
                op(pool, lambda e: e.memset(qiT[:], 0.0), wr=[t_qiT])
                qrot = [sb("qrot%d" % i, [128, 512], BF16) for i in range(2)]; t_qrot = [T(), T()]
                wsc = sb("wsc", [128, 4, 8], F32); t_wsc = T()
                PT = [sb("PT%d" % i, [128, 512], BF16) for i in range(4)]; t_PT = [T() for _ in range(4)]
                MB = sb("MB", [128, S], BF16); t_MB = T()
                junkA = sb("junkA", [128, JA], U8); t_junkA = T()
                bsa = sb("bsa", [128, 2], F32); t_bsa = T()
                bst = sb("bst", [128, 8], F32); t_bst = T()
                gluT = sb("gluT", [128, 4, 544], BF16); t_glu = T()
                gluH = sb("gluH", [128, 4, 256], BF16); t_gluH = T()
                SCb = SC[:].bitcast(BF16)
                ybf = SCb[:, 0:2048].rearrange("p (c t) -> p c t", c=4); t_ybf = t_SC
                ysq = SCb[:, 2048:4096].rearrange("p (c t) -> p c t", c=4); t_ysq = t_SC
                lnA = SC[:, 2048:2560]; t_lnA = t_SC
                lnB = SC[:, 2560:3072]; t_lnB = t_SC
                zn = SC[:, 3072:3584]; t_zn = t_SC
                sig = SC[:, 3584:4096]; t_sig = t_SC
                cdiag = RbA[:].rearrange("p a b -> p (a b)")[:, 0:3968].rearrange("p (k c) -> p k c", c=128)
                mixT = sb("mixT", [128, 8, 512], BF16); t_mixT = T()
                attn = sb("attn", [128, 512], BF16); t_attn = T()
                rs4 = sb("rs4", [128, 8], F32); t_rs4 = T()
                hm = sb("hm", [128, 256], F32)
                x1t = SC[:, 4096:5120]; t_x1t = t_SC
                hmB = sb("hmB", [128, 256], BF16)
                dma(sp, hm[:], hmask, wr=[t_small])
                op(pool, lambda e: e.tensor_copy(out=hmB[:], in_=hm[:]), rd=[t_small], wr=[t_small])

                def conv_glu(ws_a, tw_a, ws_g, tw_g, ncols, ct, dst, tdst, hcols, t_h):
                    for k in range(8):
                        op(pe, lambda e, k=k: e.matmul(ps[:, 0, 0:ncols], lhsT=ws_a[:, k, ct * 128:(ct + 1) * 128],
                                                       rhs=hTc[:, k, hcols], start=(k == 0), stop=(k == 7)),
                           rd=t_h + [tw_a], wr=[PB[0]])
                    for k in range(8):
                        op(pe, lambda e, k=k: e.matmul(ps[:, 1, 0:ncols], lhsT=ws_g[:, k, ct * 128:(ct + 1) * 128],
                                                       rhs=hTc[:, k, hcols], start=(k == 0), stop=(k == 7)),
                           rd=t_h + [tw_g], wr=[PB[1]])
                    op(act, lambda e: e.activation(out=sig[:, 0:ncols], in_=ps[:, 1, 0:ncols], func=AF.Sigmoid),
                       rd=[PB[1]], wr=[t_sig])
                    op(dve, lambda e: e.tensor_tensor(out=dst, in0=ps[:, 0, 0:ncols], in1=sig[:, 0:ncols], op=ALU.mult),
                       rd=[PB[0], t_sig], wr=[tdst])

                for hi in range(2):
                    norm_tile((64 + hi) * 128, xts[hi], t_xts[hi], xns[hi], t_xns[hi], sqj, t_sqj, sts[hi], t_sts[hi])
                    transpose_mod(xns[hi], t_xns[hi], 6 + hi, hTc[:, :, hi * 128:(hi + 1) * 128], t_hTc[hi], 0)
                wa, twa = wload(wview(w_in, 1352, 1864))
                wg, twg = wload(wview(w_in, 1864, 2376))
                for ct in range(4):
                    conv_glu(wa, twa, wg, twg, 256, ct, gluH[:, ct, :], t_gluH, slice(0, 256), [t_hTc[0], t_hTc[1]])
                    op(pool, lambda e, ct=ct: e.tensor_tensor(out=gluH[:, ct, :], in0=gluH[:, ct, :], in1=hmB[:], op=ALU.mult),
                       rd=[t_gluH, t_small], wr=[t_gluH])

                dump("gluH", gluH[:], [t_gluH], kb)
                stop_if("p2h", kb)
                for j in range(nchunks):
                    tile0 = (2 * j + 1) * 4
                    for t4 in range(4):
                        s2 = t4 % 2
                        norm_tile((tile0 + t4) * 128, xts[s2], t_xts[s2], xns[s2], t_xns[s2], sqj, t_sqj, sts[s2], t_sts[s2])
                        transpose_mod(xns[s2], t_xns[s2], 6 + s2, hTc[:, :, t4 * 128:(t4 + 1) * 128], t_hTc[t4], 0)
                    stop_if("p2n", kb)
                    for grp in range(2):
                        c0 = 0 if grp == 0 else 768
                        wq, twq = wload(wview(w_in, c0, c0 + 512))
                        stop_if("p2w", kb)
                        dstT, t_dstT = (qT, t_qT) if grp == 0 else (qiT, t_qiT)
                        for t4 in range(4):
                            bk = t4 % 2
                            for k in range(8):
                                op(pe, lambda e, k=k: e.matmul(ps[:, bk, :], lhsT=hTc[:, k, t4 * 128:(t4 + 1) * 128],
                                                               rhs=wq[:, k, :], start=(k == 0), stop=(k == 7)),
                                   rd=[t_hTc[t4], twq], wr=[PB[bk]])
                            if grp == 0 and t4 == 0:
                                stop_if("p2m", kb)
                            qr = qrot[bk]
                            src4 = ps[:, bk, :].rearrange("p (g b d) -> p g b d", g=2, b=4)
                            dst4 = qr[:].rearrange("p (b g d) -> p g b d", g=2, b=4)
                            rope4(src4, dst4, tile0 + t4, PB[bk], t_qrot[bk])
                            if grp == 0 and t4 == 0:
                                dump("qr", qr[:], [t_qrot[bk]], kb)
                                stop_if("p2a0", kb)
                            tb = 4 + bk
                            pv = psb16(tb)
                            for b in range(4):
                                op(pe, lambda e, b=b: e.transpose(out=pv[:, b * 128:(b + 1) * 128],
                                                                  in_=qr[:, b * 128:(b + 1) * 128], identity=ident[:]),
                                   rd=[t_qrot[bk], t_ident], wr=[PB[tb]])
                            pv4 = pv[:, 0:512].rearrange("p (b t) -> p b t", b=4)
                            tcols = slice(t4 * 128, (t4 + 1) * 128)
                            if grp == 0:
                                d0, d1 = qT[0:64, 0, :, tcols], qT[64:128, 1, :, tcols]
                            else:
                                d0, d1 = qiT[0:64, :, 0, tcols], qiT[64:128, :, 1, tcols]
                            op(act, lambda e: e.activation(out=d0, in_=pv4[0:64], func=AF.Copy), rd=[PB[tb]], wr=[t_dstT])
                            op(dve, lambda e: e.tensor_copy(out=d1, in_=pv4[64:128]), rd=[PB[tb]], wr=[t_dstT])
                            if grp == 0 and t4 == 0:
                                stop_if("p2a1", kb)
                            if grp == 1 and t4 == 0:
                                stop_if("p2a2", kb)
                            if grp == 1:
                                for k in range(8):
                                    op(pe, lambda e, k=k: e.matmul(ps[:, 2, 0:8], lhsT=hTc[:, k, t4 * 128:(t4 + 1) * 128],
                                                                   rhs=W1[:, k, 320:328], start=(k == 0), stop=(k == 7)),
                                       rd=[t_hTc[t4], t_W1], wr=[PB[2]])
                                op(dve, lambda e: e.tensor_scalar(
                                    out=wsc[:, t4, :].rearrange("p (b g) -> p g b", g=2),
                                    in0=ps[:, 2, 0:8].rearrange("p (g b) -> p g b", g=2),
                                    scalar1=float(8 ** -0.5 * 64 ** -0.5), scalar2=None, op0=ALU.mult),
                                   rd=[PB[2]], wr=[t_wsc])
                    if j == nchunks - 1:
                        dump("qT", qT[:].rearrange("p g b t -> p (g b) t"), [t_qT], kb)
                        dump("qiT", qiT[:].rearrange("p b g t -> p (b g) t"), [t_qiT], kb)
                        dump("wsc", wsc[:], [t_wsc], kb)
                        stop_if("p2a", kb)
                    wa, twa = wload(wview(w_in, 1352, 1864))
                    wg, twg = wload(wview(w_in, 1864, 2376))
                    for ct in range(4):
                        op(pool, lambda e, ct=ct: e.tensor_copy(out=gluT[:, ct, 0:32], in_=gluH[:, ct, j * 32:(j + 1) * 32]),
                           rd=[t_gluH], wr=[t_glu])
                        conv_glu(wa, twa, wg, twg, 512, ct, gluT[:, ct, 32:544], t_glu, slice(0, 512), t_hTc)
                    for ct in range(4):
                        op(pool, lambda e, ct=ct: e.tensor_tensor(
                            out=cdiag, in0=ident[:].unsqueeze(1).to_broadcast([128, 31, 128]),
                            in1=cw[:, ct, :].unsqueeze(2).to_broadcast([128, 31, 128]), op=ALU.mult),
                           rd=[t_ident, t_small], wr=t_Rb)
                        cbk = ct
                        for tap in range(31):
                            op(pe, lambda e, tap=tap, ct=ct: e.matmul(ps[:, cbk, :], lhsT=cdiag[:, tap, :],
                                                                     rhs=gluT[:, ct, tap + 2:tap + 514],
                                                                     start=(tap == 0), stop=(tap == 30)),
                               rd=t_Rb + [t_glu], wr=[PB[cbk]])
                        op(act, lambda e, ct=ct: e.activation(out=ybf[:, ct, :], in_=ps[:, cbk, :], func=AF.Identity,
                                                              bias=cb[:, ct:ct + 1], scale=1.0), rd=[PB[cbk], t_small], wr=[t_ybf])
                        op(act, lambda e, ct=ct: e.activation(out=ysq[:, ct, :], in_=ps[:, cbk, :], func=AF.Square,
                                                              bias=cb[:, ct:ct + 1], scale=1.0), rd=[PB[cbk], t_small], wr=[t_ysq])
                    for ct in range(4):
                        op(pe, lambda e, ct=ct: e.matmul(ps[:, 4, :], lhsT=onesm[:], rhs=ybf[:, ct, :],
                                                         start=(ct == 0), stop=(ct == 3)), rd=[t_ybf, t_ident], wr=[PB[4]])
                    for ct in range(4):
                        op(pe, lambda e, ct=ct: e.matmul(ps[:, 5, :], lhsT=onesm[:], rhs=ysq[:, ct, :],
                                                         start=(ct == 0), stop=(ct == 3)), rd=[t_ysq, t_ident], wr=[PB[5]])
                    op(act, lambda e: e.activation(out=lnA, in_=ps[:, 4, :], func=AF.Copy), rd=[PB[4]], wr=[t_lnA])
                    op(dve, lambda e: e.tensor_tensor(out=lnB, in0=lnA, in1=lnA, op=ALU.mult), rd=[t_lnA], wr=[t_lnB])
                    op(dve, lambda e: e.tensor_tensor(out=lnB, in0=ps[:, 5, :], in1=lnB, op=ALU.subtract),
                       rd=[PB[5], t_lnB], wr=[t_lnB])
                    op(dve, lambda e: e.tensor_scalar(out=lnB, in0=lnB, scalar1=0.0, scalar2=EPS, op0=ALU.max, op1=ALU.add),
                       rd=[t_lnB], wr=[t_lnB])
                    op(act, lambda e: e.activation(out=lnB, in_=lnB, func=AF.Sqrt), rd=[t_lnB], wr=[t_lnB])
                    op(dve, lambda e: e.reciprocal(out=lnB, in_=lnB), rd=[t_lnB], wr=[t_lnB])
                    for ct in range(4):
                        op(dve, lambda e, ct=ct: e.scalar_tensor_tensor(out=zn, in0=ps[:, ct, :], scalar=cb[:, ct:ct + 1],
                                                                        in1=lnA, op0=ALU.add, op1=ALU.subtract),
                           rd=[PB[ct], t_small, t_lnA], wr=[t_zn])
                        op(dve, lambda e: e.tensor_tensor(out=zn, in0=zn, in1=lnB, op=ALU.mult),
                           rd=[t_zn, t_lnB], wr=[t_zn])
                        op(act, lambda e, ct=ct: e.activation(out=mixT[:, 4 + ct, :], in_=zn, func=AF.Silu,
                                                              bias=cbn[:, ct:ct + 1], scale=cg[:, ct:ct + 1]),
                           rd=[t_zn, t_small], wr=[t_mixT])

                    if j == nchunks - 1:
                        dump("mixTc", mixT[:, 4:8, :], [t_mixT], kb)
                        stop_if("p2b", kb)
                    def qgeom(qi):
                        segs = [(c * 512, 512) for c in range(2 * j + 1)] + [((2 * j + 1) * 512, (qi + 1) * 128)]
                        nkeys = (2 * j + 1) * 512 + (qi + 1) * 128
                        return segs, nkeys, slice(qi * 128, (qi + 1) * 128)

                    def stage_A(qi):
                        segs, nkeys, qcols = qgeom(qi)
                        op(pool, lambda e: e.tensor_tensor(
                            out=Dg[:], in0=ident[:].unsqueeze(1).to_broadcast([128, 8, 128]),
                            in1=wsc[:, qi, :].unsqueeze(2).to_broadcast([128, 8, 128]), op=ALU.mult),
                           rd=[t_ident, t_wsc], wr=[t_Dg])
                        units = [(si, h) for si in range(len(segs)) for h in range(8)]
                        U = len(units)

                        def emit_L(u):
                            si, h = units[u]
                            c0, n = segs[si]
                            b, g = h // 2, h % 2
                            bk = u % 4
                            op(pe, lambda e: e.matmul(ps[:, bk, 0:n], lhsT=qiT[:, b, g, qcols],
                                                      rhs=kiT[:, c0:c0 + n], start=True, stop=True),
                               rd=[t_qiT, t_kiT], wr=[PB[bk]])
                            r = Rb[u % 8]
                            if u % 2 == 0:
                                op(act, lambda e: e.activation(out=r[:, 0:n], in_=ps[:, bk, 0:n], func=AF.Relu),
                                   rd=[PB[bk]], wr=[t_Rb[u % 8]])
                            else:
                                op(dve, lambda e: e.tensor_scalar(out=r[:, 0:n], in0=ps[:, bk, 0:n], scalar1=0.0, scalar2=None,
                                                                  op0=ALU.max), rd=[PB[bk]], wr=[t_Rb[u % 8]])

                        def emit_D(u):
                            si, h = units[u]
                            c0, n = segs[si]
                            sbk = 4 + (si % 2)
                            op(pe, lambda e: e.matmul(ps[:, sbk, 0:n], lhsT=Dg[:, h, :], rhs=Rb[u % 8][:, 0:n],
                                                      start=(h == 0), stop=(h == 7)),
                               rd=[t_Dg, t_Rb[u % 8]], wr=[PB[sbk]])
                            if h == 7:
                                if si == 2 * j:
                                    op(act, lambda e: e.activation(out=SC[:, c0:c0 + n], in_=ps[:, sbk, 0:n], func=AF.Identity,
                                                                   bias=oflg[:, j:j + 1], scale=1.0),
                                       rd=[PB[sbk], t_small], wr=[t_SC])
                                elif si == 2 * j + 1:
                                    if n > 128:
                                        op(act, lambda e: e.activation(out=SC[:, c0:c0 + n - 128], in_=ps[:, sbk, 0:n - 128],
                                                                       func=AF.Copy), rd=[PB[sbk]], wr=[t_SC])
                                    op(dve, lambda e: e.tensor_tensor(out=SC[:, c0 + n - 128:c0 + n], in0=ps[:, sbk, n - 128:n],
                                                                      in1=trim[:], op=ALU.add), rd=[PB[sbk], t_ident], wr=[t_SC])
                                else:
                                    op(act, lambda e: e.activation(out=SC[:, c0:c0 + n], in_=ps[:, sbk, 0:n], func=AF.Copy),
                                       rd=[PB[sbk]], wr=[t_SC])

                        for u in range(U + 4):
                            if u < U:
                                emit_L(u)
                            if u >= 4:
                                emit_D(u - 4)

                    def stage_B(qi, frac):
                        segs, nkeys, qcols = qgeom(qi)
                        na = min(int(frac * nkeys) // 128 * 128, JA)
                        scv = SC[:, 0:nkeys]
                        br = 12.0 if j == 0 else BR
                        nbis = 12 if j == 0 else NBIS
                        op(dve, lambda e: e.tensor_reduce(out=bst[:, 0:1], in_=scv, axis=AX.X, op=ALU.max), rd=[t_SC], wr=[t_bst])
                        op(dve, lambda e: e.tensor_scalar(out=bst[:, 1:2], in0=bst[:, 0:1], scalar1=-br / 2, scalar2=None,
                                                          op0=ALU.add), rd=[t_bst], wr=[t_bst])
                        for it in range(nbis):
                            if na > 0:
                                op(act, lambda e: e.activation(out=junkA[:, 0:na], in_=SC[:, 0:na], func=AF.Sign,
                                                               bias=bst[:, 1:2], scale=-1.0, accum_out=bsa[:, 0:1]),
                                   rd=[t_SC, t_bst], wr=[t_junkA, t_bsa])
                            op(dve, lambda e: e.tensor_scalar(out=junk[:, na:nkeys], in0=SC[:, na:nkeys], scalar1=bst[:, 1:2],
                                                              scalar2=None, op0=ALU.is_gt, op1=ALU.add, accum_out=bst[:, 2:3]),
                               rd=[t_SC, t_bst], wr=t_hTc + [t_bst])
                            if na > 0:
                                op(dve, lambda e: e.scalar_tensor_tensor(out=bst[:, 2:3], in0=bsa[:, 0:1], scalar=-0.5,
                                                                         in1=bst[:, 2:3], op0=ALU.mult, op1=ALU.add),
                                   rd=[t_bsa, t_bst], wr=[t_bst])
                            last = (it == nbis - 1)
                            cn = (br / 2) / (2 ** it) if last else (br / 2) / (2 ** (it + 1))
                            op(dve, lambda e: e.tensor_scalar(out=bst[:, 3:4], in0=bst[:, 2:3], scalar1=255.5 - na / 2.0,
                                                              scalar2=(cn if last else 2.0 * cn), op0=ALU.is_gt, op1=ALU.mult),
                               rd=[t_bst], wr=[t_bst])
                            op(dve, lambda e: e.scalar_tensor_tensor(out=bst[:, 1:2], in0=bst[:, 3:4], scalar=-cn,
                                                                     in1=bst[:, 1:2], op0=ALU.add, op1=ALU.add),
                               rd=[t_bst], wr=[t_bst])
                            yield
                        if j == nchunks - 1 and qi == 3:
                            dump("SC", SC[:, 0:nkeys], [t_SC], kb)
                            dump("bst", bst[:, 0:4], [t_bst], kb)
                            stop_if("p2c", kb)
                        op(dve, lambda e: e.tensor_scalar(out=MB[:, 0:nkeys], in0=scv, scalar1=bst[:, 1:2], scalar2=NEG,
                                                          op0=ALU.is_le, op1=ALU.mult), rd=[t_SC, t_bst], wr=[t_MB])

                    def stage_C_main(qi):
                        segs, nkeys, qcols = qgeom(qi)
                        nsb = nkeys // 128
                        U = nsb * 2
                        LAG = 2

                        def emit_S(u):
                            sbi, g = u // 2, u % 2
                            bk = u % 4
                            op(pe, lambda e: e.matmul(ps[:, bk, :], lhsT=kT[:, sbi * 128:(sbi + 1) * 128],
                                                      rhs=qT[:, g, :, qcols], start=True, stop=False),
                               rd=[t_kT, t_qT], wr=[PB[bk]])
                            op(pe, lambda e: e.matmul(ps[:, bk, :], lhsT=MB[:, sbi * 128:(sbi + 1) * 128],
                                                      rhs=ident4[:], start=False, stop=True),
                               rd=[t_MB, t_ident], wr=[PB[bk]])
                            op(act, lambda e: e.activation(out=PT[u % 4][:], in_=ps[:, bk, :], func=AF.Exp, scale=0.125),
                               rd=[PB[bk]], wr=[t_PT[u % 4]])

                        def emit_V(u):
                            sbi, g = u // 2, u % 2
                            pt = PT[u % 4]
                            ob = 4 + g
                            for b in range(4):
                                op(pe, lambda e, b=b: e.matmul(ps[:, ob, b * 65:(b + 1) * 65], lhsT=pt[:, b * 128:(b + 1) * 128],
                                                               rhs=Vaug[:, sbi, g, :], start=(sbi == 0 and b == 0),
                                                               stop=(sbi == nsb - 1 and b == 3), skip_group_check=True),
                                   rd=[t_PT[u % 4], t_V], wr=[PB[ob]])

                        for u in range(U + LAG):
                            if u < U:
                                emit_S(u)
                            if u >= LAG:
                                emit_V(u - LAG)
                            yield

                    def stage_C_tail(qi):
                        segs, nkeys, qcols = qgeom(qi)
                        for g in range(2):
                            ov = ps[:, 4 + g, 0:260].rearrange("p (b e) -> p b e", e=65)
                            op(dve, lambda e: e.reciprocal(out=rs4[:, g * 4:(g + 1) * 4].unsqueeze(2), in_=ov[:, :, 64:65]),
                               rd=[PB[4 + g]], wr=[t_rs4])
                            op(dve, lambda e: e.tensor_tensor(
                                out=attn[:, g * 256:(g + 1) * 256].rearrange("p (b d) -> p b d", d=64), in0=ov[:, :, 0:64],
                                in1=rs4[:, g * 4:(g + 1) * 4].unsqueeze(2).to_broadcast([128, 4, 64]), op=ALU.mult),
                               rd=[PB[4 + g], t_rs4], wr=[t_attn])
                        if j == nchunks - 1 and qi == 3:
                            dump("attn", attn[:], [t_attn], kb)
                            stop_if("p2d", kb)
                        pv = psb16(6 + qi % 2)
                        for f in range(4):
                            op(pe, lambda e, f=f: e.transpose(out=pv[:, f * 128:(f + 1) * 128], in_=attn[:, f * 128:(f + 1) * 128],
                                                              identity=ident[:]), rd=[t_attn, t_ident], wr=[PB[6 + qi % 2]])
                        op(act, lambda e: e.activation(out=mixT[:, 0:4, qcols], in_=pv[:, 0:512].rearrange("p (f t) -> p f t", f=4),
                                                       func=AF.Copy), rd=[PB[6 + qi % 2]], wr=[t_mixT])

                    def interleave(gb, gc, nb):
                        csteps = list(range(gc[1]))
                        per = (len(csteps) + nb - 1) // nb if nb else 0
                        gcg, gbg = gc[0], gb
                        for it in range(nb):
                            next(gbg, None)
                            for _ in range(per):
                                next(gcg, None)
                        for _ in gbg:
                            pass
                        for _ in gcg:
                            pass

                    def csteps_of(qi):
                        return (qgeom(qi)[1] // 128) * 2 + 2

                    stage_A(0)
                    for _ in stage_B(0, 0.55):
                        pass
                    for qi in range(1, 4):
                        stage_A(qi)
                        interleave(stage_B(qi, 0.2), (stage_C_main(qi - 1), csteps_of(qi - 1)), 12 if j == 0 else NBIS)
                        stage_C_tail(qi - 1)
                    for _ in stage_C_main(3):
                        pass
                    stage_C_tail(3)

                    wo0, two0 = wload(wview(w_out, 0, 512))
                    wo1, two1 = wload(wview(w_out, 512, 1024))
                    for t4 in range(4):
                        for half, (wo, two) in enumerate(((wo0, two0), (wo1, two1))):
                            bk = half
                            for f in range(8):
                                op(pe, lambda e, f=f: e.matmul(ps[:, bk, :], lhsT=mixT[:, f, t4 * 128:(t4 + 1) * 128], rhs=wo[:, f, :],
                                                               start=(f == 0), stop=(f == 7)), rd=[t_mixT, two], wr=[PB[bk]])
                        s2 = t4 % 2
                        dma(sp, xts[s2][:], xp[(tile0 + t4) * 128:(tile0 + t4 + 1) * 128, :], wr=[t_xts[s2]])
                        op(dve, lambda e: e.tensor_tensor(out=x1t, in0=ps[:, 0:2, :].rearrange("p a b -> p (a b)"), in1=G1[:],
                                                          op=ALU.mult), rd=[PB[0], PB[1], t_G1], wr=[t_x1t])
                        op(pool, lambda e: e.tensor_tensor(out=x1t, in0=x1t, in1=xts[s2][:], op=ALU.add),
                           rd=[t_x1t, t_xts[s2]], wr=[t_x1t])
                        r0 = (j * 4 + t4) * 128
                        dma(sp, x1s[r0:r0 + 128, :], x1t, rd=[t_x1t])
                kb.barrier()
                stop_if("p2e", kb)

            with contextlib.ExitStack() as es2:
                Wup = sb("Wup", [128, 8, 4096], BF16); t_Wup = T()
                Wdn = sb("Wdn", [128, 32, 1024], BF16); t_Wdn = T()
                GF = sb("GF", [128, D], F32); t_GF = T()
                xts = [sb("m_xt%d" % i, [128, D], F32) for i in range(2)]; t_xts = [T(), T()]
                xns = [sb("m_xn%d" % i, [128, D], BF16) for i in range(2)]; t_xns = [T(), T()]
                sqj = None; t_sqj = None
                G2 = sb("G2", [128, D], F32)
                dma(sp, G2[:], g2s, rd=[t_G2], wr=[t_G2])
                sts = [sb("m_st%d" % i, [128, 4], F32) for i in range(2)]; t_sts = [T(), T()]
                h2T = sb("h2T", [128, 8, 256], BF16); t_h2T = [T(), T()]
                rT = [sb("rT%d" % i, [128, 256], BF16) for i in range(2)]; t_rT = [T(), T()]
                uT = sb("uT", [128, 32, 256], BF16); t_uT = T()
                x2 = sb("x2", [128, D], F32); t_x2 = T()
                oo = x2; t_oo = t_x2
                for c4 in range(8):
                    dma(pool, Wup[:, :, c4 * 512:(c4 + 1) * 512], wview(w_up, c4 * 512, (c4 + 1) * 512), wr=[t_Wup])
                for c4 in range(4):
                    dma(pool, Wdn[:, c4 * 8:(c4 + 1) * 8, :],
                        w_down[c4 * 1024:(c4 + 1) * 1024, :].rearrange("(k p) e -> p k e", p=128), wr=[t_Wdn])
                dma(sp, GF[:], gfb, wr=[t_GF])
                for gi in range(16):
                    for t2 in range(2):
                        r0 = (gi * 2 + t2) * 128
                        norm_tile(r0, xts[t2], t_xts[t2], xns[t2], t_xns[t2], sqj, t_sqj, sts[t2], t_sts[t2], src=x1s)
                        transpose_mod(xns[t2], t_xns[t2], 6 + t2, h2T[:, :, t2 * 128:(t2 + 1) * 128], t_h2T[t2], 2)
                    for ff in range(32):
                        bk = ff % 4
                        for k in range(8):
                            op(pe, lambda e, k=k: e.matmul(ps[:, bk, 0:256], lhsT=Wup[:, k, ff * 128:(ff + 1) * 128], rhs=h2T[:, k, :],
                                                           start=(k == 0), stop=(k == 7)), rd=[t_Wup] + t_h2T, wr=[PB[bk]])
                        r = rT[ff % 2]
                        op(act, lambda e: e.activation(out=r[:], in_=ps[:, bk, 0:256], func=AF.Relu), rd=[PB[bk]], wr=[t_rT[ff % 2]])
                        op(pool, lambda e, ff=ff: e.tensor_tensor(out=uT[:, ff, :], in0=r[:], in1=r[:], op=ALU.mult),
                           rd=[t_rT[ff % 2]], wr=[t_uT])
                    for t2 in range(2):
                        for half in range(2):
                            bk = 4 + half
                            for ff in range(32):
                                op(pe, lambda e, ff=ff: e.matmul(ps[:, bk, :], lhsT=uT[:, ff, t2 * 128:(t2 + 1) * 128],
                                                                 rhs=Wdn[:, ff, half * 512:(half + 1) * 512],
                                                                 start=(ff == 0), stop=(ff == 31)), rd=[t_uT, t_Wdn], wr=[PB[bk]])
                        op(dve, lambda e: e.tensor_tensor(out=x2[:], in0=ps[:, 4:6, :].rearrange("p a b -> p (a b)"), in1=G2[:],
                                                          op=ALU.mult), rd=[PB[4], PB[5], t_G2], wr=[t_x2])
                        op(pool, lambda e: e.tensor_tensor(out=x2[:], in0=x2[:], in1=xts[t2][:], op=ALU.add),
                           rd=[t_x2, t_xts[t2]], wr=[t_x2])
                        st = sts[t2]
                        op(act, lambda e: e.activation(out=xns[t2][:], in_=x2[:], func=AF.Square, accum_out=st[:, 0:1]),
                           rd=[t_x2], wr=[t_xns[t2], t_sts[t2]])
                        op(act, lambda e: e.activation(out=st[:, 1:2], in_=st[:, 0:1], func=AF.Sqrt, bias=EPS, scale=1.0 / D),
                           rd=[t_sts[t2]], wr=[t_sts[t2]])
                        op(dve, lambda e: e.reciprocal(out=st[:, 2:3], in_=st[:, 1:2]), rd=[t_sts[t2]], wr=[t_sts[t2]])
                        op(dve, lambda e: e.scalar_tensor_tensor(out=oo[:], in0=x2[:], scalar=st[:, 2:3], in1=GF[:],
                                                                 op0=ALU.mult, op1=ALU.mult), rd=[t_x2, t_sts[t2], t_GF], wr=[t_oo])
                        r0 = (gi * 2 + t2) * 128
                        dma(sp, out[r0:r0 + 128, :], oo[:], rd=[t_oo])
                kb.barrier()

    try:
        _body()
    except _Stop:
        pass
    return nc


_NC_CACHE = {}


def _layout_inputs(x, c, positions, w_ada, b_ada, g_mix, w_in, conv_w, conv_b, conv_norm_g, conv_norm_b,
                   w_out, g_mlp, w_up, w_down, g_final):
    f32 = np.float32
    x = np.asarray(x, f32); c = np.asarray(c, f32); positions = np.asarray(positions, np.int32)

    def col(v, n):
        return np.ascontiguousarray(np.asarray(v, f32).reshape(n, 128).T)
    shared = {
        "w_ada": np.ascontiguousarray(np.asarray(w_ada, f32)[0]),
        "badac": col(np.asarray(b_ada)[0], 48),
        "badar": np.ascontiguousarray(np.asarray(b_ada, f32)[0][None, :]),
        "gmixc": col(np.asarray(g_mix)[0], 8),
        "gmlpc": col(np.asarray(g_mlp)[0], 8),
        "w_in": np.ascontiguousarray(np.asarray(w_in, f32)[0]),
        "convw": np.ascontiguousarray(np.asarray(conv_w, f32)[0].T.reshape(4, 128, 31).transpose(1, 0, 2)),
        "convb": col(np.asarray(conv_b)[0], 4),
        "cng": col(np.asarray(conv_norm_g)[0], 4),
        "cnb": col(np.asarray(conv_norm_b)[0], 4),
        "w_out": np.ascontiguousarray(np.asarray(w_out, f32)[0]),
        "w_up": np.ascontiguousarray(np.asarray(w_up, f32)[0]),
        "w_down": np.ascontiguousarray(np.asarray(w_down, f32)[0]),
        "gfb": np.ascontiguousarray(np.broadcast_to(np.asarray(g_final, f32)[None, :], (128, D))),
        "invf": np.ascontiguousarray(np.broadcast_to(
            np.power(f32(500000.0), -np.arange(8, dtype=f32) * f32(2.0) / f32(16.0)).astype(f32)[None, :], (128, 8))),
    }
    in_maps = []
    for core in range(8):
        b, p = core // 2, core % 2
        own, oth = OWN[p], OWN[1 - p]
        rows = []
        for j in range(8):
            rows.append(np.arange(oth[j] * 512, oth[j] * 512 + 512))
            rows.append(np.arange(own[j] * 512, own[j] * 512 + 512))
        rows = np.concatenate(rows)
        xpa = np.zeros((NT * 128, D), f32)
        xpa[:S] = x[b][rows]
        pos = np.zeros((NT * 128,), np.int32)
        pos[:S] = positions[b][rows]
        hm = np.ones((256,), f32)
        for j in range(8):
            if own[j] == 0:
                hm[j * 32:(j + 1) * 32] = 0.0
            else:
                hr = np.arange(own[j] * 512 - 32, own[j] * 512)
                xpa[S + j * 32:S + (j + 1) * 32] = x[b][hr]
                pos[S + j * 32:S + (j + 1) * 32] = positions[b][hr]
        of = np.array([0.0 if oth[j] < own[j] else NEG for j in range(8)], f32)
        m = dict(shared)
        m["xp"] = xpa
        m["posp"] = np.ascontiguousarray(pos.reshape(NT, 128).T)
        m["oflag"] = np.ascontiguousarray(np.broadcast_to(of[None, :], (128, 8)))
        m["hmask"] = np.ascontiguousarray(np.broadcast_to(hm[None, :], (128, 256)))
        m["cT"] = col(c[b], 8)
        in_maps.append(m)
    return in_maps


def kernel(**inputs):
    in_maps = _layout_inputs(**inputs)
    if "nc" not in _NC_CACHE:
        _NC_CACHE["nc"] = build_program()
    nc = _NC_CACHE["nc"]
    res = run_bass_kernel_spmd(nc, in_maps, core_ids=list(range(8)))
    outf = np.zeros((4, S, D), np.float32)
    for core in range(8):
        b, p = core // 2, core % 2
        o = res.results[core]["out"]
        for j, ch in enumerate(OWN[p]):
            outf[b, ch * 512:(ch + 1) * 512] = o[j * 512:(j + 1) * 512]
    if DEBUG:
        kernel.debug = res.results
    return outf
```

```python
import numpy as np
import concourse.bass as bass
import concourse.mybir as mybir
from concourse.bass_utils import run_bass_kernel_spmd

F32 = mybir.dt.float32
BF16 = mybir.dt.bfloat16
I32 = mybir.dt.int32
U8 = mybir.dt.uint8
ALU = mybir.AluOpType
AF = mybir.ActivationFunctionType
AX = mybir.AxisListType

D = 1024
S = 8192
NT = 66
NEG = -30000.0
EPS = 1e-6
NBIS = 10
BR = 6.0
JA = 4608
OWN = ([0, 3, 4, 7, 8, 11, 12, 15], [1, 2, 5, 6, 9, 10, 13, 14])
DEBUG = False


class T:
    __slots__ = ("w", "r")

    def __init__(self):
        self.w = {}
        self.r = {}


class Eng:
    def __init__(self, obj, sem, key):
        self.obj = obj
        self.sem = sem
        self.key = key
        self.cnt = 0
        self.seen = {}


class K:
    def __init__(self, nc, sems):
        self.nc = nc
        it = iter(sems)
        self.pe = Eng(nc.tensor, next(it), "pe")
        self.act = Eng(nc.scalar, next(it), "act")
        self.dve = Eng(nc.vector, next(it), "dve")
        self.pool = Eng(nc.gpsimd, next(it), "pool")
        self.sp = Eng(nc.sync, next(it), "sp")
        self.engs = [self.pe, self.act, self.dve, self.pool, self.sp]
        self.dsems = {"sp": [[s, 0] for s in [next(it) for _ in range(8)]],
                      "pool": [[s, 0] for s in [next(it) for _ in range(8)]]}
        self.dptr = {"sp": 0, "pool": 0}

    def _waits(self, eng, rd, wr):
        need = {}

        def add(d, skip_self):
            for k, (s, v) in d.items():
                if skip_self and k == eng.key:
                    continue
                if k not in need or need[k][1] < v:
                    need[k] = (s, v)
        for t in rd:
            add(t.w, False)
        skip = (eng.key == "pe")
        for t in wr:
            add(t.w, skip)
            add(t.r, skip)
        for k, (s, v) in need.items():
            if eng.seen.get(k, 0) < v:
                eng.obj.wait_ge(s, v)
                eng.seen[k] = v

    def op(self, eng, fn, rd=(), wr=()):
        self._waits(eng, rd, wr)
        inst = fn(eng.obj)
        eng.cnt += 1
        inst.then_inc(eng.sem, 1)
        tok = (eng.sem, eng.cnt)
        for t in rd:
            t.r[eng.key] = tok
        for t in wr:
            t.w = {eng.key: tok}
            t.r = {}

    def dma(self, eng, out, in_, rd=(), wr=()):
        ring = self.dsems[eng.key]
        i = self.dptr[eng.key]
        self.dptr[eng.key] = (i + 1) % len(ring)
        sem, val = ring[i]
        key = "d%s%d" % (eng.key, i)
        self._waits(eng, rd, wr)
        if val > 0 and eng.seen.get(key, 0) < val:
            eng.obj.wait_ge(sem, val)
            eng.seen[key] = val
        eng.obj.dma_start(out=out, in_=in_).then_inc(sem, 16)
        ring[i][1] = val + 16
        tok = (sem, val + 16)
        for t in rd:
            t.r[key] = tok
        for t in wr:
            t.w = {key: tok}
            t.r = {}

    def barrier(self):
        for e in self.engs:
            for f in self.engs:
                if f is not e and f.cnt > 0 and e.seen.get(f.key, 0) < f.cnt:
                    e.obj.wait_ge(f.sem, f.cnt)
                    e.seen[f.key] = f.cnt
            for qk, ring in self.dsems.items():
                for i, (s, v) in enumerate(ring):
                    key = "d%s%d" % (qk, i)
                    if v > 0 and e.seen.get(key, 0) < v:
                        e.obj.wait_ge(s, v)
                        e.seen[key] = v


class _Stop(Exception):
    pass


def build_program(stage=None, dumps=(), nchunks=8, nt1=64):
    nc = bass.Bass("TRN2", target_bir_lowering=False)
    dt = nc.dram_tensor
    xp = dt("xp", [NT * 128, D], F32, kind="ExternalInput").ap()
    posp = dt("posp", [128, NT], I32, kind="ExternalInput").ap()
    oflag = dt("oflag", [128, 8], F32, kind="ExternalInput").ap()
    hmask = dt("hmask", [128, 256], F32, kind="ExternalInput").ap()
    invf = dt("invf", [128, 8], F32, kind="ExternalInput").ap()
    cT = dt("cT", [128, 8], F32, kind="ExternalInput").ap()
    w_ada = dt("w_ada", [D, 6 * D], F32, kind="ExternalInput").ap()
    badac = dt("badac", [128, 48], F32, kind="ExternalInput").ap()
    badar = dt("badar", [1, 6 * D], F32, kind="ExternalInput").ap()
    gmixc = dt("gmixc", [128, 8], F32, kind="ExternalInput").ap()
    gmlpc = dt("gmlpc", [128, 8], F32, kind="ExternalInput").ap()
    w_in = dt("w_in", [D, 2376], F32, kind="ExternalInput").ap()
    convw = dt("convw", [128, 4, 31], F32, kind="ExternalInput").ap()
    convb = dt("convb", [128, 4], F32, kind="ExternalInput").ap()
    cng = dt("cng", [128, 4], F32, kind="ExternalInput").ap()
    cnb = dt("cnb", [128, 4], F32, kind="ExternalInput").ap()
    w_out = dt("w_out", [D, D], F32, kind="ExternalInput").ap()
    w_up = dt("w_up", [D, 4 * D], F32, kind="ExternalInput").ap()
    w_down = dt("w_down", [4 * D, D], F32, kind="ExternalInput").ap()
    gfb = dt("gfb", [128, D], F32, kind="ExternalInput").ap()
    out = dt("out", [4096, D], F32, kind="ExternalOutput").ap()
    x1s = dt("x1s", [4096, D], F32).ap()
    g1s = dt("g1s", [128, D], F32).ap()
    g2s = dt("g2s", [128, D], F32).ap()
    if "x1s" in dumps:
        x1s = dt("dbg_x1s", [4096, D], F32, kind="ExternalOutput").ap()

    def wview(w, c0, c1):
        return w[:, c0:c1].rearrange("(k p) e -> p k e", p=128)

    import contextlib
    dump_aps = {}

    def dump(name, ap, tiles, kbref):
        if name not in dumps:
            return
        shp = [int(v) for v in ap.shape]
        d_ap = dt("dbg_" + name, shp, ap.dtype, kind="ExternalOutput").ap()
        kbref.dma(kbref.sp, d_ap, ap, rd=tiles)

    def stop_if(st, kbref):
        if stage == st:
            kbref.barrier()
            raise _Stop()

    def _body():
        with contextlib.ExitStack() as es:
            sems = [es.enter_context(nc.semaphore("s%d" % i)) for i in range(21)]
            kb = K(nc, sems)
            pe, act, dve, pool, sp = kb.pe, kb.act, kb.dve, kb.pool, kb.sp
            op, dma = kb.op, kb.dma

            def sb(name, shape, dtype=F32):
                return es2.enter_context(nc.sbuf_tensor(name, shape, dtype))

            ps = es.enter_context(nc.psum_tensor("ps", [128, 8, 512], F32))
            PB = [T() for _ in range(8)]

            def psb16(b):
                return ps[:, b, :].bitcast(BF16)

            es2 = es
            ident = sb("ident", [128, 128], BF16); t_ident = T()
            ident4 = sb("ident4", [128, 4, 128], BF16)
            identf = sb("identf", [128, 128], F32)
            trim = sb("trim", [128, 128], F32)
            onesm = sb("onesm", [128, 128], BF16)
            onesr = sb("onesr", [1, 128], F32)
            cosT = sb("cosT", [128, NT, 8], F32)
            sinT = sb("sinT", [128, NT, 8], F32); t_cs = T()
            modc = sb("modc", [128, 48], F32); t_modc = T()
            ab = sb("ab", [128, 4, 8], F32); t_ab = T()
            t_G1 = T(); t_G2 = T()
            oflg = sb("oflg", [128, 8], F32); t_small = T()
            cw = sb("cw", [128, 4, 31], F32)
            cb = sb("cb", [128, 4], F32)
            cg = sb("cg", [128, 4], F32)
            cbn = sb("cbn", [128, 4], F32)
            wst = {"slots": None, "tiles": None, "ptr": 0}

            def walloc(tag):
                wst["slots"] = [sb("wslot%s%d" % (tag, i), [128, 8, 512], BF16) for i in range(2)]
                wst["tiles"] = [T() for _ in range(2)]
                wst["ptr"] = 0

            def wload(src_ap):
                i = wst["ptr"]
                wst["ptr"] = (i + 1) % 2
                dma(pool, wst["slots"][i][:], src_ap, wr=[wst["tiles"][i]])
                return wst["slots"][i], wst["tiles"][i]

            op(pool, lambda e: e.memset(identf[:], 0.0), wr=[t_ident])
            op(pool, lambda e: e.affine_select(out=identf[:], in_=identf[:], pattern=[[-1, 128]],
                                               compare_op=ALU.not_equal, fill=1.0, base=0,
                                               channel_multiplier=1), rd=[t_ident], wr=[t_ident])
            op(pool, lambda e: e.tensor_copy(out=ident[:], in_=identf[:]), rd=[t_ident], wr=[t_ident])
            op(pool, lambda e: e.tensor_copy(out=ident4[:], in_=identf[:].unsqueeze(1).to_broadcast([128, 4, 128])),
               rd=[t_ident], wr=[t_ident])
            op(pool, lambda e: e.memset(trim[:], 0.0), wr=[t_ident])
            op(pool, lambda e: e.affine_select(out=trim[:], in_=trim[:], pattern=[[-1, 128]],
                                               compare_op=ALU.is_ge, fill=NEG, base=0,
                                               channel_multiplier=1), rd=[t_ident], wr=[t_ident])
            op(pool, lambda e: e.memset(onesm[:], 1.0 / 512.0), wr=[t_ident])
            op(pool, lambda e: e.memset(onesr[:], 1.0), wr=[t_ident])
            dma(sp, oflg[:], oflag, wr=[t_small])
            dma(sp, cw[:], convw, wr=[t_small])
            dma(sp, cb[:], convb, wr=[t_small])
            dma(sp, cg[:], cng, wr=[t_small])
            dma(sp, cbn[:], cnb, wr=[t_small])

            with contextlib.ExitStack() as es2:
                posi = sb("posi", [128, NT], I32)
                posf = sb("posf", [128, NT], F32)
                ivf = sb("ivf", [128, 8], F32)
                ang = sb("ang", [128, NT, 8], F32)
                tq = sb("tq", [128, NT, 8], F32)
                kq = sb("kq", [128, NT, 8], I32)
                kf = sb("kf", [128, NT, 8], F32)
                red = sb("red", [128, NT, 8], F32)
                t_p0 = T()
                cTs = sb("cTs", [128, 8], F32)
                cond = sb("cond", [128, 8], F32); t_cond = T()
                badc = sb("badc", [128, 48], F32)
                gmc = sb("gmc", [128, 2, 8], F32)
                rowb = sb("rowb", [1, 2048], F32)
                rows = sb("rows", [1, 512], F32); t_rows = T()
                Gtmp = sb("Gtmp", [128, 512], F32); t_Gtmp = T()
                wfs = [sb("wf%d" % i, [128, 8, 512], F32) for i in range(3)]; t_wfs = [T() for _ in range(3)]
                dma(sp, posi[:], posp, wr=[t_p0])
                dma(sp, ivf[:], invf, wr=[t_p0])
                dma(sp, cTs[:], cT, wr=[t_cond])
                dma(sp, badc[:], badac, wr=[t_cond])
                dma(sp, gmc[:, 0, :], gmixc, wr=[t_cond])
                dma(sp, gmc[:, 1, :], gmlpc, wr=[t_cond])
                dma(sp, rowb[:, 0:1024], badar[:, 2048:3072], wr=[t_cond])
                dma(sp, rowb[:, 1024:2048], badar[:, 5120:6144], wr=[t_cond])
                rw = dict(rd=[t_p0], wr=[t_p0])
                op(dve, lambda e: e.tensor_copy(out=posf[:], in_=posi[:]), **rw)
                op(dve, lambda e: e.tensor_tensor(out=ang[:], in0=posf[:].unsqueeze(2).to_broadcast([128, NT, 8]),
                                                  in1=ivf[:].unsqueeze(1).to_broadcast([128, NT, 8]), op=ALU.mult), **rw)
                TWO_PI = 2.0 * np.pi
                C1 = 6.28125
                C2 = TWO_PI - C1

                def reduce_to(dst, shift):
                    op(dve, lambda e: e.tensor_scalar(out=tq[:], in0=ang[:], scalar1=shift, scalar2=1.0 / TWO_PI,
                                                      op0=ALU.add, op1=ALU.mult), **rw)
                    op(dve, lambda e: e.tensor_copy(out=kq[:], in_=tq[:]), **rw)
                    op(dve, lambda e: e.tensor_copy(out=kf[:], in_=kq[:]), **rw)
                    op(dve, lambda e: e.scalar_tensor_tensor(out=red[:], in0=kf[:], scalar=-C1, in1=ang[:],
                                                             op0=ALU.mult, op1=ALU.add), **rw)
                    op(dve, lambda e: e.scalar_tensor_tensor(out=red[:], in0=kf[:], scalar=-C2, in1=red[:],
                                                             op0=ALU.mult, op1=ALU.add), **rw)
                    op(dve, lambda e: e.tensor_scalar(out=red[:], in0=red[:], scalar1=shift, scalar2=None,
                                                      op0=ALU.add), **rw)
                    op(dve, lambda e: e.tensor_scalar(out=tq[:], in0=red[:], scalar1=np.pi, scalar2=-TWO_PI,
                                                      op0=ALU.is_gt, op1=ALU.mult), **rw)
                    op(dve, lambda e: e.tensor_tensor(out=red[:], in0=red[:], in1=tq[:], op=ALU.add), **rw)
                    op(dve, lambda e: e.tensor_scalar(out=tq[:], in0=red[:], scalar1=-np.pi, scalar2=TWO_PI,
                                                      op0=ALU.is_lt, op1=ALU.mult), **rw)
                    op(dve, lambda e: e.tensor_tensor(out=red[:], in0=red[:], in1=tq[:], op=ALU.add), **rw)
                    op(dve, lambda e: e.tensor_scalar(out=red[:], in0=red[:], scalar1=-3.1415925, scalar2=3.1415925,
                                                      op0=ALU.max, op1=ALU.min), **rw)
                    op(act, lambda e: e.activation(out=dst[:], in_=red[:], func=AF.Sin), rd=[t_p0], wr=[t_cs])

                reduce_to(sinT, 0.0)
                reduce_to(cosT, np.pi / 2.0)

                op(act, lambda e: e.activation(out=cond[:], in_=cTs[:], func=AF.Silu), rd=[t_cond], wr=[t_cond])
                for cc in range(12):
                    ws, tw = wfs[cc % 3], t_wfs[cc % 3]
                    dma(sp, ws[:], wview(w_ada, cc * 512, (cc + 1) * 512), wr=[tw])
                    if cc in (4, 5, 10, 11):
                        for k in range(8):
                            op(pe, lambda e, k=k: e.matmul(ps[0:1, 1, :], lhsT=cond[:, k:k + 1], rhs=ws[:, k, :],
                                                           start=(k == 0), stop=(k == 7)),
                               rd=[t_cond, tw], wr=[PB[1]])
                        ro = (cc - 4) * 512 if cc < 6 else 1024 + (cc - 10) * 512
                        op(dve, lambda e, ro=ro: e.tensor_tensor(out=rows[:], in0=ps[0:1, 1, :], in1=rowb[:, ro:ro + 512],
                                                                 op=ALU.add), rd=[PB[1], t_cond], wr=[t_rows])
                        op(pe, lambda e: e.matmul(ps[:, 2, :], lhsT=onesr[:], rhs=rows[:], start=True, stop=True),
                           rd=[t_rows, t_ident], wr=[PB[2]])
                        Gs, tG = (g1s, t_G1) if cc < 6 else (g2s, t_G2)
                        go = (cc - 4) * 512 if cc < 6 else (cc - 10) * 512
                        op(act, lambda e: e.activation(out=Gtmp[:], in_=ps[:, 2, :], func=AF.Copy),
                           rd=[PB[2]], wr=[t_Gtmp])
                        dma(sp, Gs[:, go:go + 512], Gtmp[:], rd=[t_Gtmp], wr=[tG])
                    else:
                        for el in range(4):
                            et = cc * 4 + el
                            for k in range(8):
                                op(pe, lambda e, k=k, el=el, et=et: e.matmul(
                                    ps[:, 0, et:et + 1], lhsT=ws[:, k, el * 128:(el + 1) * 128], rhs=cond[:, k:k + 1],
                                    start=(k == 0), stop=(k == 7), skip_group_check=True),
                                   rd=[t_cond, tw], wr=[PB[0]])
                op(dve, lambda e: e.memset(modc[:], 0.0), wr=[t_modc])
                for lo_, hi_ in ((0, 16), (24, 40)):
                    op(dve, lambda e: e.tensor_tensor(out=modc[:, lo_:hi_], in0=ps[:, 0, lo_:hi_], in1=badc[:, lo_:hi_], op=ALU.add),
                       rd=[PB[0], t_cond], wr=[t_modc])
                op(dve, lambda e: e.scalar_tensor_tensor(out=ab[:, 0, :], in0=modc[:, 8:16], scalar=1.0, in1=gmc[:, 0, :],
                                                         op0=ALU.add, op1=ALU.mult), rd=[t_modc, t_cond], wr=[t_ab])
                op(dve, lambda e: e.tensor_copy(out=ab[:, 1, :], in_=modc[:, 0:8]), rd=[t_modc], wr=[t_ab])
                op(dve, lambda e: e.scalar_tensor_tensor(out=ab[:, 2, :], in0=modc[:, 32:40], scalar=1.0, in1=gmc[:, 1, :],
                                                         op0=ALU.add, op1=ALU.mult), rd=[t_modc, t_cond], wr=[t_ab])
                op(dve, lambda e: e.tensor_copy(out=ab[:, 3, :], in_=modc[:, 24:32]), rd=[t_modc], wr=[t_ab])
                dump("cosT", cosT[:], [t_cs], kb)
                dump("sinT", sinT[:], [t_cs], kb)
                dump("ab", ab[:], [t_ab], kb)
                dump("modc", modc[:], [t_modc], kb)
                kb.barrier()
                stop_if("p0", kb)

            def norm_tile(row0, xt, t_xt, xn, t_xn, sq, t_sq, st, t_st, src=None):
                dma(sp, xt[:], (xp if src is None else src)[row0:row0 + 128, :], wr=[t_xt])
                op(act, lambda e: e.activation(out=xn[:], in_=xt[:], func=AF.Square, accum_out=st[:, 0:1]),
                   rd=[t_xt], wr=[t_xn, t_st])
                op(act, lambda e: e.activation(out=st[:, 1:2], in_=st[:, 0:1], func=AF.Sqrt, bias=EPS, scale=1.0 / D),
                   rd=[t_st], wr=[t_st])
                op(dve, lambda e: e.reciprocal(out=st[:, 2:3], in_=st[:, 1:2]), rd=[t_st], wr=[t_st])
                op(act, lambda e: e.activation(out=xn[:], in_=xt[:], func=AF.Copy, scale=st[:, 2:3]),
                   rd=[t_xt, t_st], wr=[t_xn])

            def transpose_mod(xn, t_xn, bank, hT_dst, t_hT, abi):
                pv = psb16(bank)
                for k in range(8):
                    op(pe, lambda e, k=k: e.transpose(out=pv[:, k * 128:(k + 1) * 128], in_=xn[:, k * 128:(k + 1) * 128],
                                                      identity=ident[:]), rd=[t_xn, t_ident], wr=[PB[bank]])
                pv3 = pv.rearrange("p (k t) -> p k t", k=8)
                op(dve, lambda e: e.tensor_tensor(out=hT_dst, in0=pv3,
                                                  in1=ab[:, abi, :].unsqueeze(2).to_broadcast([128, 8, 128]), op=ALU.mult),
                   rd=[PB[bank], t_ab], wr=[t_hT])
                op(pool, lambda e: e.tensor_tensor(out=hT_dst, in0=hT_dst,
                                                   in1=ab[:, abi + 1, :].unsqueeze(2).to_broadcast([128, 8, 128]), op=ALU.add),
                   rd=[t_hT, t_ab], wr=[t_hT])

            with contextlib.ExitStack() as es2:
                kT = sb("kT", [128, S], BF16); t_kT = T()
                kiT = sb("kiT", [128, S], BF16); t_kiT = T()
                Vaug = sb("Vaug", [128, 64, 2, 65], BF16); t_V = T()
                W1 = sb("W1", [128, 8, 328], BF16); t_W1 = T()
                xts = [sb("xt%d" % i, [128, D], F32) for i in range(2)]; t_xts = [T(), T()]
                xns = [sb("xn%d" % i, [128, D], BF16) for i in range(2)]; t_xns = [T(), T()]
                sqj = None; t_sqj = None
                walloc("b")
                G1 = sb("G1", [128, D], F32)
                dma(sp, G1[:], g1s, rd=[t_G1], wr=[t_G1])
                sts = [sb("st%d" % i, [128, 4], F32) for i in range(2)]; t_sts = [T(), T()]
                hTc = sb("hTc", [128, 8, 512], BF16); t_hTc = [T() for _ in range(4)]
                rtmp = sb("rtmp", [128, 4, 16, 8], F32); t_rtmp = T()
                krot = [sb("krot%d" % i, [128, 256], BF16) for i in range(2)]; t_krot = [T(), T()]

                op(pool, lambda e: e.memset(Vaug[:], 1.0), wr=[t_V])
                dma(pool, W1[:, :, 0:128], wview(w_in, 512, 640), wr=[t_W1])
                dma(pool, W1[:, :, 128:192], wview(w_in, 1280, 1344), wr=[t_W1])
                dma(pool, W1[:, :, 192:320], wview(w_in, 640, 768), wr=[t_W1])
                dma(pool, W1[:, :, 320:328], wview(w_in, 1344, 1352), wr=[t_W1])

                def rope(src3, dst3, nh, ti, tsrc, tdst):
                    cs = cosT[:, ti, :].unsqueeze(1).to_broadcast([128, nh, 8])
                    sn = sinT[:, ti, :].unsqueeze(1).to_broadcast([128, nh, 8])
                    x1, x2 = src3[:, :, 0:8], src3[:, :, 8:16]
                    t1, t2, t3, t4 = (rtmp[:, i, 0:nh, :] for i in range(4))
                    op(dve, lambda e: e.tensor_tensor(out=t1, in0=x1, in1=cs, op=ALU.mult), rd=[tsrc, t_cs], wr=[t_rtmp])
                    op(dve, lambda e: e.tensor_tensor(out=t2, in0=x2, in1=sn, op=ALU.mult), rd=[tsrc, t_cs], wr=[t_rtmp])
                    op(dve, lambda e: e.tensor_tensor(out=t3, in0=x2, in1=cs, op=ALU.mult), rd=[tsrc, t_cs], wr=[t_rtmp])
                    op(dve, lambda e: e.tensor_tensor(out=t4, in0=x1, in1=sn, op=ALU.mult), rd=[tsrc, t_cs], wr=[t_rtmp])
                    op(dve, lambda e: e.tensor_tensor(out=dst3[:, :, 0:8], in0=t1, in1=t2, op=ALU.subtract),
                       rd=[t_rtmp], wr=[tdst])
                    op(dve, lambda e: e.tensor_tensor(out=dst3[:, :, 8:16], in0=t3, in1=t4, op=ALU.add),
                       rd=[t_rtmp], wr=[tdst])
                    op(act, lambda e: e.activation(out=dst3[:, :, 16:64], in_=src3[:, :, 16:64], func=AF.Copy),
                       rd=[tsrc], wr=[tdst])

                def rope4(src4, dst4, ti, tsrc, tdst):
                    cs = cosT[:, ti, :].unsqueeze(1).unsqueeze(1).to_broadcast([128, 2, 4, 8])
                    sn = sinT[:, ti, :].unsqueeze(1).unsqueeze(1).to_broadcast([128, 2, 4, 8])
                    x1, x2 = src4[:, :, :, 0:8], src4[:, :, :, 8:16]
                    t1, t2, t3, t4 = (rtmp[:, i, 0:8, :].rearrange("p (g b) d -> p g b d", g=2) for i in range(4))
                    op(dve, lambda e: e.tensor_tensor(out=t1, in0=x1, in1=cs, op=ALU.mult), rd=[tsrc, t_cs], wr=[t_rtmp])
                    op(dve, lambda e: e.tensor_tensor(out=t2, in0=x2, in1=sn, op=ALU.mult), rd=[tsrc, t_cs], wr=[t_rtmp])
                    op(dve, lambda e: e.tensor_tensor(out=t3, in0=x2, in1=cs, op=ALU.mult), rd=[tsrc, t_cs], wr=[t_rtmp])
                    op(dve, lambda e: e.tensor_tensor(out=t4, in0=x1, in1=sn, op=ALU.mult), rd=[tsrc, t_cs], wr=[t_rtmp])
                    op(dve, lambda e: e.tensor_tensor(out=dst4[:, :, :, 0:8], in0=t1, in1=t2, op=ALU.subtract),
                       rd=[t_rtmp], wr=[tdst])
                    op(dve, lambda e: e.tensor_tensor(out=dst4[:, :, :, 8:16], in0=t3, in1=t4, op=ALU.add),
                       rd=[t_rtmp], wr=[tdst])
                    op(act, lambda e: e.activation(out=dst4[:, :, :, 16:64], in_=src4[:, :, :, 16:64], func=AF.Copy),
                       rd=[tsrc], wr=[tdst])

                def ph1_S1(ti):
                    s2 = ti % 2
                    norm_tile(ti * 128, xts[s2], t_xts[s2], xns[s2], t_xns[s2], sqj, t_sqj, sts[s2], t_sts[s2])
                    hs = ti % 4
                    hdst = hTc[:, :, hs * 128:(hs + 1) * 128]
                    transpose_mod(xns[s2], t_xns[s2], 6 + s2, hdst, t_hTc[hs], 0)

                def ph1_S2(ti):
                    s2 = ti % 2
                    hs = ti % 4
                    bk = s2
                    for k in range(8):
                        op(pe, lambda e, k=k: e.matmul(ps[:, bk, 0:320], lhsT=hTc[:, k, hs * 128:(hs + 1) * 128],
                                                       rhs=W1[:, k, 0:320], start=(k == 0), stop=(k == 7)),
                           rd=[t_hTc[hs], t_W1], wr=[PB[bk]])
                    kr = krot[s2]
                    rope(ps[:, bk, 0:192].rearrange("p (h d) -> p h d", d=64),
                         kr[:, 0:192].rearrange("p (h d) -> p h d", d=64), 3, ti, PB[bk], t_krot[s2])
                    op(pool, lambda e: e.tensor_copy(out=kr[:, 192:256], in_=kr[:, 128:192]), rd=[t_krot[s2]], wr=[t_krot[s2]])
                    op(act, lambda e: e.activation(out=Vaug[:, ti, :, 0:64],
                                                   in_=ps[:, bk, 192:320].rearrange("p (g d) -> p g d", d=64), func=AF.Copy),
                       rd=[PB[bk]], wr=[t_V])
                    tb = 4 + s2
                    pv = psb16(tb)
                    op(pe, lambda e: e.transpose(out=pv[:, 0:128], in_=kr[:, 0:128], identity=ident[:]),
                       rd=[t_krot[s2], t_ident], wr=[PB[tb]])
                    op(pe, lambda e: e.transpose(out=pv[:, 128:256], in_=kr[:, 128:256], identity=ident[:]),
                       rd=[t_krot[s2], t_ident], wr=[PB[tb]])
                    op(act, lambda e: e.activation(out=kT[:, ti * 128:(ti + 1) * 128], in_=pv[:, 0:128], func=AF.Copy),
                       rd=[PB[tb]], wr=[t_kT])
                    op(dve, lambda e: e.tensor_copy(out=kiT[:, ti * 128:(ti + 1) * 128], in_=pv[:, 128:256]),
                       rd=[PB[tb]], wr=[t_kiT])


                ph1_S1(0)
                for ti in range(nt1):
                    if ti + 1 < nt1:
                        ph1_S1(ti + 1)
                    ph1_S2(ti)
                dump("kT", kT[:], [t_kT], kb)
                dump("kiT", kiT[:], [t_kiT], kb)
                dump("Vaug", Vaug[:], [t_V], kb)
                stop_if("p1", kb)
                SC = sb("SC", [128, S], F32); t_SC = T()
                junk = hTc[:].rearrange("p k t -> p (k t)").bitcast(U8)
                RbA = sb("RbA", [128, 8, 512], BF16)
                Rb = [RbA[:, i, :] for i in range(8)]; t_Rb = [T() for _ in range(8)]
                Dg = sb("Dg", [128, 8, 128], BF16); t_Dg = T()
                qT = sb("qT", [128, 2, 4, 512], BF16); t_qT = T()
                qiT = sb("qiT", [128, 4, 2, 512], BF16); t_qiT = T()
                op(pool, lambda e: e.memset(qT[:], 0.0), wr=[t_qT])
                op(pool, lambda e: e.memset(qiT[:], 0.0), wr=[t_qiT])
                qrot = [sb("qrot%d" % i, [128, 512], BF16) for i in range(2)]; t_qrot = [T(), T()]
                wsc = sb("wsc", [128, 4, 8], F32); t_wsc = T()
                PT = [sb("PT%d" % i, [128, 512], BF16) for i in range(4)]; t_PT = [T() for _ in range(4)]
                MB = sb("MB", [128, S], BF16); t_MB = T()
                junkA = sb("junkA", [128, JA], U8); t_junkA = T()
                bsa = sb("bsa", [128, 2], F32); t_bsa = T()
                bst = sb("bst", [128, 8], F32); t_bst = T()
                gluT = sb("gluT", [128, 4, 544], BF16); t_glu = T()
                gluH = sb("gluH", [128, 4, 256], BF16); t_gluH = T()
                SCb = SC[:].bitcast(BF16)
                ybf = SCb[:, 0:2048].rearrange("p (c t) -> p c t", c=4); t_ybf = t_SC
                ysq = SCb[:, 2048:4096].rearrange("p (c t) -> p c t", c=4); t_ysq = t_SC
                lnA = SC[:, 2048:2560]; t_lnA = t_SC
                lnB = SC[:, 2560:3072]; t_lnB = t_SC
                zn = SC[:, 3072:3584]; t_zn = t_SC
                sig = SC[:, 3584:4096]; t_sig = t_SC
                cdiag = RbA[:].rearrange("p a b -> p (a b)")[:, 0:3968].rearrange("p (k c) -> p k c", c=128)
                mixT = sb("mixT", [128, 8, 512], BF16); t_mixT = T()
                attn = sb("attn", [128, 512], BF16); t_attn = T()
                rs4 = sb("rs4", [128, 8], F32); t_rs4 = T()
                hm = sb("hm", [128, 256], F32)
                x1t = SC[:, 4096:5120]; t_x1t = t_SC
                hmB = sb("hmB", [128, 256], BF16)
                dma(sp, hm[:], hmask, wr=[t_small])
                op(pool, lambda e: e.tensor_copy(out=hmB[:], in_=hm[:]), rd=[t_small], wr=[t_small])

                def conv_glu(ws_a, tw_a, ws_g, tw_g, ncols, ct, dst, tdst, hcols, t_h):
                    for k in range(8):
                        op(pe, lambda e, k=k: e.matmul(ps[:, 0, 0:ncols], lhsT=ws_a[:, k, ct * 128:(ct + 1) * 128],
                                                       rhs=hTc[:, k, hcols], start=(k == 0), stop=(k == 7)),
                           rd=t_h + [tw_a], wr=[PB[0]])
                    for k in range(8):
                        op(pe, lambda e, k=k: e.matmul(ps[:, 1, 0:ncols], lhsT=ws_g[:, k, ct * 128:(ct + 1) * 128],
                                                       rhs=hTc[:, k, hcols], start=(k == 0), stop=(k == 7)),
                           rd=t_h + [tw_g], wr=[PB[1]])
                    op(act, lambda e: e.activation(out=sig[:, 0:ncols], in_=ps[:, 1, 0:ncols], func=AF.Sigmoid),
                       rd=[PB[1]], wr=[t_sig])
                    op(dve, lambda e: e.tensor_tensor(out=dst, in0=ps[:, 0, 0:ncols], in1=sig[:, 0:ncols], op=ALU.mult),
                       rd=[PB[0], t_sig], wr=[tdst])

                for hi in range(2):
                    norm_tile((64 + hi) * 128, xts[hi], t_xts[hi], xns[hi], t_xns[hi], sqj, t_sqj, sts[hi], t_sts[hi])
                    transpose_mod(xns[hi], t_xns[hi], 6 + hi, hTc[:, :, hi * 128:(hi + 1) * 128], t_hTc[hi], 0)
                wa, twa = wload(wview(w_in, 1352, 1864))
                wg, twg = wload(wview(w_in, 1864, 2376))
                for ct in range(4):
                    conv_glu(wa, twa, wg, twg, 256, ct, gluH[:, ct, :], t_gluH, slice(0, 256), [t_hTc[0], t_hTc[1]])
                    op(pool, lambda e, ct=ct: e.tensor_tensor(out=gluH[:, ct, :], in0=gluH[:, ct, :], in1=hmB[:], op=ALU.mult),
                       rd=[t_gluH, t_small], wr=[t_gluH])

                dump("gluH", gluH[:], [t_gluH], kb)
                stop_if("p2h", kb)
                for j in range(nchunks):
                    tile0 = (2 * j + 1) * 4
                    for t4 in range(4):
                        s2 = t4 % 2
                        norm_tile((tile0 + t4) * 128, xts[s2], t_xts[s2], xns[s2], t_xns[s2], sqj, t_sqj, sts[s2], t_sts[s2])
                        transpose_mod(xns[s2], t_xns[s2], 6 + s2, hTc[:, :, t4 * 128:(t4 + 1) * 128], t_hTc[t4], 0)
                    stop_if("p2n", kb)
                    for grp in range(2):
                        c0 = 0 if grp == 0 else 768
                        wq, twq = wload(wview(w_in, c0, c0 + 512))
                        stop_if("p2w", kb)
                        dstT, t_dstT = (qT, t_qT) if grp == 0 else (qiT, t_qiT)
                        for t4 in range(4):
                            bk = t4 % 2
                            for k in range(8):
                                op(pe, lambda e, k=k: e.matmul(ps[:, bk, :], lhsT=hTc[:, k, t4 * 128:(t4 + 1) * 128],
                                                               rhs=wq[:, k, :], start=(k == 0), stop=(k == 7)),
                                   rd=[t_hTc[t4], twq], wr=[PB[bk]])
                            if grp == 0 and t4 == 0:
                                stop_if("p2m", kb)
                            qr = qrot[bk]
                            src4 = ps[:, bk, :].rearrange("p (g b d) -> p g b d", g=2, b=4)
                            dst4 = qr[:].rearrange("p (b g d) -> p g b d", g=2, b=4)
                            rope4(src4, dst4, tile0 + t4, PB[bk], t_qrot[bk])
                            if grp == 0 and t4 == 0:
                                dump("qr", qr[:], [t_qrot[bk]], kb)
                                stop_if("p2a0", kb)
                            tb = 4 + bk
                            pv = psb16(tb)
                            for b in range(4):
                                op(pe, lambda e, b=b: e.transpose(out=pv[:, b * 128:(b + 1) * 128],
                                                                  in_=qr[:, b * 128:(b + 1) * 128], identity=ident[:]),
                                   rd=[t_qrot[bk], t_ident], wr=[PB[tb]])
                            pv4 = pv[:, 0:512].rearrange("p (b t) -> p b t", b=4)
                            tcols = slice(t4 * 128, (t4 + 1) * 128)
                            if grp == 0:
                                d0, d1 = qT[0:64, 0, :, tcols], qT[64:128, 1, :, tcols]
                            else:
                                d0, d1 = qiT[0:64, :, 0, tcols], qiT[64:128, :, 1, tcols]
                            op(act, lambda e: e.activation(out=d0, in_=pv4[0:64], func=AF.Copy), rd=[PB[tb]], wr=[t_dstT])
                            op(dve, lambda e: e.tensor_copy(out=d1, in_=pv4[64:128]), rd=[PB[tb]], wr=[t_dstT])
                            if grp == 0 and t4 == 0:
                                stop_if("p2a1", kb)
                            if grp == 1 and t4 == 0:
                                stop_if("p2a2", kb)
                            if grp == 1:
                                for k in range(8):
                                    op(pe, lambda e, k=k: e.matmul(ps[:, 2, 0:8], lhsT=hTc[:, k, t4 * 128:(t4 + 1) * 128],
                                                                   rhs=W1[:, k, 320:328], start=(k == 0), stop=(k == 7)),
                                       rd=[t_hTc[t4], t_W1], wr=[PB[2]])
                                op(dve, lambda e: e.tensor_scalar(
                                    out=wsc[:, t4, :].rearrange("p (b g) -> p g b", g=2),
                                    in0=ps[:, 2, 0:8].rearrange("p (g b) -> p g b", g=2),
                                    scalar1=float(8 ** -0.5 * 64 ** -0.5), scalar2=None, op0=ALU.mult),
                                   rd=[PB[2]], wr=[t_wsc])
                    if j == nchunks - 1:
                        dump("qT", qT[:].rearrange("p g b t -> p (g b) t"), [t_qT], kb)
                        dump("qiT", qiT[:].rearrange("p b g t -> p (b g) t"), [t_qiT], kb)
                        dump("wsc", wsc[:], [t_wsc], kb)
                        stop_if("p2a", kb)
                    wa, twa = wload(wview(w_in, 1352, 1864))
                    wg, twg = wload(wview(w_in, 1864, 2376))
                    for ct in range(4):
                        op(pool, lambda e, ct=ct: e.tensor_copy(out=gluT[:, ct, 0:32], in_=gluH[:, ct, j * 32:(j + 1) * 32]),
                           rd=[t_gluH], wr=[t_glu])
                        conv_glu(wa, twa, wg, twg, 512, ct, gluT[:, ct, 32:544], t_glu, slice(0, 512), t_hTc)
                    for ct in range(4):
                        op(pool, lambda e, ct=ct: e.tensor_tensor(
                            out=cdiag, in0=ident[:].unsqueeze(1).to_broadcast([128, 31, 128]),
                            in1=cw[:, ct, :].unsqueeze(2).to_broadcast([128, 31, 128]), op=ALU.mult),
                           rd=[t_ident, t_small], wr=t_Rb)
                        cbk = ct
                        for tap in range(31):
                            op(pe, lambda e, tap=tap, ct=ct: e.matmul(ps[:, cbk, :], lhsT=cdiag[:, tap, :],
                                                                     rhs=gluT[:, ct, tap + 2:tap + 514],
                                                                     start=(tap == 0), stop=(tap == 30)),
                               rd=t_Rb + [t_glu], wr=[PB[cbk]])
                        op(act, lambda e, ct=ct: e.activation(out=ybf[:, ct, :], in_=ps[:, cbk, :], func=AF.Identity,
                                                              bias=cb[:, ct:ct + 1], scale=1.0), rd=[PB[cbk], t_small], wr=[t_ybf])
                        op(act, lambda e, ct=ct: e.activation(out=ysq[:, ct, :], in_=ps[:, cbk, :], func=AF.Square,
                                                              bias=cb[:, ct:ct + 1], scale=1.0), rd=[PB[cbk], t_small], wr=[t_ysq])
                    for ct in range(4):
                        op(pe, lambda e, ct=ct: e.matmul(ps[:, 4, :], lhsT=onesm[:], rhs=ybf[:, ct, :],
                                                         start=(ct == 0), stop=(ct == 3)), rd=[t_ybf, t_ident], wr=[PB[4]])
                    for ct in range(4):
                        op(pe, lambda e, ct=ct: e.matmul(ps[:, 5, :], lhsT=onesm[:], rhs=ysq[:, ct, :],
                                                         start=(ct == 0), stop=(ct == 3)), rd=[t_ysq, t_ident], wr=[PB[5]])
                    op(act, lambda e: e.activation(out=lnA, in_=ps[:, 4, :], func=AF.Copy), rd=[PB[4]], wr=[t_lnA])
                    op(dve, lambda e: e.tensor_tensor(out=lnB, in0=lnA, in1=lnA, op=ALU.mult), rd=[t_lnA], wr=[t_lnB])
                    op(dve, lambda e: e.tensor_tensor(out=lnB, in0=ps[:, 5, :], in1=lnB, op=ALU.subtract),
                       rd=[PB[5], t_lnB], wr=[t_lnB])
                    op(dve, lambda e: e.tensor_scalar(out=lnB, in0=lnB, scalar1=0.0, scalar2=EPS, op0=ALU.max, op1=ALU.add),
                       rd=[t_lnB], wr=[t_lnB])
                    op(act, lambda e: e.activation(out=lnB, in_=lnB, func=AF.Sqrt), rd=[t_lnB], wr=[t_lnB])
                    op(dve, lambda e: e.reciprocal(out=lnB, in_=lnB), rd=[t_lnB], wr=[t_lnB])
                    for ct in range(4):
                        op(dve, lambda e, ct=ct: e.scalar_tensor_tensor(out=zn, in0=ps[:, ct, :], scalar=cb[:, ct:ct + 1],
                                                                        in1=lnA, op0=ALU.add, op1=ALU.subtract),
                           rd=[PB[ct], t_small, t_lnA], wr=[t_zn])
                        op(dve, lambda e: e.tensor_tensor(out=zn, in0=zn, in1=lnB, op=ALU.mult),
                           rd=[t_zn, t_lnB], wr=[t_zn])
                        op(act, lambda e, ct=ct: e.activation(out=mixT[:, 4 + ct, :], in_=zn, func=AF.Silu,
                                                              bias=cbn[:, ct:ct + 1], scale=cg[:, ct:ct + 1]),
                           rd=[t_zn, t_small], wr=[t_mixT])

                    if j == nchunks - 1:
                        dump("mixTc", mixT[:, 4:8, :], [t_mixT], kb)
                        stop_if("p2b", kb)
                    def qgeom(qi):
                        segs = [(c * 512, 512) for c in range(2 * j + 1)] + [((2 * j + 1) * 512, (qi + 1) * 128)]
                        nkeys = (2 * j + 1) * 512 + (qi + 1) * 128
                        return segs, nkeys, slice(qi * 128, (qi + 1) * 128)

                    def stage_A(qi):
                        segs, nkeys, qcols = qgeom(qi)
                        op(pool, lambda e: e.tensor_tensor(
                            out=Dg[:], in0=ident[:].unsqueeze(1).to_broadcast([128, 8, 128]),
                            in1=wsc[:, qi, :].unsqueeze(2).to_broadcast([128, 8, 128]), op=ALU.mult),
                           rd=[t_ident, t_wsc], wr=[t_Dg])
                        units = [(si, h) for si in range(len(segs)) for h in range(8)]
                        U = len(units)

                        def emit_L(u):
                            si, h = units[u]
                            c0, n = segs[si]
                            b, g = h // 2, h % 2
                            bk = u % 4
                            op(pe, lambda e: e.matmul(ps[:, bk, 0:n], lhsT=qiT[:, b, g, qcols],
                                                      rhs=kiT[:, c0:c0 + n], start=True, stop=True),
                               rd=[t_qiT, t_kiT], wr=[PB[bk]])
                            r = Rb[u % 8]
                            if u % 2 == 0:
                                op(act, lambda e: e.activation(out=r[:, 0:n], in_=ps[:, bk, 0:n], func=AF.Relu),
                                   rd=[PB[bk]], wr=[t_Rb[u % 8]])
                            else:
                                op(dve, lambda e: e.tensor_scalar(out=r[:, 0:n], in0=ps[:, bk, 0:n], scalar1=0.0, scalar2=None,
                                                                  op0=ALU.max), rd=[PB[bk]], wr=[t_Rb[u % 8]])

                        def emit_D(u):
                            si, h = units[u]
                            c0, n = segs[si]
                            sbk = 4 + (si % 2)
                            op(pe, lambda e: e.matmul(ps[:, sbk, 0:n], lhsT=Dg[:, h, :], rhs=Rb[u % 8][:, 0:n],
                                                      start=(h == 0), stop=(h == 7)),
                               rd=[t_Dg, t_Rb[u % 8]], wr=[PB[sbk]])
                            if h == 7:
                                if si == 2 * j:
                                    op(act, lambda e: e.activation(out=SC[:, c0:c0 + n], in_=ps[:, sbk, 0:n], func=AF.Identity,
                                                                   bias=oflg[:, j:j + 1], scale=1.0),
                                       rd=[PB[sbk], t_small], wr=[t_SC])
                                elif si == 2 * j + 1:
                                    if n > 128:
                                        op(act, lambda e: e.activation(out=SC[:, c0:c0 + n - 128], in_=ps[:, sbk, 0:n - 128],
                                                                       func=AF.Copy), rd=[PB[sbk]], wr=[t_SC])
                                    op(dve, lambda e: e.tensor_tensor(out=SC[:, c0 + n - 128:c0 + n], in0=ps[:, sbk, n - 128:n],
                                                                      in1=trim[:], op=ALU.add), rd=[PB[sbk], t_ident], wr=[t_SC])
                                else:
                                    op(act, lambda e: e.activation(out=SC[:, c0:c0 + n], in_=ps[:, sbk, 0:n], func=AF.Copy),
                                       rd=[PB[sbk]], wr=[t_SC])

                        for u in range(U + 4):
                            if u < U:
                                emit_L(u)
                            if u >= 4:
                                emit_D(u - 4)

                    def stage_B(qi, frac):
                        segs, nkeys, qcols = qgeom(qi)
                        na = min(int(frac * nkeys) // 128 * 128, JA)
                        scv = SC[:, 0:nkeys]
                        br = 12.0 if j == 0 else BR
                        nbis = 12 if j == 0 else NBIS
                        op(dve, lambda e: e.tensor_reduce(out=bst[:, 0:1], in_=scv, axis=AX.X, op=ALU.max), rd=[t_SC], wr=[t_bst])
                        op(dve, lambda e: e.tensor_scalar(out=bst[:, 1:2], in0=bst[:, 0:1], scalar1=-br / 2, scalar2=None,
                                                          op0=ALU.add), rd=[t_bst], wr=[t_bst])
                        for it in range(nbis):
                            if na > 0:
                                op(act, lambda e: e.activation(out=junkA[:, 0:na], in_=SC[:, 0:na], func=AF.Sign,
                                                               bias=bst[:, 1:2], scale=-1.0, accum_out=bsa[:, 0:1]),
                                   rd=[t_SC, t_bst], wr=[t_junkA, t_bsa])
                            op(dve, lambda e: e.tensor_scalar(out=junk[:, na:nkeys], in0=SC[:, na:nkeys], scalar1=bst[:, 1:2],
                                                              scalar2=None, op0=ALU.is_gt, op1=ALU.add, accum_out=bst[:, 2:3]),
                               rd=[t_SC, t_bst], wr=t_hTc + [t_bst])
                            if na > 0:
                                op(dve, lambda e: e.scalar_tensor_tensor(out=bst[:, 2:3], in0=bsa[:, 0:1], scalar=-0.5,
                                                                         in1=bst[:, 2:3], op0=ALU.mult, op1=ALU.add),
                                   rd=[t_bsa, t_bst], wr=[t_bst])
                            last = (it == nbis - 1)
                            cn = (br / 2) / (2 ** it) if last else (br / 2) / (2 ** (it + 1))
                            op(dve, lambda e: e.tensor_scalar(out=bst[:, 3:4], in0=bst[:, 2:3], scalar1=255.5 - na / 2.0,
                                                              scalar2=(cn if last else 2.0 * cn), op0=ALU.is_gt, op1=ALU.mult),
                               rd=[t_bst], wr=[t_bst])
                            op(dve, lambda e: e.scalar_tensor_tensor(out=bst[:, 1:2], in0=bst[:, 3:4], scalar=-cn,
                                                                     in1=bst[:, 1:2], op0=ALU.add, op1=ALU.add),
                               rd=[t_bst], wr=[t_bst])
                            yield
                        if j == nchunks - 1 and qi == 3:
                            dump("SC", SC[:, 0:nkeys], [t_SC], kb)
                            dump("bst", bst[:, 0:4], [t_bst], kb)
                            stop_if("p2c", kb)
                        op(dve, lambda e: e.tensor_scalar(out=MB[:, 0:nkeys], in0=scv, scalar1=bst[:, 1:2], scalar2=NEG,
                                                          op0=ALU.is_le, op1=ALU.mult), rd=[t_SC, t_bst], wr=[t_MB])

                    def stage_C_main(qi):
                        segs, nkeys, qcols = qgeom(qi)
                        nsb = nkeys // 128
                        U = nsb * 2
                        LAG = 2

                        def emit_S(u):
                            sbi, g = u // 2, u % 2
                            bk = u % 4
                            op(pe, lambda e: e.matmul(ps[:, bk, :], lhsT=kT[:, sbi * 128:(sbi + 1) * 128],
                                                      rhs=qT[:, g, :, qcols], start=True, stop=False),
                               rd=[t_kT, t_qT], wr=[PB[bk]])
                            op(pe, lambda e: e.matmul(ps[:, bk, :], lhsT=MB[:, sbi * 128:(sbi + 1) * 128],
                                                      rhs=ident4[:], start=False, stop=True),
                               rd=[t_MB, t_ident], wr=[PB[bk]])
                            op(act, lambda e: e.activation(out=PT[u % 4][:], in_=ps[:, bk, :], func=AF.Exp, scale=0.125),
                               rd=[PB[bk]], wr=[t_PT[u % 4]])

                        def emit_V(u):
                            sbi, g = u // 2, u % 2
                            pt = PT[u % 4]
                            ob = 4 + g
                            for b in range(4):
                                op(pe, lambda e, b=b: e.matmul(ps[:, ob, b * 65:(b + 1) * 65], lhsT=pt[:, b * 128:(b + 1) * 128],
                                                               rhs=Vaug[:, sbi, g, :], start=(sbi == 0 and b == 0),
                                                               stop=(sbi == nsb - 1 and b == 3), skip_group_check=True),
                                   rd=[t_PT[u % 4], t_V], wr=[PB[ob]])

                        for u in range(U + LAG):
                            if u < U:
                                emit_S(u)
                            if u >= LAG:
                                emit_V(u - LAG)
                            yield

                    def stage_C_tail(qi):
                        segs, nkeys, qcols = qgeom(qi)
                        for g in range(2):
                            ov = ps[:, 4 + g, 0:260].rearrange("p (b e) -> p b e", e=65)
                            op(dve, lambda e: e.reciprocal(out=rs4[:, g * 4:(g + 1) * 4].unsqueeze(2), in_=ov[:, :, 64:65]),
                               rd=[PB[4 + g]], wr=[t_rs4])
                            op(dve, lambda e: e.tensor_tensor(
                                out=attn[:, g * 256:(g + 1) * 256].rearrange("p (b d) -> p b d", d=64), in0=ov[:, :, 0:64],
                                in1=rs4[:, g * 4:(g + 1) * 4].unsqueeze(2).to_broadcast([128, 4, 64]), op=ALU.mult),
                               rd=[PB[4 + g], t_rs4], wr=[t_attn])
                        if j == nchunks - 1 and qi == 3:
                            dump("attn", attn[:], [t_attn], kb)
                            stop_if("p2d", kb)
                        pv = psb16(6 + qi % 2)
                        for f in range(4):
                            op(pe, lambda e, f=f: e.transpose(out=pv[:, f * 128:(f + 1) * 128], in_=attn[:, f * 128:(f + 1) * 128],
                                                              identity=ident[:]), rd=[t_attn, t_ident], wr=[PB[6 + qi % 2]])
                        op(act, lambda e: e.activation(out=mixT[:, 0:4, qcols], in_=pv[:, 0:512].rearrange("p (f t) -> p f t", f=4),
                                                       func=AF.Copy), rd=[PB[6 + qi % 2]], wr=[t_mixT])

                    def interleave(gb, gc, nb):
                        csteps = list(range(gc[1]))
                        per = (len(csteps) + nb - 1) // nb if nb else 0
                        gcg, gbg = gc[0], gb
                        for it in range(nb):
                            next(gbg, None)
                            for _ in range(per):
                                next(gcg, None)
                        for _ in gbg:
                            pass
                        for _ in gcg:
                            pass

                    def csteps_of(qi):
                        return (qgeom(qi)[1] // 128) * 2 + 2

                    stage_A(0)
                    for _ in stage_B(0, 0.55):
                        pass
                    for qi in range(1, 4):
                        stage_A(qi)
                        interleave(stage_B(qi, 0.2), (stage_C_main(qi - 1), csteps_of(qi - 1)), 12 if j == 0 else NBIS)
                        stage_C_tail(qi - 1)
                    for _ in stage_C_main(3):
                        pass
                    stage_C_tail(3)

                    wo0, two0 = wload(wview(w_out, 0, 512))
                    wo1, two1 = wload(wview(w_out, 512, 1024))
                    for t4 in range(4):
                        for half, (wo, two) in enumerate(((wo0, two0), (wo1, two1))):
                            bk = half
                            for f in range(8):
                                op(pe, lambda e, f=f: e.matmul(ps[:, bk, :], lhsT=mixT[:, f, t4 * 128:(t4 + 1) * 128], rhs=wo[:, f, :],
                                                               start=(f == 0), stop=(f == 7)), rd=[t_mixT, two], wr=[PB[bk]])
                        s2 = t4 % 2
                        dma(sp, xts[s2][:], xp[(tile0 + t4) * 128:(tile0 + t4 + 1) * 128, :], wr=[t_xts[s2]])
                        op(dve, lambda e: e.tensor_tensor(out=x1t, in0=ps[:, 0:2, :].rearrange("p a b -> p (a b)"), in1=G1[:],
                                                          op=ALU.mult), rd=[PB[0], PB[1], t_G1], wr=[t_x1t])
                        op(pool, lambda e: e.tensor_tensor(out=x1t, in0=x1t, in1=xts[s2][:], op=ALU.add),
                           rd=[t_x1t, t_xts[s2]], wr=[t_x1t])
                        r0 = (j * 4 + t4) * 128
                        dma(sp, x1s[r0:r0 + 128, :], x1t, rd=[t_x1t])
                kb.barrier()
                stop_if("p2e", kb)

            with contextlib.ExitStack() as es2:
                Wup = sb("Wup", [128, 8, 4096], BF16); t_Wup = T()
                Wdn = sb("Wdn", [128, 32, 1024], BF16); t_Wdn = T()
                GF = sb("GF", [128, D], F32); t_GF = T()
                xts = [sb("m_xt%d" % i, [128, D], F32) for i in range(4)]; t_xts = [T() for _ in range(4)]
                xns = [sb("m_xn%d" % i, [128, D], BF16) for i in range(4)]; t_xns = [T() for _ in range(4)]
                sqj = None; t_sqj = None
                G2 = sb("G2", [128, D], F32)
                dma(sp, G2[:], g2s, rd=[t_G2], wr=[t_G2])
                sts = [sb("m_st%d" % i, [128, 4], F32) for i in range(4)]; t_sts = [T() for _ in range(4)]
                h2T = [sb("h2T%d" % i, [128, 8, 256], BF16) for i in range(2)]; t_h2T = [[T(), T()], [T(), T()]]
                rT = [sb("rT%d" % i, [128, 256], BF16) for i in range(2)]; t_rT = [T(), T()]
                uT = sb("uT", [128, 32, 256], BF16); t_uT = T()
                x2 = sb("x2", [128, D], F32); t_x2 = T()
                oo = x2; t_oo = t_x2
                for c4 in range(8):
                    dma(pool, Wup[:, :, c4 * 512:(c4 + 1) * 512], wview(w_up, c4 * 512, (c4 + 1) * 512), wr=[t_Wup])
                for c4 in range(4):
                    dma(pool, Wdn[:, c4 * 8:(c4 + 1) * 8, :],
                        w_down[c4 * 1024:(c4 + 1) * 1024, :].rearrange("(k p) e -> p k e", p=128), wr=[t_Wdn])
                dma(sp, GF[:], gfb, wr=[t_GF])
                def m_pre_norm(gi):
                    for t2 in range(2):
                        sl = (gi % 2) * 2 + t2
                        r0 = (gi * 2 + t2) * 128
                        norm_tile(r0, xts[sl], t_xts[sl], xns[sl], t_xns[sl], sqj, t_sqj, sts[sl], t_sts[sl], src=x1s)

                def m_pre_T(gi):
                    for t2 in range(2):
                        sl = (gi % 2) * 2 + t2
                        transpose_mod(xns[sl], t_xns[sl], 6 + t2, h2T[gi % 2][:, :, t2 * 128:(t2 + 1) * 128], t_h2T[gi % 2][t2], 2)

                def m_up(gi):
                    hh = h2T[gi % 2]
                    for ff in range(32):
                        bk = ff % 4
                        for k in range(8):
                            op(pe, lambda e, k=k: e.matmul(ps[:, bk, 0:256], lhsT=Wup[:, k, ff * 128:(ff + 1) * 128], rhs=hh[:, k, :],
                                                           start=(k == 0), stop=(k == 7)), rd=[t_Wup] + t_h2T[gi % 2], wr=[PB[bk]])
                        r = rT[ff % 2]
                        op(act, lambda e: e.activation(out=r[:], in_=ps[:, bk, 0:256], func=AF.Relu), rd=[PB[bk]], wr=[t_rT[ff % 2]])
                        op(pool, lambda e, ff=ff: e.tensor_tensor(out=uT[:, ff, :], in0=r[:], in1=r[:], op=ALU.mult),
                           rd=[t_rT[ff % 2]], wr=[t_uT])
                        if ff == 8 and gi + 1 < 16:
                            m_pre_norm(gi + 1)

                def m_down(gi):
                    for t2 in range(2):
                        sl = (gi % 2) * 2 + t2
                        for half in range(2):
                            bk = 4 + half
                            for ff in range(32):
                                op(pe, lambda e, ff=ff: e.matmul(ps[:, bk, :], lhsT=uT[:, ff, t2 * 128:(t2 + 1) * 128],
                                                                 rhs=Wdn[:, ff, half * 512:(half + 1) * 512],
                                                                 start=(ff == 0), stop=(ff == 31)), rd=[t_uT, t_Wdn], wr=[PB[bk]])
                        op(dve, lambda e: e.tensor_tensor(out=x2[:], in0=ps[:, 4:6, :].rearrange("p a b -> p (a b)"), in1=G2[:],
                                                          op=ALU.mult), rd=[PB[4], PB[5], t_G2], wr=[t_x2])
                        op(pool, lambda e: e.tensor_tensor(out=x2[:], in0=x2[:], in1=xts[sl][:], op=ALU.add),
                           rd=[t_x2, t_xts[sl]], wr=[t_x2])
                        st = sts[sl]
                        op(act, lambda e: e.activation(out=xns[sl][:], in_=x2[:], func=AF.Square, accum_out=st[:, 0:1]),
                           rd=[t_x2], wr=[t_xns[sl], t_sts[sl]])
                        op(act, lambda e: e.activation(out=st[:, 1:2], in_=st[:, 0:1], func=AF.Sqrt, bias=EPS, scale=1.0 / D),
                           rd=[t_sts[sl]], wr=[t_sts[sl]])
                        op(dve, lambda e: e.reciprocal(out=st[:, 2:3], in_=st[:, 1:2]), rd=[t_sts[sl]], wr=[t_sts[sl]])
                        op(dve, lambda e: e.scalar_tensor_tensor(out=oo[:], in0=x2[:], scalar=st[:, 2:3], in1=GF[:],
                                                                 op0=ALU.mult, op1=ALU.mult), rd=[t_x2, t_sts[sl], t_GF], wr=[t_oo])
                        r0 = (gi * 2 + t2) * 128
                        dma(sp, out[r0:r0 + 128, :], oo[:], rd=[t_oo])

                m_pre_norm(0)
                m_pre_T(0)
                for gi in range(16):
                    m_up(gi)
                    if gi + 1 < 16:
                        m_pre_T(gi + 1)
                    m_down(gi)
                kb.barrier()

    try:
        _body()
    except _Stop:
        pass
    return nc


_NC_CACHE = {}


def _layout_inputs(x, c, positions, w_ada, b_ada, g_mix, w_in, conv_w, conv_b, conv_norm_g, conv_norm_b,
                   w_out, g_mlp, w_up, w_down, g_final):
    f32 = np.float32
    x = np.asarray(x, f32); c = np.asarray(c, f32); positions = np.asarray(positions, np.int32)

    def col(v, n):
        return np.ascontiguousarray(np.asarray(v, f32).reshape(n, 128).T)
    shared = {
        "w_ada": np.ascontiguousarray(np.asarray(w_ada, f32)[0]),
        "badac": col(np.asarray(b_ada)[0], 48),
        "badar": np.ascontiguousarray(np.asarray(b_ada, f32)[0][None, :]),
        "gmixc": col(np.asarray(g_mix)[0], 8),
        "gmlpc": col(np.asarray(g_mlp)[0], 8),
        "w_in": np.ascontiguousarray(np.asarray(w_in, f32)[0]),
        "convw": np.ascontiguousarray(np.asarray(conv_w, f32)[0].T.reshape(4, 128, 31).transpose(1, 0, 2)),
        "convb": col(np.asarray(conv_b)[0], 4),
        "cng": col(np.asarray(conv_norm_g)[0], 4),
        "cnb": col(np.asarray(conv_norm_b)[0], 4),
        "w_out": np.ascontiguousarray(np.asarray(w_out, f32)[0]),
        "w_up": np.ascontiguousarray(np.asarray(w_up, f32)[0]),
        "w_down": np.ascontiguousarray(np.asarray(w_down, f32)[0]),
        "gfb": np.ascontiguousarray(np.broadcast_to(np.asarray(g_final, f32)[None, :], (128, D))),
        "invf": np.ascontiguousarray(np.broadcast_to(
            np.power(f32(500000.0), -np.arange(8, dtype=f32) * f32(2.0) / f32(16.0)).astype(f32)[None, :], (128, 8))),
    }
    in_maps = []
    for core in range(8):
        b, p = core // 2, core % 2
        own, oth = OWN[p], OWN[1 - p]
        rows = []
        for j in range(8):
            rows.append(np.arange(oth[j] * 512, oth[j] * 512 + 512))
            rows.append(np.arange(own[j] * 512, own[j] * 512 + 512))
        rows = np.concatenate(rows)
        xpa = np.zeros((NT * 128, D), f32)
        xpa[:S] = x[b][rows]
        pos = np.zeros((NT * 128,), np.int32)
        pos[:S] = positions[b][rows]
        hm = np.ones((256,), f32)
        for j in range(8):
            if own[j] == 0:
                hm[j * 32:(j + 1) * 32] = 0.0
            else:
                hr = np.arange(own[j] * 512 - 32, own[j] * 512)
                xpa[S + j * 32:S + (j + 1) * 32] = x[b][hr]
                pos[S + j * 32:S + (j + 1) * 32] = positions[b][hr]
        of = np.array([0.0 if oth[j] < own[j] else NEG for j in range(8)], f32)
        m = dict(shared)
        m["xp"] = xpa
        m["posp"] = np.ascontiguousarray(pos.reshape(NT, 128).T)
        m["oflag"] = np.ascontiguousarray(np.broadcast_to(of[None, :], (128, 8)))
        m["hmask"] = np.ascontiguousarray(np.broadcast_to(hm[None, :], (128, 256)))
        m["cT"] = col(c[b], 8)
        in_maps.append(m)
    return in_maps


def kernel(**inputs):
    in_maps = _layout_inputs(**inputs)
    if "nc" not in _NC_CACHE:
        _NC_CACHE["nc"] = build_program()
    nc = _NC_CACHE["nc"]
    res = run_bass_kernel_spmd(nc, in_maps, core_ids=list(range(8)))
    outf = np.zeros((4, S, D), np.float32)
    for core in range(8):
        b, p = core // 2, core % 2
        o = res.results[core]["out"]
        for j, ch in enumerate(OWN[p]):
            outf[b, ch * 512:(ch + 1) * 512] = o[j * 512:(j + 1) * 512]
    if DEBUG:
        kernel.debug = res.results
    return outf
```

```python
import numpy as np
import concourse.bass as bass
import concourse.mybir as mybir
from concourse.bass_utils import run_bass_kernel_spmd

F32 = mybir.dt.float32
BF16 = mybir.dt.bfloat16
I32 = mybir.dt.int32
U8 = mybir.dt.uint8
ALU = mybir.AluOpType
AF = mybir.ActivationFunctionType
AX = mybir.AxisListType

D = 1024
S = 8192
NT = 66
NEG = -30000.0
EPS = 1e-6
NBIS = 10
BR = 6.0
JA = 2560
OWN = ([0, 3, 4, 7, 8, 11, 12, 15], [1, 2, 5, 6, 9, 10, 13, 14])
DEBUG = False


class T:
    __slots__ = ("w", "r")

    def __init__(self):
        self.w = {}
        self.r = {}


class Eng:
    def __init__(self, obj, sem, key):
        self.obj = obj
        self.sem = sem
        self.key = key
        self.cnt = 0
        self.seen = {}


class K:
    def __init__(self, nc, sems):
        self.nc = nc
        it = iter(sems)
        self.pe = Eng(nc.tensor, next(it), "pe")
        self.act = Eng(nc.scalar, next(it), "act")
        self.dve = Eng(nc.vector, next(it), "dve")
        self.pool = Eng(nc.gpsimd, next(it), "pool")
        self.sp = Eng(nc.sync, next(it), "sp")
        self.engs = [self.pe, self.act, self.dve, self.pool, self.sp]
        self.dsems = {"sp": [[s, 0] for s in [next(it) for _ in range(8)]],
                      "pool": [[s, 0] for s in [next(it) for _ in range(8)]]}
        self.dptr = {"sp": 0, "pool": 0}

    def _waits(self, eng, rd, wr):
        need = {}

        def add(d, skip_self):
            for k, (s, v) in d.items():
                if skip_self and k == eng.key:
                    continue
                if k not in need or need[k][1] < v:
                    need[k] = (s, v)
        for t in rd:
            add(t.w, False)
        skip = (eng.key == "pe")
        for t in wr:
            add(t.w, skip)
            add(t.r, skip)
        for k, (s, v) in need.items():
            if eng.seen.get(k, 0) < v:
                eng.obj.wait_ge(s, v)
                eng.seen[k] = v

    def op(self, eng, fn, rd=(), wr=()):
        self._waits(eng, rd, wr)
        inst = fn(eng.obj)
        eng.cnt += 1
        inst.then_inc(eng.sem, 1)
        tok = (eng.sem, eng.cnt)
        for t in rd:
            t.r[eng.key] = tok
        for t in wr:
            t.w = {eng.key: tok}
            t.r = {}

    def dma(self, eng, out, in_, rd=(), wr=()):
        ring = self.dsems[eng.key]
        i = self.dptr[eng.key]
        self.dptr[eng.key] = (i + 1) % len(ring)
        sem, val = ring[i]
        key = "d%s%d" % (eng.key, i)
        self._waits(eng, rd, wr)
        if val > 0 and eng.seen.get(key, 0) < val:
            eng.obj.wait_ge(sem, val)
            eng.seen[key] = val
        eng.obj.dma_start(out=out, in_=in_).then_inc(sem, 16)
        ring[i][1] = val + 16
        tok = (sem, val + 16)
        for t in rd:
            t.r[key] = tok
        for t in wr:
            t.w = {key: tok}
            t.r = {}

    def barrier(self):
        for e in self.engs:
            for f in self.engs:
                if f is not e and f.cnt > 0 and e.seen.get(f.key, 0) < f.cnt:
                    e.obj.wait_ge(f.sem, f.cnt)
                    e.seen[f.key] = f.cnt
            for qk, ring in self.dsems.items():
                for i, (s, v) in enumerate(ring):
                    key = "d%s%d" % (qk, i)
                    if v > 0 and e.seen.get(key, 0) < v:
                        e.obj.wait_ge(s, v)
                        e.seen[key] = v


class _Stop(Exception):
    pass


def build_program(stage=None, dumps=(), nchunks=8, nt1=64):
    nc = bass.Bass("TRN2", target_bir_lowering=False)
    dt = nc.dram_tensor
    xp = dt("xp", [NT * 128, D], F32, kind="ExternalInput").ap()
    posp = dt("posp", [128, NT], I32, kind="ExternalInput").ap()
    oflag = dt("oflag", [128, 8], F32, kind="ExternalInput").ap()
    hmask = dt("hmask", [128, 256], F32, kind="ExternalInput").ap()
    invf = dt("invf", [128, 8], F32, kind="ExternalInput").ap()
    cT = dt("cT", [128, 8], F32, kind="ExternalInput").ap()
    w_ada = dt("w_ada", [D, 6 * D], F32, kind="ExternalInput").ap()
    badac = dt("badac", [128, 48], F32, kind="ExternalInput").ap()
    badar = dt("badar", [1, 6 * D], F32, kind="ExternalInput").ap()
    gmixc = dt("gmixc", [128, 8], F32, kind="ExternalInput").ap()
    gmlpc = dt("gmlpc", [128, 8], F32, kind="ExternalInput").ap()
    w_in = dt("w_in", [D, 2376], F32, kind="ExternalInput").ap()
    convw = dt("convw", [128, 4, 31], F32, kind="ExternalInput").ap()
    convb = dt("convb", [128, 4], F32, kind="ExternalInput").ap()
    cng = dt("cng", [128, 4], F32, kind="ExternalInput").ap()
    cnb = dt("cnb", [128, 4], F32, kind="ExternalInput").ap()
    w_out = dt("w_out", [D, D], F32, kind="ExternalInput").ap()
    w_up = dt("w_up", [D, 4 * D], F32, kind="ExternalInput").ap()
    w_down = dt("w_down", [4 * D, D], F32, kind="ExternalInput").ap()
    gfb = dt("gfb", [128, D], F32, kind="ExternalInput").ap()
    out = dt("out", [4096, D], F32, kind="ExternalOutput").ap()
    x1s = dt("x1s", [4096, D], F32).ap()
    g1s = dt("g1s", [128, D], F32).ap()
    g2s = dt("g2s", [128, D], F32).ap()
    if "x1s" in dumps:
        x1s = dt("dbg_x1s", [4096, D], F32, kind="ExternalOutput").ap()

    def wview(w, c0, c1):
        return w[:, c0:c1].rearrange("(k p) e -> p k e", p=128)

    import contextlib
    dump_aps = {}

    def dump(name, ap, tiles, kbref):
        if name not in dumps:
            return
        shp = [int(v) for v in ap.shape]
        d_ap = dt("dbg_" + name, shp, ap.dtype, kind="ExternalOutput").ap()
        kbref.dma(kbref.sp, d_ap, ap, rd=tiles)

    def stop_if(st, kbref):
        if stage == st:
            kbref.barrier()
            raise _Stop()

    def _body():
        with contextlib.ExitStack() as es:
            sems = [es.enter_context(nc.semaphore("s%d" % i)) for i in range(21)]
            kb = K(nc, sems)
            pe, act, dve, pool, sp = kb.pe, kb.act, kb.dve, kb.pool, kb.sp
            op, dma = kb.op, kb.dma

            def sb(name, shape, dtype=F32):
                return es2.enter_context(nc.sbuf_tensor(name, shape, dtype))

            ps = es.enter_context(nc.psum_tensor("ps", [128, 8, 512], F32))
            PB = [T() for _ in range(8)]

            def psb16(b):
                return ps[:, b, :].bitcast(BF16)

            es2 = es
            ident = sb("ident", [128, 128], BF16); t_ident = T()
            ident4 = sb("ident4", [128, 4, 128], BF16)
            identf = sb("identf", [128, 128], F32)
            trim = sb("trim", [128, 128], F32)
            onesm = sb("onesm", [128, 128], BF16)
            onesr = sb("onesr", [1, 128], F32)
            cosT = sb("cosT", [128, NT, 8], F32)
            sinT = sb("sinT", [128, NT, 8], F32); t_cs = T()
            modc = sb("modc", [128, 48], F32); t_modc = T()
            ab = sb("ab", [128, 4, 8], F32); t_ab = T()
            t_G1 = T(); t_G2 = T()
            oflg = sb("oflg", [128, 8], F32); t_small = T()
            cw = sb("cw", [128, 4, 31], F32)
            cb = sb("cb", [128, 4], F32)
            cg = sb("cg", [128, 4], F32)
            cbn = sb("cbn", [128, 4], F32)
            wst = {"slots": None, "tiles": None, "ptr": 0}

            def walloc(tag):
                wst["slots"] = [sb("wslot%s%d" % (tag, i), [128, 8, 512], BF16) for i in range(2)]
                wst["tiles"] = [T() for _ in range(2)]
                wst["ptr"] = 0

            def wload(src_ap):
                i = wst["ptr"]
                wst["ptr"] = (i + 1) % 2
                dma(pool, wst["slots"][i][:], src_ap, wr=[wst["tiles"][i]])
                return wst["slots"][i], wst["tiles"][i]

            op(pool, lambda e: e.memset(identf[:], 0.0), wr=[t_ident])
            op(pool, lambda e: e.affine_select(out=identf[:], in_=identf[:], pattern=[[-1, 128]],
                                               compare_op=ALU.not_equal, fill=1.0, base=0,
                                               channel_multiplier=1), rd=[t_ident], wr=[t_ident])
            op(pool, lambda e: e.tensor_copy(out=ident[:], in_=identf[:]), rd=[t_ident], wr=[t_ident])
            op(pool, lambda e: e.tensor_copy(out=ident4[:], in_=identf[:].unsqueeze(1).to_broadcast([128, 4, 128])),
               rd=[t_ident], wr=[t_ident])
            op(pool, lambda e: e.memset(trim[:], 0.0), wr=[t_ident])
            op(pool, lambda e: e.affine_select(out=trim[:], in_=trim[:], pattern=[[-1, 128]],
                                               compare_op=ALU.is_ge, fill=NEG, base=0,
                                               channel_multiplier=1), rd=[t_ident], wr=[t_ident])
            op(pool, lambda e: e.memset(onesm[:], 1.0 / 512.0), wr=[t_ident])
            op(pool, lambda e: e.memset(onesr[:], 1.0), wr=[t_ident])
            dma(sp, oflg[:], oflag, wr=[t_small])
            dma(sp, cw[:], convw, wr=[t_small])
            dma(sp, cb[:], convb, wr=[t_small])
            dma(sp, cg[:], cng, wr=[t_small])
            dma(sp, cbn[:], cnb, wr=[t_small])

            with contextlib.ExitStack() as es2:
                posi = sb("posi", [128, NT], I32)
                posf = sb("posf", [128, NT], F32)
                ivf = sb("ivf", [128, 8], F32)
                ang = sb("ang", [128, NT, 8], F32)
                tq = sb("tq", [128, NT, 8], F32)
                kq = sb("kq", [128, NT, 8], I32)
                kf = sb("kf", [128, NT, 8], F32)
                red = sb("red", [128, NT, 8], F32)
                t_p0 = T()
                cTs = sb("cTs", [128, 8], F32)
                cond = sb("cond", [128, 8], F32); t_cond = T()
                badc = sb("badc", [128, 48], F32)
                gmc = sb("gmc", [128, 2, 8], F32)
                rowb = sb("rowb", [1, 6144], F32)
                rows2 = [sb("rows%d" % i, [1, 512], F32) for i in range(2)]; t_rows2 = [T(), T()]
                Gtmp = sb("Gtmp", [128, 512], F32); t_Gtmp = T()
                wfs = [sb("wf%d" % i, [128, 8, 512], F32) for i in range(3)]; t_wfs = [T() for _ in range(3)]
                dma(sp, posi[:], posp, wr=[t_p0])
                dma(sp, ivf[:], invf, wr=[t_p0])
                dma(sp, cTs[:], cT, wr=[t_cond])
                dma(sp, badc[:], badac, wr=[t_cond])
                dma(sp, gmc[:, 0, :], gmixc, wr=[t_cond])
                dma(sp, gmc[:, 1, :], gmlpc, wr=[t_cond])
                dma(sp, rowb[:], badar, wr=[t_cond])
                rw = dict(rd=[t_p0], wr=[t_p0])
                op(dve, lambda e: e.tensor_copy(out=posf[:], in_=posi[:]), **rw)
                op(dve, lambda e: e.tensor_tensor(out=ang[:], in0=posf[:].unsqueeze(2).to_broadcast([128, NT, 8]),
                                                  in1=ivf[:].unsqueeze(1).to_broadcast([128, NT, 8]), op=ALU.mult), **rw)
                TWO_PI = 2.0 * np.pi
                C1 = 6.28125
                C2 = TWO_PI - C1

                def reduce_to(dst, shift):
                    op(dve, lambda e: e.tensor_scalar(out=tq[:], in0=ang[:], scalar1=shift, scalar2=1.0 / TWO_PI,
                                                      op0=ALU.add, op1=ALU.mult), **rw)
                    op(dve, lambda e: e.tensor_copy(out=kq[:], in_=tq[:]), **rw)
                    op(dve, lambda e: e.tensor_copy(out=kf[:], in_=kq[:]), **rw)
                    op(dve, lambda e: e.scalar_tensor_tensor(out=red[:], in0=kf[:], scalar=-C1, in1=ang[:],
                                                             op0=ALU.mult, op1=ALU.add), **rw)
                    op(dve, lambda e: e.scalar_tensor_tensor(out=red[:], in0=kf[:], scalar=-C2, in1=red[:],
                                                             op0=ALU.mult, op1=ALU.add), **rw)
                    op(dve, lambda e: e.tensor_scalar(out=red[:], in0=red[:], scalar1=shift, scalar2=None,
                                                      op0=ALU.add), **rw)
                    op(dve, lambda e: e.tensor_scalar(out=tq[:], in0=red[:], scalar1=np.pi, scalar2=-TWO_PI,
                                                      op0=ALU.is_gt, op1=ALU.mult), **rw)
                    op(dve, lambda e: e.tensor_tensor(out=red[:], in0=red[:], in1=tq[:], op=ALU.add), **rw)
                    op(dve, lambda e: e.tensor_scalar(out=tq[:], in0=red[:], scalar1=-np.pi, scalar2=TWO_PI,
                                                      op0=ALU.is_lt, op1=ALU.mult), **rw)
                    op(dve, lambda e: e.tensor_tensor(out=red[:], in0=red[:], in1=tq[:], op=ALU.add), **rw)
                    op(dve, lambda e: e.tensor_scalar(out=red[:], in0=red[:], scalar1=-3.1415925, scalar2=3.1415925,
                                                      op0=ALU.max, op1=ALU.min), **rw)
                    op(act, lambda e: e.activation(out=dst[:], in_=red[:], func=AF.Sin), rd=[t_p0], wr=[t_cs])

                reduce_to(sinT, 0.0)
                reduce_to(cosT, np.pi / 2.0)

                op(act, lambda e: e.activation(out=cond[:], in_=cTs[:], func=AF.Silu), rd=[t_cond], wr=[t_cond])
                for cc in range(12):
                    ws, tw = wfs[cc % 3], t_wfs[cc % 3]
                    dma(sp, ws[:], wview(w_ada, cc * 512, (cc + 1) * 512), wr=[tw])
                    rb_, trb_ = rows2[cc % 2], t_rows2[cc % 2]
                    pbk = 1 + cc % 2
                    for k in range(8):
                        op(pe, lambda e, k=k: e.matmul(ps[0:1, pbk, :], lhsT=cond[:, k:k + 1], rhs=ws[:, k, :],
                                                       start=(k == 0), stop=(k == 7)),
                           rd=[t_cond, tw], wr=[PB[pbk]])
                    op(dve, lambda e: e.tensor_tensor(out=rb_[:], in0=ps[0:1, pbk, :], in1=rowb[:, cc * 512:(cc + 1) * 512],
                                                      op=ALU.add), rd=[PB[pbk], t_cond], wr=[trb_])
                    if cc in (4, 5, 10, 11):
                        op(pe, lambda e: e.matmul(ps[:, 3, :], lhsT=onesr[:], rhs=rb_[:], start=True, stop=True),
                           rd=[trb_, t_ident], wr=[PB[3]])
                        Gs, tG = (g1s, t_G1) if cc < 6 else (g2s, t_G2)
                        go = (cc - 4) * 512 if cc < 6 else (cc - 10) * 512
                        op(act, lambda e: e.activation(out=Gtmp[:], in_=ps[:, 3, :], func=AF.Copy),
                           rd=[PB[3]], wr=[t_Gtmp])
                        dma(sp, Gs[:, go:go + 512], Gtmp[:], rd=[t_Gtmp], wr=[tG])
                    else:
                        for el in range(4):
                            et = cc * 4 + el
                            op(pe, lambda e, el=el, et=et: e.matmul(ps[:, 0, et:et + 1], lhsT=rb_[0:1, el * 128:(el + 1) * 128],
                                                                   rhs=onesr[0:1, 0:1], start=True, stop=True, skip_group_check=True),
                               rd=[trb_, t_ident], wr=[PB[0]])
                op(dve, lambda e: e.memset(modc[:], 0.0), wr=[t_modc])
                for lo_, hi_ in ((0, 16), (24, 40)):
                    op(dve, lambda e: e.tensor_copy(out=modc[:, lo_:hi_], in_=ps[:, 0, lo_:hi_]),
                       rd=[PB[0]], wr=[t_modc])
                op(dve, lambda e: e.scalar_tensor_tensor(out=ab[:, 0, :], in0=modc[:, 8:16], scalar=1.0, in1=gmc[:, 0, :],
                                                         op0=ALU.add, op1=ALU.mult), rd=[t_modc, t_cond], wr=[t_ab])
                op(dve, lambda e: e.tensor_copy(out=ab[:, 1, :], in_=modc[:, 0:8]), rd=[t_modc], wr=[t_ab])
                op(dve, lambda e: e.scalar_tensor_tensor(out=ab[:, 2, :], in0=modc[:, 32:40], scalar=1.0, in1=gmc[:, 1, :],
                                                         op0=ALU.add, op1=ALU.mult), rd=[t_modc, t_cond], wr=[t_ab])
                op(dve, lambda e: e.tensor_copy(out=ab[:, 3, :], in_=modc[:, 24:32]), rd=[t_modc], wr=[t_ab])
                dump("cosT", cosT[:], [t_cs], kb)
                dump("sinT", sinT[:], [t_cs], kb)
                dump("ab", ab[:], [t_ab], kb)
                dump("modc", modc[:], [t_modc], kb)
                kb.barrier()
                stop_if("p0", kb)

            def norm_tile(row0, xt, t_xt, xn, t_xn, sq, t_sq, st, t_st, src=None):
                dma(sp, xt[:], (xp if src is None else src)[row0:row0 + 128, :], wr=[t_xt])
                op(act, lambda e: e.activation(out=xn[:], in_=xt[:], func=AF.Square, accum_out=st[:, 0:1]),
                   rd=[t_xt], wr=[t_xn, t_st])
                op(act, lambda e: e.activation(out=st[:, 1:2], in_=st[:, 0:1], func=AF.Sqrt, bias=EPS, scale=1.0 / D),
                   rd=[t_st], wr=[t_st])
                op(dve, lambda e: e.reciprocal(out=st[:, 2:3], in_=st[:, 1:2]), rd=[t_st], wr=[t_st])
                op(act, lambda e: e.activation(out=xn[:], in_=xt[:], func=AF.Copy, scale=st[:, 2:3]),
                   rd=[t_xt, t_st], wr=[t_xn])

            def transpose_mod(xn, t_xn, bank, hT_dst, t_hT, abi):
                pv = psb16(bank)
                for k in range(8):
                    op(pe, lambda e, k=k: e.transpose(out=pv[:, k * 128:(k + 1) * 128], in_=xn[:, k * 128:(k + 1) * 128],
                                                      identity=ident[:]), rd=[t_xn, t_ident], wr=[PB[bank]])
                pv3 = pv.rearrange("p (k t) -> p k t", k=8)
                op(dve, lambda e: e.tensor_tensor(out=hT_dst, in0=pv3,
                                                  in1=ab[:, abi, :].unsqueeze(2).to_broadcast([128, 8, 128]), op=ALU.mult),
                   rd=[PB[bank], t_ab], wr=[t_hT])
                op(pool, lambda e: e.tensor_tensor(out=hT_dst, in0=hT_dst,
                                                   in1=ab[:, abi + 1, :].unsqueeze(2).to_broadcast([128, 8, 128]), op=ALU.add),
                   rd=[t_hT, t_ab], wr=[t_hT])

            with contextlib.ExitStack() as es2:
                kT = sb("kT", [128, S], BF16); t_kT = T()
                kiT = sb("kiT", [128, S], BF16); t_kiT = T()
                Vaug = sb("Vaug", [128, 64, 2, 65], BF16); t_V = T()
                W1 = sb("W1", [128, 8, 328], BF16); t_W1 = T()
                xts = [sb("xt%d" % i, [128, D], F32) for i in range(2)]; t_xts = [T(), T()]
                xns = [sb("xn%d" % i, [128, D], BF16) for i in range(2)]; t_xns = [T(), T()]
                sqj = None; t_sqj = None
                walloc("b")
                G1 = sb("G1", [128, D], F32)
                dma(sp, G1[:], g1s, rd=[t_G1], wr=[t_G1])
                sts = [sb("st%d" % i, [128, 4], F32) for i in range(2)]; t_sts = [T(), T()]
                hTc = sb("hTc", [128, 8, 512], BF16); t_hTc = [T() for _ in range(4)]
                rtmp = sb("rtmp", [128, 4, 16, 8], F32); t_rtmp = T()
                krot = [sb("krot%d" % i, [128, 256], BF16) for i in range(2)]; t_krot = [T(), T()]

                op(pool, lambda e: e.memset(Vaug[:], 1.0), wr=[t_V])
                dma(pool, W1[:, :, 0:128], wview(w_in, 512, 640), wr=[t_W1])
                dma(pool, W1[:, :, 128:192], wview(w_in, 1280, 1344), wr=[t_W1])
                dma(pool, W1[:, :, 192:320], wview(w_in, 640, 768), wr=[t_W1])
                dma(pool, W1[:, :, 320:328], wview(w_in, 1344, 1352), wr=[t_W1])

                def rope(src3, dst3, nh, ti, tsrc, tdst):
                    cs = cosT[:, ti, :].unsqueeze(1).to_broadcast([128, nh, 8])
                    sn = sinT[:, ti, :].unsqueeze(1).to_broadcast([128, nh, 8])
                    x1, x2 = src3[:, :, 0:8], src3[:, :, 8:16]
                    t1, t2, t3, t4 = (rtmp[:, i, 0:nh, :] for i in range(4))
                    op(dve, lambda e: e.tensor_tensor(out=t1, in0=x1, in1=cs, op=ALU.mult), rd=[tsrc, t_cs], wr=[t_rtmp])
                    op(dve, lambda e: e.tensor_tensor(out=t2, in0=x2, in1=sn, op=ALU.mult), rd=[tsrc, t_cs], wr=[t_rtmp])
                    op(dve, lambda e: e.tensor_tensor(out=t3, in0=x2, in1=cs, op=ALU.mult), rd=[tsrc, t_cs], wr=[t_rtmp])
                    op(dve, lambda e: e.tensor_tensor(out=t4, in0=x1, in1=sn, op=ALU.mult), rd=[tsrc, t_cs], wr=[t_rtmp])
                    op(dve, lambda e: e.tensor_tensor(out=dst3[:, :, 0:8], in0=t1, in1=t2, op=ALU.subtract),
                       rd=[t_rtmp], wr=[tdst])
                    op(dve, lambda e: e.tensor_tensor(out=dst3[:, :, 8:16], in0=t3, in1=t4, op=ALU.add),
                       rd=[t_rtmp], wr=[tdst])
                    op(act, lambda e: e.activation(out=dst3[:, :, 16:64], in_=src3[:, :, 16:64], func=AF.Copy),
                       rd=[tsrc], wr=[tdst])

                def rope4(src4, dst4, ti, tsrc, tdst):
                    cs = cosT[:, ti, :].unsqueeze(1).unsqueeze(1).to_broadcast([128, 2, 4, 8])
                    sn = sinT[:, ti, :].unsqueeze(1).unsqueeze(1).to_broadcast([128, 2, 4, 8])
                    x1, x2 = src4[:, :, :, 0:8], src4[:, :, :, 8:16]
                    t1, t2, t3, t4 = (rtmp[:, i, 0:8, :].rearrange("p (g b) d -> p g b d", g=2) for i in range(4))
                    op(dve, lambda e: e.tensor_tensor(out=t1, in0=x1, in1=cs, op=ALU.mult), rd=[tsrc, t_cs], wr=[t_rtmp])
                    op(dve, lambda e: e.tensor_tensor(out=t2, in0=x2, in1=sn, op=ALU.mult), rd=[tsrc, t_cs], wr=[t_rtmp])
                    op(dve, lambda e: e.tensor_tensor(out=t3, in0=x2, in1=cs, op=ALU.mult), rd=[tsrc, t_cs], wr=[t_rtmp])
                    op(dve, lambda e: e.tensor_tensor(out=t4, in0=x1, in1=sn, op=ALU.mult), rd=[tsrc, t_cs], wr=[t_rtmp])
                    op(dve, lambda e: e.tensor_tensor(out=dst4[:, :, :, 0:8], in0=t1, in1=t2, op=ALU.subtract),
                       rd=[t_rtmp], wr=[tdst])
                    op(dve, lambda e: e.tensor_tensor(out=dst4[:, :, :, 8:16], in0=t3, in1=t4, op=ALU.add),
                       rd=[t_rtmp], wr=[tdst])
                    op(act, lambda e: e.activation(out=dst4[:, :, :, 16:64], in_=src4[:, :, :, 16:64], func=AF.Copy),
                       rd=[tsrc], wr=[tdst])

                def ph1_S1(ti):
                    s2 = ti % 2
                    norm_tile(ti * 128, xts[s2], t_xts[s2], xns[s2], t_xns[s2], sqj, t_sqj, sts[s2], t_sts[s2])
                    hs = ti % 4
                    hdst = hTc[:, :, hs * 128:(hs + 1) * 128]
                    transpose_mod(xns[s2], t_xns[s2], 6 + s2, hdst, t_hTc[hs], 0)

                def ph1_S2(ti):
                    s2 = ti % 2
                    hs = ti % 4
                    bk = s2
                    for k in range(8):
                        op(pe, lambda e, k=k: e.matmul(ps[:, bk, 0:320], lhsT=hTc[:, k, hs * 128:(hs + 1) * 128],
                                                       rhs=W1[:, k, 0:320], start=(k == 0), stop=(k == 7)),
                           rd=[t_hTc[hs], t_W1], wr=[PB[bk]])
                    kr = krot[s2]
                    rope(ps[:, bk, 0:192].rearrange("p (h d) -> p h d", d=64),
                         kr[:, 0:192].rearrange("p (h d) -> p h d", d=64), 3, ti, PB[bk], t_krot[s2])
                    op(pool, lambda e: e.tensor_copy(out=kr[:, 192:256], in_=kr[:, 128:192]), rd=[t_krot[s2]], wr=[t_krot[s2]])
                    op(act, lambda e: e.activation(out=Vaug[:, ti, :, 0:64],
                                                   in_=ps[:, bk, 192:320].rearrange("p (g d) -> p g d", d=64), func=AF.Copy),
                       rd=[PB[bk]], wr=[t_V])
                    tb = 4 + s2
                    pv = psb16(tb)
                    op(pe, lambda e: e.transpose(out=pv[:, 0:128], in_=kr[:, 0:128], identity=ident[:]),
                       rd=[t_krot[s2], t_ident], wr=[PB[tb]])
                    op(pe, lambda e: e.transpose(out=pv[:, 128:256], in_=kr[:, 128:256], identity=ident[:]),
                       rd=[t_krot[s2], t_ident], wr=[PB[tb]])
                    op(act, lambda e: e.activation(out=kT[:, ti * 128:(ti + 1) * 128], in_=pv[:, 0:128], func=AF.Copy),
                       rd=[PB[tb]], wr=[t_kT])
                    op(dve, lambda e: e.tensor_copy(out=kiT[:, ti * 128:(ti + 1) * 128], in_=pv[:, 128:256]),
                       rd=[PB[tb]], wr=[t_kiT])


                ph1_S1(0)
                for ti in range(nt1):
                    if ti + 1 < nt1:
                        ph1_S1(ti + 1)
                    ph1_S2(ti)
                dump("kT", kT[:], [t_kT], kb)
                dump("kiT", kiT[:], [t_kiT], kb)
                dump("Vaug", Vaug[:], [t_V], kb)
                stop_if("p1", kb)
                SC = sb("SC", [128, S], F32); t_SC = T()
                junk = hTc[:].rearrange("p k t -> p (k t)").bitcast(U8)
                RbA = sb("RbA", [128, 8, 512], BF16)
                Rb = [RbA[:, i, :] for i in range(8)]; t_Rb = [T() for _ in range(8)]
                Dg = sb("Dg", [128, 8, 128], BF16); t_Dg = T()
                qT = sb("qT", [128, 2, 4, 512], BF16); t_qT = T()
                qiT = sb("qiT", [128, 4, 2, 512], BF16); t_qiT = T()
                op(pool, lambda e: e.memset(qT[:], 0.0), wr=[t_qT])
                op(pool, lambda e: e.memset(qiT[:], 0.0), wr=[t_qiT])
                qrot = [sb("qrot%d" % i, [128, 512], BF16) for i in range(2)]; t_qrot = [T(), T()]
                wsc = sb("wsc", [128, 4, 8], F32); t_wsc = T()
                PT = [sb("PT%d" % i, [128, 512], BF16) for i in range(4)]; t_PT = [T() for _ in range(4)]
                MB = sb("MB", [128, S], BF16); t_MB = T()
                junkA = sb("junkA", [128, JA], U8); t_junkA = T()
                bsa = sb("bsa", [128, 2], F32); t_bsa = T()
                bst = sb("bst", [128, 8], F32); t_bst = T()
                gluT = sb("gluT", [128, 4, 544], BF16); t_glu = T()
                gluH = sb("gluH", [128, 4, 256], BF16); t_gluH = T()
                SCb = SC[:].bitcast(BF16)
                ybf = SCb[:, 0:2048].rearrange("p (c t) -> p c t", c=4); t_ybf = t_SC
                ysq = SCb[:, 2048:4096].rearrange("p (c t) -> p c t", c=4); t_ysq = t_SC
                lnA = SC[:, 2048:2560]; t_lnA = t_SC
                lnB = SC[:, 2560:3072]; t_lnB = t_SC
                zn = SC[:, 3072:3584]; t_zn = t_SC
                sig = SC[:, 3584:4096]; t_sig = t_SC
                cdiag = RbA[:].rearrange("p a b -> p (a b)")[:, 0:3968].rearrange("p (k c) -> p k c", c=128)
                mixT = sb("mixT", [128, 8, 512], BF16); t_mixT = T()
                attn = sb("attn", [128, 512], BF16); t_attn = T()
                rs4 = sb("rs4", [128, 8], F32); t_rs4 = T()
                x1t = SC[:, 4096:5120]; t_x1t = t_SC
                hmB = sb("hmB", [128, 256], BF16)
                hm = sb("hm", [128, 256], F32)
                dma(sp, hm[:], hmask, wr=[t_small])
                op(pool, lambda e: e.tensor_copy(out=hmB[:], in_=hm[:]), rd=[t_small], wr=[t_small])

                def conv_glu(ws_a, tw_a, ws_g, tw_g, ncols, ct, dst, tdst, hcols, t_h):
                    for k in range(8):
                        op(pe, lambda e, k=k: e.matmul(ps[:, 0, 0:ncols], lhsT=ws_a[:, k, ct * 128:(ct + 1) * 128],
                                                       rhs=hTc[:, k, hcols], start=(k == 0), stop=(k == 7)),
                           rd=t_h + [tw_a], wr=[PB[0]])
                    for k in range(8):
                        op(pe, lambda e, k=k: e.matmul(ps[:, 1, 0:ncols], lhsT=ws_g[:, k, ct * 128:(ct + 1) * 128],
                                                       rhs=hTc[:, k, hcols], start=(k == 0), stop=(k == 7)),
                           rd=t_h + [tw_g], wr=[PB[1]])
                    op(act, lambda e: e.activation(out=sig[:, 0:ncols], in_=ps[:, 1, 0:ncols], func=AF.Sigmoid),
                       rd=[PB[1]], wr=[t_sig])
                    op(dve, lambda e: e.tensor_tensor(out=dst, in0=ps[:, 0, 0:ncols], in1=sig[:, 0:ncols], op=ALU.mult),
                       rd=[PB[0], t_sig], wr=[tdst])

                for hi in range(2):
                    norm_tile((64 + hi) * 128, xts[hi], t_xts[hi], xns[hi], t_xns[hi], sqj, t_sqj, sts[hi], t_sts[hi])
                    transpose_mod(xns[hi], t_xns[hi], 6 + hi, hTc[:, :, hi * 128:(hi + 1) * 128], t_hTc[hi], 0)
                wa, twa = wload(wview(w_in, 1352, 1864))
                wg, twg = wload(wview(w_in, 1864, 2376))
                for ct in range(4):
                    conv_glu(wa, twa, wg, twg, 256, ct, gluH[:, ct, :], t_gluH, slice(0, 256), [t_hTc[0], t_hTc[1]])
                    op(pool, lambda e, ct=ct: e.tensor_tensor(out=gluH[:, ct, :], in0=gluH[:, ct, :], in1=hmB[:], op=ALU.mult),
                       rd=[t_gluH, t_small], wr=[t_gluH])

                dump("gluH", gluH[:], [t_gluH], kb)
                stop_if("p2h", kb)
                for j in range(nchunks):
                    tile0 = (2 * j + 1) * 4
                    for t4 in range(4):
                        s2 = t4 % 2
                        norm_tile((tile0 + t4) * 128, xts[s2], t_xts[s2], xns[s2], t_xns[s2], sqj, t_sqj, sts[s2], t_sts[s2])
                        transpose_mod(xns[s2], t_xns[s2], 6 + s2, hTc[:, :, t4 * 128:(t4 + 1) * 128], t_hTc[t4], 0)
                    stop_if("p2n", kb)
                    for grp in range(2):
                        c0 = 0 if grp == 0 else 768
                        wq, twq = wload(wview(w_in, c0, c0 + 512))
                        stop_if("p2w", kb)
                        dstT, t_dstT = (qT, t_qT) if grp == 0 else (qiT, t_qiT)
                        for t4 in range(4):
                            bk = t4 % 2
                            for k in range(8):
                                op(pe, lambda e, k=k: e.matmul(ps[:, bk, :], lhsT=hTc[:, k, t4 * 128:(t4 + 1) * 128],
                                                               rhs=wq[:, k, :], start=(k == 0), stop=(k == 7)),
                                   rd=[t_hTc[t4], twq], wr=[PB[bk]])
                            if grp == 0 and t4 == 0:
                                stop_if("p2m", kb)
                            qr = qrot[bk]
                            src4 = ps[:, bk, :].rearrange("p (g b d) -> p g b d", g=2, b=4)
                            dst4 = qr[:].rearrange("p (b g d) -> p g b d", g=2, b=4)
                            rope4(src4, dst4, tile0 + t4, PB[bk], t_qrot[bk])
                            if grp == 0 and t4 == 0:
                                dump("qr", qr[:], [t_qrot[bk]], kb)
                                stop_if("p2a0", kb)
                            tb = 4 + bk
                            pv = psb16(tb)
                            for b in range(4):
                                op(pe, lambda e, b=b: e.transpose(out=pv[:, b * 128:(b + 1) * 128],
                                                                  in_=qr[:, b * 128:(b + 1) * 128], identity=ident[:]),
                                   rd=[t_qrot[bk], t_ident], wr=[PB[tb]])
                            pv4 = pv[:, 0:512].rearrange("p (b t) -> p b t", b=4)
                            tcols = slice(t4 * 128, (t4 + 1) * 128)
                            if grp == 0:
                                d0, d1 = qT[0:64, 0, :, tcols], qT[64:128, 1, :, tcols]
                            else:
                                d0, d1 = qiT[0:64, :, 0, tcols], qiT[64:128, :, 1, tcols]
                            op(act, lambda e: e.activation(out=d0, in_=pv4[0:64], func=AF.Copy), rd=[PB[tb]], wr=[t_dstT])
                            op(dve, lambda e: e.tensor_copy(out=d1, in_=pv4[64:128]), rd=[PB[tb]], wr=[t_dstT])
                            if grp == 0 and t4 == 0:
                                stop_if("p2a1", kb)
                            if grp == 1 and t4 == 0:
                                stop_if("p2a2", kb)
                            if grp == 1:
                                for k in range(8):
                                    op(pe, lambda e, k=k: e.matmul(ps[:, 2, 0:8], lhsT=hTc[:, k, t4 * 128:(t4 + 1) * 128],
                                                                   rhs=W1[:, k, 320:328], start=(k == 0), stop=(k == 7)),
                                       rd=[t_hTc[t4], t_W1], wr=[PB[2]])
                                op(dve, lambda e: e.tensor_scalar(
                                    out=wsc[:, t4, :].rearrange("p (b g) -> p g b", g=2),
                                    in0=ps[:, 2, 0:8].rearrange("p (g b) -> p g b", g=2),
                                    scalar1=float(8 ** -0.5 * 64 ** -0.5), scalar2=None, op0=ALU.mult),
                                   rd=[PB[2]], wr=[t_wsc])
                    if j == nchunks - 1:
                        dump("qT", qT[:].rearrange("p g b t -> p (g b) t"), [t_qT], kb)
                        dump("qiT", qiT[:].rearrange("p b g t -> p (b g) t"), [t_qiT], kb)
                        dump("wsc", wsc[:], [t_wsc], kb)
                        stop_if("p2a", kb)
                    wa, twa = wload(wview(w_in, 1352, 1864))
                    wg, twg = wload(wview(w_in, 1864, 2376))
                    for ct in range(4):
                        op(pool, lambda e, ct=ct: e.tensor_copy(out=gluT[:, ct, 0:32], in_=gluH[:, ct, j * 32:(j + 1) * 32]),
                           rd=[t_gluH], wr=[t_glu])
                        conv_glu(wa, twa, wg, twg, 512, ct, gluT[:, ct, 32:544], t_glu, slice(0, 512), t_hTc)
                    for ct in range(4):
                        op(pool, lambda e, ct=ct: e.tensor_tensor(
                            out=cdiag, in0=ident[:].unsqueeze(1).to_broadcast([128, 31, 128]),
                            in1=cw[:, ct, :].unsqueeze(2).to_broadcast([128, 31, 128]), op=ALU.mult),
                           rd=[t_ident, t_small], wr=t_Rb)
                        cbk = ct
                        for tap in range(31):
                            op(pe, lambda e, tap=tap, ct=ct: e.matmul(ps[:, cbk, :], lhsT=cdiag[:, tap, :],
                                                                     rhs=gluT[:, ct, tap + 2:tap + 514],
                                                                     start=(tap == 0), stop=(tap == 30)),
                               rd=t_Rb + [t_glu], wr=[PB[cbk]])
                        op(act, lambda e, ct=ct: e.activation(out=ybf[:, ct, :], in_=ps[:, cbk, :], func=AF.Identity,
                                                              bias=cb[:, ct:ct + 1], scale=1.0), rd=[PB[cbk], t_small], wr=[t_ybf])
                        op(act, lambda e, ct=ct: e.activation(out=ysq[:, ct, :], in_=ps[:, cbk, :], func=AF.Square,
                                                              bias=cb[:, ct:ct + 1], scale=1.0), rd=[PB[cbk], t_small], wr=[t_ysq])
                    for ct in range(4):
                        op(pe, lambda e, ct=ct: e.matmul(ps[:, 4, :], lhsT=onesm[:], rhs=ybf[:, ct, :],
                                                         start=(ct == 0), stop=(ct == 3)), rd=[t_ybf, t_ident], wr=[PB[4]])
                    for ct in range(4):
                        op(pe, lambda e, ct=ct: e.matmul(ps[:, 5, :], lhsT=onesm[:], rhs=ysq[:, ct, :],
                                                         start=(ct == 0), stop=(ct == 3)), rd=[t_ysq, t_ident], wr=[PB[5]])
                    op(act, lambda e: e.activation(out=lnA, in_=ps[:, 4, :], func=AF.Copy), rd=[PB[4]], wr=[t_lnA])
                    op(dve, lambda e: e.tensor_tensor(out=lnB, in0=lnA, in1=lnA, op=ALU.mult), rd=[t_lnA], wr=[t_lnB])
                    op(dve, lambda e: e.tensor_tensor(out=lnB, in0=ps[:, 5, :], in1=lnB, op=ALU.subtract),
                       rd=[PB[5], t_lnB], wr=[t_lnB])
                    op(dve, lambda e: e.tensor_scalar(out=lnB, in0=lnB, scalar1=0.0, scalar2=EPS, op0=ALU.max, op1=ALU.add),
                       rd=[t_lnB], wr=[t_lnB])
                    op(act, lambda e: e.activation(out=lnB, in_=lnB, func=AF.Sqrt), rd=[t_lnB], wr=[t_lnB])
                    op(dve, lambda e: e.reciprocal(out=lnB, in_=lnB), rd=[t_lnB], wr=[t_lnB])
                    for ct in range(4):
                        op(dve, lambda e, ct=ct: e.scalar_tensor_tensor(out=zn, in0=ps[:, ct, :], scalar=cb[:, ct:ct + 1],
                                                                        in1=lnA, op0=ALU.add, op1=ALU.subtract),
                           rd=[PB[ct], t_small, t_lnA], wr=[t_zn])
                        op(dve, lambda e: e.tensor_tensor(out=zn, in0=zn, in1=lnB, op=ALU.mult),
                           rd=[t_zn, t_lnB], wr=[t_zn])
                        op(act, lambda e, ct=ct: e.activation(out=mixT[:, 4 + ct, :], in_=zn, func=AF.Silu,
                                                              bias=cbn[:, ct:ct + 1], scale=cg[:, ct:ct + 1]),
                           rd=[t_zn, t_small], wr=[t_mixT])

                    if j == nchunks - 1:
                        dump("mixTc", mixT[:, 4:8, :], [t_mixT], kb)
                        stop_if("p2b", kb)
                    def qgeom(qi):
                        segs = [(c * 512, 512) for c in range(2 * j + 1)] + [((2 * j + 1) * 512, (qi + 1) * 128)]
                        nkeys = (2 * j + 1) * 512 + (qi + 1) * 128
                        return segs, nkeys, slice(qi * 128, (qi + 1) * 128)

                    def stage_A(qi):
                        segs, nkeys, qcols = qgeom(qi)
                        op(pool, lambda e: e.tensor_tensor(
                            out=Dg[:], in0=ident[:].unsqueeze(1).to_broadcast([128, 8, 128]),
                            in1=wsc[:, qi, :].unsqueeze(2).to_broadcast([128, 8, 128]), op=ALU.mult),
                           rd=[t_ident, t_wsc], wr=[t_Dg])
                        units = [(si, h) for si in range(len(segs)) for h in range(8)]
                        U = len(units)

                        def emit_L(u):
                            si, h = units[u]
                            c0, n = segs[si]
                            b, g = h // 2, h % 2
                            bk = u % 4
                            op(pe, lambda e: e.matmul(ps[:, bk, 0:n], lhsT=qiT[:, b, g, qcols],
                                                      rhs=kiT[:, c0:c0 + n], start=True, stop=True),
                               rd=[t_qiT, t_kiT], wr=[PB[bk]])
                            r = Rb[u % 8]
                            if u % 2 == 0:
                                op(act, lambda e: e.activation(out=r[:, 0:n], in_=ps[:, bk, 0:n], func=AF.Relu),
                                   rd=[PB[bk]], wr=[t_Rb[u % 8]])
                            else:
                                op(dve, lambda e: e.tensor_scalar(out=r[:, 0:n], in0=ps[:, bk, 0:n], scalar1=0.0, scalar2=None,
                                                                  op0=ALU.max), rd=[PB[bk]], wr=[t_Rb[u % 8]])

                        def emit_D(u):
                            si, h = units[u]
                            c0, n = segs[si]
                            sbk = 4 + (si % 2)
                            op(pe, lambda e: e.matmul(ps[:, sbk, 0:n], lhsT=Dg[:, h, :], rhs=Rb[u % 8][:, 0:n],
                                                      start=(h == 0), stop=(h == 7)),
                               rd=[t_Dg, t_Rb[u % 8]], wr=[PB[sbk]])
                            if h == 7:
                                if si == 2 * j:
                                    op(act, lambda e: e.activation(out=SC[:, c0:c0 + n], in_=ps[:, sbk, 0:n], func=AF.Identity,
                                                                   bias=oflg[:, j:j + 1], scale=1.0),
                                       rd=[PB[sbk], t_small], wr=[t_SC])
                                elif si == 2 * j + 1:
                                    if n > 128:
                                        op(act, lambda e: e.activation(out=SC[:, c0:c0 + n - 128], in_=ps[:, sbk, 0:n - 128],
                                                                       func=AF.Copy), rd=[PB[sbk]], wr=[t_SC])
                                    op(dve, lambda e: e.tensor_tensor(out=SC[:, c0 + n - 128:c0 + n], in0=ps[:, sbk, n - 128:n],
                                                                      in1=trim[:], op=ALU.add), rd=[PB[sbk], t_ident], wr=[t_SC])
                                else:
                                    op(act, lambda e: e.activation(out=SC[:, c0:c0 + n], in_=ps[:, sbk, 0:n], func=AF.Copy),
                                       rd=[PB[sbk]], wr=[t_SC])

                        for u in range(U + 4):
                            if u < U:
                                emit_L(u)
                            if u >= 4:
                                emit_D(u - 4)

                    def stage_B(qi, frac):
                        segs, nkeys, qcols = qgeom(qi)
                        na = min(int(frac * nkeys) // 128 * 128, JA)
                        scv = SC[:, 0:nkeys]
                        br = 12.0 if j == 0 else BR
                        nbis = 12 if j == 0 else NBIS
                        op(dve, lambda e: e.tensor_reduce(out=bst[:, 0:1], in_=scv, axis=AX.X, op=ALU.max), rd=[t_SC], wr=[t_bst])
                        op(dve, lambda e: e.tensor_scalar(out=bst[:, 1:2], in0=bst[:, 0:1], scalar1=-br / 2, scalar2=None,
                                                          op0=ALU.add), rd=[t_bst], wr=[t_bst])
                        for it in range(nbis):
                            if na > 0:
                                op(act, lambda e: e.activation(out=junkA[:, 0:na], in_=SC[:, 0:na], func=AF.Sign,
                                                               bias=bst[:, 1:2], scale=-1.0, accum_out=bsa[:, 0:1]),
                                   rd=[t_SC, t_bst], wr=[t_junkA, t_bsa])
                            op(dve, lambda e: e.tensor_scalar(out=junk[:, na:nkeys], in0=SC[:, na:nkeys], scalar1=bst[:, 1:2],
                                                              scalar2=None, op0=ALU.is_gt, op1=ALU.add, accum_out=bst[:, 2:3]),
                               rd=[t_SC, t_bst], wr=t_hTc + [t_bst])
                            if na > 0:
                                op(dve, lambda e: e.scalar_tensor_tensor(out=bst[:, 2:3], in0=bsa[:, 0:1], scalar=-0.5,
                                                                         in1=bst[:, 2:3], op0=ALU.mult, op1=ALU.add),
                                   rd=[t_bsa, t_bst], wr=[t_bst])
                            last = (it == nbis - 1)
                            cn = (br / 2) / (2 ** it) if last else (br / 2) / (2 ** (it + 1))
                            op(dve, lambda e: e.tensor_scalar(out=bst[:, 3:4], in0=bst[:, 2:3], scalar1=255.5 - na / 2.0,
                                                              scalar2=(cn if last else 2.0 * cn), op0=ALU.is_gt, op1=ALU.mult),
                               rd=[t_bst], wr=[t_bst])
                            op(dve, lambda e: e.scalar_tensor_tensor(out=bst[:, 1:2], in0=bst[:, 3:4], scalar=-cn,
                                                                     in1=bst[:, 1:2], op0=ALU.add, op1=ALU.add),
                               rd=[t_bst], wr=[t_bst])
                            yield
                        if j == nchunks - 1 and qi == 3:
                            dump("SC", SC[:, 0:nkeys], [t_SC], kb)
                            dump("bst", bst[:, 0:4], [t_bst], kb)
                            stop_if("p2c", kb)
                        op(dve, lambda e: e.tensor_scalar(out=MB[:, 0:nkeys], in0=scv, scalar1=bst[:, 1:2], scalar2=NEG,
                                                          op0=ALU.is_le, op1=ALU.mult), rd=[t_SC, t_bst], wr=[t_MB])

                    def stage_C_main(qi):
                        segs, nkeys, qcols = qgeom(qi)
                        nsb = nkeys // 128
                        U = nsb * 2
                        LAG = 2

                        def emit_S(u):
                            sbi, g = u // 2, u % 2
                            bk = u % 4
                            op(pe, lambda e: e.matmul(ps[:, bk, :], lhsT=kT[:, sbi * 128:(sbi + 1) * 128],
                                                      rhs=qT[:, g, :, qcols], start=True, stop=False),
                               rd=[t_kT, t_qT], wr=[PB[bk]])
                            op(pe, lambda e: e.matmul(ps[:, bk, :], lhsT=MB[:, sbi * 128:(sbi + 1) * 128],
                                                      rhs=ident4[:], start=False, stop=True),
                               rd=[t_MB, t_ident], wr=[PB[bk]])
                            op(act, lambda e: e.activation(out=PT[u % 4][:], in_=ps[:, bk, :], func=AF.Exp, scale=0.125),
                               rd=[PB[bk]], wr=[t_PT[u % 4]])

                        def emit_V(u):
                            sbi, g = u // 2, u % 2
                            pt = PT[u % 4]
                            ob = 4 + g
                            for b in range(4):
                                op(pe, lambda e, b=b: e.matmul(ps[:, ob, b * 65:(b + 1) * 65], lhsT=pt[:, b * 128:(b + 1) * 128],
                                                               rhs=Vaug[:, sbi, g, :], start=(sbi == 0 and b == 0),
                                                               stop=(sbi == nsb - 1 and b == 3), skip_group_check=True),
                                   rd=[t_PT[u % 4], t_V], wr=[PB[ob]])

                        for u in range(U + LAG):
                            if u < U:
                                emit_S(u)
                            if u >= LAG:
                                emit_V(u - LAG)
                            yield

                    def stage_C_tail(qi):
                        segs, nkeys, qcols = qgeom(qi)
                        for g in range(2):
                            ov = ps[:, 4 + g, 0:260].rearrange("p (b e) -> p b e", e=65)
                            op(dve, lambda e: e.reciprocal(out=rs4[:, g * 4:(g + 1) * 4].unsqueeze(2), in_=ov[:, :, 64:65]),
                               rd=[PB[4 + g]], wr=[t_rs4])
                            op(dve, lambda e: e.tensor_tensor(
                                out=attn[:, g * 256:(g + 1) * 256].rearrange("p (b d) -> p b d", d=64), in0=ov[:, :, 0:64],
                                in1=rs4[:, g * 4:(g + 1) * 4].unsqueeze(2).to_broadcast([128, 4, 64]), op=ALU.mult),
                               rd=[PB[4 + g], t_rs4], wr=[t_attn])
                        if j == nchunks - 1 and qi == 3:
                            dump("attn", attn[:], [t_attn], kb)
                            stop_if("p2d", kb)
                        pv = psb16(6 + qi % 2)
                        for f in range(4):
                            op(pe, lambda e, f=f: e.transpose(out=pv[:, f * 128:(f + 1) * 128], in_=attn[:, f * 128:(f + 1) * 128],
                                                              identity=ident[:]), rd=[t_attn, t_ident], wr=[PB[6 + qi % 2]])
                        op(act, lambda e: e.activation(out=mixT[:, 0:4, qcols], in_=pv[:, 0:512].rearrange("p (f t) -> p f t", f=4),
                                                       func=AF.Copy), rd=[PB[6 + qi % 2]], wr=[t_mixT])

                    def interleave(gb, gc, nb):
                        csteps = list(range(gc[1]))
                        per = (len(csteps) + nb - 1) // nb if nb else 0
                        gcg, gbg = gc[0], gb
                        for it in range(nb):
                            next(gbg, None)
                            for _ in range(per):
                                next(gcg, None)
                        for _ in gbg:
                            pass
                        for _ in gcg:
                            pass

                    def csteps_of(qi):
                        return (qgeom(qi)[1] // 128) * 2 + 2

                    stage_A(0)
                    for _ in stage_B(0, 0.55):
                        pass
                    for qi in range(1, 4):
                        stage_A(qi)
                        interleave(stage_B(qi, 0.12), (stage_C_main(qi - 1), csteps_of(qi - 1)), 12 if j == 0 else NBIS)
                        stage_C_tail(qi - 1)
                    for _ in stage_C_main(3):
                        pass
                    stage_C_tail(3)

                    wo0, two0 = wload(wview(w_out, 0, 512))
                    wo1, two1 = wload(wview(w_out, 512, 1024))
                    for t4 in range(4):
                        for half, (wo, two) in enumerate(((wo0, two0), (wo1, two1))):
                            bk = half
                            for f in range(8):
                                op(pe, lambda e, f=f: e.matmul(ps[:, bk, :], lhsT=mixT[:, f, t4 * 128:(t4 + 1) * 128], rhs=wo[:, f, :],
                                                               start=(f == 0), stop=(f == 7)), rd=[t_mixT, two], wr=[PB[bk]])
                        s2 = t4 % 2
                        dma(sp, xts[s2][:], xp[(tile0 + t4) * 128:(tile0 + t4 + 1) * 128, :], wr=[t_xts[s2]])
                        op(dve, lambda e: e.tensor_tensor(out=x1t, in0=ps[:, 0:2, :].rearrange("p a b -> p (a b)"), in1=G1[:],
                                                          op=ALU.mult), rd=[PB[0], PB[1], t_G1], wr=[t_x1t])
                        op(pool, lambda e: e.tensor_tensor(out=x1t, in0=x1t, in1=xts[s2][:], op=ALU.add),
                           rd=[t_x1t, t_xts[s2]], wr=[t_x1t])
                        r0 = (j * 4 + t4) * 128
                        dma(sp, x1s[r0:r0 + 128, :], x1t, rd=[t_x1t])
                kb.barrier()
                stop_if("p2e", kb)

            with contextlib.ExitStack() as es2:
                Wup = sb("Wup", [128, 8, 4096], BF16); t_Wup = T()
                Wdn = sb("Wdn", [128, 32, 1024], BF16); t_Wdn = T()
                GF = sb("GF", [128, D], F32); t_GF = T()
                xts = [sb("m_xt%d" % i, [128, D], F32) for i in range(4)]; t_xts = [T() for _ in range(4)]
                xns = [sb("m_xn%d" % i, [128, D], BF16) for i in range(4)]; t_xns = [T() for _ in range(4)]
                sqj = None; t_sqj = None
                G2 = sb("G2", [128, D], F32)
                dma(sp, G2[:], g2s, rd=[t_G2], wr=[t_G2])
                sts = [sb("m_st%d" % i, [128, 4], F32) for i in range(4)]; t_sts = [T() for _ in range(4)]
                h2T = [sb("h2T%d" % i, [128, 8, 256], BF16) for i in range(2)]; t_h2T = [[T(), T()], [T(), T()]]
                rT = [sb("rT%d" % i, [128, 256], BF16) for i in range(2)]; t_rT = [T(), T()]
                uT = sb("uT", [128, 32, 256], BF16); t_uT = T()
                x2 = sb("x2", [128, D], F32); t_x2 = T()
                oo = x2; t_oo = t_x2
                for c4 in range(8):
                    dma(pool, Wup[:, :, c4 * 512:(c4 + 1) * 512], wview(w_up, c4 * 512, (c4 + 1) * 512), wr=[t_Wup])
                for c4 in range(4):
                    dma(pool, Wdn[:, c4 * 8:(c4 + 1) * 8, :],
                        w_down[c4 * 1024:(c4 + 1) * 1024, :].rearrange("(k p) e -> p k e", p=128), wr=[t_Wdn])
                dma(sp, GF[:], gfb, wr=[t_GF])
                def m_pre_norm(gi):
                    for t2 in range(2):
                        sl = (gi % 2) * 2 + t2
                        r0 = (gi * 2 + t2) * 128
                        norm_tile(r0, xts[sl], t_xts[sl], xns[sl], t_xns[sl], sqj, t_sqj, sts[sl], t_sts[sl], src=x1s)

                def m_pre_T(gi):
                    for t2 in range(2):
                        sl = (gi % 2) * 2 + t2
                        transpose_mod(xns[sl], t_xns[sl], 6 + t2, h2T[gi % 2][:, :, t2 * 128:(t2 + 1) * 128], t_h2T[gi % 2][t2], 2)

                def m_up(gi):
                    hh = h2T[gi % 2]
                    for ff in range(32):
                        bk = ff % 4
                        for k in range(8):
                            op(pe, lambda e, k=k: e.matmul(ps[:, bk, 0:256], lhsT=Wup[:, k, ff * 128:(ff + 1) * 128], rhs=hh[:, k, :],
                                                           start=(k == 0), stop=(k == 7)), rd=[t_Wup] + t_h2T[gi % 2], wr=[PB[bk]])
                        r = rT[ff % 2]
                        op(act, lambda e: e.activation(out=r[:], in_=ps[:, bk, 0:256], func=AF.Relu), rd=[PB[bk]], wr=[t_rT[ff % 2]])
                        op(pool, lambda e, ff=ff: e.tensor_tensor(out=uT[:, ff, :], in0=r[:], in1=r[:], op=ALU.mult),
                           rd=[t_rT[ff % 2]], wr=[t_uT])
                        if ff == 8 and gi + 1 < 16:
                            m_pre_norm(gi + 1)

                def m_down(gi):
                    for t2 in range(2):
                        sl = (gi % 2) * 2 + t2
                        for half in range(2):
                            bk = 4 + half
                            for ff in range(32):
                                op(pe, lambda e, ff=ff: e.matmul(ps[:, bk, :], lhsT=uT[:, ff, t2 * 128:(t2 + 1) * 128],
                                                                 rhs=Wdn[:, ff, half * 512:(half + 1) * 512],
                                                                 start=(ff == 0), stop=(ff == 31)), rd=[t_uT, t_Wdn], wr=[PB[bk]])
                        op(dve, lambda e: e.tensor_tensor(out=x2[:], in0=ps[:, 4:6, :].rearrange("p a b -> p (a b)"), in1=G2[:],
                                                          op=ALU.mult), rd=[PB[4], PB[5], t_G2], wr=[t_x2])
                        op(pool, lambda e: e.tensor_tensor(out=x2[:], in0=x2[:], in1=xts[sl][:], op=ALU.add),
                           rd=[t_x2, t_xts[sl]], wr=[t_x2])
                        st = sts[sl]
                        op(act, lambda e: e.activation(out=xns[sl][:], in_=x2[:], func=AF.Square, accum_out=st[:, 0:1]),
                           rd=[t_x2], wr=[t_xns[sl], t_sts[sl]])
                        op(act, lambda e: e.activation(out=st[:, 1:2], in_=st[:, 0:1], func=AF.Sqrt, bias=EPS, scale=1.0 / D),
                           rd=[t_sts[sl]], wr=[t_sts[sl]])
                        op(dve, lambda e: e.reciprocal(out=st[:, 2:3], in_=st[:, 1:2]), rd=[t_sts[sl]], wr=[t_sts[sl]])
                        op(dve, lambda e: e.scalar_tensor_tensor(out=oo[:], in0=x2[:], scalar=st[:, 2:3], in1=GF[:],
                                                                 op0=ALU.mult, op1=ALU.mult), rd=[t_x2, t_sts[sl], t_GF], wr=[t_oo])
                        r0 = (gi * 2 + t2) * 128
                        dma(sp, out[r0:r0 + 128, :], oo[:], rd=[t_oo])

                m_pre_norm(0)
                m_pre_T(0)
                for gi in range(16):
                    m_up(gi)
                    if gi + 1 < 16:
                        m_pre_T(gi + 1)
                    m_down(gi)
                kb.barrier()

    try:
        _body()
    except _Stop:
        pass
    return nc


_NC_CACHE = {}


def _layout_inputs(x, c, positions, w_ada, b_ada, g_mix, w_in, conv_w, conv_b, conv_norm_g, conv_norm_b,
                   w_out, g_mlp, w_up, w_down, g_final):
    f32 = np.float32
    x = np.asarray(x, f32); c = np.asarray(c, f32); positions = np.asarray(positions, np.int32)

    def col(v, n):
        return np.ascontiguousarray(np.asarray(v, f32).reshape(n, 128).T)
    shared = {
        "w_ada": np.ascontiguousarray(np.asarray(w_ada, f32)[0]),
        "badac": col(np.asarray(b_ada)[0], 48),
        "badar": np.ascontiguousarray(np.asarray(b_ada, f32)[0][None, :]),
        "gmixc": col(np.asarray(g_mix)[0], 8),
        "gmlpc": col(np.asarray(g_mlp)[0], 8),
        "w_in": np.ascontiguousarray(np.asarray(w_in, f32)[0]),
        "convw": np.ascontiguousarray(np.asarray(conv_w, f32)[0].T.reshape(4, 128, 31).transpose(1, 0, 2)),
        "convb": col(np.asarray(conv_b)[0], 4),
        "cng": col(np.asarray(conv_norm_g)[0], 4),
        "cnb": col(np.asarray(conv_norm_b)[0], 4),
        "w_out": np.ascontiguousarray(np.asarray(w_out, f32)[0]),
        "w_up": np.ascontiguousarray(np.asarray(w_up, f32)[0]),
        "w_down": np.ascontiguousarray(np.asarray(w_down, f32)[0]),
        "gfb": np.ascontiguousarray(np.broadcast_to(np.asarray(g_final, f32)[None, :], (128, D))),
        "invf": np.ascontiguousarray(np.broadcast_to(
            np.power(f32(500000.0), -np.arange(8, dtype=f32) * f32(2.0) / f32(16.0)).astype(f32)[None, :], (128, 8))),
    }
    in_maps = []
    for core in range(8):
        b, p = core // 2, core % 2
        own, oth = OWN[p], OWN[1 - p]
        rows = []
        for j in range(8):
            rows.append(np.arange(oth[j] * 512, oth[j] * 512 + 512))
            rows.append(np.arange(own[j] * 512, own[j] * 512 + 512))
        rows = np.concatenate(rows)
        xpa = np.zeros((NT * 128, D), f32)
        xpa[:S] = x[b][rows]
        pos = np.zeros((NT * 128,), np.int32)
        pos[:S] = positions[b][rows]
        hm = np.ones((256,), f32)
        for j in range(8):
            if own[j] == 0:
                hm[j * 32:(j + 1) * 32] = 0.0
            else:
                hr = np.arange(own[j] * 512 - 32, own[j] * 512)
                xpa[S + j * 32:S + (j + 1) * 32] = x[b][hr]
                pos[S + j * 32:S + (j + 1) * 32] = positions[b][hr]
        of = np.array([0.0 if oth[j] < own[j] else NEG for j in range(8)], f32)
        m = dict(shared)
        m["xp"] = xpa
        m["posp"] = np.ascontiguousarray(pos.reshape(NT, 128).T)
        m["oflag"] = np.ascontiguousarray(np.broadcast_to(of[None, :], (128, 8)))
        m["hmask"] = np.ascontiguousarray(np.broadcast_to(hm[None, :], (128, 256)))
        m["cT"] = col(c[b], 8)
        in_maps.append(m)
    return in_maps


def kernel(**inputs):
    in_maps = _layout_inputs(**inputs)
    if "nc" not in _NC_CACHE:
        _NC_CACHE["nc"] = build_program()
    nc = _NC_CACHE["nc"]
    res = run_bass_kernel_spmd(nc, in_maps, core_ids=list(range(8)))
    outf = np.zeros((4, S, D), np.float32)
    for core in range(8):
        b, p = core // 2, core % 2
        o = res.results[core]["out"]
        for j, ch in enumerate(OWN[p]):
            outf[b, ch * 512:(ch + 1) * 512] = o[j * 512:(j + 1) * 512]
    if DEBUG:
        kernel.debug = res.results
    return outf
```

```python
import numpy as np
import concourse.bass as bass
import concourse.mybir as mybir
from concourse.bass_utils import run_bass_kernel_spmd

F32 = mybir.dt.float32
BF16 = mybir.dt.bfloat16
I32 = mybir.dt.int32
U8 = mybir.dt.uint8
ALU = mybir.AluOpType
AF = mybir.ActivationFunctionType
AX = mybir.AxisListType

D = 1024
S = 8192
NT = 66
NEG = -30000.0
EPS = 1e-6
NBIS = 10
BR = 6.0
JA = 2560
OWN = ([0, 3, 4, 7, 8, 11, 12, 15], [1, 2, 5, 6, 9, 10, 13, 14])
DEBUG = False


class T:
    __slots__ = ("w", "r")

    def __init__(self):
        self.w = {}
        self.r = {}


class Eng:
    def __init__(self, obj, sem, key):
        self.obj = obj
        self.sem = sem
        self.key = key
        self.cnt = 0
        self.seen = {}


class K:
    def __init__(self, nc, sems):
        self.nc = nc
        it = iter(sems)
        self.pe = Eng(nc.tensor, next(it), "pe")
        self.act = Eng(nc.scalar, next(it), "act")
        self.dve = Eng(nc.vector, next(it), "dve")
        self.pool = Eng(nc.gpsimd, next(it), "pool")
        self.sp = Eng(nc.sync, next(it), "sp")
        self.engs = [self.pe, self.act, self.dve, self.pool, self.sp]
        self.dsems = {"sp": [[s, 0] for s in [next(it) for _ in range(8)]],
                      "pool": [[s, 0] for s in [next(it) for _ in range(8)]]}
        self.dptr = {"sp": 0, "pool": 0}

    def _waits(self, eng, rd, wr):
        need = {}

        def add(d, skip_self):
            for k, (s, v) in d.items():
                if skip_self and k == eng.key:
                    continue
                if k not in need or need[k][1] < v:
                    need[k] = (s, v)
        for t in rd:
            add(t.w, False)
        skip = (eng.key == "pe")
        for t in wr:
            add(t.w, skip)
            add(t.r, skip)
        for k, (s, v) in need.items():
            if eng.seen.get(k, 0) < v:
                eng.obj.wait_ge(s, v)
                eng.seen[k] = v

    def op(self, eng, fn, rd=(), wr=()):
        self._waits(eng, rd, wr)
        inst = fn(eng.obj)
        eng.cnt += 1
        inst.then_inc(eng.sem, 1)
        tok = (eng.sem, eng.cnt)
        for t in rd:
            t.r[eng.key] = tok
        for t in wr:
            t.w = {eng.key: tok}
            t.r = {}

    def dma(self, eng, out, in_, rd=(), wr=()):
        ring = self.dsems[eng.key]
        i = self.dptr[eng.key]
        self.dptr[eng.key] = (i + 1) % len(ring)
        sem, val = ring[i]
        key = "d%s%d" % (eng.key, i)
        self._waits(eng, rd, wr)
        if val > 0 and eng.seen.get(key, 0) < val:
            eng.obj.wait_ge(sem, val)
            eng.seen[key] = val
        eng.obj.dma_start(out=out, in_=in_).then_inc(sem, 16)
        ring[i][1] = val + 16
        tok = (sem, val + 16)
        for t in rd:
            t.r[key] = tok
        for t in wr:
            t.w = {key: tok}
            t.r = {}

    def barrier(self):
        for e in self.engs:
            for f in self.engs:
                if f is not e and f.cnt > 0 and e.seen.get(f.key, 0) < f.cnt:
                    e.obj.wait_ge(f.sem, f.cnt)
                    e.seen[f.key] = f.cnt
            for qk, ring in self.dsems.items():
                for i, (s, v) in enumerate(ring):
                    key = "d%s%d" % (qk, i)
                    if v > 0 and e.seen.get(key, 0) < v:
                        e.obj.wait_ge(s, v)
                        e.seen[key] = v


class _Stop(Exception):
    pass


def build_program(stage=None, dumps=(), nchunks=8, nt1=64):
    nc = bass.Bass("TRN2", target_bir_lowering=False)
    dt = nc.dram_tensor
    xp = dt("xp", [NT * 128, D], F32, kind="ExternalInput").ap()
    posp = dt("posp", [128, NT], I32, kind="ExternalInput").ap()
    oflag = dt("oflag", [128, 8], F32, kind="ExternalInput").ap()
    hmask = dt("hmask", [128, 256], F32, kind="ExternalInput").ap()
    invf = dt("invf", [128, 8], F32, kind="ExternalInput").ap()
    cT = dt("cT", [128, 8], F32, kind="ExternalInput").ap()
    w_ada = dt("w_ada", [D, 6 * D], F32, kind="ExternalInput").ap()
    badac = dt("badac", [128, 48], F32, kind="ExternalInput").ap()
    badar = dt("badar", [1, 6 * D], F32, kind="ExternalInput").ap()
    gmixc = dt("gmixc", [128, 8], F32, kind="ExternalInput").ap()
    gmlpc = dt("gmlpc", [128, 8], F32, kind="ExternalInput").ap()
    w_in = dt("w_in", [D, 2376], F32, kind="ExternalInput").ap()
    convw = dt("convw", [128, 4, 31], F32, kind="ExternalInput").ap()
    convb = dt("convb", [128, 4], F32, kind="ExternalInput").ap()
    cng = dt("cng", [128, 4], F32, kind="ExternalInput").ap()
    cnb = dt("cnb", [128, 4], F32, kind="ExternalInput").ap()
    w_out = dt("w_out", [D, D], F32, kind="ExternalInput").ap()
    w_up = dt("w_up", [D, 4 * D], F32, kind="ExternalInput").ap()
    w_down = dt("w_down", [4 * D, D], F32, kind="ExternalInput").ap()
    gfb = dt("gfb", [128, D], F32, kind="ExternalInput").ap()
    out = dt("out", [4096, D], F32, kind="ExternalOutput").ap()
    x1s = dt("x1s", [4096, D], F32).ap()
    g1s = dt("g1s", [128, D], F32).ap()
    g2s = dt("g2s", [128, D], F32).ap()
    if "x1s" in dumps:
        x1s = dt("dbg_x1s", [4096, D], F32, kind="ExternalOutput").ap()

    def wview(w, c0, c1):
        return w[:, c0:c1].rearrange("(k p) e -> p k e", p=128)

    import contextlib
    dump_aps = {}

    def dump(name, ap, tiles, kbref):
        if name not in dumps:
            return
        shp = [int(v) for v in ap.shape]
        d_ap = dt("dbg_" + name, shp, ap.dtype, kind="ExternalOutput").ap()
        kbref.dma(kbref.sp, d_ap, ap, rd=tiles)

    def stop_if(st, kbref):
        if stage == st:
            kbref.barrier()
            raise _Stop()

    def _body():
        with contextlib.ExitStack() as es:
            sems = [es.enter_context(nc.semaphore("s%d" % i)) for i in range(21)]
            kb = K(nc, sems)
            pe, act, dve, pool, sp = kb.pe, kb.act, kb.dve, kb.pool, kb.sp
            op, dma = kb.op, kb.dma

            def sb(name, shape, dtype=F32):
                return es2.enter_context(nc.sbuf_tensor(name, shape, dtype))

            ps = es.enter_context(nc.psum_tensor("ps", [128, 8, 512], F32))
            PB = [T() for _ in range(8)]

            def psb16(b):
                return ps[:, b, :].bitcast(BF16)

            es2 = es
            ident = sb("ident", [128, 128], BF16); t_ident = T()
            ident4 = sb("ident4", [128, 4, 128], BF16)
            identf = sb("identf", [128, 128], F32)
            trim = sb("trim", [128, 128], F32)
            onesm = sb("onesm", [128, 128], BF16)
            onesr = sb("onesr", [1, 128], F32)
            cosT = sb("cosT", [128, NT, 8], F32)
            sinT = sb("sinT", [128, NT, 8], F32); t_cs = T()
            modc = sb("modc", [128, 48], F32); t_modc = T()
            ab = sb("ab", [128, 4, 8], F32); t_ab = T()
            t_G1 = T(); t_G2 = T()
            oflg = sb("oflg", [128, 8], F32); t_small = T()
            cw = sb("cw", [128, 4, 31], F32)
            cb = sb("cb", [128, 4], F32)
            cg = sb("cg", [128, 4], F32)
            cbn = sb("cbn", [128, 4], F32)
            wst = {"slots": None, "tiles": None, "ptr": 0}

            def walloc(tag):
                wst["slots"] = [sb("wslot%s%d" % (tag, i), [128, 8, 512], BF16) for i in range(2)]
                wst["tiles"] = [T() for _ in range(2)]
                wst["ptr"] = 0

            def wload(src_ap):
                i = wst["ptr"]
                wst["ptr"] = (i + 1) % 2
                dma(pool, wst["slots"][i][:], src_ap, wr=[wst["tiles"][i]])
                return wst["slots"][i], wst["tiles"][i]

            op(pool, lambda e: e.memset(identf[:], 0.0), wr=[t_ident])
            op(pool, lambda e: e.affine_select(out=identf[:], in_=identf[:], pattern=[[-1, 128]],
                                               compare_op=ALU.not_equal, fill=1.0, base=0,
                                               channel_multiplier=1), rd=[t_ident], wr=[t_ident])
            op(pool, lambda e: e.tensor_copy(out=ident[:], in_=identf[:]), rd=[t_ident], wr=[t_ident])
            op(pool, lambda e: e.tensor_copy(out=ident4[:], in_=identf[:].unsqueeze(1).to_broadcast([128, 4, 128])),
               rd=[t_ident], wr=[t_ident])
            op(pool, lambda e: e.memset(trim[:], 0.0), wr=[t_ident])
            op(pool, lambda e: e.affine_select(out=trim[:], in_=trim[:], pattern=[[-1, 128]],
                                               compare_op=ALU.is_ge, fill=NEG, base=0,
                                               channel_multiplier=1), rd=[t_ident], wr=[t_ident])
            op(pool, lambda e: e.memset(onesm[:], 1.0 / 512.0), wr=[t_ident])
            op(pool, lambda e: e.memset(onesr[:], 1.0), wr=[t_ident])
            dma(sp, oflg[:], oflag, wr=[t_small])
            dma(sp, cw[:], convw, wr=[t_small])
            dma(sp, cb[:], convb, wr=[t_small])
            dma(sp, cg[:], cng, wr=[t_small])
            dma(sp, cbn[:], cnb, wr=[t_small])

            with contextlib.ExitStack() as es2:
                posi = sb("posi", [128, NT], I32)
                posf = sb("posf", [128, NT], F32)
                ivf = sb("ivf", [128, 8], F32)
                ang = sb("ang", [128, NT, 8], F32)
                tq = sb("tq", [128, NT, 8], F32)
                kq = sb("kq", [128, NT, 8], I32)
                kf = sb("kf", [128, NT, 8], F32)
                red = sb("red", [128, NT, 8], F32)
                t_p0 = T()
                cTs = sb("cTs", [128, 8], F32)
                cond = sb("cond", [128, 8], F32); t_cond = T()
                badc = sb("badc", [128, 48], F32)
                gmc = sb("gmc", [128, 2, 8], F32)
                rowb = sb("rowb", [1, 6144], F32)
                rows2 = [sb("rows%d" % i, [1, 512], F32) for i in range(2)]; t_rows2 = [T(), T()]
                Gtmp = sb("Gtmp", [128, 512], F32); t_Gtmp = T()
                wfs = [sb("wf%d" % i, [128, 8, 512], F32) for i in range(3)]; t_wfs = [T() for _ in range(3)]
                dma(sp, posi[:], posp, wr=[t_p0])
                dma(sp, ivf[:], invf, wr=[t_p0])
                dma(sp, cTs[:], cT, wr=[t_cond])
                dma(sp, badc[:], badac, wr=[t_cond])
                dma(sp, gmc[:, 0, :], gmixc, wr=[t_cond])
                dma(sp, gmc[:, 1, :], gmlpc, wr=[t_cond])
                dma(sp, rowb[:], badar, wr=[t_cond])
                rw = dict(rd=[t_p0], wr=[t_p0])
                op(dve, lambda e: e.tensor_copy(out=posf[:], in_=posi[:]), **rw)
                op(dve, lambda e: e.tensor_tensor(out=ang[:], in0=posf[:].unsqueeze(2).to_broadcast([128, NT, 8]),
                                                  in1=ivf[:].unsqueeze(1).to_broadcast([128, NT, 8]), op=ALU.mult), **rw)
                TWO_PI = 2.0 * np.pi
                C1 = 6.28125
                C2 = TWO_PI - C1

                def reduce_to(dst, shift):
                    op(dve, lambda e: e.tensor_scalar(out=tq[:], in0=ang[:], scalar1=shift, scalar2=1.0 / TWO_PI,
                                                      op0=ALU.add, op1=ALU.mult), **rw)
                    op(dve, lambda e: e.tensor_copy(out=kq[:], in_=tq[:]), **rw)
                    op(dve, lambda e: e.tensor_copy(out=kf[:], in_=kq[:]), **rw)
                    op(dve, lambda e: e.scalar_tensor_tensor(out=red[:], in0=kf[:], scalar=-C1, in1=ang[:],
                                                             op0=ALU.mult, op1=ALU.add), **rw)
                    op(dve, lambda e: e.scalar_tensor_tensor(out=red[:], in0=kf[:], scalar=-C2, in1=red[:],
                                                             op0=ALU.mult, op1=ALU.add), **rw)
                    op(dve, lambda e: e.tensor_scalar(out=red[:], in0=red[:], scalar1=shift, scalar2=None,
                                                      op0=ALU.add), **rw)
                    op(dve, lambda e: e.tensor_scalar(out=tq[:], in0=red[:], scalar1=np.pi, scalar2=-TWO_PI,
                                                      op0=ALU.is_gt, op1=ALU.mult), **rw)
                    op(dve, lambda e: e.tensor_tensor(out=red[:], in0=red[:], in1=tq[:], op=ALU.add), **rw)
                    op(dve, lambda e: e.tensor_scalar(out=tq[:], in0=red[:], scalar1=-np.pi, scalar2=TWO_PI,
                                                      op0=ALU.is_lt, op1=ALU.mult), **rw)
                    op(dve, lambda e: e.tensor_tensor(out=red[:], in0=red[:], in1=tq[:], op=ALU.add), **rw)
                    op(dve, lambda e: e.tensor_scalar(out=red[:], in0=red[:], scalar1=-3.1415925, scalar2=3.1415925,
                                                      op0=ALU.max, op1=ALU.min), **rw)
                    op(act, lambda e: e.activation(out=dst[:], in_=red[:], func=AF.Sin), rd=[t_p0], wr=[t_cs])

                reduce_to(sinT, 0.0)
                reduce_to(cosT, np.pi / 2.0)

                op(act, lambda e: e.activation(out=cond[:], in_=cTs[:], func=AF.Silu), rd=[t_cond], wr=[t_cond])
                for cc in range(12):
                    ws, tw = wfs[cc % 3], t_wfs[cc % 3]
                    dma(sp, ws[:], wview(w_ada, cc * 512, (cc + 1) * 512), wr=[tw])
                    rb_, trb_ = rows2[cc % 2], t_rows2[cc % 2]
                    pbk = 1 + cc % 2
                    for k in range(8):
                        op(pe, lambda e, k=k: e.matmul(ps[0:1, pbk, :], lhsT=cond[:, k:k + 1], rhs=ws[:, k, :],
                                                       start=(k == 0), stop=(k == 7)),
                           rd=[t_cond, tw], wr=[PB[pbk]])
                    op(dve, lambda e: e.tensor_tensor(out=rb_[:], in0=ps[0:1, pbk, :], in1=rowb[:, cc * 512:(cc + 1) * 512],
                                                      op=ALU.add), rd=[PB[pbk], t_cond], wr=[trb_])
                    if cc in (4, 5, 10, 11):
                        op(pe, lambda e: e.matmul(ps[:, 3, :], lhsT=onesr[:], rhs=rb_[:], start=True, stop=True),
                           rd=[trb_, t_ident], wr=[PB[3]])
                        Gs, tG = (g1s, t_G1) if cc < 6 else (g2s, t_G2)
                        go = (cc - 4) * 512 if cc < 6 else (cc - 10) * 512
                        op(act, lambda e: e.activation(out=Gtmp[:], in_=ps[:, 3, :], func=AF.Copy),
                           rd=[PB[3]], wr=[t_Gtmp])
                        dma(sp, Gs[:, go:go + 512], Gtmp[:], rd=[t_Gtmp], wr=[tG])
                    else:
                        for el in range(4):
                            et = cc * 4 + el
                            op(pe, lambda e, el=el, et=et: e.matmul(ps[:, 0, et:et + 1], lhsT=rb_[0:1, el * 128:(el + 1) * 128],
                                                                   rhs=onesr[0:1, 0:1], start=True, stop=True, skip_group_check=True),
                               rd=[trb_, t_ident], wr=[PB[0]])
                op(dve, lambda e: e.memset(modc[:], 0.0), wr=[t_modc])
                for lo_, hi_ in ((0, 16), (24, 40)):
                    op(dve, lambda e: e.tensor_copy(out=modc[:, lo_:hi_], in_=ps[:, 0, lo_:hi_]),
                       rd=[PB[0]], wr=[t_modc])
                op(dve, lambda e: e.scalar_tensor_tensor(out=ab[:, 0, :], in0=modc[:, 8:16], scalar=1.0, in1=gmc[:, 0, :],
                                                         op0=ALU.add, op1=ALU.mult), rd=[t_modc, t_cond], wr=[t_ab])
                op(dve, lambda e: e.tensor_copy(out=ab[:, 1, :], in_=modc[:, 0:8]), rd=[t_modc], wr=[t_ab])
                op(dve, lambda e: e.scalar_tensor_tensor(out=ab[:, 2, :], in0=modc[:, 32:40], scalar=1.0, in1=gmc[:, 1, :],
                                                         op0=ALU.add, op1=ALU.mult), rd=[t_modc, t_cond], wr=[t_ab])
                op(dve, lambda e: e.tensor_copy(out=ab[:, 3, :], in_=modc[:, 24:32]), rd=[t_modc], wr=[t_ab])
                dump("cosT", cosT[:], [t_cs], kb)
                dump("sinT", sinT[:], [t_cs], kb)
                dump("ab", ab[:], [t_ab], kb)
                dump("modc", modc[:], [t_modc], kb)
                kb.barrier()
                stop_if("p0", kb)

            def norm_tile(row0, xt, t_xt, xn, t_xn, sq, t_sq, st, t_st, src=None):
                dma(sp, xt[:], (xp if src is None else src)[row0:row0 + 128, :], wr=[t_xt])
                op(act, lambda e: e.activation(out=xn[:], in_=xt[:], func=AF.Square, accum_out=st[:, 0:1]),
                   rd=[t_xt], wr=[t_xn, t_st])
                op(act, lambda e: e.activation(out=st[:, 1:2], in_=st[:, 0:1], func=AF.Sqrt, bias=EPS, scale=1.0 / D),
                   rd=[t_st], wr=[t_st])
                op(dve, lambda e: e.reciprocal(out=st[:, 2:3], in_=st[:, 1:2]), rd=[t_st], wr=[t_st])
                op(act, lambda e: e.activation(out=xn[:], in_=xt[:], func=AF.Copy, scale=st[:, 2:3]),
                   rd=[t_xt, t_st], wr=[t_xn])

            def transpose_mod(xn, t_xn, bank, hT_dst, t_hT, abi):
                pv = psb16(bank)
                for k in range(8):
                    op(pe, lambda e, k=k: e.transpose(out=pv[:, k * 128:(k + 1) * 128], in_=xn[:, k * 128:(k + 1) * 128],
                                                      identity=ident[:]), rd=[t_xn, t_ident], wr=[PB[bank]])
                pv3 = pv.rearrange("p (k t) -> p k t", k=8)
                op(dve, lambda e: e.tensor_tensor(out=hT_dst, in0=pv3,
                                                  in1=ab[:, abi, :].unsqueeze(2).to_broadcast([128, 8, 128]), op=ALU.mult),
                   rd=[PB[bank], t_ab], wr=[t_hT])
                op(pool, lambda e: e.tensor_tensor(out=hT_dst, in0=hT_dst,
                                                   in1=ab[:, abi + 1, :].unsqueeze(2).to_broadcast([128, 8, 128]), op=ALU.add),
                   rd=[t_hT, t_ab], wr=[t_hT])

            with contextlib.ExitStack() as es2:
                kT = sb("kT", [128, S], BF16); t_kT = T()
                kiT = sb("kiT", [128, S], BF16); t_kiT = T()
                Vaug = sb("Vaug", [128, 64, 2, 65], BF16); t_V = T()
                W1 = sb("W1", [128, 8, 328], BF16); t_W1 = T()
                xts = [sb("xt%d" % i, [128, D], F32) for i in range(2)]; t_xts = [T(), T()]
                xns = [sb("xn%d" % i, [128, D], BF16) for i in range(2)]; t_xns = [T(), T()]
                sqj = None; t_sqj = None
                walloc("b")
                G1 = sb("G1", [128, D], F32)
                dma(sp, G1[:], g1s, rd=[t_G1], wr=[t_G1])
                sts = [sb("st%d" % i, [128, 4], F32) for i in range(2)]; t_sts = [T(), T()]
                hTc = sb("hTc", [128, 8, 512], BF16); t_hTc = [T() for _ in range(4)]
                rtmp = sb("rtmp", [128, 4, 16, 8], F32); t_rtmp = T(); t_rt4 = [T() for _ in range(4)]
                krot = [sb("krot%d" % i, [128, 256], BF16) for i in range(2)]; t_krot = [T(), T()]

                op(pool, lambda e: e.memset(Vaug[:], 1.0), wr=[t_V])
                dma(pool, W1[:, :, 0:128], wview(w_in, 512, 640), wr=[t_W1])
                dma(pool, W1[:, :, 128:192], wview(w_in, 1280, 1344), wr=[t_W1])
                dma(pool, W1[:, :, 192:320], wview(w_in, 640, 768), wr=[t_W1])
                dma(pool, W1[:, :, 320:328], wview(w_in, 1344, 1352), wr=[t_W1])

                def rope(src3, dst3, nh, ti, tsrc, tdst):
                    cs = cosT[:, ti, :].unsqueeze(1).to_broadcast([128, nh, 8])
                    sn = sinT[:, ti, :].unsqueeze(1).to_broadcast([128, nh, 8])
                    x1, x2 = src3[:, :, 0:8], src3[:, :, 8:16]
                    t1, t2, t3, t4 = (rtmp[:, i, 0:nh, :] for i in range(4))
                    op(dve, lambda e: e.tensor_tensor(out=t1, in0=x1, in1=cs, op=ALU.mult), rd=[tsrc, t_cs], wr=[t_rt4[0]])
                    op(dve, lambda e: e.tensor_tensor(out=t2, in0=x2, in1=sn, op=ALU.mult), rd=[tsrc, t_cs], wr=[t_rt4[1]])
                    op(dve, lambda e: e.tensor_tensor(out=t3, in0=x2, in1=cs, op=ALU.mult), rd=[tsrc, t_cs], wr=[t_rt4[2]])
                    op(dve, lambda e: e.tensor_tensor(out=t4, in0=x1, in1=sn, op=ALU.mult), rd=[tsrc, t_cs], wr=[t_rt4[3]])
                    op(dve, lambda e: e.tensor_tensor(out=dst3[:, :, 0:8], in0=t1, in1=t2, op=ALU.subtract),
                       rd=[t_rt4[0], t_rt4[1]], wr=[tdst])
                    op(dve, lambda e: e.tensor_tensor(out=dst3[:, :, 8:16], in0=t3, in1=t4, op=ALU.add),
                       rd=[t_rt4[2], t_rt4[3]], wr=[tdst])
                    op(act, lambda e: e.activation(out=dst3[:, :, 16:64], in_=src3[:, :, 16:64], func=AF.Copy),
                       rd=[tsrc], wr=[tdst])

                def rope4(src4, dst4, ti, tsrc, tdst):
                    cs = cosT[:, ti, :].unsqueeze(1).unsqueeze(1).to_broadcast([128, 2, 4, 8])
                    sn = sinT[:, ti, :].unsqueeze(1).unsqueeze(1).to_broadcast([128, 2, 4, 8])
                    x1, x2 = src4[:, :, :, 0:8], src4[:, :, :, 8:16]
                    t1, t2, t3, t4 = (rtmp[:, i, 0:8, :].rearrange("p (g b) d -> p g b d", g=2) for i in range(4))
                    op(dve, lambda e: e.tensor_tensor(out=t1, in0=x1, in1=cs, op=ALU.mult), rd=[tsrc, t_cs], wr=[t_rt4[0]])
                    op(dve, lambda e: e.tensor_tensor(out=t2, in0=x2, in1=sn, op=ALU.mult), rd=[tsrc, t_cs], wr=[t_rt4[1]])
                    op(dve, lambda e: e.tensor_tensor(out=t3, in0=x2, in1=cs, op=ALU.mult), rd=[tsrc, t_cs], wr=[t_rt4[2]])
                    op(dve, lambda e: e.tensor_tensor(out=t4, in0=x1, in1=sn, op=ALU.mult), rd=[tsrc, t_cs], wr=[t_rt4[3]])
                    op(dve, lambda e: e.tensor_tensor(out=dst4[:, :, :, 0:8], in0=t1, in1=t2, op=ALU.subtract),
                       rd=[t_rt4[0], t_rt4[1]], wr=[tdst])
                    op(dve, lambda e: e.tensor_tensor(out=dst4[:, :, :, 8:16], in0=t3, in1=t4, op=ALU.add),
                       rd=[t_rt4[2], t_rt4[3]], wr=[tdst])
                    op(act, lambda e: e.activation(out=dst4[:, :, :, 16:64], in_=src4[:, :, :, 16:64], func=AF.Copy),
                       rd=[tsrc], wr=[tdst])

                def ph1_S1(ti):
                    s2 = ti % 2
                    norm_tile(ti * 128, xts[s2], t_xts[s2], xns[s2], t_xns[s2], sqj, t_sqj, sts[s2], t_sts[s2])
                    hs = ti % 4
                    hdst = hTc[:, :, hs * 128:(hs + 1) * 128]
                    transpose_mod(xns[s2], t_xns[s2], 6 + s2, hdst, t_hTc[hs], 0)

                def ph1_S2(ti):
                    s2 = ti % 2
                    hs = ti % 4
                    bk = s2
                    for k in range(8):
                        op(pe, lambda e, k=k: e.matmul(ps[:, bk, 0:320], lhsT=hTc[:, k, hs * 128:(hs + 1) * 128],
                                                       rhs=W1[:, k, 0:320], start=(k == 0), stop=(k == 7)),
                           rd=[t_hTc[hs], t_W1], wr=[PB[bk]])
                    kr = krot[s2]
                    rope(ps[:, bk, 0:192].rearrange("p (h d) -> p h d", d=64),
                         kr[:, 0:192].rearrange("p (h d) -> p h d", d=64), 3, ti, PB[bk], t_krot[s2])
                    op(pool, lambda e: e.tensor_copy(out=kr[:, 192:256], in_=kr[:, 128:192]), rd=[t_krot[s2]], wr=[t_krot[s2]])
                    op(act, lambda e: e.activation(out=Vaug[:, ti, :, 0:64],
                                                   in_=ps[:, bk, 192:320].rearrange("p (g d) -> p g d", d=64), func=AF.Copy),
                       rd=[PB[bk]], wr=[t_V])
                    tb = 4 + s2
                    pv = psb16(tb)
                    op(pe, lambda e: e.transpose(out=pv[:, 0:128], in_=kr[:, 0:128], identity=ident[:]),
                       rd=[t_krot[s2], t_ident], wr=[PB[tb]])
                    op(pe, lambda e: e.transpose(out=pv[:, 128:256], in_=kr[:, 128:256], identity=ident[:]),
                       rd=[t_krot[s2], t_ident], wr=[PB[tb]])
                    op(act, lambda e: e.activation(out=kT[:, ti * 128:(ti + 1) * 128], in_=pv[:, 0:128], func=AF.Copy),
                       rd=[PB[tb]], wr=[t_kT])
                    op(dve, lambda e: e.tensor_copy(out=kiT[:, ti * 128:(ti + 1) * 128], in_=pv[:, 128:256]),
                       rd=[PB[tb]], wr=[t_kiT])


                ph1_S1(0)
                for ti in range(nt1):
                    if ti + 1 < nt1:
                        ph1_S1(ti + 1)
                    ph1_S2(ti)
                dump("kT", kT[:], [t_kT], kb)
                dump("kiT", kiT[:], [t_kiT], kb)
                dump("Vaug", Vaug[:], [t_V], kb)
                stop_if("p1", kb)
                SC = sb("SC", [128, S], F32); t_SC = T()
                junk = hTc[:].rearrange("p k t -> p (k t)").bitcast(U8)
                RbA = sb("RbA", [128, 8, 512], BF16)
                Rb = [RbA[:, i, :] for i in range(8)]; t_Rb = [T() for _ in range(8)]
                Dg = sb("Dg", [128, 8, 128], BF16); t_Dg = T()
                qT = sb("qT", [128, 2, 4, 512], BF16); t_qT = T()
                qiT = sb("qiT", [128, 4, 2, 512], BF16); t_qiT = T()
                op(pool, lambda e: e.memset(qT[:], 0.0), wr=[t_qT])
                op(pool, lambda e: e.memset(qiT[:], 0.0), wr=[t_qiT])
                qrot = [sb("qrot%d" % i, [128, 512], BF16) for i in range(2)]; t_qrot = [T(), T()]
                wsc = sb("wsc", [128, 4, 8], F32); t_wsc = T()
                PT = [sb("PT%d" % i, [128, 512], BF16) for i in range(4)]; t_PT = [T() for _ in range(4)]
                MB = sb("MB", [128, S], BF16); t_MB = T()
                junkA = sb("junkA", [128, JA], U8); t_junkA = T()
                bsa = sb("bsa", [128, 2], F32); t_bsa = T()
                bst = sb("bst", [128, 8], F32); t_bst = T()
                gluT = sb("gluT", [128, 4, 544], BF16); t_glu = T()
                gluH = sb("gluH", [128, 4, 256], BF16); t_gluH = T()
                SCb = SC[:].bitcast(BF16)
                ybf = SCb[:, 0:2048].rearrange("p (c t) -> p c t", c=4); t_ybf = t_SC
                ysq = SCb[:, 2048:4096].rearrange("p (c t) -> p c t", c=4); t_ysq = t_SC
                lnA = SC[:, 2048:2560]; t_lnA = t_SC
                lnB = SC[:, 2560:3072]; t_lnB = t_SC
                zn = SC[:, 3072:3584]; t_zn = t_SC
                sig = SC[:, 3584:4096]; t_sig = t_SC
                cdiag = RbA[:].rearrange("p a b -> p (a b)")[:, 0:3968].rearrange("p (k c) -> p k c", c=128)
                mixT = sb("mixT", [128, 8, 512], BF16); t_mixT = T()
                attn = sb("attn", [128, 512], BF16); t_attn = T()
                rs4 = sb("rs4", [128, 8], F32); t_rs4 = T()
                x1t = SC[:, 4096:5120]; t_x1t = t_SC
                hmB = sb("hmB", [128, 256], BF16)
                hm = sb("hm", [128, 256], F32)
                dma(sp, hm[:], hmask, wr=[t_small])
                op(pool, lambda e: e.tensor_copy(out=hmB[:], in_=hm[:]), rd=[t_small], wr=[t_small])

                def conv_glu(ws_a, tw_a, ws_g, tw_g, ncols, ct, dst, tdst, hcols, t_h):
                    for k in range(8):
                        op(pe, lambda e, k=k: e.matmul(ps[:, 0, 0:ncols], lhsT=ws_a[:, k, ct * 128:(ct + 1) * 128],
                                                       rhs=hTc[:, k, hcols], start=(k == 0), stop=(k == 7)),
                           rd=t_h + [tw_a], wr=[PB[0]])
                    for k in range(8):
                        op(pe, lambda e, k=k: e.matmul(ps[:, 1, 0:ncols], lhsT=ws_g[:, k, ct * 128:(ct + 1) * 128],
                                                       rhs=hTc[:, k, hcols], start=(k == 0), stop=(k == 7)),
                           rd=t_h + [tw_g], wr=[PB[1]])
                    op(act, lambda e: e.activation(out=sig[:, 0:ncols], in_=ps[:, 1, 0:ncols], func=AF.Sigmoid),
                       rd=[PB[1]], wr=[t_sig])
                    op(dve, lambda e: e.tensor_tensor(out=dst, in0=ps[:, 0, 0:ncols], in1=sig[:, 0:ncols], op=ALU.mult),
                       rd=[PB[0], t_sig], wr=[tdst])

                for hi in range(2):
                    norm_tile((64 + hi) * 128, xts[hi], t_xts[hi], xns[hi], t_xns[hi], sqj, t_sqj, sts[hi], t_sts[hi])
                    transpose_mod(xns[hi], t_xns[hi], 6 + hi, hTc[:, :, hi * 128:(hi + 1) * 128], t_hTc[hi], 0)
                wa, twa = wload(wview(w_in, 1352, 1864))
                wg, twg = wload(wview(w_in, 1864, 2376))
                for ct in range(4):
                    conv_glu(wa, twa, wg, twg, 256, ct, gluH[:, ct, :], t_gluH, slice(0, 256), [t_hTc[0], t_hTc[1]])
                    op(pool, lambda e, ct=ct: e.tensor_tensor(out=gluH[:, ct, :], in0=gluH[:, ct, :], in1=hmB[:], op=ALU.mult),
                       rd=[t_gluH, t_small], wr=[t_gluH])

                dump("gluH", gluH[:], [t_gluH], kb)
                stop_if("p2h", kb)
                for j in range(nchunks):
                    tile0 = (2 * j + 1) * 4
                    for t4 in range(4):
                        s2 = t4 % 2
                        norm_tile((tile0 + t4) * 128, xts[s2], t_xts[s2], xns[s2], t_xns[s2], sqj, t_sqj, sts[s2], t_sts[s2])
                        transpose_mod(xns[s2], t_xns[s2], 6 + s2, hTc[:, :, t4 * 128:(t4 + 1) * 128], t_hTc[t4], 0)
                    stop_if("p2n", kb)
                    for grp in range(2):
                        c0 = 0 if grp == 0 else 768
                        wq, twq = wload(wview(w_in, c0, c0 + 512))
                        stop_if("p2w", kb)
                        dstT, t_dstT = (qT, t_qT) if grp == 0 else (qiT, t_qiT)
                        for t4 in range(4):
                            bk = t4 % 2
                            for k in range(8):
                                op(pe, lambda e, k=k: e.matmul(ps[:, bk, :], lhsT=hTc[:, k, t4 * 128:(t4 + 1) * 128],
                                                               rhs=wq[:, k, :], start=(k == 0), stop=(k == 7)),
                                   rd=[t_hTc[t4], twq], wr=[PB[bk]])
                            if grp == 0 and t4 == 0:
                                stop_if("p2m", kb)
                            qr = qrot[bk]
                            src4 = ps[:, bk, :].rearrange("p (g b d) -> p g b d", g=2, b=4)
                            dst4 = qr[:].rearrange("p (b g d) -> p g b d", g=2, b=4)
                            rope4(src4, dst4, tile0 + t4, PB[bk], t_qrot[bk])
                            if grp == 0 and t4 == 0:
                                dump("qr", qr[:], [t_qrot[bk]], kb)
                                stop_if("p2a0", kb)
                            tb = 4 + bk
                            pv = psb16(tb)
                            for b in range(4):
                                op(pe, lambda e, b=b: e.transpose(out=pv[:, b * 128:(b + 1) * 128],
                                                                  in_=qr[:, b * 128:(b + 1) * 128], identity=ident[:]),
                                   rd=[t_qrot[bk], t_ident], wr=[PB[tb]])
                            pv4 = pv[:, 0:512].rearrange("p (b t) -> p b t", b=4)
                            tcols = slice(t4 * 128, (t4 + 1) * 128)
                            if grp == 0:
                                d0, d1 = qT[0:64, 0, :, tcols], qT[64:128, 1, :, tcols]
                            else:
                                d0, d1 = qiT[0:64, :, 0, tcols], qiT[64:128, :, 1, tcols]
                            op(act, lambda e: e.activation(out=d0, in_=pv4[0:64], func=AF.Copy), rd=[PB[tb]], wr=[t_dstT])
                            op(dve, lambda e: e.tensor_copy(out=d1, in_=pv4[64:128]), rd=[PB[tb]], wr=[t_dstT])
                            if grp == 0 and t4 == 0:
                                stop_if("p2a1", kb)
                            if grp == 1 and t4 == 0:
                                stop_if("p2a2", kb)
                            if grp == 1:
                                for k in range(8):
                                    op(pe, lambda e, k=k: e.matmul(ps[:, 2, 0:8], lhsT=hTc[:, k, t4 * 128:(t4 + 1) * 128],
                                                                   rhs=W1[:, k, 320:328], start=(k == 0), stop=(k == 7)),
                                       rd=[t_hTc[t4], t_W1], wr=[PB[2]])
                                op(dve, lambda e: e.tensor_scalar(
                                    out=wsc[:, t4, :].rearrange("p (b g) -> p g b", g=2),
                                    in0=ps[:, 2, 0:8].rearrange("p (g b) -> p g b", g=2),
                                    scalar1=float(8 ** -0.5 * 64 ** -0.5), scalar2=None, op0=ALU.mult),
                                   rd=[PB[2]], wr=[t_wsc])
                    if j == nchunks - 1:
                        dump("qT", qT[:].rearrange("p g b t -> p (g b) t"), [t_qT], kb)
                        dump("qiT", qiT[:].rearrange("p b g t -> p (b g) t"), [t_qiT], kb)
                        dump("wsc", wsc[:], [t_wsc], kb)
                        stop_if("p2a", kb)
                    wa, twa = wload(wview(w_in, 1352, 1864))
                    wg, twg = wload(wview(w_in, 1864, 2376))
                    for ct in range(4):
                        op(pool, lambda e, ct=ct: e.tensor_copy(out=gluT[:, ct, 0:32], in_=gluH[:, ct, j * 32:(j + 1) * 32]),
                           rd=[t_gluH], wr=[t_glu])
                        conv_glu(wa, twa, wg, twg, 512, ct, gluT[:, ct, 32:544], t_glu, slice(0, 512), t_hTc)
                    for ct in range(4):
                        op(pool, lambda e, ct=ct: e.tensor_tensor(
                            out=cdiag, in0=ident[:].unsqueeze(1).to_broadcast([128, 31, 128]),
                            in1=cw[:, ct, :].unsqueeze(2).to_broadcast([128, 31, 128]), op=ALU.mult),
                           rd=[t_ident, t_small], wr=t_Rb)
                        cbk = ct
                        for tap in range(31):
                            op(pe, lambda e, tap=tap, ct=ct: e.matmul(ps[:, cbk, :], lhsT=cdiag[:, tap, :],
                                                                     rhs=gluT[:, ct, tap + 2:tap + 514],
                                                                     start=(tap == 0), stop=(tap == 30)),
                               rd=t_Rb + [t_glu], wr=[PB[cbk]])
                        op(act, lambda e, ct=ct: e.activation(out=ybf[:, ct, :], in_=ps[:, cbk, :], func=AF.Identity,
                                                              bias=cb[:, ct:ct + 1], scale=1.0), rd=[PB[cbk], t_small], wr=[t_ybf])
                        op(act, lambda e, ct=ct: e.activation(out=ysq[:, ct, :], in_=ps[:, cbk, :], func=AF.Square,
                                                              bias=cb[:, ct:ct + 1], scale=1.0), rd=[PB[cbk], t_small], wr=[t_ysq])
                    for ct in range(4):
                        op(pe, lambda e, ct=ct: e.matmul(ps[:, 4, :], lhsT=onesm[:], rhs=ybf[:, ct, :],
                                                         start=(ct == 0), stop=(ct == 3)), rd=[t_ybf, t_ident], wr=[PB[4]])
                    for ct in range(4):
                        op(pe, lambda e, ct=ct: e.matmul(ps[:, 5, :], lhsT=onesm[:], rhs=ysq[:, ct, :],
                                                         start=(ct == 0), stop=(ct == 3)), rd=[t_ysq, t_ident], wr=[PB[5]])
                    op(act, lambda e: e.activation(out=lnA, in_=ps[:, 4, :], func=AF.Copy), rd=[PB[4]], wr=[t_lnA])
                    op(dve, lambda e: e.tensor_tensor(out=lnB, in0=lnA, in1=lnA, op=ALU.mult), rd=[t_lnA], wr=[t_lnB])
                    op(dve, lambda e: e.tensor_tensor(out=lnB, in0=ps[:, 5, :], in1=lnB, op=ALU.subtract),
                       rd=[PB[5], t_lnB], wr=[t_lnB])
                    op(dve, lambda e: e.tensor_scalar(out=lnB, in0=lnB, scalar1=0.0, scalar2=EPS, op0=ALU.max, op1=ALU.add),
                       rd=[t_lnB], wr=[t_lnB])
                    op(act, lambda e: e.activation(out=lnB, in_=lnB, func=AF.Sqrt), rd=[t_lnB], wr=[t_lnB])
                    op(dve, lambda e: e.reciprocal(out=lnB, in_=lnB), rd=[t_lnB], wr=[t_lnB])
                    for ct in range(4):
                        op(dve, lambda e, ct=ct: e.scalar_tensor_tensor(out=zn, in0=ps[:, ct, :], scalar=cb[:, ct:ct + 1],
                                                                        in1=lnA, op0=ALU.add, op1=ALU.subtract),
                           rd=[PB[ct], t_small, t_lnA], wr=[t_zn])
                        op(dve, lambda e: e.tensor_tensor(out=zn, in0=zn, in1=lnB, op=ALU.mult),
                           rd=[t_zn, t_lnB], wr=[t_zn])
                        op(act, lambda e, ct=ct: e.activation(out=mixT[:, 4 + ct, :], in_=zn, func=AF.Silu,
                                                              bias=cbn[:, ct:ct + 1], scale=cg[:, ct:ct + 1]),
                           rd=[t_zn, t_small], wr=[t_mixT])

                    if j == nchunks - 1:
                        dump("mixTc", mixT[:, 4:8, :], [t_mixT], kb)
                        stop_if("p2b", kb)
                    def qgeom(qi):
                        segs = [(c * 512, 512) for c in range(2 * j + 1)] + [((2 * j + 1) * 512, (qi + 1) * 128)]
                        nkeys = (2 * j + 1) * 512 + (qi + 1) * 128
                        return segs, nkeys, slice(qi * 128, (qi + 1) * 128)

                    def stage_A(qi):
                        segs, nkeys, qcols = qgeom(qi)
                        op(pool, lambda e: e.tensor_tensor(
                            out=Dg[:], in0=ident[:].unsqueeze(1).to_broadcast([128, 8, 128]),
                            in1=wsc[:, qi, :].unsqueeze(2).to_broadcast([128, 8, 128]), op=ALU.mult),
                           rd=[t_ident, t_wsc], wr=[t_Dg])
                        units = [(si, h) for si in range(len(segs)) for h in range(8)]
                        U = len(units)

                        def emit_L(u):
                            si, h = units[u]
                            c0, n = segs[si]
                            b, g = h // 2, h % 2
                            bk = u % 4
                            op(pe, lambda e: e.matmul(ps[:, bk, 0:n], lhsT=qiT[:, b, g, qcols],
                                                      rhs=kiT[:, c0:c0 + n], start=True, stop=True),
                               rd=[t_qiT, t_kiT], wr=[PB[bk]])
                            r = Rb[u % 8]
                            if u % 2 == 0:
                                op(act, lambda e: e.activation(out=r[:, 0:n], in_=ps[:, bk, 0:n], func=AF.Relu),
                                   rd=[PB[bk]], wr=[t_Rb[u % 8]])
                            else:
                                op(dve, lambda e: e.tensor_scalar(out=r[:, 0:n], in0=ps[:, bk, 0:n], scalar1=0.0, scalar2=None,
                                                                  op0=ALU.max), rd=[PB[bk]], wr=[t_Rb[u % 8]])

                        def emit_D(u):
                            si, h = units[u]
                            c0, n = segs[si]
                            sbk = 4 + (si % 2)
                            op(pe, lambda e: e.matmul(ps[:, sbk, 0:n], lhsT=Dg[:, h, :], rhs=Rb[u % 8][:, 0:n],
                                                      start=(h == 0), stop=(h == 7)),
                               rd=[t_Dg, t_Rb[u % 8]], wr=[PB[sbk]])
                            if h == 7:
                                if si == 2 * j:
                                    op(act, lambda e: e.activation(out=SC[:, c0:c0 + n], in_=ps[:, sbk, 0:n], func=AF.Identity,
                                                                   bias=oflg[:, j:j + 1], scale=1.0),
                                       rd=[PB[sbk], t_small], wr=[t_SC])
                                elif si == 2 * j + 1:
                                    if n > 128:
                                        op(act, lambda e: e.activation(out=SC[:, c0:c0 + n - 128], in_=ps[:, sbk, 0:n - 128],
                                                                       func=AF.Copy), rd=[PB[sbk]], wr=[t_SC])
                                    op(dve, lambda e: e.tensor_tensor(out=SC[:, c0 + n - 128:c0 + n], in0=ps[:, sbk, n - 128:n],
                                                                      in1=trim[:], op=ALU.add), rd=[PB[sbk], t_ident], wr=[t_SC])
                                else:
                                    op(act, lambda e: e.activation(out=SC[:, c0:c0 + n], in_=ps[:, sbk, 0:n], func=AF.Copy),
                                       rd=[PB[sbk]], wr=[t_SC])

                        for u in range(U + 4):
                            if u < U:
                                emit_L(u)
                            if u >= 4:
                                emit_D(u - 4)

                    def stage_B(qi, frac):
                        segs, nkeys, qcols = qgeom(qi)
                        na = min(int(frac * nkeys) // 128 * 128, JA)
                        scv = SC[:, 0:nkeys]
                        br = 12.0 if j == 0 else BR
                        nbis = 12 if j == 0 else NBIS
                        op(dve, lambda e: e.tensor_reduce(out=bst[:, 0:1], in_=scv, axis=AX.X, op=ALU.max), rd=[t_SC], wr=[t_bst])
                        op(dve, lambda e: e.tensor_scalar(out=bst[:, 1:2], in0=bst[:, 0:1], scalar1=-br / 2, scalar2=None,
                                                          op0=ALU.add), rd=[t_bst], wr=[t_bst])
                        for it in range(nbis):
                            if na > 0:
                                op(act, lambda e: e.activation(out=junkA[:, 0:na], in_=SC[:, 0:na], func=AF.Sign,
                                                               bias=bst[:, 1:2], scale=-1.0, accum_out=bsa[:, 0:1]),
                                   rd=[t_SC, t_bst], wr=[t_junkA, t_bsa])
                            op(dve, lambda e: e.tensor_scalar(out=junk[:, na:nkeys], in0=SC[:, na:nkeys], scalar1=bst[:, 1:2],
                                                              scalar2=None, op0=ALU.is_gt, op1=ALU.add, accum_out=bst[:, 2:3]),
                               rd=[t_SC, t_bst], wr=t_hTc + [t_bst])
                            if na > 0:
                                op(dve, lambda e: e.scalar_tensor_tensor(out=bst[:, 2:3], in0=bsa[:, 0:1], scalar=-0.5,
                                                                         in1=bst[:, 2:3], op0=ALU.mult, op1=ALU.add),
                                   rd=[t_bsa, t_bst], wr=[t_bst])
                            last = (it == nbis - 1)
                            cn = (br / 2) / (2 ** it) if last else (br / 2) / (2 ** (it + 1))
                            op(dve, lambda e: e.tensor_scalar(out=bst[:, 3:4], in0=bst[:, 2:3], scalar1=255.5 - na / 2.0,
                                                              scalar2=(cn if last else 2.0 * cn), op0=ALU.is_gt, op1=ALU.mult),
                               rd=[t_bst], wr=[t_bst])
                            op(dve, lambda e: e.scalar_tensor_tensor(out=bst[:, 1:2], in0=bst[:, 3:4], scalar=-cn,
                                                                     in1=bst[:, 1:2], op0=ALU.add, op1=ALU.add),
                               rd=[t_bst], wr=[t_bst])
                            yield
                        if j == nchunks - 1 and qi == 3:
                            dump("SC", SC[:, 0:nkeys], [t_SC], kb)
                            dump("bst", bst[:, 0:4], [t_bst], kb)
                            stop_if("p2c", kb)
                        op(dve, lambda e: e.tensor_scalar(out=MB[:, 0:nkeys], in0=scv, scalar1=bst[:, 1:2], scalar2=NEG,
                                                          op0=ALU.is_le, op1=ALU.mult), rd=[t_SC, t_bst], wr=[t_MB])

                    def stage_C_main(qi):
                        segs, nkeys, qcols = qgeom(qi)
                        nsb = nkeys // 128
                        U = nsb * 2
                        LAG = 2

                        def emit_S(u):
                            sbi, g = u // 2, u % 2
                            bk = u % 4
                            op(pe, lambda e: e.matmul(ps[:, bk, :], lhsT=kT[:, sbi * 128:(sbi + 1) * 128],
                                                      rhs=qT[:, g, :, qcols], start=True, stop=False),
                               rd=[t_kT, t_qT], wr=[PB[bk]])
                            op(pe, lambda e: e.matmul(ps[:, bk, :], lhsT=MB[:, sbi * 128:(sbi + 1) * 128],
                                                      rhs=ident4[:], start=False, stop=True),
                               rd=[t_MB, t_ident], wr=[PB[bk]])
                            op(act, lambda e: e.activation(out=PT[u % 4][:], in_=ps[:, bk, :], func=AF.Exp, scale=0.125),
                               rd=[PB[bk]], wr=[t_PT[u % 4]])

                        def emit_V(u):
                            sbi, g = u // 2, u % 2
                            pt = PT[u % 4]
                            ob = 4 + g
                            for b in range(4):
                                op(pe, lambda e, b=b: e.matmul(ps[:, ob, b * 65:(b + 1) * 65], lhsT=pt[:, b * 128:(b + 1) * 128],
                                                               rhs=Vaug[:, sbi, g, :], start=(sbi == 0 and b == 0),
                                                               stop=(sbi == nsb - 1 and b == 3), skip_group_check=True),
                                   rd=[t_PT[u % 4], t_V], wr=[PB[ob]])

                        for u in range(U + LAG):
                            if u < U:
                                emit_S(u)
                            if u >= LAG:
                                emit_V(u - LAG)
                            yield

                    def stage_C_tail(qi):
                        segs, nkeys, qcols = qgeom(qi)
                        for g in range(2):
                            ov = ps[:, 4 + g, 0:260].rearrange("p (b e) -> p b e", e=65)
                            op(dve, lambda e: e.reciprocal(out=rs4[:, g * 4:(g + 1) * 4].unsqueeze(2), in_=ov[:, :, 64:65]),
                               rd=[PB[4 + g]], wr=[t_rs4])
                            op(dve, lambda e: e.tensor_tensor(
                                out=attn[:, g * 256:(g + 1) * 256].rearrange("p (b d) -> p b d", d=64), in0=ov[:, :, 0:64],
                                in1=rs4[:, g * 4:(g + 1) * 4].unsqueeze(2).to_broadcast([128, 4, 64]), op=ALU.mult),
                               rd=[PB[4 + g], t_rs4], wr=[t_attn])
                        if j == nchunks - 1 and qi == 3:
                            dump("attn", attn[:], [t_attn], kb)
                            stop_if("p2d", kb)
                        pv = psb16(6 + qi % 2)
                        for f in range(4):
                            op(pe, lambda e, f=f: e.transpose(out=pv[:, f * 128:(f + 1) * 128], in_=attn[:, f * 128:(f + 1) * 128],
                                                              identity=ident[:]), rd=[t_attn, t_ident], wr=[PB[6 + qi % 2]])
                        op(act, lambda e: e.activation(out=mixT[:, 0:4, qcols], in_=pv[:, 0:512].rearrange("p (f t) -> p f t", f=4),
                                                       func=AF.Copy), rd=[PB[6 + qi % 2]], wr=[t_mixT])

                    def interleave(gb, gc, nb):
                        csteps = list(range(gc[1]))
                        per = (len(csteps) + nb - 1) // nb if nb else 0
                        gcg, gbg = gc[0], gb
                        for it in range(nb):
                            next(gbg, None)
                            for _ in range(per):
                                next(gcg, None)
                        for _ in gbg:
                            pass
                        for _ in gcg:
                            pass

                    def csteps_of(qi):
                        return (qgeom(qi)[1] // 128) * 2 + 2

                    stage_A(0)
                    for _ in stage_B(0, 0.55):
                        pass
                    for qi in range(1, 4):
                        stage_A(qi)
                        interleave(stage_B(qi, 0.12), (stage_C_main(qi - 1), csteps_of(qi - 1)), 12 if j == 0 else NBIS)
                        stage_C_tail(qi - 1)
                    for _ in stage_C_main(3):
                        pass
                    stage_C_tail(3)

                    wo0, two0 = wload(wview(w_out, 0, 512))
                    wo1, two1 = wload(wview(w_out, 512, 1024))
                    for t4 in range(4):
                        for half, (wo, two) in enumerate(((wo0, two0), (wo1, two1))):
                            bk = half
                            for f in range(8):
                                op(pe, lambda e, f=f: e.matmul(ps[:, bk, :], lhsT=mixT[:, f, t4 * 128:(t4 + 1) * 128], rhs=wo[:, f, :],
                                                               start=(f == 0), stop=(f == 7)), rd=[t_mixT, two], wr=[PB[bk]])
                        s2 = t4 % 2
                        dma(sp, xts[s2][:], xp[(tile0 + t4) * 128:(tile0 + t4 + 1) * 128, :], wr=[t_xts[s2]])
                        op(dve, lambda e: e.tensor_tensor(out=x1t, in0=ps[:, 0:2, :].rearrange("p a b -> p (a b)"), in1=G1[:],
                                                          op=ALU.mult), rd=[PB[0], PB[1], t_G1], wr=[t_x1t])
                        op(pool, lambda e: e.tensor_tensor(out=x1t, in0=x1t, in1=xts[s2][:], op=ALU.add),
                           rd=[t_x1t, t_xts[s2]], wr=[t_x1t])
                        r0 = (j * 4 + t4) * 128
                        dma(sp, x1s[r0:r0 + 128, :], x1t, rd=[t_x1t])
                kb.barrier()
                stop_if("p2e", kb)

            with contextlib.ExitStack() as es2:
                Wup = sb("Wup", [128, 8, 4096], BF16); t_Wup = T()
                Wdn = sb("Wdn", [128, 32, 1024], BF16); t_Wdn = T()
                GF = sb("GF", [128, D], F32); t_GF = T()
                xts = [sb("m_xt%d" % i, [128, D], F32) for i in range(4)]; t_xts = [T() for _ in range(4)]
                xns = [sb("m_xn%d" % i, [128, D], BF16) for i in range(4)]; t_xns = [T() for _ in range(4)]
                sqj = None; t_sqj = None
                G2 = sb("G2", [128, D], F32)
                dma(sp, G2[:], g2s, rd=[t_G2], wr=[t_G2])
                sts = [sb("m_st%d" % i, [128, 4], F32) for i in range(4)]; t_sts = [T() for _ in range(4)]
                h2T = [sb("h2T%d" % i, [128, 8, 256], BF16) for i in range(2)]; t_h2T = [[T(), T()], [T(), T()]]
                rT = [sb("rT%d" % i, [128, 256], BF16) for i in range(2)]; t_rT = [T(), T()]
                uT = sb("uT", [128, 32, 256], BF16); t_uT = T()
                x2 = sb("x2", [128, D], F32); t_x2 = T()
                oo = x2; t_oo = t_x2
                for c4 in range(8):
                    dma(pool, Wup[:, :, c4 * 512:(c4 + 1) * 512], wview(w_up, c4 * 512, (c4 + 1) * 512), wr=[t_Wup])
                for c4 in range(4):
                    dma(pool, Wdn[:, c4 * 8:(c4 + 1) * 8, :],
                        w_down[c4 * 1024:(c4 + 1) * 1024, :].rearrange("(k p) e -> p k e", p=128), wr=[t_Wdn])
                dma(sp, GF[:], gfb, wr=[t_GF])
                def m_pre_norm(gi):
                    for t2 in range(2):
                        sl = (gi % 2) * 2 + t2
                        r0 = (gi * 2 + t2) * 128
                        norm_tile(r0, xts[sl], t_xts[sl], xns[sl], t_xns[sl], sqj, t_sqj, sts[sl], t_sts[sl], src=x1s)

                def m_pre_T(gi):
                    for t2 in range(2):
                        sl = (gi % 2) * 2 + t2
                        transpose_mod(xns[sl], t_xns[sl], 6 + t2, h2T[gi % 2][:, :, t2 * 128:(t2 + 1) * 128], t_h2T[gi % 2][t2], 2)

                def m_up(gi):
                    hh = h2T[gi % 2]
                    for ff in range(32):
                        bk = ff % 4
                        for k in range(8):
                            op(pe, lambda e, k=k: e.matmul(ps[:, bk, 0:256], lhsT=Wup[:, k, ff * 128:(ff + 1) * 128], rhs=hh[:, k, :],
                                                           start=(k == 0), stop=(k == 7)), rd=[t_Wup] + t_h2T[gi % 2], wr=[PB[bk]])
                        r = rT[ff % 2]
                        op(act, lambda e: e.activation(out=r[:], in_=ps[:, bk, 0:256], func=AF.Relu), rd=[PB[bk]], wr=[t_rT[ff % 2]])
                        op(pool, lambda e, ff=ff: e.tensor_tensor(out=uT[:, ff, :], in0=r[:], in1=r[:], op=ALU.mult),
                           rd=[t_rT[ff % 2]], wr=[t_uT])
                        if ff == 8 and gi + 1 < 16:
                            m_pre_norm(gi + 1)

                def m_down(gi):
                    for t2 in range(2):
                        sl = (gi % 2) * 2 + t2
                        for half in range(2):
                            bk = 4 + half
                            for ff in range(32):
                                op(pe, lambda e, ff=ff: e.matmul(ps[:, bk, :], lhsT=uT[:, ff, t2 * 128:(t2 + 1) * 128],
                                                                 rhs=Wdn[:, ff, half * 512:(half + 1) * 512],
                                                                 start=(ff == 0), stop=(ff == 31)), rd=[t_uT, t_Wdn], wr=[PB[bk]])
                        op(dve, lambda e: e.tensor_tensor(out=x2[:], in0=ps[:, 4:6, :].rearrange("p a b -> p (a b)"), in1=G2[:],
                                                          op=ALU.mult), rd=[PB[4], PB[5], t_G2], wr=[t_x2])
                        op(pool, lambda e: e.tensor_tensor(out=x2[:], in0=x2[:], in1=xts[sl][:], op=ALU.add),
                           rd=[t_x2, t_xts[sl]], wr=[t_x2])
                        st = sts[sl]
                        op(act, lambda e: e.activation(out=xns[sl][:], in_=x2[:], func=AF.Square, accum_out=st[:, 0:1]),
                           rd=[t_x2], wr=[t_xns[sl], t_sts[sl]])
                        op(act, lambda e: e.activation(out=st[:, 1:2], in_=st[:, 0:1], func=AF.Sqrt, bias=EPS, scale=1.0 / D),
                           rd=[t_sts[sl]], wr=[t_sts[sl]])
                        op(dve, lambda e: e.reciprocal(out=st[:, 2:3], in_=st[:, 1:2]), rd=[t_sts[sl]], wr=[t_sts[sl]])
                        op(dve, lambda e: e.scalar_tensor_tensor(out=oo[:], in0=x2[:], scalar=st[:, 2:3], in1=GF[:],
                                                                 op0=ALU.mult, op1=ALU.mult), rd=[t_x2, t_sts[sl], t_GF], wr=[t_oo])
                        r0 = (gi * 2 + t2) * 128
                        dma(sp, out[r0:r0 + 128, :], oo[:], rd=[t_oo])

                m_pre_norm(0)
                m_pre_T(0)
                for gi in range(16):
                    m_up(gi)
                    if gi + 1 < 16:
                        m_pre_T(gi + 1)
                    m_down(gi)
                kb.barrier()

    try:
        _body()
    except _Stop:
        pass
    return nc


_NC_CACHE = {}


def _layout_inputs(x, c, positions, w_ada, b_ada, g_mix, w_in, conv_w, conv_b, conv_norm_g, conv_norm_b,
                   w_out, g_mlp, w_up, w_down, g_final):
    f32 = np.float32
    x = np.asarray(x, f32); c = np.asarray(c, f32); positions = np.asarray(positions, np.int32)

    def col(v, n):
        return np.ascontiguousarray(np.asarray(v, f32).reshape(n, 128).T)
    shared = {
        "w_ada": np.ascontiguousarray(np.asarray(w_ada, f32)[0]),
        "badac": col(np.asarray(b_ada)[0], 48),
        "badar": np.ascontiguousarray(np.asarray(b_ada, f32)[0][None, :]),
        "gmixc": col(np.asarray(g_mix)[0], 8),
        "gmlpc": col(np.asarray(g_mlp)[0], 8),
        "w_in": np.ascontiguousarray(np.asarray(w_in, f32)[0]),
        "convw": np.ascontiguousarray(np.asarray(conv_w, f32)[0].T.reshape(4, 128, 31).transpose(1, 0, 2)),
        "convb": col(np.asarray(conv_b)[0], 4),
        "cng": col(np.asarray(conv_norm_g)[0], 4),
        "cnb": col(np.asarray(conv_norm_b)[0], 4),
        "w_out": np.ascontiguousarray(np.asarray(w_out, f32)[0]),
        "w_up": np.ascontiguousarray(np.asarray(w_up, f32)[0]),
        "w_down": np.ascontiguousarray(np.asarray(w_down, f32)[0]),
        "gfb": np.ascontiguousarray(np.broadcast_to(np.asarray(g_final, f32)[None, :], (128, D))),
        "invf": np.ascontiguousarray(np.broadcast_to(
            np.power(f32(500000.0), -np.arange(8, dtype=f32) * f32(2.0) / f32(16.0)).astype(f32)[None, :], (128, 8))),
    }
    in_maps = []
    for core in range(8):
        b, p = core // 2, core % 2
        own, oth = OWN[p], OWN[1 - p]
        rows = []
        for j in range(8):
            rows.append(np.arange(oth[j] * 512, oth[j] * 512 + 512))
            rows.append(np.arange(own[j] * 512, own[j] * 512 + 512))
        rows = np.concatenate(rows)
        xpa = np.zeros((NT * 128, D), f32)
        xpa[:S] = x[b][rows]
        pos = np.zeros((NT * 128,), np.int32)
        pos[:S] = positions[b][rows]
        hm = np.ones((256,), f32)
        for j in range(8):
            if own[j] == 0:
                hm[j * 32:(j + 1) * 32] = 0.0
            else:
                hr = np.arange(own[j] * 512 - 32, own[j] * 512)
                xpa[S + j * 32:S + (j + 1) * 32] = x[b][hr]
                pos[S + j * 32:S + (j + 1) * 32] = positions[b][hr]
        of = np.array([0.0 if oth[j] < own[j] else NEG for j in range(8)], f32)
        m = dict(shared)
        m["xp"] = xpa
        m["posp"] = np.ascontiguousarray(pos.reshape(NT, 128).T)
        m["oflag"] = np.ascontiguousarray(np.broadcast_to(of[None, :], (128, 8)))
        m["hmask"] = np.ascontiguousarray(np.broadcast_to(hm[None, :], (128, 256)))
        m["cT"] = col(c[b], 8)
        in_maps.append(m)
    return in_maps


def kernel(**inputs):
    in_maps = _layout_inputs(**inputs)
    if "nc" not in _NC_CACHE:
        _NC_CACHE["nc"] = build_program()
    nc = _NC_CACHE["nc"]
    res = run_bass_kernel_spmd(nc, in_maps, core_ids=list(range(8)))
    outf = np.zeros((4, S, D), np.float32)
    for core in range(8):
        b, p = core // 2, core % 2
        o = res.results[core]["out"]
        for j, ch in enumerate(OWN[p]):
            outf[b, ch * 512:(ch + 1) * 512] = o[j * 512:(j + 1) * 512]
    if DEBUG:
        kernel.debug = res.results
    return outf
```

```python
import numpy as np
import concourse.bass as bass
import concourse.mybir as mybir
from concourse.bass_utils import run_bass_kernel_spmd

F32 = mybir.dt.float32
BF16 = mybir.dt.bfloat16
I32 = mybir.dt.int32
U8 = mybir.dt.uint8
ALU = mybir.AluOpType
AF = mybir.ActivationFunctionType
AX = mybir.AxisListType

D = 1024
S = 8192
NT = 66
NEG = -30000.0
EPS = 1e-6
NBIS = 10
BR = 6.0
JA = 2560
OWN = ([0, 3, 4, 7, 8, 11, 12, 15], [1, 2, 5, 6, 9, 10, 13, 14])
DEBUG = False


class T:
    __slots__ = ("w", "r")

    def __init__(self):
        self.w = {}
        self.r = {}


class Eng:
    def __init__(self, obj, sem, key):
        self.obj = obj
        self.sem = sem
        self.key = key
        self.cnt = 0
        self.seen = {}


class K:
    def __init__(self, nc, sems):
        self.nc = nc
        it = iter(sems)
        self.pe = Eng(nc.tensor, next(it), "pe")
        self.act = Eng(nc.scalar, next(it), "act")
        self.dve = Eng(nc.vector, next(it), "dve")
        self.pool = Eng(nc.gpsimd, next(it), "pool")
        self.sp = Eng(nc.sync, next(it), "sp")
        self.engs = [self.pe, self.act, self.dve, self.pool, self.sp]
        self.dsems = {"sp": [[s, 0] for s in [next(it) for _ in range(8)]],
                      "pool": [[s, 0] for s in [next(it) for _ in range(8)]]}
        self.dptr = {"sp": 0, "pool": 0}

    def _waits(self, eng, rd, wr):
        need = {}

        def add(d, skip_self):
            for k, (s, v) in d.items():
                if skip_self and k == eng.key:
                    continue
                if k not in need or need[k][1] < v:
                    need[k] = (s, v)
        for t in rd:
            add(t.w, False)
        skip = (eng.key == "pe")
        for t in wr:
            add(t.w, skip)
            add(t.r, skip)
        for k, (s, v) in need.items():
            if eng.seen.get(k, 0) < v:
                eng.obj.wait_ge(s, v)
                eng.seen[k] = v

    def op(self, eng, fn, rd=(), wr=()):
        self._waits(eng, rd, wr)
        inst = fn(eng.obj)
        eng.cnt += 1
        inst.then_inc(eng.sem, 1)
        tok = (eng.sem, eng.cnt)
        for t in rd:
            t.r[eng.key] = tok
        for t in wr:
            t.w = {eng.key: tok}
            t.r = {}

    def dma(self, eng, out, in_, rd=(), wr=()):
        ring = self.dsems[eng.key]
        i = self.dptr[eng.key]
        self.dptr[eng.key] = (i + 1) % len(ring)
        sem, val = ring[i]
        key = "d%s%d" % (eng.key, i)
        self._waits(eng, rd, wr)
        if val > 0 and eng.seen.get(key, 0) < val:
            eng.obj.wait_ge(sem, val)
            eng.seen[key] = val
        eng.obj.dma_start(out=out, in_=in_).then_inc(sem, 16)
        ring[i][1] = val + 16
        tok = (sem, val + 16)
        for t in rd:
            t.r[key] = tok
        for t in wr:
            t.w = {key: tok}
            t.r = {}

    def barrier(self):
        for e in self.engs:
            for f in self.engs:
                if f is not e and f.cnt > 0 and e.seen.get(f.key, 0) < f.cnt:
                    e.obj.wait_ge(f.sem, f.cnt)
                    e.seen[f.key] = f.cnt
            for qk, ring in self.dsems.items():
                for i, (s, v) in enumerate(ring):
                    key = "d%s%d" % (qk, i)
                    if v > 0 and e.seen.get(key, 0) < v:
                        e.obj.wait_ge(s, v)
                        e.seen[key] = v


class _Stop(Exception):
    pass


def build_program(stage=None, dumps=(), nchunks=8, nt1=64):
    nc = bass.Bass("TRN2", target_bir_lowering=False)
    dt = nc.dram_tensor
    xp = dt("xp", [NT * 128, D], F32, kind="ExternalInput").ap()
    posp = dt("posp", [128, NT], I32, kind="ExternalInput").ap()
    oflag = dt("oflag", [128, 8], F32, kind="ExternalInput").ap()
    hmask = dt("hmask", [128, 256], F32, kind="ExternalInput").ap()
    invf = dt("invf", [128, 8], F32, kind="ExternalInput").ap()
    cT = dt("cT", [128, 8], F32, kind="ExternalInput").ap()
    w_ada = dt("w_ada", [D, 6 * D], F32, kind="ExternalInput").ap()
    badac = dt("badac", [128, 48], F32, kind="ExternalInput").ap()
    badar = dt("badar", [1, 6 * D], F32, kind="ExternalInput").ap()
    gmixc = dt("gmixc", [128, 8], F32, kind="ExternalInput").ap()
    gmlpc = dt("gmlpc", [128, 8], F32, kind="ExternalInput").ap()
    w_in = dt("w_in", [D, 2376], F32, kind="ExternalInput").ap()
    convw = dt("convw", [128, 4, 31], F32, kind="ExternalInput").ap()
    convb = dt("convb", [128, 4], F32, kind="ExternalInput").ap()
    cng = dt("cng", [128, 4], F32, kind="ExternalInput").ap()
    cnb = dt("cnb", [128, 4], F32, kind="ExternalInput").ap()
    w_out = dt("w_out", [D, D], F32, kind="ExternalInput").ap()
    w_up = dt("w_up", [D, 4 * D], F32, kind="ExternalInput").ap()
    w_down = dt("w_down", [4 * D, D], F32, kind="ExternalInput").ap()
    gfb = dt("gfb", [128, D], F32, kind="ExternalInput").ap()
    out = dt("out", [4096, D], F32, kind="ExternalOutput").ap()
    x1s = dt("x1s", [4096, D], F32).ap()
    g1s = dt("g1s", [128, D], F32).ap()
    g2s = dt("g2s", [128, D], F32).ap()
    if "x1s" in dumps:
        x1s = dt("dbg_x1s", [4096, D], F32, kind="ExternalOutput").ap()

    def wview(w, c0, c1):
        return w[:, c0:c1].rearrange("(k p) e -> p k e", p=128)

    import contextlib
    dump_aps = {}

    def dump(name, ap, tiles, kbref):
        if name not in dumps:
            return
        shp = [int(v) for v in ap.shape]
        d_ap = dt("dbg_" + name, shp, ap.dtype, kind="ExternalOutput").ap()
        kbref.dma(kbref.sp, d_ap, ap, rd=tiles)

    def stop_if(st, kbref):
        if stage == st:
            kbref.barrier()
            raise _Stop()

    def _body():
        with contextlib.ExitStack() as es:
            sems = [es.enter_context(nc.semaphore("s%d" % i)) for i in range(21)]
            kb = K(nc, sems)
            pe, act, dve, pool, sp = kb.pe, kb.act, kb.dve, kb.pool, kb.sp
            op, dma = kb.op, kb.dma

            def sb(name, shape, dtype=F32):
                return es2.enter_context(nc.sbuf_tensor(name, shape, dtype))

            ps = es.enter_context(nc.psum_tensor("ps", [128, 8, 512], F32))
            PB = [T() for _ in range(8)]

            def psb16(b):
                return ps[:, b, :].bitcast(BF16)

            es2 = es
            ident = sb("ident", [128, 128], BF16); t_ident = T()
            ident4 = sb("ident4", [128, 4, 128], BF16)
            identf = sb("identf", [128, 128], F32)
            trim = sb("trim", [128, 128], F32)
            onesm = sb("onesm", [128, 128], BF16)
            onesr = sb("onesr", [1, 128], F32)
            cosT = sb("cosT", [128, NT, 8], F32)
            sinT = sb("sinT", [128, NT, 8], F32); t_cs = T()
            modc = sb("modc", [128, 48], F32); t_modc = T()
            ab = sb("ab", [128, 4, 8], F32); t_ab = T()
            t_G1 = T(); t_G2 = T()
            oflg = sb("oflg", [128, 8], F32); t_small = T()
            cw = sb("cw", [128, 4, 31], F32)
            cb = sb("cb", [128, 4], F32)
            cg = sb("cg", [128, 4], F32)
            cbn = sb("cbn", [128, 4], F32)
            wst = {"slots": None, "tiles": None, "ptr": 0}

            def walloc(tag):
                wst["slots"] = [sb("wslot%s%d" % (tag, i), [128, 8, 512], BF16) for i in range(2)]
                wst["tiles"] = [T() for _ in range(2)]
                wst["ptr"] = 0

            def wload(src_ap):
                i = wst["ptr"]
                wst["ptr"] = (i + 1) % 2
                dma(pool, wst["slots"][i][:], src_ap, wr=[wst["tiles"][i]])
                return wst["slots"][i], wst["tiles"][i]

            op(pool, lambda e: e.memset(identf[:], 0.0), wr=[t_ident])
            op(pool, lambda e: e.affine_select(out=identf[:], in_=identf[:], pattern=[[-1, 128]],
                                               compare_op=ALU.not_equal, fill=1.0, base=0,
                                               channel_multiplier=1), rd=[t_ident], wr=[t_ident])
            op(pool, lambda e: e.tensor_copy(out=ident[:], in_=identf[:]), rd=[t_ident], wr=[t_ident])
            op(pool, lambda e: e.tensor_copy(out=ident4[:], in_=identf[:].unsqueeze(1).to_broadcast([128, 4, 128])),
               rd=[t_ident], wr=[t_ident])
            op(pool, lambda e: e.memset(trim[:], 0.0), wr=[t_ident])
            op(pool, lambda e: e.affine_select(out=trim[:], in_=trim[:], pattern=[[-1, 128]],
                                               compare_op=ALU.is_ge, fill=NEG, base=0,
                                               channel_multiplier=1), rd=[t_ident], wr=[t_ident])
            op(pool, lambda e: e.memset(onesm[:], 1.0 / 512.0), wr=[t_ident])
            op(pool, lambda e: e.memset(onesr[:], 1.0), wr=[t_ident])
            dma(sp, oflg[:], oflag, wr=[t_small])
            dma(sp, cw[:], convw, wr=[t_small])
            dma(sp, cb[:], convb, wr=[t_small])
            dma(sp, cg[:], cng, wr=[t_small])
            dma(sp, cbn[:], cnb, wr=[t_small])

            with contextlib.ExitStack() as es2:
                posi = sb("posi", [128, NT], I32)
                posf = sb("posf", [128, NT], F32)
                ivf = sb("ivf", [128, 8], F32)
                ang = sb("ang", [128, NT, 8], F32)
                tq = sb("tq", [128, NT, 8], F32)
                kq = sb("kq", [128, NT, 8], I32)
                kf = sb("kf", [128, NT, 8], F32)
                red = sb("red", [128, NT, 8], F32)
                t_p0 = T()
                cTs = sb("cTs", [128, 8], F32)
                cond = sb("cond", [128, 8], F32); t_cond = T()
                badc = sb("badc", [128, 48], F32)
                gmc = sb("gmc", [128, 2, 8], F32)
                rowb = sb("rowb", [1, 6144], F32)
                rows2 = [sb("rows%d" % i, [1, 512], F32) for i in range(2)]; t_rows2 = [T(), T()]
                Gtmp = sb("Gtmp", [128, 512], F32); t_Gtmp = T()
                wfs = [sb("wf%d" % i, [128, 8, 512], F32) for i in range(3)]; t_wfs = [T() for _ in range(3)]
                dma(sp, posi[:], posp, wr=[t_p0])
                dma(sp, ivf[:], invf, wr=[t_p0])
                dma(sp, cTs[:], cT, wr=[t_cond])
                dma(sp, badc[:], badac, wr=[t_cond])
                dma(sp, gmc[:, 0, :], gmixc, wr=[t_cond])
                dma(sp, gmc[:, 1, :], gmlpc, wr=[t_cond])
                dma(sp, rowb[:], badar, wr=[t_cond])
                rw = dict(rd=[t_p0], wr=[t_p0])
                op(dve, lambda e: e.tensor_copy(out=posf[:], in_=posi[:]), **rw)
                op(dve, lambda e: e.tensor_tensor(out=ang[:], in0=posf[:].unsqueeze(2).to_broadcast([128, NT, 8]),
                                                  in1=ivf[:].unsqueeze(1).to_broadcast([128, NT, 8]), op=ALU.mult), **rw)
                TWO_PI = 2.0 * np.pi
                C1 = 6.28125
                C2 = TWO_PI - C1

                def reduce_to(dst, shift):
                    op(dve, lambda e: e.tensor_scalar(out=tq[:], in0=ang[:], scalar1=shift, scalar2=1.0 / TWO_PI,
                                                      op0=ALU.add, op1=ALU.mult), **rw)
                    op(dve, lambda e: e.tensor_copy(out=kq[:], in_=tq[:]), **rw)
                    op(dve, lambda e: e.tensor_copy(out=kf[:], in_=kq[:]), **rw)
                    op(dve, lambda e: e.scalar_tensor_tensor(out=red[:], in0=kf[:], scalar=-C1, in1=ang[:],
                                                             op0=ALU.mult, op1=ALU.add), **rw)
                    op(dve, lambda e: e.scalar_tensor_tensor(out=red[:], in0=kf[:], scalar=-C2, in1=red[:],
                                                             op0=ALU.mult, op1=ALU.add), **rw)
                    op(dve, lambda e: e.tensor_scalar(out=red[:], in0=red[:], scalar1=shift, scalar2=None,
                                                      op0=ALU.add), **rw)
                    op(dve, lambda e: e.tensor_scalar(out=tq[:], in0=red[:], scalar1=np.pi, scalar2=-TWO_PI,
                                                      op0=ALU.is_gt, op1=ALU.mult), **rw)
                    op(dve, lambda e: e.tensor_tensor(out=red[:], in0=red[:], in1=tq[:], op=ALU.add), **rw)
                    op(dve, lambda e: e.tensor_scalar(out=tq[:], in0=red[:], scalar1=-np.pi, scalar2=TWO_PI,
                                                      op0=ALU.is_lt, op1=ALU.mult), **rw)
                    op(dve, lambda e: e.tensor_tensor(out=red[:], in0=red[:], in1=tq[:], op=ALU.add), **rw)
                    op(dve, lambda e: e.tensor_scalar(out=red[:], in0=red[:], scalar1=-3.1415925, scalar2=3.1415925,
                                                      op0=ALU.max, op1=ALU.min), **rw)
                    op(act, lambda e: e.activation(out=dst[:], in_=red[:], func=AF.Sin), rd=[t_p0], wr=[t_cs])

                reduce_to(sinT, 0.0)
                reduce_to(cosT, np.pi / 2.0)

                op(act, lambda e: e.activation(out=cond[:], in_=cTs[:], func=AF.Silu), rd=[t_cond], wr=[t_cond])
                for cc in range(12):
                    ws, tw = wfs[cc % 3], t_wfs[cc % 3]
                    dma(sp, ws[:], wview(w_ada, cc * 512, (cc + 1) * 512), wr=[tw])
                    rb_, trb_ = rows2[cc % 2], t_rows2[cc % 2]
                    pbk = 1 + cc % 2
                    for k in range(8):
                        op(pe, lambda e, k=k: e.matmul(ps[0:1, pbk, :], lhsT=cond[:, k:k + 1], rhs=ws[:, k, :],
                                                       start=(k == 0), stop=(k == 7)),
                           rd=[t_cond, tw], wr=[PB[pbk]])
                    op(dve, lambda e: e.tensor_tensor(out=rb_[:], in0=ps[0:1, pbk, :], in1=rowb[:, cc * 512:(cc + 1) * 512],
                                                      op=ALU.add), rd=[PB[pbk], t_cond], wr=[trb_])
                    if cc in (4, 5, 10, 11):
                        op(pe, lambda e: e.matmul(ps[:, 3, :], lhsT=onesr[:], rhs=rb_[:], start=True, stop=True),
                           rd=[trb_, t_ident], wr=[PB[3]])
                        Gs, tG = (g1s, t_G1) if cc < 6 else (g2s, t_G2)
                        go = (cc - 4) * 512 if cc < 6 else (cc - 10) * 512
                        op(act, lambda e: e.activation(out=Gtmp[:], in_=ps[:, 3, :], func=AF.Copy),
                           rd=[PB[3]], wr=[t_Gtmp])
                        dma(sp, Gs[:, go:go + 512], Gtmp[:], rd=[t_Gtmp], wr=[tG])
                    else:
                        for el in range(4):
                            et = cc * 4 + el
                            op(pe, lambda e, el=el, et=et: e.matmul(ps[:, 0, et:et + 1], lhsT=rb_[0:1, el * 128:(el + 1) * 128],
                                                                   rhs=onesr[0:1, 0:1], start=True, stop=True, skip_group_check=True),
                               rd=[trb_, t_ident], wr=[PB[0]])
                op(dve, lambda e: e.memset(modc[:], 0.0), wr=[t_modc])
                for lo_, hi_ in ((0, 16), (24, 40)):
                    op(dve, lambda e: e.tensor_copy(out=modc[:, lo_:hi_], in_=ps[:, 0, lo_:hi_]),
                       rd=[PB[0]], wr=[t_modc])
                op(dve, lambda e: e.scalar_tensor_tensor(out=ab[:, 0, :], in0=modc[:, 8:16], scalar=1.0, in1=gmc[:, 0, :],
                                                         op0=ALU.add, op1=ALU.mult), rd=[t_modc, t_cond], wr=[t_ab])
                op(dve, lambda e: e.tensor_copy(out=ab[:, 1, :], in_=modc[:, 0:8]), rd=[t_modc], wr=[t_ab])
                op(dve, lambda e: e.scalar_tensor_tensor(out=ab[:, 2, :], in0=modc[:, 32:40], scalar=1.0, in1=gmc[:, 1, :],
                                                         op0=ALU.add, op1=ALU.mult), rd=[t_modc, t_cond], wr=[t_ab])
                op(dve, lambda e: e.tensor_copy(out=ab[:, 3, :], in_=modc[:, 24:32]), rd=[t_modc], wr=[t_ab])
                dump("cosT", cosT[:], [t_cs], kb)
                dump("sinT", sinT[:], [t_cs], kb)
                dump("ab", ab[:], [t_ab], kb)
                dump("modc", modc[:], [t_modc], kb)
                kb.barrier()
                stop_if("p0", kb)

            def norm_tile(row0, xt, t_xt, xn, t_xn, sq, t_sq, st, t_st, src=None):
                dma(sp, xt[:], (xp if src is None else src)[row0:row0 + 128, :], wr=[t_xt])
                op(act, lambda e: e.activation(out=xn[:], in_=xt[:], func=AF.Square, accum_out=st[:, 0:1]),
                   rd=[t_xt], wr=[t_xn, t_st])
                op(act, lambda e: e.activation(out=st[:, 1:2], in_=st[:, 0:1], func=AF.Sqrt, bias=EPS, scale=1.0 / D),
                   rd=[t_st], wr=[t_st])
                op(dve, lambda e: e.reciprocal(out=st[:, 2:3], in_=st[:, 1:2]), rd=[t_st], wr=[t_st])
                op(act, lambda e: e.activation(out=xn[:], in_=xt[:], func=AF.Copy, scale=st[:, 2:3]),
                   rd=[t_xt, t_st], wr=[t_xn])

            def transpose_mod(xn, t_xn, bank, hT_dst, t_hT, abi):
                pv = psb16(bank)
                for k in range(8):
                    op(pe, lambda e, k=k: e.transpose(out=pv[:, k * 128:(k + 1) * 128], in_=xn[:, k * 128:(k + 1) * 128],
                                                      identity=ident[:]), rd=[t_xn, t_ident], wr=[PB[bank]])
                pv3 = pv.rearrange("p (k t) -> p k t", k=8)
                op(dve, lambda e: e.tensor_tensor(out=hT_dst, in0=pv3,
                                                  in1=ab[:, abi, :].unsqueeze(2).to_broadcast([128, 8, 128]), op=ALU.mult),
                   rd=[PB[bank], t_ab], wr=[t_hT])
                op(pool, lambda e: e.tensor_tensor(out=hT_dst, in0=hT_dst,
                                                   in1=ab[:, abi + 1, :].unsqueeze(2).to_broadcast([128, 8, 128]), op=ALU.add),
                   rd=[t_hT, t_ab], wr=[t_hT])

            with contextlib.ExitStack() as es2:
                kT = sb("kT", [128, S], BF16); t_kT = T()
                kiT = sb("kiT", [128, S], BF16); t_kiT = T()
                Vaug = sb("Vaug", [128, 64, 2, 65], BF16); t_V = T()
                W1 = sb("W1", [128, 8, 328], BF16); t_W1 = T()
                xts = [sb("xt%d" % i, [128, D], F32) for i in range(2)]; t_xts = [T(), T()]
                xns = [sb("xn%d" % i, [128, D], BF16) for i in range(2)]; t_xns = [T(), T()]
                sqj = None; t_sqj = None
                walloc("b")
                G1 = sb("G1", [128, D], F32)
                dma(sp, G1[:], g1s, rd=[t_G1], wr=[t_G1])
                sts = [sb("st%d" % i, [128, 4], F32) for i in range(2)]; t_sts = [T(), T()]
                hTc = sb("hTc", [128, 8, 512], BF16); t_hTc = [T() for _ in range(4)]
                rtmp = sb("rtmp", [128, 4, 16, 8], F32); t_rtmp = T()
                krot = [sb("krot%d" % i, [128, 256], BF16) for i in range(2)]; t_krot = [T(), T()]

                op(pool, lambda e: e.memset(Vaug[:], 1.0), wr=[t_V])
                dma(pool, W1[:, :, 0:128], wview(w_in, 512, 640), wr=[t_W1])
                dma(pool, W1[:, :, 128:192], wview(w_in, 1280, 1344), wr=[t_W1])
                dma(pool, W1[:, :, 192:320], wview(w_in, 640, 768), wr=[t_W1])
                dma(pool, W1[:, :, 320:328], wview(w_in, 1344, 1352), wr=[t_W1])

                def rope(src3, dst3, nh, ti, tsrc, tdst):
                    cs = cosT[:, ti, :].unsqueeze(1).to_broadcast([128, nh, 8])
                    sn = sinT[:, ti, :].unsqueeze(1).to_broadcast([128, nh, 8])
                    x1, x2 = src3[:, :, 0:8], src3[:, :, 8:16]
                    t1, t2, t3, t4 = (rtmp[:, i, 0:nh, :] for i in range(4))
                    op(dve, lambda e: e.tensor_tensor(out=t1, in0=x1, in1=cs, op=ALU.mult), rd=[tsrc, t_cs], wr=[t_rtmp])
                    op(dve, lambda e: e.tensor_tensor(out=t2, in0=x2, in1=sn, op=ALU.mult), rd=[tsrc, t_cs], wr=[t_rtmp])
                    op(dve, lambda e: e.tensor_tensor(out=t3, in0=x2, in1=cs, op=ALU.mult), rd=[tsrc, t_cs], wr=[t_rtmp])
                    op(dve, lambda e: e.tensor_tensor(out=t4, in0=x1, in1=sn, op=ALU.mult), rd=[tsrc, t_cs], wr=[t_rtmp])
                    op(dve, lambda e: e.tensor_tensor(out=dst3[:, :, 0:8], in0=t1, in1=t2, op=ALU.subtract),
                       rd=[t_rtmp], wr=[tdst])
                    op(dve, lambda e: e.tensor_tensor(out=dst3[:, :, 8:16], in0=t3, in1=t4, op=ALU.add),
                       rd=[t_rtmp], wr=[tdst])
                    op(act, lambda e: e.activation(out=dst3[:, :, 16:64], in_=src3[:, :, 16:64], func=AF.Copy),
                       rd=[tsrc], wr=[tdst])

                def rope4(src4, dst4, ti, tsrc, tdst):
                    cs = cosT[:, ti, :].unsqueeze(1).unsqueeze(1).to_broadcast([128, 2, 4, 8])
                    sn = sinT[:, ti, :].unsqueeze(1).unsqueeze(1).to_broadcast([128, 2, 4, 8])
                    x1, x2 = src4[:, :, :, 0:8], src4[:, :, :, 8:16]
                    t1, t2, t3, t4 = (rtmp[:, i, 0:8, :].rearrange("p (g b) d -> p g b d", g=2) for i in range(4))
                    op(dve, lambda e: e.tensor_tensor(out=t1, in0=x1, in1=cs, op=ALU.mult), rd=[tsrc, t_cs], wr=[t_rtmp])
                    op(dve, lambda e: e.tensor_tensor(out=t2, in0=x2, in1=sn, op=ALU.mult), rd=[tsrc, t_cs], wr=[t_rtmp])
                    op(dve, lambda e: e.tensor_tensor(out=t3, in0=x2, in1=cs, op=ALU.mult), rd=[tsrc, t_cs], wr=[t_rtmp])
                    op(dve, lambda e: e.tensor_tensor(out=t4, in0=x1, in1=sn, op=ALU.mult), rd=[tsrc, t_cs], wr=[t_rtmp])
                    op(dve, lambda e: e.tensor_tensor(out=dst4[:, :, :, 0:8], in0=t1, in1=t2, op=ALU.subtract),
                       rd=[t_rtmp], wr=[tdst])
                    op(dve, lambda e: e.tensor_tensor(out=dst4[:, :, :, 8:16], in0=t3, in1=t4, op=ALU.add),
                       rd=[t_rtmp], wr=[tdst])
                    op(act, lambda e: e.activation(out=dst4[:, :, :, 16:64], in_=src4[:, :, :, 16:64], func=AF.Copy),
                       rd=[tsrc], wr=[tdst])

                def ph1_S1(ti):
                    s2 = ti % 2
                    norm_tile(ti * 128, xts[s2], t_xts[s2], xns[s2], t_xns[s2], sqj, t_sqj, sts[s2], t_sts[s2])
                    hs = ti % 4
                    hdst = hTc[:, :, hs * 128:(hs + 1) * 128]
                    transpose_mod(xns[s2], t_xns[s2], 6 + s2, hdst, t_hTc[hs], 0)

                def ph1_S2(ti):
                    s2 = ti % 2
                    hs = ti % 4
                    bk = s2
                    for k in range(8):
                        op(pe, lambda e, k=k: e.matmul(ps[:, bk, 0:320], lhsT=hTc[:, k, hs * 128:(hs + 1) * 128],
                                                       rhs=W1[:, k, 0:320], start=(k == 0), stop=(k == 7)),
                           rd=[t_hTc[hs], t_W1], wr=[PB[bk]])
                    kr = krot[s2]
                    rope(ps[:, bk, 0:192].rearrange("p (h d) -> p h d", d=64),
                         kr[:, 0:192].rearrange("p (h d) -> p h d", d=64), 3, ti, PB[bk], t_krot[s2])
                    op(pool, lambda e: e.tensor_copy(out=kr[:, 192:256], in_=kr[:, 128:192]), rd=[t_krot[s2]], wr=[t_krot[s2]])
                    op(act, lambda e: e.activation(out=Vaug[:, ti, :, 0:64],
                                                   in_=ps[:, bk, 192:320].rearrange("p (g d) -> p g d", d=64), func=AF.Copy),
                       rd=[PB[bk]], wr=[t_V])
                    tb = 4 + s2
                    pv = psb16(tb)
                    op(pe, lambda e: e.transpose(out=pv[:, 0:128], in_=kr[:, 0:128], identity=ident[:]),
                       rd=[t_krot[s2], t_ident], wr=[PB[tb]])
                    op(pe, lambda e: e.transpose(out=pv[:, 128:256], in_=kr[:, 128:256], identity=ident[:]),
                       rd=[t_krot[s2], t_ident], wr=[PB[tb]])
                    op(act, lambda e: e.activation(out=kT[:, ti * 128:(ti + 1) * 128], in_=pv[:, 0:128], func=AF.Copy),
                       rd=[PB[tb]], wr=[t_kT])
                    op(dve, lambda e: e.tensor_copy(out=kiT[:, ti * 128:(ti + 1) * 128], in_=pv[:, 128:256]),
                       rd=[PB[tb]], wr=[t_kiT])


                ph1_S1(0)
                for ti in range(nt1):
                    if ti + 1 < nt1:
                        ph1_S1(ti + 1)
                    ph1_S2(ti)
                dump("kT", kT[:], [t_kT], kb)
                dump("kiT", kiT[:], [t_kiT], kb)
                dump("Vaug", Vaug[:], [t_V], kb)
                stop_if("p1", kb)
                SC = sb("SC", [128, S], F32); t_SC = T()
                junk = hTc[:].rearrange("p k t -> p (k t)").bitcast(U8)
                RbA = sb("RbA", [128, 8, 512], BF16)
                Rb = [RbA[:, i, :] for i in range(8)]; t_Rb = [T() for _ in range(8)]
                Dg = sb("Dg", [128, 8, 128], BF16); t_Dg = T()
                qT = sb("qT", [128, 2, 4, 512], BF16); t_qT = T()
                qiT = sb("qiT", [128, 4, 2, 512], BF16); t_qiT = T()
                op(pool, lambda e: e.memset(qT[:], 0.0), wr=[t_qT])
                op(pool, lambda e: e.memset(qiT[:], 0.0), wr=[t_qiT])
                qrot = [sb("qrot%d" % i, [128, 512], BF16) for i in range(2)]; t_qrot = [T(), T()]
                wsc = sb("wsc", [128, 4, 8], F32); t_wsc = T()
                PT = [sb("PT%d" % i, [128, 512], BF16) for i in range(4)]; t_PT = [T() for _ in range(4)]
                MB = sb("MB", [128, S], BF16); t_MB = T()
                junkA = sb("junkA", [128, JA], U8); t_junkA = T()
                bsa = sb("bsa", [128, 2], F32); t_bsa = T()
                bst = sb("bst", [128, 8], F32); t_bst = T()
                gluT = sb("gluT", [128, 4, 544], BF16); t_glu = T()
                gluH = sb("gluH", [128, 4, 256], BF16); t_gluH = T()
                SCb = SC[:].bitcast(BF16)
                ybf = SCb[:, 0:2048].rearrange("p (c t) -> p c t", c=4); t_ybf = t_SC
                ysq = SCb[:, 2048:4096].rearrange("p (c t) -> p c t", c=4); t_ysq = t_SC
                lnA = SC[:, 2048:2560]; t_lnA = t_SC
                lnB = SC[:, 2560:3072]; t_lnB = t_SC
                zn = SC[:, 3072:3584]; t_zn = t_SC
                sig = SC[:, 3584:4096]; t_sig = t_SC
                cdiag = RbA[:].rearrange("p a b -> p (a b)")[:, 0:3968].rearrange("p (k c) -> p k c", c=128)
                mixT = sb("mixT", [128, 8, 512], BF16); t_mixT = T()
                attn = sb("attn", [128, 512], BF16); t_attn = T()
                rs4 = sb("rs4", [128, 8], F32); t_rs4 = T()
                x1t = SC[:, 4096:5120]; t_x1t = t_SC
                hmB = sb("hmB", [128, 256], BF16)
                hm = sb("hm", [128, 256], F32)
                dma(sp, hm[:], hmask, wr=[t_small])
                op(pool, lambda e: e.tensor_copy(out=hmB[:], in_=hm[:]), rd=[t_small], wr=[t_small])

                def conv_glu_mm(ws_a, tw_a, ws_g, tw_g, ncols, ct, hcols, t_h):
                    b0 = (ct % 2) * 2
                    for k in range(8):
                        op(pe, lambda e, k=k: e.matmul(ps[:, b0, 0:ncols], lhsT=ws_a[:, k, ct * 128:(ct + 1) * 128],
                                                       rhs=hTc[:, k, hcols], start=(k == 0), stop=(k == 7)),
                           rd=t_h + [tw_a], wr=[PB[b0]])
                    for k in range(8):
                        op(pe, lambda e, k=k: e.matmul(ps[:, b0 + 1, 0:ncols], lhsT=ws_g[:, k, ct * 128:(ct + 1) * 128],
                                                       rhs=hTc[:, k, hcols], start=(k == 0), stop=(k == 7)),
                           rd=t_h + [tw_g], wr=[PB[b0 + 1]])

                def conv_glu_ev(ncols, ct, dst, tdst):
                    b0 = (ct % 2) * 2
                    op(act, lambda e: e.activation(out=sig[:, 0:ncols], in_=ps[:, b0 + 1, 0:ncols], func=AF.Sigmoid),
                       rd=[PB[b0 + 1]], wr=[t_sig])
                    op(dve, lambda e: e.tensor_tensor(out=dst, in0=ps[:, b0, 0:ncols], in1=sig[:, 0:ncols], op=ALU.mult),
                       rd=[PB[b0], t_sig], wr=[tdst])

                def conv_glu(ws_a, tw_a, ws_g, tw_g, ncols, ct, dst, tdst, hcols, t_h):
                    conv_glu_mm(ws_a, tw_a, ws_g, tw_g, ncols, ct, hcols, t_h)
                    conv_glu_ev(ncols, ct, dst, tdst)

                for hi in range(2):
                    norm_tile((64 + hi) * 128, xts[hi], t_xts[hi], xns[hi], t_xns[hi], sqj, t_sqj, sts[hi], t_sts[hi])
                    transpose_mod(xns[hi], t_xns[hi], 6 + hi, hTc[:, :, hi * 128:(hi + 1) * 128], t_hTc[hi], 0)
                wa, twa = wload(wview(w_in, 1352, 1864))
                wg, twg = wload(wview(w_in, 1864, 2376))
                for ct in range(4):
                    conv_glu(wa, twa, wg, twg, 256, ct, gluH[:, ct, :], t_gluH, slice(0, 256), [t_hTc[0], t_hTc[1]])
                    op(pool, lambda e, ct=ct: e.tensor_tensor(out=gluH[:, ct, :], in0=gluH[:, ct, :], in1=hmB[:], op=ALU.mult),
                       rd=[t_gluH, t_small], wr=[t_gluH])

                dump("gluH", gluH[:], [t_gluH], kb)
                stop_if("p2h", kb)
                for j in range(nchunks):
                    tile0 = (2 * j + 1) * 4
                    for t4 in range(4):
                        s2 = t4 % 2
                        norm_tile((tile0 + t4) * 128, xts[s2], t_xts[s2], xns[s2], t_xns[s2], sqj, t_sqj, sts[s2], t_sts[s2])
                        transpose_mod(xns[s2], t_xns[s2], 6 + s2, hTc[:, :, t4 * 128:(t4 + 1) * 128], t_hTc[t4], 0)
                    wqi, twqi = wload(wview(w_in, 768, 1280))
                    wq_box = {}

                    def u_mm(grp, t4, bk):
                        wq, twq = wq_box["w"] if grp == 0 else (wqi, twqi)
                        for k in range(8):
                            op(pe, lambda e, k=k: e.matmul(ps[:, bk, :], lhsT=hTc[:, k, t4 * 128:(t4 + 1) * 128],
                                                           rhs=wq[:, k, :], start=(k == 0), stop=(k == 7)),
                               rd=[t_hTc[t4], twq], wr=[PB[bk]])
                        if grp == 1:
                            for k in range(8):
                                op(pe, lambda e, k=k: e.matmul(ps[:, 2, 0:8], lhsT=hTc[:, k, t4 * 128:(t4 + 1) * 128],
                                                               rhs=W1[:, k, 320:328], start=(k == 0), stop=(k == 7)),
                                   rd=[t_hTc[t4], t_W1], wr=[PB[2]])
                            op(dve, lambda e: e.tensor_scalar(
                                out=wsc[:, t4, :].rearrange("p (b g) -> p g b", g=2),
                                in0=ps[:, 2, 0:8].rearrange("p (g b) -> p g b", g=2),
                                scalar1=float(8 ** -0.5 * 64 ** -0.5), scalar2=None, op0=ALU.mult),
                               rd=[PB[2]], wr=[t_wsc])

                    def u_rope(grp, t4, bk):
                        qr = qrot[bk]
                        src4 = ps[:, bk, :].rearrange("p (g b d) -> p g b d", g=2, b=4)
                        dst4 = qr[:].rearrange("p (b g d) -> p g b d", g=2, b=4)
                        rope4(src4, dst4, tile0 + t4, PB[bk], t_qrot[bk])

                    def u_tr(grp, t4, bk):
                        qr = qrot[bk]
                        t_dstT = t_qT if grp == 0 else t_qiT
                        tb = 4 + bk
                        pv = psb16(tb)
                        for b in range(4):
                            op(pe, lambda e, b=b: e.transpose(out=pv[:, b * 128:(b + 1) * 128],
                                                              in_=qr[:, b * 128:(b + 1) * 128], identity=ident[:]),
                               rd=[t_qrot[bk], t_ident], wr=[PB[tb]])
                        pv4 = pv[:, 0:512].rearrange("p (b t) -> p b t", b=4)
                        tcols = slice(t4 * 128, (t4 + 1) * 128)
                        if grp == 0:
                            d0, d1 = qT[0:64, 0, :, tcols], qT[64:128, 1, :, tcols]
                        else:
                            d0, d1 = qiT[0:64, :, 0, tcols], qiT[64:128, :, 1, tcols]
                        op(act, lambda e: e.activation(out=d0, in_=pv4[0:64], func=AF.Copy), rd=[PB[tb]], wr=[t_dstT])
                        op(dve, lambda e: e.tensor_copy(out=d1, in_=pv4[64:128]), rd=[PB[tb]], wr=[t_dstT])

                    def units_gen(grp):
                        u_mm(grp, 0, 0)
                        yield
                        for t4 in range(4):
                            if t4 + 1 < 4:
                                u_mm(grp, t4 + 1, (t4 + 1) % 2)
                                yield
                            u_rope(grp, t4, t4 % 2)
                            yield
                            u_tr(grp, t4, t4 % 2)
                            yield

                    for _ in units_gen(1):
                        pass
                    q_units = units_gen(0)
                    wa, twa = wload(wview(w_in, 1352, 1864))
                    wg, twg = wload(wview(w_in, 1864, 2376))
                    op(pool, lambda e: e.tensor_copy(out=gluT[:, :, 0:32], in_=gluH[:, :, j * 32:(j + 1) * 32]),
                       rd=[t_gluH], wr=[t_glu])
                    conv_glu_mm(wa, twa, wg, twg, 512, 0, slice(0, 512), t_hTc)
                    for ct in range(4):
                        if ct + 1 < 4:
                            conv_glu_mm(wa, twa, wg, twg, 512, ct + 1, slice(0, 512), t_hTc)
                        conv_glu_ev(512, ct, gluT[:, ct, 32:544], t_glu)
                    for ct in range(4):
                        op(pool, lambda e, ct=ct: e.tensor_tensor(
                            out=cdiag, in0=ident[:].unsqueeze(1).to_broadcast([128, 31, 128]),
                            in1=cw[:, ct, :].unsqueeze(2).to_broadcast([128, 31, 128]), op=ALU.mult),
                           rd=[t_ident, t_small], wr=t_Rb)
                        cbk = ct
                        for tap in range(31):
                            op(pe, lambda e, tap=tap, ct=ct: e.matmul(ps[:, cbk, :], lhsT=cdiag[:, tap, :],
                                                                     rhs=gluT[:, ct, tap + 2:tap + 514],
                                                                     start=(tap == 0), stop=(tap == 30)),
                               rd=t_Rb + [t_glu], wr=[PB[cbk]])
                        op(act, lambda e, ct=ct: e.activation(out=ybf[:, ct, :], in_=ps[:, cbk, :], func=AF.Identity,
                                                              bias=cb[:, ct:ct + 1], scale=1.0), rd=[PB[cbk], t_small], wr=[t_ybf])
                        op(act, lambda e, ct=ct: e.activation(out=ysq[:, ct, :], in_=ps[:, cbk, :], func=AF.Square,
                                                              bias=cb[:, ct:ct + 1], scale=1.0), rd=[PB[cbk], t_small], wr=[t_ysq])
                    for ct in range(4):
                        op(pe, lambda e, ct=ct: e.matmul(ps[:, 4, :], lhsT=onesm[:], rhs=ybf[:, ct, :],
                                                         start=(ct == 0), stop=(ct == 3)), rd=[t_ybf, t_ident], wr=[PB[4]])
                    for ct in range(4):
                        op(pe, lambda e, ct=ct: e.matmul(ps[:, 5, :], lhsT=onesm[:], rhs=ysq[:, ct, :],
                                                         start=(ct == 0), stop=(ct == 3)), rd=[t_ysq, t_ident], wr=[PB[5]])
                    op(act, lambda e: e.activation(out=lnA, in_=ps[:, 4, :], func=AF.Copy), rd=[PB[4]], wr=[t_lnA])
                    op(dve, lambda e: e.tensor_tensor(out=lnB, in0=lnA, in1=lnA, op=ALU.mult), rd=[t_lnA], wr=[t_lnB])
                    op(dve, lambda e: e.tensor_tensor(out=lnB, in0=ps[:, 5, :], in1=lnB, op=ALU.subtract),
                       rd=[PB[5], t_lnB], wr=[t_lnB])
                    op(dve, lambda e: e.tensor_scalar(out=lnB, in0=lnB, scalar1=0.0, scalar2=EPS, op0=ALU.max, op1=ALU.add),
                       rd=[t_lnB], wr=[t_lnB])
                    op(act, lambda e: e.activation(out=lnB, in_=lnB, func=AF.Sqrt), rd=[t_lnB], wr=[t_lnB])
                    op(dve, lambda e: e.reciprocal(out=lnB, in_=lnB), rd=[t_lnB], wr=[t_lnB])
                    for ct in range(4):
                        op(dve, lambda e, ct=ct: e.scalar_tensor_tensor(out=zn, in0=ps[:, ct, :], scalar=cb[:, ct:ct + 1],
                                                                        in1=lnA, op0=ALU.add, op1=ALU.subtract),
                           rd=[PB[ct], t_small, t_lnA], wr=[t_zn])
                        op(dve, lambda e: e.tensor_tensor(out=zn, in0=zn, in1=lnB, op=ALU.mult),
                           rd=[t_zn, t_lnB], wr=[t_zn])
                        op(act, lambda e, ct=ct: e.activation(out=mixT[:, 4 + ct, :], in_=zn, func=AF.Silu,
                                                              bias=cbn[:, ct:ct + 1], scale=cg[:, ct:ct + 1]),
                           rd=[t_zn, t_small], wr=[t_mixT])

                    if j == nchunks - 1:
                        dump("mixTc", mixT[:, 4:8, :], [t_mixT], kb)
                        stop_if("p2b", kb)
                    def qgeom(qi):
                        segs = [(c * 512, 512) for c in range(2 * j + 1)] + [((2 * j + 1) * 512, (qi + 1) * 128)]
                        nkeys = (2 * j + 1) * 512 + (qi + 1) * 128
                        return segs, nkeys, slice(qi * 128, (qi + 1) * 128)

                    def stage_A(qi):
                        segs, nkeys, qcols = qgeom(qi)
                        op(pool, lambda e: e.tensor_tensor(
                            out=Dg[:], in0=ident[:].unsqueeze(1).to_broadcast([128, 8, 128]),
                            in1=wsc[:, qi, :].unsqueeze(2).to_broadcast([128, 8, 128]), op=ALU.mult),
                           rd=[t_ident, t_wsc], wr=[t_Dg])
                        units = [(si, h) for si in range(len(segs)) for h in range(8)]
                        U = len(units)

                        def emit_L(u):
                            si, h = units[u]
                            c0, n = segs[si]
                            b, g = h // 2, h % 2
                            bk = u % 4
                            op(pe, lambda e: e.matmul(ps[:, bk, 0:n], lhsT=qiT[:, b, g, qcols],
                                                      rhs=kiT[:, c0:c0 + n], start=True, stop=True),
                               rd=[t_qiT, t_kiT], wr=[PB[bk]])
                            r = Rb[u % 8]
                            if u % 2 == 0:
                                op(act, lambda e: e.activation(out=r[:, 0:n], in_=ps[:, bk, 0:n], func=AF.Relu),
                                   rd=[PB[bk]], wr=[t_Rb[u % 8]])
                            else:
                                op(dve, lambda e: e.tensor_scalar(out=r[:, 0:n], in0=ps[:, bk, 0:n], scalar1=0.0, scalar2=None,
                                                                  op0=ALU.max), rd=[PB[bk]], wr=[t_Rb[u % 8]])

                        def emit_D(u):
                            si, h = units[u]
                            c0, n = segs[si]
                            sbk = 4 + (si % 2)
                            op(pe, lambda e: e.matmul(ps[:, sbk, 0:n], lhsT=Dg[:, h, :], rhs=Rb[u % 8][:, 0:n],
                                                      start=(h == 0), stop=(h == 7)),
                               rd=[t_Dg, t_Rb[u % 8]], wr=[PB[sbk]])
                            if h == 7:
                                if si == 2 * j:
                                    op(act, lambda e: e.activation(out=SC[:, c0:c0 + n], in_=ps[:, sbk, 0:n], func=AF.Identity,
                                                                   bias=oflg[:, j:j + 1], scale=1.0),
                                       rd=[PB[sbk], t_small], wr=[t_SC])
                                elif si == 2 * j + 1:
                                    if n > 128:
                                        op(act, lambda e: e.activation(out=SC[:, c0:c0 + n - 128], in_=ps[:, sbk, 0:n - 128],
                                                                       func=AF.Copy), rd=[PB[sbk]], wr=[t_SC])
                                    op(dve, lambda e: e.tensor_tensor(out=SC[:, c0 + n - 128:c0 + n], in0=ps[:, sbk, n - 128:n],
                                                                      in1=trim[:], op=ALU.add), rd=[PB[sbk], t_ident], wr=[t_SC])
                                else:
                                    op(act, lambda e: e.activation(out=SC[:, c0:c0 + n], in_=ps[:, sbk, 0:n], func=AF.Copy),
                                       rd=[PB[sbk]], wr=[t_SC])

                        for u in range(U + 4):
                            if u < U:
                                emit_L(u)
                            if u >= 4:
                                emit_D(u - 4)

                    def stage_B(qi, frac, jk=None, jk_t=None):
                        segs, nkeys, qcols = qgeom(qi)
                        na = min(int(frac * nkeys) // 128 * 128, JA)
                        scv = SC[:, 0:nkeys]
                        br = 12.0 if j == 0 else BR
                        nbis = 12 if j == 0 else NBIS
                        op(dve, lambda e: e.tensor_reduce(out=bst[:, 0:1], in_=scv, axis=AX.X, op=ALU.max), rd=[t_SC], wr=[t_bst])
                        op(dve, lambda e: e.tensor_scalar(out=bst[:, 1:2], in0=bst[:, 0:1], scalar1=-br / 2, scalar2=None,
                                                          op0=ALU.add), rd=[t_bst], wr=[t_bst])
                        for it in range(nbis):
                            if na > 0:
                                op(act, lambda e: e.activation(out=junkA[:, 0:na], in_=SC[:, 0:na], func=AF.Sign,
                                                               bias=bst[:, 1:2], scale=-1.0, accum_out=bsa[:, 0:1]),
                                   rd=[t_SC, t_bst], wr=[t_junkA, t_bsa])
                            jk_ = junk if jk is None else jk
                            jkt_ = t_hTc if jk is None else jk_t
                            op(dve, lambda e: e.tensor_scalar(out=jk_[:, na:nkeys], in0=SC[:, na:nkeys], scalar1=bst[:, 1:2],
                                                              scalar2=None, op0=ALU.is_gt, op1=ALU.add, accum_out=bst[:, 2:3]),
                               rd=[t_SC, t_bst], wr=jkt_ + [t_bst])
                            if na > 0:
                                op(dve, lambda e: e.scalar_tensor_tensor(out=bst[:, 2:3], in0=bsa[:, 0:1], scalar=-0.5,
                                                                         in1=bst[:, 2:3], op0=ALU.mult, op1=ALU.add),
                                   rd=[t_bsa, t_bst], wr=[t_bst])
                            last = (it == nbis - 1)
                            cn = (br / 2) / (2 ** it) if last else (br / 2) / (2 ** (it + 1))
                            op(dve, lambda e: e.tensor_scalar(out=bst[:, 3:4], in0=bst[:, 2:3], scalar1=255.5 - na / 2.0,
                                                              scalar2=(cn if last else 2.0 * cn), op0=ALU.is_gt, op1=ALU.mult),
                               rd=[t_bst], wr=[t_bst])
                            op(dve, lambda e: e.scalar_tensor_tensor(out=bst[:, 1:2], in0=bst[:, 3:4], scalar=-cn,
                                                                     in1=bst[:, 1:2], op0=ALU.add, op1=ALU.add),
                               rd=[t_bst], wr=[t_bst])
                            yield
                        if j == nchunks - 1 and qi == 3:
                            dump("SC", SC[:, 0:nkeys], [t_SC], kb)
                            dump("bst", bst[:, 0:4], [t_bst], kb)
                            stop_if("p2c", kb)
                        op(dve, lambda e: e.tensor_scalar(out=MB[:, 0:nkeys], in0=scv, scalar1=bst[:, 1:2], scalar2=NEG,
                                                          op0=ALU.is_le, op1=ALU.mult), rd=[t_SC, t_bst], wr=[t_MB])

                    def stage_C_main(qi):
                        segs, nkeys, qcols = qgeom(qi)
                        nsb = nkeys // 128
                        U = nsb * 2
                        LAG = 2

                        def emit_S(u):
                            sbi, g = u // 2, u % 2
                            bk = u % 4
                            op(pe, lambda e: e.matmul(ps[:, bk, :], lhsT=kT[:, sbi * 128:(sbi + 1) * 128],
                                                      rhs=qT[:, g, :, qcols], start=True, stop=False),
                               rd=[t_kT, t_qT], wr=[PB[bk]])
                            op(pe, lambda e: e.matmul(ps[:, bk, :], lhsT=MB[:, sbi * 128:(sbi + 1) * 128],
                                                      rhs=ident4[:], start=False, stop=True),
                               rd=[t_MB, t_ident], wr=[PB[bk]])
                            op(act, lambda e: e.activation(out=PT[u % 4][:], in_=ps[:, bk, :], func=AF.Exp, scale=0.125),
                               rd=[PB[bk]], wr=[t_PT[u % 4]])

                        def emit_V(u):
                            sbi, g = u // 2, u % 2
                            pt = PT[u % 4]
                            ob = 4 + g
                            for b in range(4):
                                op(pe, lambda e, b=b: e.matmul(ps[:, ob, b * 65:(b + 1) * 65], lhsT=pt[:, b * 128:(b + 1) * 128],
                                                               rhs=Vaug[:, sbi, g, :], start=(sbi == 0 and b == 0),
                                                               stop=(sbi == nsb - 1 and b == 3), skip_group_check=True),
                                   rd=[t_PT[u % 4], t_V], wr=[PB[ob]])

                        for u in range(U + LAG):
                            if u < U:
                                emit_S(u)
                            if u >= LAG:
                                emit_V(u - LAG)
                            yield

                    def stage_C_tail(qi):
                        segs, nkeys, qcols = qgeom(qi)
                        for g in range(2):
                            ov = ps[:, 4 + g, 0:260].rearrange("p (b e) -> p b e", e=65)
                            op(dve, lambda e: e.reciprocal(out=rs4[:, g * 4:(g + 1) * 4].unsqueeze(2), in_=ov[:, :, 64:65]),
                               rd=[PB[4 + g]], wr=[t_rs4])
                            op(dve, lambda e: e.tensor_tensor(
                                out=attn[:, g * 256:(g + 1) * 256].rearrange("p (b d) -> p b d", d=64), in0=ov[:, :, 0:64],
                                in1=rs4[:, g * 4:(g + 1) * 4].unsqueeze(2).to_broadcast([128, 4, 64]), op=ALU.mult),
                               rd=[PB[4 + g], t_rs4], wr=[t_attn])
                        if j == nchunks - 1 and qi == 3:
                            dump("attn", attn[:], [t_attn], kb)
                            stop_if("p2d", kb)
                        pv = psb16(6 + qi % 2)
                        for f in range(4):
                            op(pe, lambda e, f=f: e.transpose(out=pv[:, f * 128:(f + 1) * 128], in_=attn[:, f * 128:(f + 1) * 128],
                                                              identity=ident[:]), rd=[t_attn, t_ident], wr=[PB[6 + qi % 2]])
                        op(act, lambda e: e.activation(out=mixT[:, 0:4, qcols], in_=pv[:, 0:512].rearrange("p (f t) -> p f t", f=4),
                                                       func=AF.Copy), rd=[PB[6 + qi % 2]], wr=[t_mixT])

                    def interleave(gb, gc, nb):
                        csteps = list(range(gc[1]))
                        per = (len(csteps) + nb - 1) // nb if nb else 0
                        gcg, gbg = gc[0], gb
                        for it in range(nb):
                            next(gbg, None)
                            for _ in range(per):
                                next(gcg, None)
                        for _ in gbg:
                            pass
                        for _ in gcg:
                            pass

                    def csteps_of(qi):
                        return (qgeom(qi)[1] // 128) * 2 + 2

                    wq_box["w"] = wload(wview(w_in, 0, 512))
                    stage_A(0)
                    for _ in stage_B(0, 0.55, jk=MB[:].bitcast(U8), jk_t=[t_MB]):
                        next(q_units, None)
                        next(q_units, None)
                    for _ in q_units:
                        pass
                    for qi in range(1, 4):
                        stage_A(qi)
                        interleave(stage_B(qi, 0.12), (stage_C_main(qi - 1), csteps_of(qi - 1)), 12 if j == 0 else NBIS)
                        stage_C_tail(qi - 1)
                    for _ in stage_C_main(3):
                        pass
                    stage_C_tail(3)

                    wo0, two0 = wload(wview(w_out, 0, 512))
                    wo1, two1 = wload(wview(w_out, 512, 1024))
                    for t4 in range(4):
                        for half, (wo, two) in enumerate(((wo0, two0), (wo1, two1))):
                            bk = half
                            for f in range(8):
                                op(pe, lambda e, f=f: e.matmul(ps[:, bk, :], lhsT=mixT[:, f, t4 * 128:(t4 + 1) * 128], rhs=wo[:, f, :],
                                                               start=(f == 0), stop=(f == 7)), rd=[t_mixT, two], wr=[PB[bk]])
                        s2 = t4 % 2
                        dma(sp, xts[s2][:], xp[(tile0 + t4) * 128:(tile0 + t4 + 1) * 128, :], wr=[t_xts[s2]])
                        op(dve, lambda e: e.tensor_tensor(out=x1t, in0=ps[:, 0:2, :].rearrange("p a b -> p (a b)"), in1=G1[:],
                                                          op=ALU.mult), rd=[PB[0], PB[1], t_G1], wr=[t_x1t])
                        op(pool, lambda e: e.tensor_tensor(out=x1t, in0=x1t, in1=xts[s2][:], op=ALU.add),
                           rd=[t_x1t, t_xts[s2]], wr=[t_x1t])
                        r0 = (j * 4 + t4) * 128
                        dma(sp, x1s[r0:r0 + 128, :], x1t, rd=[t_x1t])
                kb.barrier()
                stop_if("p2e", kb)

            with contextlib.ExitStack() as es2:
                Wup = sb("Wup", [128, 8, 4096], BF16); t_Wup = T()
                Wdn = sb("Wdn", [128, 32, 1024], BF16); t_Wdn = T()
                GF = sb("GF", [128, D], F32); t_GF = T()
                xts = [sb("m_xt%d" % i, [128, D], F32) for i in range(4)]; t_xts = [T() for _ in range(4)]
                xns = [sb("m_xn%d" % i, [128, D], BF16) for i in range(4)]; t_xns = [T() for _ in range(4)]
                sqj = None; t_sqj = None
                G2 = sb("G2", [128, D], F32)
                dma(sp, G2[:], g2s, rd=[t_G2], wr=[t_G2])
                sts = [sb("m_st%d" % i, [128, 4], F32) for i in range(4)]; t_sts = [T() for _ in range(4)]
                h2T = [sb("h2T%d" % i, [128, 8, 256], BF16) for i in range(2)]; t_h2T = [[T(), T()], [T(), T()]]
                rT = [sb("rT%d" % i, [128, 256], BF16) for i in range(2)]; t_rT = [T(), T()]
                uT = sb("uT", [128, 32, 256], BF16); t_uT = T()
                x2 = sb("x2", [128, D], F32); t_x2 = T()
                oo = x2; t_oo = t_x2
                for c4 in range(8):
                    dma(pool, Wup[:, :, c4 * 512:(c4 + 1) * 512], wview(w_up, c4 * 512, (c4 + 1) * 512), wr=[t_Wup])
                for c4 in range(4):
                    dma(pool, Wdn[:, c4 * 8:(c4 + 1) * 8, :],
                        w_down[c4 * 1024:(c4 + 1) * 1024, :].rearrange("(k p) e -> p k e", p=128), wr=[t_Wdn])
                dma(sp, GF[:], gfb, wr=[t_GF])
                def m_pre_norm(gi):
                    for t2 in range(2):
                        sl = (gi % 2) * 2 + t2
                        r0 = (gi * 2 + t2) * 128
                        norm_tile(r0, xts[sl], t_xts[sl], xns[sl], t_xns[sl], sqj, t_sqj, sts[sl], t_sts[sl], src=x1s)

                def m_pre_T(gi):
                    for t2 in range(2):
                        sl = (gi % 2) * 2 + t2
                        transpose_mod(xns[sl], t_xns[sl], 6 + t2, h2T[gi % 2][:, :, t2 * 128:(t2 + 1) * 128], t_h2T[gi % 2][t2], 2)

                def m_up(gi):
                    hh = h2T[gi % 2]
                    for ff in range(32):
                        bk = ff % 4
                        for k in range(8):
                            op(pe, lambda e, k=k: e.matmul(ps[:, bk, 0:256], lhsT=Wup[:, k, ff * 128:(ff + 1) * 128], rhs=hh[:, k, :],
                                                           start=(k == 0), stop=(k == 7)), rd=[t_Wup] + t_h2T[gi % 2], wr=[PB[bk]])
                        r = rT[ff % 2]
                        op(act, lambda e: e.activation(out=r[:], in_=ps[:, bk, 0:256], func=AF.Relu), rd=[PB[bk]], wr=[t_rT[ff % 2]])
                        op(pool, lambda e, ff=ff: e.tensor_tensor(out=uT[:, ff, :], in0=r[:], in1=r[:], op=ALU.mult),
                           rd=[t_rT[ff % 2]], wr=[t_uT])
                        if ff == 8 and gi + 1 < 16:
                            m_pre_norm(gi + 1)

                def m_down(gi):
                    for t2 in range(2):
                        sl = (gi % 2) * 2 + t2
                        for half in range(2):
                            bk = 4 + half
                            for ff in range(32):
                                op(pe, lambda e, ff=ff: e.matmul(ps[:, bk, :], lhsT=uT[:, ff, t2 * 128:(t2 + 1) * 128],
                                                                 rhs=Wdn[:, ff, half * 512:(half + 1) * 512],
                                                                 start=(ff == 0), stop=(ff == 31)), rd=[t_uT, t_Wdn], wr=[PB[bk]])
                        op(dve, lambda e: e.tensor_tensor(out=x2[:], in0=ps[:, 4:6, :].rearrange("p a b -> p (a b)"), in1=G2[:],
                                                          op=ALU.mult), rd=[PB[4], PB[5], t_G2], wr=[t_x2])
                        op(pool, lambda e: e.tensor_tensor(out=x2[:], in0=x2[:], in1=xts[sl][:], op=ALU.add),
                           rd=[t_x2, t_xts[sl]], wr=[t_x2])
                        st = sts[sl]
                        op(act, lambda e: e.activation(out=xns[sl][:], in_=x2[:], func=AF.Square, accum_out=st[:, 0:1]),
                           rd=[t_x2], wr=[t_xns[sl], t_sts[sl]])
                        op(act, lambda e: e.activation(out=st[:, 1:2], in_=st[:, 0:1], func=AF.Sqrt, bias=EPS, scale=1.0 / D),
                           rd=[t_sts[sl]], wr=[t_sts[sl]])
                        op(dve, lambda e: e.reciprocal(out=st[:, 2:3], in_=st[:, 1:2]), rd=[t_sts[sl]], wr=[t_sts[sl]])
                        op(dve, lambda e: e.scalar_tensor_tensor(out=oo[:], in0=x2[:], scalar=st[:, 2:3], in1=GF[:],
                                                                 op0=ALU.mult, op1=ALU.mult), rd=[t_x2, t_sts[sl], t_GF], wr=[t_oo])
                        r0 = (gi * 2 + t2) * 128
                        dma(sp, out[r0:r0 + 128, :], oo[:], rd=[t_oo])

                m_pre_norm(0)
                m_pre_T(0)
                for gi in range(16):
                    m_up(gi)
                    if gi + 1 < 16:
                        m_pre_T(gi + 1)
                    m_down(gi)
                kb.barrier()

    try:
        _body()
    except _Stop:
        pass
    return nc


_NC_CACHE = {}


def _layout_inputs(x, c, positions, w_ada, b_ada, g_mix, w_in, conv_w, conv_b, conv_norm_g, conv_norm_b,
                   w_out, g_mlp, w_up, w_down, g_final):
    f32 = np.float32
    x = np.asarray(x, f32); c = np.asarray(c, f32); positions = np.asarray(positions, np.int32)

    def col(v, n):
        return np.ascontiguousarray(np.asarray(v, f32).reshape(n, 128).T)
    shared = {
        "w_ada": np.ascontiguousarray(np.asarray(w_ada, f32)[0]),
        "badac": col(np.asarray(b_ada)[0], 48),
        "badar": np.ascontiguousarray(np.asarray(b_ada, f32)[0][None, :]),
        "gmixc": col(np.asarray(g_mix)[0], 8),
        "gmlpc": col(np.asarray(g_mlp)[0], 8),
        "w_in": np.ascontiguousarray(np.asarray(w_in, f32)[0]),
        "convw": np.ascontiguousarray(np.asarray(conv_w, f32)[0].T.reshape(4, 128, 31).transpose(1, 0, 2)),
        "convb": col(np.asarray(conv_b)[0], 4),
        "cng": col(np.asarray(conv_norm_g)[0], 4),
        "cnb": col(np.asarray(conv_norm_b)[0], 4),
        "w_out": np.ascontiguousarray(np.asarray(w_out, f32)[0]),
        "w_up": np.ascontiguousarray(np.asarray(w_up, f32)[0]),
        "w_down": np.ascontiguousarray(np.asarray(w_down, f32)[0]),
        "gfb": np.ascontiguousarray(np.broadcast_to(np.asarray(g_final, f32)[None, :], (128, D))),
        "invf": np.ascontiguousarray(np.broadcast_to(
            np.power(f32(500000.0), -np.arange(8, dtype=f32) * f32(2.0) / f32(16.0)).astype(f32)[None, :], (128, 8))),
    }
    in_maps = []
    for core in range(8):
        b, p = core // 2, core % 2
        own, oth = OWN[p], OWN[1 - p]
        rows = []
        for j in range(8):
            rows.append(np.arange(oth[j] * 512, oth[j] * 512 + 512))
            rows.append(np.arange(own[j] * 512, own[j] * 512 + 512))
        rows = np.concatenate(rows)
        xpa = np.zeros((NT * 128, D), f32)
        xpa[:S] = x[b][rows]
        pos = np.zeros((NT * 128,), np.int32)
        pos[:S] = positions[b][rows]
        hm = np.ones((256,), f32)
        for j in range(8):
            if own[j] == 0:
                hm[j * 32:(j + 1) * 32] = 0.0
            else:
                hr = np.arange(own[j] * 512 - 32, own[j] * 512)
                xpa[S + j * 32:S + (j + 1) * 32] = x[b][hr]
                pos[S + j * 32:S + (j + 1) * 32] = positions[b][hr]
        of = np.array([0.0 if oth[j] < own[j] else NEG for j in range(8)], f32)
        m = dict(shared)
        m["xp"] = xpa
        m["posp"] = np.ascontiguousarray(pos.reshape(NT, 128).T)
        m["oflag"] = np.ascontiguousarray(np.broadcast_to(of[None, :], (128, 8)))
        m["hmask"] = np.ascontiguousarray(np.broadcast_to(hm[None, :], (128, 256)))
        m["cT"] = col(c[b], 8)
        in_maps.append(m)
    return in_maps


def kernel(**inputs):
    in_maps = _layout_inputs(**inputs)
    if "nc" not in _NC_CACHE:
        _NC_CACHE["nc"] = build_program()
    nc = _NC_CACHE["nc"]
    res = run_bass_kernel_spmd(nc, in_maps, core_ids=list(range(8)))
    outf = np.zeros((4, S, D), np.float32)
    for core in range(8):
        b, p = core // 2, core % 2
        o = res.results[core]["out"]
        for j, ch in enumerate(OWN[p]):
            outf[b, ch * 512:(ch + 1) * 512] = o[j * 512:(j + 1) * 512]
    if DEBUG:
        kernel.debug = res.results
    return outf
```

```python
import numpy as np
import concourse.bass as bass
import concourse.mybir as mybir
from concourse.bass_utils import run_bass_kernel_spmd

F32 = mybir.dt.float32
BF16 = mybir.dt.bfloat16
I32 = mybir.dt.int32
U8 = mybir.dt.uint8
ALU = mybir.AluOpType
AF = mybir.ActivationFunctionType
AX = mybir.AxisListType

D = 1024
S = 8192
NT = 66
NEG = -30000.0
EPS = 1e-6
NBIS = 10
BR = 6.0
JA = 2560
OWN = ([0, 3, 4, 7, 8, 11, 12, 15], [1, 2, 5, 6, 9, 10, 13, 14])
DEBUG = False


class T:
    __slots__ = ("w", "r")

    def __init__(self):
        self.w = {}
        self.r = {}


class Eng:
    def __init__(self, obj, sem, key):
        self.obj = obj
        self.sem = sem
        self.key = key
        self.cnt = 0
        self.seen = {}


class K:
    def __init__(self, nc, sems):
        self.nc = nc
        it = iter(sems)
        self.pe = Eng(nc.tensor, next(it), "pe")
        self.act = Eng(nc.scalar, next(it), "act")
        self.dve = Eng(nc.vector, next(it), "dve")
        self.pool = Eng(nc.gpsimd, next(it), "pool")
        self.sp = Eng(nc.sync, next(it), "sp")
        self.engs = [self.pe, self.act, self.dve, self.pool, self.sp]
        self.dsems = {"sp": [[s, 0] for s in [next(it) for _ in range(8)]],
                      "pool": [[s, 0] for s in [next(it) for _ in range(8)]]}
        self.dptr = {"sp": 0, "pool": 0}

    def _waits(self, eng, rd, wr):
        need = {}

        def add(d, skip_self):
            for k, (s, v) in d.items():
                if skip_self and k == eng.key:
                    continue
                if k not in need or need[k][1] < v:
                    need[k] = (s, v)
        for t in rd:
            add(t.w, False)
        skip = (eng.key == "pe")
        for t in wr:
            add(t.w, skip)
            add(t.r, skip)
        for k, (s, v) in need.items():
            if eng.seen.get(k, 0) < v:
                eng.obj.wait_ge(s, v)
                eng.seen[k] = v

    def op(self, eng, fn, rd=(), wr=()):
        self._waits(eng, rd, wr)
        inst = fn(eng.obj)
        eng.cnt += 1
        inst.then_inc(eng.sem, 1)
        tok = (eng.sem, eng.cnt)
        for t in rd:
            t.r[eng.key] = tok
        for t in wr:
            t.w = {eng.key: tok}
            t.r = {}

    def dma(self, eng, out, in_, rd=(), wr=()):
        ring = self.dsems[eng.key]
        i = self.dptr[eng.key]
        self.dptr[eng.key] = (i + 1) % len(ring)
        sem, val = ring[i]
        key = "d%s%d" % (eng.key, i)
        self._waits(eng, rd, wr)
        if val > 0 and eng.seen.get(key, 0) < val:
            eng.obj.wait_ge(sem, val)
            eng.seen[key] = val
        eng.obj.dma_start(out=out, in_=in_).then_inc(sem, 16)
        ring[i][1] = val + 16
        tok = (sem, val + 16)
        for t in rd:
            t.r[key] = tok
        for t in wr:
            t.w = {key: tok}
            t.r = {}

    def barrier(self):
        for e in self.engs:
            for f in self.engs:
                if f is not e and f.cnt > 0 and e.seen.get(f.key, 0) < f.cnt:
                    e.obj.wait_ge(f.sem, f.cnt)
                    e.seen[f.key] = f.cnt
            for qk, ring in self.dsems.items():
                for i, (s, v) in enumerate(ring):
                    key = "d%s%d" % (qk, i)
                    if v > 0 and e.seen.get(key, 0) < v:
                        e.obj.wait_ge(s, v)
                        e.seen[key] = v


class _Stop(Exception):
    pass


def build_program(stage=None, dumps=(), nchunks=8, nt1=64):
    nc = bass.Bass("TRN2", target_bir_lowering=False)
    dt = nc.dram_tensor
    xp = dt("xp", [NT * 128, D], F32, kind="ExternalInput").ap()
    posp = dt("posp", [128, NT], I32, kind="ExternalInput").ap()
    oflag = dt("oflag", [128, 8], F32, kind="ExternalInput").ap()
    hmask = dt("hmask", [128, 256], F32, kind="ExternalInput").ap()
    invf = dt("invf", [128, 8], F32, kind="ExternalInput").ap()
    cT = dt("cT", [128, 8], F32, kind="ExternalInput").ap()
    w_ada = dt("w_ada", [D, 6 * D], F32, kind="ExternalInput").ap()
    badac = dt("badac", [128, 48], F32, kind="ExternalInput").ap()
    badar = dt("badar", [1, 6 * D], F32, kind="ExternalInput").ap()
    gmixc = dt("gmixc", [128, 8], F32, kind="ExternalInput").ap()
    gmlpc = dt("gmlpc", [128, 8], F32, kind="ExternalInput").ap()
    w_in = dt("w_in", [D, 2376], F32, kind="ExternalInput").ap()
    convw = dt("convw", [128, 4, 31], F32, kind="ExternalInput").ap()
    convb = dt("convb", [128, 4], F32, kind="ExternalInput").ap()
    cng = dt("cng", [128, 4], F32, kind="ExternalInput").ap()
    cnb = dt("cnb", [128, 4], F32, kind="ExternalInput").ap()
    w_out = dt("w_out", [D, D], F32, kind="ExternalInput").ap()
    w_up = dt("w_up", [D, 4 * D], F32, kind="ExternalInput").ap()
    w_down = dt("w_down", [4 * D, D], F32, kind="ExternalInput").ap()
    gfb = dt("gfb", [128, D], F32, kind="ExternalInput").ap()
    out = dt("out", [4096, D], F32, kind="ExternalOutput").ap()
    x1s = dt("x1s", [4096, D], F32).ap()
    g1s = dt("g1s", [128, D], F32).ap()
    g2s = dt("g2s", [128, D], F32).ap()
    if "x1s" in dumps:
        x1s = dt("dbg_x1s", [4096, D], F32, kind="ExternalOutput").ap()

    def wview(w, c0, c1):
        return w[:, c0:c1].rearrange("(k p) e -> p k e", p=128)

    import contextlib
    dump_aps = {}

    def dump(name, ap, tiles, kbref):
        if name not in dumps:
            return
        shp = [int(v) for v in ap.shape]
        d_ap = dt("dbg_" + name, shp, ap.dtype, kind="ExternalOutput").ap()
        kbref.dma(kbref.sp, d_ap, ap, rd=tiles)

    def stop_if(st, kbref):
        if stage == st:
            kbref.barrier()
            raise _Stop()

    def _body():
        with contextlib.ExitStack() as es:
            sems = [es.enter_context(nc.semaphore("s%d" % i)) for i in range(21)]
            kb = K(nc, sems)
            pe, act, dve, pool, sp = kb.pe, kb.act, kb.dve, kb.pool, kb.sp
            op, dma = kb.op, kb.dma

            def sb(name, shape, dtype=F32):
                return es2.enter_context(nc.sbuf_tensor(name, shape, dtype))

            ps = es.enter_context(nc.psum_tensor("ps", [128, 8, 512], F32))
            PB = [T() for _ in range(8)]

            def psb16(b):
                return ps[:, b, :].bitcast(BF16)

            es2 = es
            ident = sb("ident", [128, 128], BF16); t_ident = T()
            ident4 = sb("ident4", [128, 4, 128], BF16)
            identf = sb("identf", [128, 128], F32)
            trim = sb("trim", [128, 128], F32)
            onesm = sb("onesm", [128, 128], BF16)
            onesr = sb("onesr", [1, 128], F32)
            cosT = sb("cosT", [128, NT, 8], F32)
            sinT = sb("sinT", [128, NT, 8], F32); t_cs = T()
            modc = sb("modc", [128, 48], F32); t_modc = T()
            ab = sb("ab", [128, 4, 8], F32); t_ab = T()
            t_G1 = T(); t_G2 = T()
            oflg = sb("oflg", [128, 8], F32); t_small = T()
            cw = sb("cw", [128, 4, 31], F32)
            cb = sb("cb", [128, 4], F32)
            cg = sb("cg", [128, 4], F32)
            cbn = sb("cbn", [128, 4], F32)
            wst = {"slots": None, "tiles": None, "ptr": 0}

            def walloc(tag):
                wst["slots"] = [sb("wslot%s%d" % (tag, i), [128, 8, 512], BF16) for i in range(2)]
                wst["tiles"] = [T() for _ in range(2)]
                wst["ptr"] = 0

            def wload(src_ap):
                i = wst["ptr"]
                wst["ptr"] = (i + 1) % 2
                dma(pool, wst["slots"][i][:], src_ap, wr=[wst["tiles"][i]])
                return wst["slots"][i], wst["tiles"][i]

            op(pool, lambda e: e.memset(identf[:], 0.0), wr=[t_ident])
            op(pool, lambda e: e.affine_select(out=identf[:], in_=identf[:], pattern=[[-1, 128]],
                                               compare_op=ALU.not_equal, fill=1.0, base=0,
                                               channel_multiplier=1), rd=[t_ident], wr=[t_ident])
            op(pool, lambda e: e.tensor_copy(out=ident[:], in_=identf[:]), rd=[t_ident], wr=[t_ident])
            op(pool, lambda e: e.tensor_copy(out=ident4[:], in_=identf[:].unsqueeze(1).to_broadcast([128, 4, 128])),
               rd=[t_ident], wr=[t_ident])
            op(pool, lambda e: e.memset(trim[:], 0.0), wr=[t_ident])
            op(pool, lambda e: e.affine_select(out=trim[:], in_=trim[:], pattern=[[-1, 128]],
                                               compare_op=ALU.is_ge, fill=NEG, base=0,
                                               channel_multiplier=1), rd=[t_ident], wr=[t_ident])
            op(pool, lambda e: e.memset(onesm[:], 1.0 / 512.0), wr=[t_ident])
            op(pool, lambda e: e.memset(onesr[:], 1.0), wr=[t_ident])
            dma(sp, oflg[:], oflag, wr=[t_small])
            dma(sp, cw[:], convw, wr=[t_small])
            dma(sp, cb[:], convb, wr=[t_small])
            dma(sp, cg[:], cng, wr=[t_small])
            dma(sp, cbn[:], cnb, wr=[t_small])

            with contextlib.ExitStack() as es2:
                posi = sb("posi", [128, NT], I32)
                posf = sb("posf", [128, NT], F32)
                ivf = sb("ivf", [128, 8], F32)
                ang = sb("ang", [128, NT, 8], F32)
                tq = sb("tq", [128, NT, 8], F32)
                kq = sb("kq", [128, NT, 8], I32)
                kf = sb("kf", [128, NT, 8], F32)
                red = sb("red", [128, NT, 8], F32)
                t_p0 = T()
                cTs = sb("cTs", [128, 8], F32)
                cond = sb("cond", [128, 8], F32); t_cond = T()
                badc = sb("badc", [128, 48], F32)
                gmc = sb("gmc", [128, 2, 8], F32)
                rowb = sb("rowb", [1, 6144], F32)
                rows2 = [sb("rows%d" % i, [1, 512], F32) for i in range(2)]; t_rows2 = [T(), T()]
                Gtmp = sb("Gtmp", [128, 512], F32); t_Gtmp = T()
                wfs = [sb("wf%d" % i, [128, 8, 512], F32) for i in range(3)]; t_wfs = [T() for _ in range(3)]
                dma(sp, posi[:], posp, wr=[t_p0])
                dma(sp, ivf[:], invf, wr=[t_p0])
                dma(sp, cTs[:], cT, wr=[t_cond])
                dma(sp, badc[:], badac, wr=[t_cond])
                dma(sp, gmc[:, 0, :], gmixc, wr=[t_cond])
                dma(sp, gmc[:, 1, :], gmlpc, wr=[t_cond])
                dma(sp, rowb[:], badar, wr=[t_cond])
                rw = dict(rd=[t_p0], wr=[t_p0])
                op(dve, lambda e: e.tensor_copy(out=posf[:], in_=posi[:]), **rw)
                op(dve, lambda e: e.tensor_tensor(out=ang[:], in0=posf[:].unsqueeze(2).to_broadcast([128, NT, 8]),
                                                  in1=ivf[:].unsqueeze(1).to_broadcast([128, NT, 8]), op=ALU.mult), **rw)
                TWO_PI = 2.0 * np.pi
                C1 = 6.28125
                C2 = TWO_PI - C1

                def reduce_to(dst, shift):
                    op(dve, lambda e: e.tensor_scalar(out=tq[:], in0=ang[:], scalar1=shift, scalar2=1.0 / TWO_PI,
                                                      op0=ALU.add, op1=ALU.mult), **rw)
                    op(dve, lambda e: e.tensor_copy(out=kq[:], in_=tq[:]), **rw)
                    op(dve, lambda e: e.tensor_copy(out=kf[:], in_=kq[:]), **rw)
                    op(dve, lambda e: e.scalar_tensor_tensor(out=red[:], in0=kf[:], scalar=-C1, in1=ang[:],
                                                             op0=ALU.mult, op1=ALU.add), **rw)
                    op(dve, lambda e: e.scalar_tensor_tensor(out=red[:], in0=kf[:], scalar=-C2, in1=red[:],
                                                             op0=ALU.mult, op1=ALU.add), **rw)
                    op(dve, lambda e: e.tensor_scalar(out=red[:], in0=red[:], scalar1=shift, scalar2=None,
                                                      op0=ALU.add), **rw)
                    op(dve, lambda e: e.tensor_scalar(out=tq[:], in0=red[:], scalar1=np.pi, scalar2=-TWO_PI,
                                                      op0=ALU.is_gt, op1=ALU.mult), **rw)
                    op(dve, lambda e: e.tensor_tensor(out=red[:], in0=red[:], in1=tq[:], op=ALU.add), **rw)
                    op(dve, lambda e: e.tensor_scalar(out=tq[:], in0=red[:], scalar1=-np.pi, scalar2=TWO_PI,
                                                      op0=ALU.is_lt, op1=ALU.mult), **rw)
                    op(dve, lambda e: e.tensor_tensor(out=red[:], in0=red[:], in1=tq[:], op=ALU.add), **rw)
                    op(dve, lambda e: e.tensor_scalar(out=red[:], in0=red[:], scalar1=-3.1415925, scalar2=3.1415925,
                                                      op0=ALU.max, op1=ALU.min), **rw)
                    op(act, lambda e: e.activation(out=dst[:], in_=red[:], func=AF.Sin), rd=[t_p0], wr=[t_cs])

                reduce_to(sinT, 0.0)
                reduce_to(cosT, np.pi / 2.0)

                op(act, lambda e: e.activation(out=cond[:], in_=cTs[:], func=AF.Silu), rd=[t_cond], wr=[t_cond])
                for cc in range(12):
                    ws, tw = wfs[cc % 3], t_wfs[cc % 3]
                    dma(sp, ws[:], wview(w_ada, cc * 512, (cc + 1) * 512), wr=[tw])
                    rb_, trb_ = rows2[cc % 2], t_rows2[cc % 2]
                    pbk = 1 + cc % 2
                    for k in range(8):
                        op(pe, lambda e, k=k: e.matmul(ps[0:1, pbk, :], lhsT=cond[:, k:k + 1], rhs=ws[:, k, :],
                                                       start=(k == 0), stop=(k == 7)),
                           rd=[t_cond, tw], wr=[PB[pbk]])
                    op(dve, lambda e: e.tensor_tensor(out=rb_[:], in0=ps[0:1, pbk, :], in1=rowb[:, cc * 512:(cc + 1) * 512],
                                                      op=ALU.add), rd=[PB[pbk], t_cond], wr=[trb_])
                    if cc in (4, 5, 10, 11):
                        op(pe, lambda e: e.matmul(ps[:, 3, :], lhsT=onesr[:], rhs=rb_[:], start=True, stop=True),
                           rd=[trb_, t_ident], wr=[PB[3]])
                        Gs, tG = (g1s, t_G1) if cc < 6 else (g2s, t_G2)
                        go = (cc - 4) * 512 if cc < 6 else (cc - 10) * 512
                        op(act, lambda e: e.activation(out=Gtmp[:], in_=ps[:, 3, :], func=AF.Copy),
                           rd=[PB[3]], wr=[t_Gtmp])
                        dma(sp, Gs[:, go:go + 512], Gtmp[:], rd=[t_Gtmp], wr=[tG])
                    else:
                        for el in range(4):
                            et = cc * 4 + el
                            op(pe, lambda e, el=el, et=et: e.matmul(ps[:, 0, et:et + 1], lhsT=rb_[0:1, el * 128:(el + 1) * 128],
                                                                   rhs=onesr[0:1, 0:1], start=True, stop=True, skip_group_check=True),
                               rd=[trb_, t_ident], wr=[PB[0]])
                op(dve, lambda e: e.memset(modc[:], 0.0), wr=[t_modc])
                for lo_, hi_ in ((0, 16), (24, 40)):
                    op(dve, lambda e: e.tensor_copy(out=modc[:, lo_:hi_], in_=ps[:, 0, lo_:hi_]),
                       rd=[PB[0]], wr=[t_modc])
                op(dve, lambda e: e.scalar_tensor_tensor(out=ab[:, 0, :], in0=modc[:, 8:16], scalar=1.0, in1=gmc[:, 0, :],
                                                         op0=ALU.add, op1=ALU.mult), rd=[t_modc, t_cond], wr=[t_ab])
                op(dve, lambda e: e.tensor_copy(out=ab[:, 1, :], in_=modc[:, 0:8]), rd=[t_modc], wr=[t_ab])
                op(dve, lambda e: e.scalar_tensor_tensor(out=ab[:, 2, :], in0=modc[:, 32:40], scalar=1.0, in1=gmc[:, 1, :],
                                                         op0=ALU.add, op1=ALU.mult), rd=[t_modc, t_cond], wr=[t_ab])
                op(dve, lambda e: e.tensor_copy(out=ab[:, 3, :], in_=modc[:, 24:32]), rd=[t_modc], wr=[t_ab])
                dump("cosT", cosT[:], [t_cs], kb)
                dump("sinT", sinT[:], [t_cs], kb)
                dump("ab", ab[:], [t_ab], kb)
                dump("modc", modc[:], [t_modc], kb)
                kb.barrier()
                stop_if("p0", kb)

            def norm_tile(row0, xt, t_xt, xn, t_xn, sq, t_sq, st, t_st, src=None):
                dma(sp, xt[:], (xp if src is None else src)[row0:row0 + 128, :], wr=[t_xt])
                op(act, lambda e: e.activation(out=xn[:], in_=xt[:], func=AF.Square, accum_out=st[:, 0:1]),
                   rd=[t_xt], wr=[t_xn, t_st])
                op(act, lambda e: e.activation(out=st[:, 1:2], in_=st[:, 0:1], func=AF.Sqrt, bias=EPS, scale=1.0 / D),
                   rd=[t_st], wr=[t_st])
                op(dve, lambda e: e.reciprocal(out=st[:, 2:3], in_=st[:, 1:2]), rd=[t_st], wr=[t_st])
                op(act, lambda e: e.activation(out=xn[:], in_=xt[:], func=AF.Copy, scale=st[:, 2:3]),
                   rd=[t_xt, t_st], wr=[t_xn])

            def transpose_mod(xn, t_xn, bank, hT_dst, t_hT, abi):
                pv = psb16(bank)
                for k in range(8):
                    op(pe, lambda e, k=k: e.transpose(out=pv[:, k * 128:(k + 1) * 128], in_=xn[:, k * 128:(k + 1) * 128],
                                                      identity=ident[:]), rd=[t_xn, t_ident], wr=[PB[bank]])
                pv3 = pv.rearrange("p (k t) -> p k t", k=8)
                op(dve, lambda e: e.tensor_tensor(out=hT_dst, in0=pv3,
                                                  in1=ab[:, abi, :].unsqueeze(2).to_broadcast([128, 8, 128]), op=ALU.mult),
                   rd=[PB[bank], t_ab], wr=[t_hT])
                op(pool, lambda e: e.tensor_tensor(out=hT_dst, in0=hT_dst,
                                                   in1=ab[:, abi + 1, :].unsqueeze(2).to_broadcast([128, 8, 128]), op=ALU.add),
                   rd=[t_hT, t_ab], wr=[t_hT])

            def transpose_only(xn, t_xn, bank):
                pv = psb16(bank)
                for k in range(8):
                    op(pe, lambda e, k=k: e.transpose(out=pv[:, k * 128:(k + 1) * 128], in_=xn[:, k * 128:(k + 1) * 128],
                                                      identity=ident[:]), rd=[t_xn, t_ident], wr=[PB[bank]])

            def mod_only(bank, hT_dst, t_hT, abi):
                pv3 = psb16(bank).rearrange("p (k t) -> p k t", k=8)
                op(dve, lambda e: e.tensor_tensor(out=hT_dst, in0=pv3,
                                                  in1=ab[:, abi, :].unsqueeze(2).to_broadcast([128, 8, 128]), op=ALU.mult),
                   rd=[PB[bank], t_ab], wr=[t_hT])
                op(pool, lambda e: e.tensor_tensor(out=hT_dst, in0=hT_dst,
                                                   in1=ab[:, abi + 1, :].unsqueeze(2).to_broadcast([128, 8, 128]), op=ALU.add),
                   rd=[t_hT, t_ab], wr=[t_hT])

            with contextlib.ExitStack() as es2:
                kT = sb("kT", [128, S], BF16); t_kT = T()
                kiT = sb("kiT", [128, S], BF16); t_kiT = T()
                Vaug = sb("Vaug", [128, 64, 2, 65], BF16); t_V = T()
                W1 = sb("W1", [128, 8, 328], BF16); t_W1 = T()
                xts = [sb("xt%d" % i, [128, D], F32) for i in range(2)]; t_xts = [T(), T()]
                xns = [sb("xn%d" % i, [128, D], BF16) for i in range(2)]; t_xns = [T(), T()]
                sqj = None; t_sqj = None
                walloc("b")
                G1 = sb("G1", [128, D], F32)
                dma(sp, G1[:], g1s, rd=[t_G1], wr=[t_G1])
                sts = [sb("st%d" % i, [128, 4], F32) for i in range(2)]; t_sts = [T(), T()]
                hTc = sb("hTc", [128, 8, 512], BF16); t_hTc = [T() for _ in range(4)]
                rtmp = sb("rtmp", [128, 4, 16, 8], F32); t_rtmp = T()
                krot = [sb("krot%d" % i, [128, 256], BF16) for i in range(2)]; t_krot = [T(), T()]

                op(pool, lambda e: e.memset(Vaug[:], 1.0), wr=[t_V])
                dma(pool, W1[:, :, 0:128], wview(w_in, 512, 640), wr=[t_W1])
                dma(pool, W1[:, :, 128:192], wview(w_in, 1280, 1344), wr=[t_W1])
                dma(pool, W1[:, :, 192:320], wview(w_in, 640, 768), wr=[t_W1])
                dma(pool, W1[:, :, 320:328], wview(w_in, 1344, 1352), wr=[t_W1])

                def rope(src3, dst3, nh, ti, tsrc, tdst):
                    cs = cosT[:, ti, :].unsqueeze(1).to_broadcast([128, nh, 8])
                    sn = sinT[:, ti, :].unsqueeze(1).to_broadcast([128, nh, 8])
                    x1, x2 = src3[:, :, 0:8], src3[:, :, 8:16]
                    t1, t2, t3, t4 = (rtmp[:, i, 0:nh, :] for i in range(4))
                    op(dve, lambda e: e.tensor_tensor(out=t1, in0=x1, in1=cs, op=ALU.mult), rd=[tsrc, t_cs], wr=[t_rtmp])
                    op(dve, lambda e: e.tensor_tensor(out=t2, in0=x2, in1=sn, op=ALU.mult), rd=[tsrc, t_cs], wr=[t_rtmp])
                    op(dve, lambda e: e.tensor_tensor(out=t3, in0=x2, in1=cs, op=ALU.mult), rd=[tsrc, t_cs], wr=[t_rtmp])
                    op(dve, lambda e: e.tensor_tensor(out=t4, in0=x1, in1=sn, op=ALU.mult), rd=[tsrc, t_cs], wr=[t_rtmp])
                    op(dve, lambda e: e.tensor_tensor(out=dst3[:, :, 0:8], in0=t1, in1=t2, op=ALU.subtract),
                       rd=[t_rtmp], wr=[tdst])
                    op(dve, lambda e: e.tensor_tensor(out=dst3[:, :, 8:16], in0=t3, in1=t4, op=ALU.add),
                       rd=[t_rtmp], wr=[tdst])
                    op(act, lambda e: e.activation(out=dst3[:, :, 16:64], in_=src3[:, :, 16:64], func=AF.Copy),
                       rd=[tsrc], wr=[tdst])

                def rope4(src4, dst4, ti, tsrc, tdst):
                    cs = cosT[:, ti, :].unsqueeze(1).unsqueeze(1).to_broadcast([128, 2, 4, 8])
                    sn = sinT[:, ti, :].unsqueeze(1).unsqueeze(1).to_broadcast([128, 2, 4, 8])
                    x1, x2 = src4[:, :, :, 0:8], src4[:, :, :, 8:16]
                    t1, t2, t3, t4 = (rtmp[:, i, 0:8, :].rearrange("p (g b) d -> p g b d", g=2) for i in range(4))
                    op(dve, lambda e: e.tensor_tensor(out=t1, in0=x1, in1=cs, op=ALU.mult), rd=[tsrc, t_cs], wr=[t_rtmp])
                    op(dve, lambda e: e.tensor_tensor(out=t2, in0=x2, in1=sn, op=ALU.mult), rd=[tsrc, t_cs], wr=[t_rtmp])
                    op(dve, lambda e: e.tensor_tensor(out=t3, in0=x2, in1=cs, op=ALU.mult), rd=[tsrc, t_cs], wr=[t_rtmp])
                    op(dve, lambda e: e.tensor_tensor(out=t4, in0=x1, in1=sn, op=ALU.mult), rd=[tsrc, t_cs], wr=[t_rtmp])
                    op(dve, lambda e: e.tensor_tensor(out=dst4[:, :, :, 0:8], in0=t1, in1=t2, op=ALU.subtract),
                       rd=[t_rtmp], wr=[tdst])
                    op(dve, lambda e: e.tensor_tensor(out=dst4[:, :, :, 8:16], in0=t3, in1=t4, op=ALU.add),
                       rd=[t_rtmp], wr=[tdst])
                    op(act, lambda e: e.activation(out=dst4[:, :, :, 16:64], in_=src4[:, :, :, 16:64], func=AF.Copy),
                       rd=[tsrc], wr=[tdst])

                def ph1_S1(ti):
                    s2 = ti % 2
                    norm_tile(ti * 128, xts[s2], t_xts[s2], xns[s2], t_xns[s2], sqj, t_sqj, sts[s2], t_sts[s2])
                    hs = ti % 4
                    hdst = hTc[:, :, hs * 128:(hs + 1) * 128]
                    transpose_mod(xns[s2], t_xns[s2], 6 + s2, hdst, t_hTc[hs], 0)

                def ph1_S2(ti):
                    s2 = ti % 2
                    hs = ti % 4
                    bk = s2
                    for k in range(8):
                        op(pe, lambda e, k=k: e.matmul(ps[:, bk, 0:320], lhsT=hTc[:, k, hs * 128:(hs + 1) * 128],
                                                       rhs=W1[:, k, 0:320], start=(k == 0), stop=(k == 7)),
                           rd=[t_hTc[hs], t_W1], wr=[PB[bk]])
                    kr = krot[s2]
                    rope(ps[:, bk, 0:192].rearrange("p (h d) -> p h d", d=64),
                         kr[:, 0:192].rearrange("p (h d) -> p h d", d=64), 3, ti, PB[bk], t_krot[s2])
                    op(pool, lambda e: e.tensor_copy(out=kr[:, 192:256], in_=kr[:, 128:192]), rd=[t_krot[s2]], wr=[t_krot[s2]])
                    op(act, lambda e: e.activation(out=Vaug[:, ti, :, 0:64],
                                                   in_=ps[:, bk, 192:320].rearrange("p (g d) -> p g d", d=64), func=AF.Copy),
                       rd=[PB[bk]], wr=[t_V])
                    tb = 4 + s2
                    pv = psb16(tb)
                    op(pe, lambda e: e.transpose(out=pv[:, 0:128], in_=kr[:, 0:128], identity=ident[:]),
                       rd=[t_krot[s2], t_ident], wr=[PB[tb]])
                    op(pe, lambda e: e.transpose(out=pv[:, 128:256], in_=kr[:, 128:256], identity=ident[:]),
                       rd=[t_krot[s2], t_ident], wr=[PB[tb]])
                    op(act, lambda e: e.activation(out=kT[:, ti * 128:(ti + 1) * 128], in_=pv[:, 0:128], func=AF.Copy),
                       rd=[PB[tb]], wr=[t_kT])
                    op(dve, lambda e: e.tensor_copy(out=kiT[:, ti * 128:(ti + 1) * 128], in_=pv[:, 128:256]),
                       rd=[PB[tb]], wr=[t_kiT])


                ph1_S1(0)
                for ti in range(nt1):
                    if ti + 1 < nt1:
                        ph1_S1(ti + 1)
                    ph1_S2(ti)
                dump("kT", kT[:], [t_kT], kb)
                dump("kiT", kiT[:], [t_kiT], kb)
                dump("Vaug", Vaug[:], [t_V], kb)
                stop_if("p1", kb)
                SC = sb("SC", [128, S], F32); t_SC = T()
                junk = hTc[:].rearrange("p k t -> p (k t)").bitcast(U8)
                RbA = sb("RbA", [128, 8, 512], BF16)
                Rb = [RbA[:, i, :] for i in range(8)]; t_Rb = [T() for _ in range(8)]
                Dg = sb("Dg", [128, 8, 128], BF16); t_Dg = T()
                qT = sb("qT", [128, 2, 4, 512], BF16); t_qT = T()
                qiT = sb("qiT", [128, 4, 2, 512], BF16); t_qiT = T()
                op(pool, lambda e: e.memset(qT[:], 0.0), wr=[t_qT])
                op(pool, lambda e: e.memset(qiT[:], 0.0), wr=[t_qiT])
                qrot = [sb("qrot%d" % i, [128, 512], BF16) for i in range(2)]; t_qrot = [T(), T()]
                wsc = sb("wsc", [128, 4, 8], F32); t_wsc = T()
                PT = [sb("PT%d" % i, [128, 512], BF16) for i in range(4)]; t_PT = [T() for _ in range(4)]
                MB = sb("MB", [128, S], BF16); t_MB = T()
                junkA = sb("junkA", [128, JA], U8); t_junkA = T()
                bsa = sb("bsa", [128, 2], F32); t_bsa = T()
                bst = sb("bst", [128, 8], F32); t_bst = T()
                gluT = sb("gluT", [128, 4, 544], BF16); t_glu = T()
                gluH = sb("gluH", [128, 4, 256], BF16); t_gluH = T()
                SCb = SC[:].bitcast(BF16)
                ybf = SCb[:, 0:2048].rearrange("p (c t) -> p c t", c=4); t_ybf = t_SC
                ysq = SCb[:, 2048:4096].rearrange("p (c t) -> p c t", c=4); t_ysq = t_SC
                lnA = SC[:, 2048:2560]; t_lnA = t_SC
                lnB = SC[:, 2560:3072]; t_lnB = t_SC
                zn = SC[:, 3072:3584]; t_zn = t_SC
                sig = SC[:, 3584:4096]; t_sig = t_SC
                RbF = RbA[:].rearrange("p a b -> p (a b)")
                cdh = [RbF[:, 0:2048].rearrange("p (k c) -> p k c", c=128), RbF[:, 2048:3968].rearrange("p (k c) -> p k c", c=128)]
                t_cdh = [t_Rb[0:4], t_Rb[4:8]]
                mixT = sb("mixT", [128, 8, 512], BF16); t_mixT = T()
                attn = sb("attn", [128, 512], BF16); t_attn = T()
                rs4 = sb("rs4", [128, 8], F32); t_rs4 = T()
                x1t = SC[:, 4096:5120]; t_x1t = t_SC
                hmB = sb("hmB", [128, 256], BF16)
                hm = sb("hm", [128, 256], F32)
                dma(sp, hm[:], hmask, wr=[t_small])
                op(pool, lambda e: e.tensor_copy(out=hmB[:], in_=hm[:]), rd=[t_small], wr=[t_small])

                def conv_glu_mm(ws_a, tw_a, ws_g, tw_g, ncols, ct, hcols, t_h):
                    b0 = (ct % 2) * 2
                    for k in range(8):
                        op(pe, lambda e, k=k: e.matmul(ps[:, b0, 0:ncols], lhsT=ws_a[:, k, ct * 128:(ct + 1) * 128],
                                                       rhs=hTc[:, k, hcols], start=(k == 0), stop=(k == 7)),
                           rd=t_h + [tw_a], wr=[PB[b0]])
                    for k in range(8):
                        op(pe, lambda e, k=k: e.matmul(ps[:, b0 + 1, 0:ncols], lhsT=ws_g[:, k, ct * 128:(ct + 1) * 128],
                                                       rhs=hTc[:, k, hcols], start=(k == 0), stop=(k == 7)),
                           rd=t_h + [tw_g], wr=[PB[b0 + 1]])

                def conv_glu_ev(ncols, ct, dst, tdst):
                    b0 = (ct % 2) * 2
                    op(act, lambda e: e.activation(out=sig[:, 0:ncols], in_=ps[:, b0 + 1, 0:ncols], func=AF.Sigmoid),
                       rd=[PB[b0 + 1]], wr=[t_sig])
                    op(dve, lambda e: e.tensor_tensor(out=dst, in0=ps[:, b0, 0:ncols], in1=sig[:, 0:ncols], op=ALU.mult),
                       rd=[PB[b0], t_sig], wr=[tdst])

                def conv_glu(ws_a, tw_a, ws_g, tw_g, ncols, ct, dst, tdst, hcols, t_h):
                    conv_glu_mm(ws_a, tw_a, ws_g, tw_g, ncols, ct, hcols, t_h)
                    conv_glu_ev(ncols, ct, dst, tdst)

                for hi in range(2):
                    norm_tile((64 + hi) * 128, xts[hi], t_xts[hi], xns[hi], t_xns[hi], sqj, t_sqj, sts[hi], t_sts[hi])
                    transpose_mod(xns[hi], t_xns[hi], 6 + hi, hTc[:, :, hi * 128:(hi + 1) * 128], t_hTc[hi], 0)
                wa, twa = wload(wview(w_in, 1352, 1864))
                wg, twg = wload(wview(w_in, 1864, 2376))
                for ct in range(4):
                    conv_glu(wa, twa, wg, twg, 256, ct, gluH[:, ct, :], t_gluH, slice(0, 256), [t_hTc[0], t_hTc[1]])
                    op(pool, lambda e, ct=ct: e.tensor_tensor(out=gluH[:, ct, :], in0=gluH[:, ct, :], in1=hmB[:], op=ALU.mult),
                       rd=[t_gluH, t_small], wr=[t_gluH])

                dump("gluH", gluH[:], [t_gluH], kb)
                stop_if("p2h", kb)
                for j in range(nchunks):
                    tile0 = (2 * j + 1) * 4
                    def norm_stages(jn):
                        tl0 = (2 * jn + 1) * 4

                        def nA(t4):
                            s2 = t4 % 2
                            norm_tile((tl0 + t4) * 128, xts[s2], t_xts[s2], xns[s2], t_xns[s2], sqj, t_sqj, sts[s2], t_sts[s2])

                        def nT(t4):
                            s2 = t4 % 2
                            transpose_only(xns[s2], t_xns[s2], 6 + s2)

                        def nM(t4):
                            mod_only(6 + t4 % 2, hTc[:, :, t4 * 128:(t4 + 1) * 128], t_hTc[t4], 0)

                        order = [(nA, 0), (nA, 1), (nT, 0), (nM, 0), (nA, 2), (nT, 1), (nM, 1), (nA, 3), (nT, 2), (nM, 2),
                                 (nT, 3), (nM, 3)]
                        for fn, t4 in order:
                            fn(t4)
                            yield

                    if j == 0:
                        for _ in norm_stages(0):
                            pass
                    wqi, twqi = wload(wview(w_in, 768, 1280))
                    wq_box = {}

                    def u_mm(grp, t4, bk):
                        wq, twq = wq_box["w"] if grp == 0 else (wqi, twqi)
                        for k in range(8):
                            op(pe, lambda e, k=k: e.matmul(ps[:, bk, :], lhsT=hTc[:, k, t4 * 128:(t4 + 1) * 128],
                                                           rhs=wq[:, k, :], start=(k == 0), stop=(k == 7)),
                               rd=[t_hTc[t4], twq], wr=[PB[bk]])
                        if grp == 1:
                            for k in range(8):
                                op(pe, lambda e, k=k: e.matmul(ps[:, 2, 0:8], lhsT=hTc[:, k, t4 * 128:(t4 + 1) * 128],
                                                               rhs=W1[:, k, 320:328], start=(k == 0), stop=(k == 7)),
                                   rd=[t_hTc[t4], t_W1], wr=[PB[2]])
                            op(dve, lambda e: e.tensor_scalar(
                                out=wsc[:, t4, :].rearrange("p (b g) -> p g b", g=2),
                                in0=ps[:, 2, 0:8].rearrange("p (g b) -> p g b", g=2),
                                scalar1=float(8 ** -0.5 * 64 ** -0.5), scalar2=None, op0=ALU.mult),
                               rd=[PB[2]], wr=[t_wsc])

                    def u_rope(grp, t4, bk):
                        qr = qrot[bk]
                        src4 = ps[:, bk, :].rearrange("p (g b d) -> p g b d", g=2, b=4)
                        dst4 = qr[:].rearrange("p (b g d) -> p g b d", g=2, b=4)
                        rope4(src4, dst4, tile0 + t4, PB[bk], t_qrot[bk])

                    def u_trP(grp, t4, bk):
                        qr = qrot[bk]
                        tb = 4 + bk
                        pv = psb16(tb)
                        for b in range(4):
                            op(pe, lambda e, b=b: e.transpose(out=pv[:, b * 128:(b + 1) * 128],
                                                              in_=qr[:, b * 128:(b + 1) * 128], identity=ident[:]),
                               rd=[t_qrot[bk], t_ident], wr=[PB[tb]])

                    def u_trC(grp, t4, bk):
                        t_dstT = t_qT if grp == 0 else t_qiT
                        tb = 4 + bk
                        pv = psb16(tb)
                        pv4 = pv[:, 0:512].rearrange("p (b t) -> p b t", b=4)
                        tcols = slice(t4 * 128, (t4 + 1) * 128)
                        if grp == 0:
                            d0, d1 = qT[0:64, 0, :, tcols], qT[64:128, 1, :, tcols]
                        else:
                            d0, d1 = qiT[0:64, :, 0, tcols], qiT[64:128, :, 1, tcols]
                        op(act, lambda e: e.activation(out=d0, in_=pv4[0:64], func=AF.Copy), rd=[PB[tb]], wr=[t_dstT])
                        if grp == 0:
                            op(act, lambda e: e.activation(out=d1, in_=pv4[64:128], func=AF.Copy), rd=[PB[tb]], wr=[t_dstT])
                        else:
                            op(dve, lambda e: e.tensor_copy(out=d1, in_=pv4[64:128]), rd=[PB[tb]], wr=[t_dstT])

                    def units_gen(grp):
                        order = [("mm", 0), ("mm", 1), ("rope", 0), ("trP", 0), ("mm", 2), ("rope", 1), ("trC", 0), ("trP", 1),
                                 ("mm", 3), ("rope", 2), ("trC", 1), ("trP", 2), ("rope", 3), ("trC", 2), ("trP", 3), ("trC", 3)]
                        fns = {"mm": u_mm, "rope": u_rope, "trP": u_trP, "trC": u_trC}
                        for kind, t4 in order:
                            fns[kind](grp, t4, t4 % 2)
                            yield

                    for _ in units_gen(1):
                        pass
                    q_units = units_gen(0)
                    wa, twa = wload(wview(w_in, 1352, 1864))
                    wg, twg = wload(wview(w_in, 1864, 2376))
                    op(pool, lambda e: e.tensor_copy(out=gluT[:, :, 0:32], in_=gluH[:, :, j * 32:(j + 1) * 32]),
                       rd=[t_gluH], wr=[t_glu])
                    conv_glu_mm(wa, twa, wg, twg, 512, 0, slice(0, 512), t_hTc)
                    for ct in range(4):
                        if ct + 1 < 4:
                            conv_glu_mm(wa, twa, wg, twg, 512, ct + 1, slice(0, 512), t_hTc)
                        conv_glu_ev(512, ct, gluT[:, ct, 32:544], t_glu)
                    for ct in range(4):
                        cbk = ct
                        for hf, (t_lo, t_hi) in enumerate(((0, 16), (16, 31))):
                            nt_ = t_hi - t_lo
                            op(pool, lambda e, ct=ct: e.tensor_tensor(
                                out=cdh[hf], in0=ident[:].unsqueeze(1).to_broadcast([128, nt_, 128]),
                                in1=cw[:, ct, t_lo:t_hi].unsqueeze(2).to_broadcast([128, nt_, 128]), op=ALU.mult),
                               rd=[t_ident, t_small], wr=t_cdh[hf])
                        for tap in range(31):
                            hf, tl = (0, tap) if tap < 16 else (1, tap - 16)
                            op(pe, lambda e, tap=tap, ct=ct: e.matmul(ps[:, cbk, :], lhsT=cdh[hf][:, tl, :],
                                                                     rhs=gluT[:, ct, tap + 2:tap + 514],
                                                                     start=(tap == 0), stop=(tap == 30)),
                               rd=t_cdh[hf] + [t_glu], wr=[PB[cbk]])
                        op(act, lambda e, ct=ct: e.activation(out=ybf[:, ct, :], in_=ps[:, cbk, :], func=AF.Identity,
                                                              bias=cb[:, ct:ct + 1], scale=1.0), rd=[PB[cbk], t_small], wr=[t_ybf])
                        op(act, lambda e, ct=ct: e.activation(out=ysq[:, ct, :], in_=ps[:, cbk, :], func=AF.Square,
                                                              bias=cb[:, ct:ct + 1], scale=1.0), rd=[PB[cbk], t_small], wr=[t_ysq])
                    for ct in range(4):
                        op(pe, lambda e, ct=ct: e.matmul(ps[:, 4, :], lhsT=onesm[:], rhs=ybf[:, ct, :],
                                                         start=(ct == 0), stop=(ct == 3)), rd=[t_ybf, t_ident], wr=[PB[4]])
                    for ct in range(4):
                        op(pe, lambda e, ct=ct: e.matmul(ps[:, 5, :], lhsT=onesm[:], rhs=ysq[:, ct, :],
                                                         start=(ct == 0), stop=(ct == 3)), rd=[t_ysq, t_ident], wr=[PB[5]])
                    op(act, lambda e: e.activation(out=lnA, in_=ps[:, 4, :], func=AF.Copy), rd=[PB[4]], wr=[t_lnA])
                    op(dve, lambda e: e.tensor_tensor(out=lnB, in0=lnA, in1=lnA, op=ALU.mult), rd=[t_lnA], wr=[t_lnB])
                    op(dve, lambda e: e.tensor_tensor(out=lnB, in0=ps[:, 5, :], in1=lnB, op=ALU.subtract),
                       rd=[PB[5], t_lnB], wr=[t_lnB])
                    op(dve, lambda e: e.tensor_scalar(out=lnB, in0=lnB, scalar1=0.0, scalar2=EPS, op0=ALU.max, op1=ALU.add),
                       rd=[t_lnB], wr=[t_lnB])
                    op(act, lambda e: e.activation(out=lnB, in_=lnB, func=AF.Sqrt), rd=[t_lnB], wr=[t_lnB])
                    op(dve, lambda e: e.reciprocal(out=lnB, in_=lnB), rd=[t_lnB], wr=[t_lnB])
                    for ct in range(4):
                        op(dve, lambda e, ct=ct: e.scalar_tensor_tensor(out=zn, in0=ps[:, ct, :], scalar=cb[:, ct:ct + 1],
                                                                        in1=lnA, op0=ALU.add, op1=ALU.subtract),
                           rd=[PB[ct], t_small, t_lnA], wr=[t_zn])
                        op(dve, lambda e: e.tensor_tensor(out=zn, in0=zn, in1=lnB, op=ALU.mult),
                           rd=[t_zn, t_lnB], wr=[t_zn])
                        op(act, lambda e, ct=ct: e.activation(out=mixT[:, 4 + ct, :], in_=zn, func=AF.Silu,
                                                              bias=cbn[:, ct:ct + 1], scale=cg[:, ct:ct + 1]),
                           rd=[t_zn, t_small], wr=[t_mixT])

                    if j == nchunks - 1:
                        dump("mixTc", mixT[:, 4:8, :], [t_mixT], kb)
                        stop_if("p2b", kb)
                    def qgeom(qi):
                        segs = [(c * 512, 512) for c in range(2 * j + 1)] + [((2 * j + 1) * 512, (qi + 1) * 128)]
                        nkeys = (2 * j + 1) * 512 + (qi + 1) * 128
                        return segs, nkeys, slice(qi * 128, (qi + 1) * 128)

                    def stage_A(qi):
                        segs, nkeys, qcols = qgeom(qi)
                        op(pool, lambda e: e.tensor_tensor(
                            out=Dg[:], in0=ident[:].unsqueeze(1).to_broadcast([128, 8, 128]),
                            in1=wsc[:, qi, :].unsqueeze(2).to_broadcast([128, 8, 128]), op=ALU.mult),
                           rd=[t_ident, t_wsc], wr=[t_Dg])
                        units = [(si, h) for si in range(len(segs)) for h in range(8)]
                        U = len(units)

                        def emit_L(u):
                            si, h = units[u]
                            c0, n = segs[si]
                            b, g = h // 2, h % 2
                            bk = u % 4
                            op(pe, lambda e: e.matmul(ps[:, bk, 0:n], lhsT=qiT[:, b, g, qcols],
                                                      rhs=kiT[:, c0:c0 + n], start=True, stop=True),
                               rd=[t_qiT, t_kiT], wr=[PB[bk]])
                            r = Rb[u % 8]
                            if u % 2 == 0:
                                op(act, lambda e: e.activation(out=r[:, 0:n], in_=ps[:, bk, 0:n], func=AF.Relu),
                                   rd=[PB[bk]], wr=[t_Rb[u % 8]])
                            else:
                                op(dve, lambda e: e.tensor_scalar(out=r[:, 0:n], in0=ps[:, bk, 0:n], scalar1=0.0, scalar2=None,
                                                                  op0=ALU.max), rd=[PB[bk]], wr=[t_Rb[u % 8]])

                        def emit_D(u):
                            si, h = units[u]
                            c0, n = segs[si]
                            sbk = 4 + (si % 2)
                            op(pe, lambda e: e.matmul(ps[:, sbk, 0:n], lhsT=Dg[:, h, :], rhs=Rb[u % 8][:, 0:n],
                                                      start=(h == 0), stop=(h == 7)),
                               rd=[t_Dg, t_Rb[u % 8]], wr=[PB[sbk]])
                            if h == 7:
                                if si == 2 * j:
                                    op(act, lambda e: e.activation(out=SC[:, c0:c0 + n], in_=ps[:, sbk, 0:n], func=AF.Identity,
                                                                   bias=oflg[:, j:j + 1], scale=1.0),
                                       rd=[PB[sbk], t_small], wr=[t_SC])
                                elif si == 2 * j + 1:
                                    if n > 128:
                                        op(act, lambda e: e.activation(out=SC[:, c0:c0 + n - 128], in_=ps[:, sbk, 0:n - 128],
                                                                       func=AF.Copy), rd=[PB[sbk]], wr=[t_SC])
                                    op(dve, lambda e: e.tensor_tensor(out=SC[:, c0 + n - 128:c0 + n], in0=ps[:, sbk, n - 128:n],
                                                                      in1=trim[:], op=ALU.add), rd=[PB[sbk], t_ident], wr=[t_SC])
                                else:
                                    op(act, lambda e: e.activation(out=SC[:, c0:c0 + n], in_=ps[:, sbk, 0:n], func=AF.Copy),
                                       rd=[PB[sbk]], wr=[t_SC])

                        for u in range(U + 4):
                            if u < U:
                                emit_L(u)
                            if u >= 4:
                                emit_D(u - 4)

                    def stage_B(qi, frac, jk=None, jk_t=None):
                        segs, nkeys, qcols = qgeom(qi)
                        na = min(int(frac * nkeys) // 128 * 128, JA)
                        scv = SC[:, 0:nkeys]
                        br = 12.0 if j == 0 else BR
                        nbis = 12 if j == 0 else NBIS
                        op(dve, lambda e: e.tensor_reduce(out=bst[:, 0:1], in_=scv, axis=AX.X, op=ALU.max), rd=[t_SC], wr=[t_bst])
                        op(dve, lambda e: e.tensor_scalar(out=bst[:, 1:2], in0=bst[:, 0:1], scalar1=-br / 2, scalar2=None,
                                                          op0=ALU.add), rd=[t_bst], wr=[t_bst])
                        for it in range(nbis):
                            if na > 0:
                                op(act, lambda e: e.activation(out=junkA[:, 0:na], in_=SC[:, 0:na], func=AF.Sign,
                                                               bias=bst[:, 1:2], scale=-1.0, accum_out=bsa[:, 0:1]),
                                   rd=[t_SC, t_bst], wr=[t_junkA, t_bsa])
                            jk_ = junk if jk is None else jk
                            jkt_ = t_hTc if jk is None else jk_t
                            op(dve, lambda e: e.tensor_scalar(out=jk_[:, na:nkeys], in0=SC[:, na:nkeys], scalar1=bst[:, 1:2],
                                                              scalar2=None, op0=ALU.is_gt, op1=ALU.add, accum_out=bst[:, 2:3]),
                               rd=[t_SC, t_bst], wr=jkt_ + [t_bst])
                            if na > 0:
                                op(dve, lambda e: e.scalar_tensor_tensor(out=bst[:, 2:3], in0=bsa[:, 0:1], scalar=-0.5,
                                                                         in1=bst[:, 2:3], op0=ALU.mult, op1=ALU.add),
                                   rd=[t_bsa, t_bst], wr=[t_bst])
                            last = (it == nbis - 1)
                            cn = (br / 2) / (2 ** it) if last else (br / 2) / (2 ** (it + 1))
                            op(dve, lambda e: e.tensor_scalar(out=bst[:, 3:4], in0=bst[:, 2:3], scalar1=255.5 - na / 2.0,
                                                              scalar2=(cn if last else 2.0 * cn), op0=ALU.is_gt, op1=ALU.mult),
                               rd=[t_bst], wr=[t_bst])
                            op(dve, lambda e: e.scalar_tensor_tensor(out=bst[:, 1:2], in0=bst[:, 3:4], scalar=-cn,
                                                                     in1=bst[:, 1:2], op0=ALU.add, op1=ALU.add),
                               rd=[t_bst], wr=[t_bst])
                            yield
                        if j == nchunks - 1 and qi == 3:
                            dump("SC", SC[:, 0:nkeys], [t_SC], kb)
                            dump("bst", bst[:, 0:4], [t_bst], kb)
                            stop_if("p2c", kb)
                        op(dve, lambda e: e.tensor_scalar(out=MB[:, 0:nkeys], in0=scv, scalar1=bst[:, 1:2], scalar2=NEG,
                                                          op0=ALU.is_le, op1=ALU.mult), rd=[t_SC, t_bst], wr=[t_MB])

                    def stage_C_main(qi):
                        segs, nkeys, qcols = qgeom(qi)
                        nsb = nkeys // 128
                        U = nsb * 2
                        LAG = 2

                        def emit_S(u):
                            sbi, g = u // 2, u % 2
                            bk = u % 4
                            op(pe, lambda e: e.matmul(ps[:, bk, :], lhsT=kT[:, sbi * 128:(sbi + 1) * 128],
                                                      rhs=qT[:, g, :, qcols], start=True, stop=False),
                               rd=[t_kT, t_qT], wr=[PB[bk]])
                            op(pe, lambda e: e.matmul(ps[:, bk, :], lhsT=MB[:, sbi * 128:(sbi + 1) * 128],
                                                      rhs=ident4[:], start=False, stop=True),
                               rd=[t_MB, t_ident], wr=[PB[bk]])
                            op(act, lambda e: e.activation(out=PT[u % 4][:], in_=ps[:, bk, :], func=AF.Exp, scale=0.125),
                               rd=[PB[bk]], wr=[t_PT[u % 4]])

                        def emit_V(u):
                            sbi, g = u // 2, u % 2
                            pt = PT[u % 4]
                            ob = 4 + g
                            for b in range(4):
                                op(pe, lambda e, b=b: e.matmul(ps[:, ob, b * 65:(b + 1) * 65], lhsT=pt[:, b * 128:(b + 1) * 128],
                                                               rhs=Vaug[:, sbi, g, :], start=(sbi == 0 and b == 0),
                                                               stop=(sbi == nsb - 1 and b == 3), skip_group_check=True),
                                   rd=[t_PT[u % 4], t_V], wr=[PB[ob]])

                        for u in range(U + LAG):
                            if u < U:
                                emit_S(u)
                            if u >= LAG:
                                emit_V(u - LAG)
                            yield

                    def stage_C_tail(qi):
                        segs, nkeys, qcols = qgeom(qi)
                        for g in range(2):
                            ov = ps[:, 4 + g, 0:260].rearrange("p (b e) -> p b e", e=65)
                            op(dve, lambda e: e.reciprocal(out=rs4[:, g * 4:(g + 1) * 4].unsqueeze(2), in_=ov[:, :, 64:65]),
                               rd=[PB[4 + g]], wr=[t_rs4])
                            op(dve, lambda e: e.tensor_tensor(
                                out=attn[:, g * 256:(g + 1) * 256].rearrange("p (b d) -> p b d", d=64), in0=ov[:, :, 0:64],
                                in1=rs4[:, g * 4:(g + 1) * 4].unsqueeze(2).to_broadcast([128, 4, 64]), op=ALU.mult),
                               rd=[PB[4 + g], t_rs4], wr=[t_attn])
                        if j == nchunks - 1 and qi == 3:
                            dump("attn", attn[:], [t_attn], kb)
                            stop_if("p2d", kb)
                        pv = psb16(6 + qi % 2)
                        for f in range(4):
                            op(pe, lambda e, f=f: e.transpose(out=pv[:, f * 128:(f + 1) * 128], in_=attn[:, f * 128:(f + 1) * 128],
                                                              identity=ident[:]), rd=[t_attn, t_ident], wr=[PB[6 + qi % 2]])
                        op(act, lambda e: e.activation(out=mixT[:, 0:4, qcols], in_=pv[:, 0:512].rearrange("p (f t) -> p f t", f=4),
                                                       func=AF.Copy), rd=[PB[6 + qi % 2]], wr=[t_mixT])

                    def interleave(gb, gc, nb):
                        csteps = list(range(gc[1]))
                        per = (len(csteps) + nb - 1) // nb if nb else 0
                        gcg, gbg = gc[0], gb
                        for it in range(nb):
                            next(gbg, None)
                            for _ in range(per):
                                next(gcg, None)
                        for _ in gbg:
                            pass
                        for _ in gcg:
                            pass

                    def csteps_of(qi):
                        return (qgeom(qi)[1] // 128) * 2 + 2

                    wq_box["w"] = wload(wview(w_in, 0, 512))
                    stage_A(0)
                    for _ in stage_B(0, 0.55, jk=MB[:].bitcast(U8), jk_t=[t_MB]):
                        next(q_units, None)
                        next(q_units, None)
                    for _ in q_units:
                        pass
                    for qi in range(1, 4):
                        stage_A(qi)
                        interleave(stage_B(qi, 0.12), (stage_C_main(qi - 1), csteps_of(qi - 1)), 12 if j == 0 else NBIS)
                        stage_C_tail(qi - 1)
                    ng = norm_stages(j + 1) if j + 1 < nchunks else iter(())
                    per = max(1, csteps_of(3) // 14)
                    for i_, _ in enumerate(stage_C_main(3)):
                        if i_ % per == per - 1:
                            next(ng, None)
                    for _ in ng:
                        pass
                    stage_C_tail(3)

                    wo0, two0 = wload(wview(w_out, 0, 512))
                    wo1, two1 = wload(wview(w_out, 512, 1024))
                    for t4 in range(4):
                        for half, (wo, two) in enumerate(((wo0, two0), (wo1, two1))):
                            bk = half
                            for f in range(8):
                                op(pe, lambda e, f=f: e.matmul(ps[:, bk, :], lhsT=mixT[:, f, t4 * 128:(t4 + 1) * 128], rhs=wo[:, f, :],
                                                               start=(f == 0), stop=(f == 7)), rd=[t_mixT, two], wr=[PB[bk]])
                        s2 = t4 % 2
                        dma(sp, xts[s2][:], xp[(tile0 + t4) * 128:(tile0 + t4 + 1) * 128, :], wr=[t_xts[s2]])
                        op(dve, lambda e: e.tensor_tensor(out=x1t, in0=ps[:, 0:2, :].rearrange("p a b -> p (a b)"), in1=G1[:],
                                                          op=ALU.mult), rd=[PB[0], PB[1], t_G1], wr=[t_x1t])
                        op(pool, lambda e: e.tensor_tensor(out=x1t, in0=x1t, in1=xts[s2][:], op=ALU.add),
                           rd=[t_x1t, t_xts[s2]], wr=[t_x1t])
                        r0 = (j * 4 + t4) * 128
                        dma(sp, x1s[r0:r0 + 128, :], x1t, rd=[t_x1t])
                kb.barrier()
                stop_if("p2e", kb)

            with contextlib.ExitStack() as es2:
                Wup = sb("Wup", [128, 8, 4096], BF16); t_Wup = T()
                Wdn = sb("Wdn", [128, 32, 1024], BF16); t_Wdn = T()
                GF = sb("GF", [128, D], F32); t_GF = T()
                xts = [sb("m_xt%d" % i, [128, D], F32) for i in range(4)]; t_xts = [T() for _ in range(4)]
                xns = [sb("m_xn%d" % i, [128, D], BF16) for i in range(4)]; t_xns = [T() for _ in range(4)]
                sqj = None; t_sqj = None
                G2 = sb("G2", [128, D], F32)
                dma(sp, G2[:], g2s, rd=[t_G2], wr=[t_G2])
                sts = [sb("m_st%d" % i, [128, 4], F32) for i in range(4)]; t_sts = [T() for _ in range(4)]
                h2T = [sb("h2T%d" % i, [128, 8, 256], BF16) for i in range(2)]; t_h2T = [[T(), T()], [T(), T()]]
                rT = [sb("rT%d" % i, [128, 256], BF16) for i in range(2)]; t_rT = [T(), T()]
                uT = sb("uT", [128, 32, 256], BF16); t_uT = T()
                x2 = sb("x2", [128, D], F32); t_x2 = T()
                oo = x2; t_oo = t_x2
                for c4 in range(8):
                    dma(pool, Wup[:, :, c4 * 512:(c4 + 1) * 512], wview(w_up, c4 * 512, (c4 + 1) * 512), wr=[t_Wup])
                for c4 in range(4):
                    dma(pool, Wdn[:, c4 * 8:(c4 + 1) * 8, :],
                        w_down[c4 * 1024:(c4 + 1) * 1024, :].rearrange("(k p) e -> p k e", p=128), wr=[t_Wdn])
                dma(sp, GF[:], gfb, wr=[t_GF])
                def m_pre_norm(gi):
                    for t2 in range(2):
                        sl = (gi % 2) * 2 + t2
                        r0 = (gi * 2 + t2) * 128
                        norm_tile(r0, xts[sl], t_xts[sl], xns[sl], t_xns[sl], sqj, t_sqj, sts[sl], t_sts[sl], src=x1s)

                def m_pre_T(gi):
                    for t2 in range(2):
                        sl = (gi % 2) * 2 + t2
                        transpose_mod(xns[sl], t_xns[sl], 6 + t2, h2T[gi % 2][:, :, t2 * 128:(t2 + 1) * 128], t_h2T[gi % 2][t2], 2)

                def m_up(gi):
                    hh = h2T[gi % 2]
                    for ff in range(32):
                        bk = ff % 4
                        for k in range(8):
                            op(pe, lambda e, k=k: e.matmul(ps[:, bk, 0:256], lhsT=Wup[:, k, ff * 128:(ff + 1) * 128], rhs=hh[:, k, :],
                                                           start=(k == 0), stop=(k == 7)), rd=[t_Wup] + t_h2T[gi % 2], wr=[PB[bk]])
                        r = rT[ff % 2]
                        op(act, lambda e: e.activation(out=r[:], in_=ps[:, bk, 0:256], func=AF.Relu), rd=[PB[bk]], wr=[t_rT[ff % 2]])
                        op(pool, lambda e, ff=ff: e.tensor_tensor(out=uT[:, ff, :], in0=r[:], in1=r[:], op=ALU.mult),
                           rd=[t_rT[ff % 2]], wr=[t_uT])
                        if ff == 8 and gi + 1 < 16:
                            m_pre_norm(gi + 1)

                def m_down(gi):
                    for t2 in range(2):
                        sl = (gi % 2) * 2 + t2
                        for half in range(2):
                            bk = 4 + half
                            for ff in range(32):
                                op(pe, lambda e, ff=ff: e.matmul(ps[:, bk, :], lhsT=uT[:, ff, t2 * 128:(t2 + 1) * 128],
                                                                 rhs=Wdn[:, ff, half * 512:(half + 1) * 512],
                                                                 start=(ff == 0), stop=(ff == 31)), rd=[t_uT, t_Wdn], wr=[PB[bk]])
                        op(dve, lambda e: e.tensor_tensor(out=x2[:], in0=ps[:, 4:6, :].rearrange("p a b -> p (a b)"), in1=G2[:],
                                                          op=ALU.mult), rd=[PB[4], PB[5], t_G2], wr=[t_x2])
                        op(pool, lambda e: e.tensor_tensor(out=x2[:], in0=x2[:], in1=xts[sl][:], op=ALU.add),
                           rd=[t_x2, t_xts[sl]], wr=[t_x2])
                        st = sts[sl]
                        op(act, lambda e: e.activation(out=xns[sl][:], in_=x2[:], func=AF.Square, accum_out=st[:, 0:1]),
                           rd=[t_x2], wr=[t_xns[sl], t_sts[sl]])
                        op(act, lambda e: e.activation(out=st[:, 1:2], in_=st[:, 0:1], func=AF.Sqrt, bias=EPS, scale=1.0 / D),
                           rd=[t_sts[sl]], wr=[t_sts[sl]])
                        op(dve, lambda e: e.reciprocal(out=st[:, 2:3], in_=st[:, 1:2]), rd=[t_sts[sl]], wr=[t_sts[sl]])
                        op(dve, lambda e: e.scalar_tensor_tensor(out=oo[:], in0=x2[:], scalar=st[:, 2:3], in1=GF[:],
                                                                 op0=ALU.mult, op1=ALU.mult), rd=[t_x2, t_sts[sl], t_GF], wr=[t_oo])
                        r0 = (gi * 2 + t2) * 128
                        dma(sp, out[r0:r0 + 128, :], oo[:], rd=[t_oo])

                m_pre_norm(0)
                m_pre_T(0)
                for gi in range(16):
                    m_up(gi)
                    if gi + 1 < 16:
                        m_pre_T(gi + 1)
                    m_down(gi)
                kb.barrier()

    try:
        _body()
    except _Stop:
        pass
    return nc


_NC_CACHE = {}


def _layout_inputs(x, c, positions, w_ada, b_ada, g_mix, w_in, conv_w, conv_b, conv_norm_g, conv_norm_b,
                   w_out, g_mlp, w_up, w_down, g_final):
    f32 = np.float32
    x = np.asarray(x, f32); c = np.asarray(c, f32); positions = np.asarray(positions, np.int32)

    def col(v, n):
        return np.ascontiguousarray(np.asarray(v, f32).reshape(n, 128).T)
    shared = {
        "w_ada": np.ascontiguousarray(np.asarray(w_ada, f32)[0]),
        "badac": col(np.asarray(b_ada)[0], 48),
        "badar": np.ascontiguousarray(np.asarray(b_ada, f32)[0][None, :]),
        "gmixc": col(np.asarray(g_mix)[0], 8),
        "gmlpc": col(np.asarray(g_mlp)[0], 8),
        "w_in": np.ascontiguousarray(np.asarray(w_in, f32)[0]),
        "convw": np.ascontiguousarray(np.asarray(conv_w, f32)[0].T.reshape(4, 128, 31).transpose(1, 0, 2)),
        "convb": col(np.asarray(conv_b)[0], 4),
        "cng": col(np.asarray(conv_norm_g)[0], 4),
        "cnb": col(np.asarray(conv_norm_b)[0], 4),
        "w_out": np.ascontiguousarray(np.asarray(w_out, f32)[0]),
        "w_up": np.ascontiguousarray(np.asarray(w_up, f32)[0]),
        "w_down": np.ascontiguousarray(np.asarray(w_down, f32)[0]),
        "gfb": np.ascontiguousarray(np.broadcast_to(np.asarray(g_final, f32)[None, :], (128, D))),
        "invf": np.ascontiguousarray(np.broadcast_to(
            np.power(f32(500000.0), -np.arange(8, dtype=f32) * f32(2.0) / f32(16.0)).astype(f32)[None, :], (128, 8))),
    }
    in_maps = []
    for core in range(8):
        b, p = core // 2, core % 2
        own, oth = OWN[p], OWN[1 - p]
        rows = []
        for j in range(8):
            rows.append(np.arange(oth[j] * 512, oth[j] * 512 + 512))
            rows.append(np.arange(own[j] * 512, own[j] * 512 + 512))
        rows = np.concatenate(rows)
        xpa = np.zeros((NT * 128, D), f32)
        xpa[:S] = x[b][rows]
        pos = np.zeros((NT * 128,), np.int32)
        pos[:S] = positions[b][rows]
        hm = np.ones((256,), f32)
        for j in range(8):
            if own[j] == 0:
                hm[j * 32:(j + 1) * 32] = 0.0
            else:
                hr = np.arange(own[j] * 512 - 32, own[j] * 512)
                xpa[S + j * 32:S + (j + 1) * 32] = x[b][hr]
                pos[S + j * 32:S + (j + 1) * 32] = positions[b][hr]
        of = np.array([0.0 if oth[j] < own[j] else NEG for j in range(8)], f32)
        m = dict(shared)
        m["xp"] = xpa
        m["posp"] = np.ascontiguousarray(pos.reshape(NT, 128).T)
        m["oflag"] = np.ascontiguousarray(np.broadcast_to(of[None, :], (128, 8)))
        m["hmask"] = np.ascontiguousarray(np.broadcast_to(hm[None, :], (128, 256)))
        m["cT"] = col(c[b], 8)
        in_maps.append(m)
    return in_maps


def kernel(**inputs):
    in_maps = _layout_inputs(**inputs)
    if "nc" not in _NC_CACHE:
        _NC_CACHE["nc"] = build_program()
    nc = _NC_CACHE["nc"]
    res = run_bass_kernel_spmd(nc, in_maps, core_ids=list(range(8)))
    outf = np.zeros((4, S, D), np.float32)
    for core in range(8):
        b, p = core // 2, core % 2
        o = res.results[core]["out"]
        for j, ch in enumerate(OWN[p]):
            outf[b, ch * 512:(ch + 1) * 512] = o[j * 512:(j + 1) * 512]
    if DEBUG:
        kernel.debug = res.results
    return outf
```

```python
import numpy as np
import concourse.bass as bass
import concourse.mybir as mybir
from concourse.bass_utils import run_bass_kernel_spmd

F32 = mybir.dt.float32
BF16 = mybir.dt.bfloat16
I32 = mybir.dt.int32
U8 = mybir.dt.uint8
ALU = mybir.AluOpType
AF = mybir.ActivationFunctionType
AX = mybir.AxisListType

D = 1024
S = 8192
NT = 66
NEG = -30000.0
EPS = 1e-6
NBIS = 9
BR = 6.0
JA = 2560
OWN = ([0, 3, 4, 7, 8, 11, 12, 15], [1, 2, 5, 6, 9, 10, 13, 14])
DEBUG = False


class T:
    __slots__ = ("w", "r")

    def __init__(self):
        self.w = {}
        self.r = {}


class Eng:
    def __init__(self, obj, sem, key):
        self.obj = obj
        self.sem = sem
        self.key = key
        self.cnt = 0
        self.seen = {}


class K:
    def __init__(self, nc, sems):
        self.nc = nc
        it = iter(sems)
        self.pe = Eng(nc.tensor, next(it), "pe")
        self.act = Eng(nc.scalar, next(it), "act")
        self.dve = Eng(nc.vector, next(it), "dve")
        self.pool = Eng(nc.gpsimd, next(it), "pool")
        self.sp = Eng(nc.sync, next(it), "sp")
        self.engs = [self.pe, self.act, self.dve, self.pool, self.sp]
        self.dsems = {"sp": [[s, 0] for s in [next(it) for _ in range(8)]],
                      "pool": [[s, 0] for s in [next(it) for _ in range(8)]]}
        self.dptr = {"sp": 0, "pool": 0}

    def _waits(self, eng, rd, wr):
        need = {}

        def add(d, skip_self):
            for k, (s, v) in d.items():
                if skip_self and k == eng.key:
                    continue
                if k not in need or need[k][1] < v:
                    need[k] = (s, v)
        for t in rd:
            add(t.w, False)
        skip = (eng.key == "pe")
        for t in wr:
            add(t.w, skip)
            add(t.r, skip)
        for k, (s, v) in need.items():
            if eng.seen.get(k, 0) < v:
                eng.obj.wait_ge(s, v)
                eng.seen[k] = v

    def op(self, eng, fn, rd=(), wr=()):
        self._waits(eng, rd, wr)
        inst = fn(eng.obj)
        eng.cnt += 1
        inst.then_inc(eng.sem, 1)
        tok = (eng.sem, eng.cnt)
        for t in rd:
            t.r[eng.key] = tok
        for t in wr:
            t.w = {eng.key: tok}
            t.r = {}

    def dma(self, eng, out, in_, rd=(), wr=()):
        ring = self.dsems[eng.key]
        i = self.dptr[eng.key]
        self.dptr[eng.key] = (i + 1) % len(ring)
        sem, val = ring[i]
        key = "d%s%d" % (eng.key, i)
        self._waits(eng, rd, wr)
        if val > 0 and eng.seen.get(key, 0) < val:
            eng.obj.wait_ge(sem, val)
            eng.seen[key] = val
        eng.obj.dma_start(out=out, in_=in_).then_inc(sem, 16)
        ring[i][1] = val + 16
        tok = (sem, val + 16)
        for t in rd:
            t.r[key] = tok
        for t in wr:
            t.w = {key: tok}
            t.r = {}

    def barrier(self):
        for e in self.engs:
            for f in self.engs:
                if f is not e and f.cnt > 0 and e.seen.get(f.key, 0) < f.cnt:
                    e.obj.wait_ge(f.sem, f.cnt)
                    e.seen[f.key] = f.cnt
            for qk, ring in self.dsems.items():
                for i, (s, v) in enumerate(ring):
                    key = "d%s%d" % (qk, i)
                    if v > 0 and e.seen.get(key, 0) < v:
                        e.obj.wait_ge(s, v)
                        e.seen[key] = v


class _Stop(Exception):
    pass


def build_program(stage=None, dumps=(), nchunks=8, nt1=64):
    nc = bass.Bass("TRN2", target_bir_lowering=False)
    dt = nc.dram_tensor
    xp = dt("xp", [NT * 128, D], F32, kind="ExternalInput").ap()
    posp = dt("posp", [128, NT], I32, kind="ExternalInput").ap()
    oflag = dt("oflag", [128, 8], F32, kind="ExternalInput").ap()
    hmask = dt("hmask", [128, 256], F32, kind="ExternalInput").ap()
    invf = dt("invf", [128, 8], F32, kind="ExternalInput").ap()
    cT = dt("cT", [128, 8], F32, kind="ExternalInput").ap()
    w_ada = dt("w_ada", [D, 6 * D], F32, kind="ExternalInput").ap()
    badac = dt("badac", [128, 48], F32, kind="ExternalInput").ap()
    badar = dt("badar", [1, 6 * D], F32, kind="ExternalInput").ap()
    gmixc = dt("gmixc", [128, 8], F32, kind="ExternalInput").ap()
    gmlpc = dt("gmlpc", [128, 8], F32, kind="ExternalInput").ap()
    w_in = dt("w_in", [D, 2376], F32, kind="ExternalInput").ap()
    convw = dt("convw", [128, 4, 31], F32, kind="ExternalInput").ap()
    convb = dt("convb", [128, 4], F32, kind="ExternalInput").ap()
    cng = dt("cng", [128, 4], F32, kind="ExternalInput").ap()
    cnb = dt("cnb", [128, 4], F32, kind="ExternalInput").ap()
    w_out = dt("w_out", [D, D], F32, kind="ExternalInput").ap()
    w_up = dt("w_up", [D, 4 * D], F32, kind="ExternalInput").ap()
    w_down = dt("w_down", [4 * D, D], F32, kind="ExternalInput").ap()
    gfb = dt("gfb", [128, D], F32, kind="ExternalInput").ap()
    out = dt("out", [4096, D], F32, kind="ExternalOutput").ap()
    x1s = dt("x1s", [4096, D], F32).ap()
    g1s = dt("g1s", [128, D], F32).ap()
    g2s = dt("g2s", [128, D], F32).ap()
    if "x1s" in dumps:
        x1s = dt("dbg_x1s", [4096, D], F32, kind="ExternalOutput").ap()

    def wview(w, c0, c1):
        return w[:, c0:c1].rearrange("(k p) e -> p k e", p=128)

    import contextlib
    dump_aps = {}

    def dump(name, ap, tiles, kbref):
        if name not in dumps:
            return
        shp = [int(v) for v in ap.shape]
        d_ap = dt("dbg_" + name, shp, ap.dtype, kind="ExternalOutput").ap()
        kbref.dma(kbref.sp, d_ap, ap, rd=tiles)

    def stop_if(st, kbref):
        if stage == st:
            kbref.barrier()
            raise _Stop()

    def _body():
        with contextlib.ExitStack() as es:
            sems = [es.enter_context(nc.semaphore("s%d" % i)) for i in range(21)]
            kb = K(nc, sems)
            pe, act, dve, pool, sp = kb.pe, kb.act, kb.dve, kb.pool, kb.sp
            op, dma = kb.op, kb.dma

            def sb(name, shape, dtype=F32):
                return es2.enter_context(nc.sbuf_tensor(name, shape, dtype))

            ps = es.enter_context(nc.psum_tensor("ps", [128, 8, 512], F32))
            PB = [T() for _ in range(8)]

            def psb16(b):
                return ps[:, b, :].bitcast(BF16)

            es2 = es
            ident = sb("ident", [128, 128], BF16); t_ident = T()
            ident4 = sb("ident4", [128, 4, 128], BF16)
            identf = sb("identf", [128, 128], F32)
            trim = sb("trim", [128, 128], F32)
            onesm = sb("onesm", [128, 128], BF16)
            onesr = sb("onesr", [1, 128], F32)
            cosT = sb("cosT", [128, NT, 8], F32)
            sinT = sb("sinT", [128, NT, 8], F32); t_cs = T()
            modc = sb("modc", [128, 48], F32); t_modc = T()
            ab = sb("ab", [128, 4, 8], F32); t_ab = T()
            t_G1 = T(); t_G2 = T()
            oflg = sb("oflg", [128, 8], F32); t_small = T()
            cw = sb("cw", [128, 4, 31], F32)
            cb = sb("cb", [128, 4], F32)
            cg = sb("cg", [128, 4], F32)
            cbn = sb("cbn", [128, 4], F32)
            wst = {"slots": None, "tiles": None, "ptr": 0}

            def walloc(tag):
                wst["slots"] = [sb("wslot%s%d" % (tag, i), [128, 8, 512], BF16) for i in range(2)]
                wst["tiles"] = [T() for _ in range(2)]
                wst["ptr"] = 0

            def wload(src_ap):
                i = wst["ptr"]
                wst["ptr"] = (i + 1) % 2
                dma(pool, wst["slots"][i][:], src_ap, wr=[wst["tiles"][i]])
                return wst["slots"][i], wst["tiles"][i]

            op(pool, lambda e: e.memset(identf[:], 0.0), wr=[t_ident])
            op(pool, lambda e: e.affine_select(out=identf[:], in_=identf[:], pattern=[[-1, 128]],
                                               compare_op=ALU.not_equal, fill=1.0, base=0,
                                               channel_multiplier=1), rd=[t_ident], wr=[t_ident])
            op(pool, lambda e: e.tensor_copy(out=ident[:], in_=identf[:]), rd=[t_ident], wr=[t_ident])
            op(pool, lambda e: e.tensor_copy(out=ident4[:], in_=identf[:].unsqueeze(1).to_broadcast([128, 4, 128])),
               rd=[t_ident], wr=[t_ident])
            op(pool, lambda e: e.memset(trim[:], 0.0), wr=[t_ident])
            op(pool, lambda e: e.affine_select(out=trim[:], in_=trim[:], pattern=[[-1, 128]],
                                               compare_op=ALU.is_ge, fill=NEG, base=0,
                                               channel_multiplier=1), rd=[t_ident], wr=[t_ident])
            op(pool, lambda e: e.memset(onesm[:], 1.0 / 512.0), wr=[t_ident])
            op(pool, lambda e: e.memset(onesr[:], 1.0), wr=[t_ident])
            dma(sp, oflg[:], oflag, wr=[t_small])
            dma(sp, cw[:], convw, wr=[t_small])
            dma(sp, cb[:], convb, wr=[t_small])
            dma(sp, cg[:], cng, wr=[t_small])
            dma(sp, cbn[:], cnb, wr=[t_small])

            with contextlib.ExitStack() as es2:
                posi = sb("posi", [128, NT], I32)
                posf = sb("posf", [128, NT], F32)
                ivf = sb("ivf", [128, 8], F32)
                ang = sb("ang", [128, NT, 8], F32)
                tq = sb("tq", [128, NT, 8], F32)
                kq = sb("kq", [128, NT, 8], I32)
                kf = sb("kf", [128, NT, 8], F32)
                red = sb("red", [128, NT, 8], F32)
                t_p0 = T()
                cTs = sb("cTs", [128, 8], F32)
                cond = sb("cond", [128, 8], F32); t_cond = T()
                badc = sb("badc", [128, 48], F32)
                gmc = sb("gmc", [128, 2, 8], F32)
                rowb = sb("rowb", [1, 6144], F32)
                rows2 = [sb("rows%d" % i, [1, 512], F32) for i in range(2)]; t_rows2 = [T(), T()]
                Gtmp = sb("Gtmp", [128, 512], F32); t_Gtmp = T()
                wfs = [sb("wf%d" % i, [128, 8, 512], F32) for i in range(3)]; t_wfs = [T() for _ in range(3)]
                dma(sp, posi[:], posp, wr=[t_p0])
                dma(sp, ivf[:], invf, wr=[t_p0])
                dma(sp, cTs[:], cT, wr=[t_cond])
                dma(sp, badc[:], badac, wr=[t_cond])
                dma(sp, gmc[:, 0, :], gmixc, wr=[t_cond])
                dma(sp, gmc[:, 1, :], gmlpc, wr=[t_cond])
                dma(sp, rowb[:], badar, wr=[t_cond])
                rw = dict(rd=[t_p0], wr=[t_p0])
                op(dve, lambda e: e.tensor_copy(out=posf[:], in_=posi[:]), **rw)
                op(dve, lambda e: e.tensor_tensor(out=ang[:], in0=posf[:].unsqueeze(2).to_broadcast([128, NT, 8]),
                                                  in1=ivf[:].unsqueeze(1).to_broadcast([128, NT, 8]), op=ALU.mult), **rw)
                TWO_PI = 2.0 * np.pi
                C1 = 6.28125
                C2 = TWO_PI - C1

                def reduce_to(dst, shift):
                    op(dve, lambda e: e.tensor_scalar(out=tq[:], in0=ang[:], scalar1=shift, scalar2=1.0 / TWO_PI,
                                                      op0=ALU.add, op1=ALU.mult), **rw)
                    op(dve, lambda e: e.tensor_copy(out=kq[:], in_=tq[:]), **rw)
                    op(dve, lambda e: e.tensor_copy(out=kf[:], in_=kq[:]), **rw)
                    op(dve, lambda e: e.scalar_tensor_tensor(out=red[:], in0=kf[:], scalar=-C1, in1=ang[:],
                                                             op0=ALU.mult, op1=ALU.add), **rw)
                    op(dve, lambda e: e.scalar_tensor_tensor(out=red[:], in0=kf[:], scalar=-C2, in1=red[:],
                                                             op0=ALU.mult, op1=ALU.add), **rw)
                    op(dve, lambda e: e.tensor_scalar(out=red[:], in0=red[:], scalar1=shift, scalar2=None,
                                                      op0=ALU.add), **rw)
                    op(dve, lambda e: e.tensor_scalar(out=tq[:], in0=red[:], scalar1=np.pi, scalar2=-TWO_PI,
                                                      op0=ALU.is_gt, op1=ALU.mult), **rw)
                    op(dve, lambda e: e.tensor_tensor(out=red[:], in0=red[:], in1=tq[:], op=ALU.add), **rw)
                    op(dve, lambda e: e.tensor_scalar(out=tq[:], in0=red[:], scalar1=-np.pi, scalar2=TWO_PI,
                                                      op0=ALU.is_lt, op1=ALU.mult), **rw)
                    op(dve, lambda e: e.tensor_tensor(out=red[:], in0=red[:], in1=tq[:], op=ALU.add), **rw)
                    op(dve, lambda e: e.tensor_scalar(out=red[:], in0=red[:], scalar1=-3.1415925, scalar2=3.1415925,
                                                      op0=ALU.max, op1=ALU.min), **rw)
                    op(act, lambda e: e.activation(out=dst[:], in_=red[:], func=AF.Sin), rd=[t_p0], wr=[t_cs])

                reduce_to(sinT, 0.0)
                reduce_to(cosT, np.pi / 2.0)

                op(act, lambda e: e.activation(out=cond[:], in_=cTs[:], func=AF.Silu), rd=[t_cond], wr=[t_cond])
                for cc in range(12):
                    ws, tw = wfs[cc % 3], t_wfs[cc % 3]
                    dma(sp, ws[:], wview(w_ada, cc * 512, (cc + 1) * 512), wr=[tw])
                    rb_, trb_ = rows2[cc % 2], t_rows2[cc % 2]
                    pbk = 1 + cc % 2
                    for k in range(8):
                        op(pe, lambda e, k=k: e.matmul(ps[0:1, pbk, :], lhsT=cond[:, k:k + 1], rhs=ws[:, k, :],
                                                       start=(k == 0), stop=(k == 7)),
                           rd=[t_cond, tw], wr=[PB[pbk]])
                    op(dve, lambda e: e.tensor_tensor(out=rb_[:], in0=ps[0:1, pbk, :], in1=rowb[:, cc * 512:(cc + 1) * 512],
                                                      op=ALU.add), rd=[PB[pbk], t_cond], wr=[trb_])
                    if cc in (4, 5, 10, 11):
                        op(pe, lambda e: e.matmul(ps[:, 3, :], lhsT=onesr[:], rhs=rb_[:], start=True, stop=True),
                           rd=[trb_, t_ident], wr=[PB[3]])
                        Gs, tG = (g1s, t_G1) if cc < 6 else (g2s, t_G2)
                        go = (cc - 4) * 512 if cc < 6 else (cc - 10) * 512
                        op(act, lambda e: e.activation(out=Gtmp[:], in_=ps[:, 3, :], func=AF.Copy),
                           rd=[PB[3]], wr=[t_Gtmp])
                        dma(sp, Gs[:, go:go + 512], Gtmp[:], rd=[t_Gtmp], wr=[tG])
                    else:
                        for el in range(4):
                            et = cc * 4 + el
                            op(pe, lambda e, el=el, et=et: e.matmul(ps[:, 0, et:et + 1], lhsT=rb_[0:1, el * 128:(el + 1) * 128],
                                                                   rhs=onesr[0:1, 0:1], start=True, stop=True, skip_group_check=True),
                               rd=[trb_, t_ident], wr=[PB[0]])
                op(dve, lambda e: e.memset(modc[:], 0.0), wr=[t_modc])
                for lo_, hi_ in ((0, 16), (24, 40)):
                    op(dve, lambda e: e.tensor_copy(out=modc[:, lo_:hi_], in_=ps[:, 0, lo_:hi_]),
                       rd=[PB[0]], wr=[t_modc])
                op(dve, lambda e: e.scalar_tensor_tensor(out=ab[:, 0, :], in0=modc[:, 8:16], scalar=1.0, in1=gmc[:, 0, :],
                                                         op0=ALU.add, op1=ALU.mult), rd=[t_modc, t_cond], wr=[t_ab])
                op(dve, lambda e: e.tensor_copy(out=ab[:, 1, :], in_=modc[:, 0:8]), rd=[t_modc], wr=[t_ab])
                op(dve, lambda e: e.scalar_tensor_tensor(out=ab[:, 2, :], in0=modc[:, 32:40], scalar=1.0, in1=gmc[:, 1, :],
                                                         op0=ALU.add, op1=ALU.mult), rd=[t_modc, t_cond], wr=[t_ab])
                op(dve, lambda e: e.tensor_copy(out=ab[:, 3, :], in_=modc[:, 24:32]), rd=[t_modc], wr=[t_ab])
                dump("cosT", cosT[:], [t_cs], kb)
                dump("sinT", sinT[:], [t_cs], kb)
                dump("ab", ab[:], [t_ab], kb)
                dump("modc", modc[:], [t_modc], kb)
                kb.barrier()
                stop_if("p0", kb)

            def norm_dma(row0, xt, t_xt, src=None):
                dma(sp, xt[:], (xp if src is None else src)[row0:row0 + 128, :], wr=[t_xt])

            def norm_stats(xt, t_xt, xn, t_xn, st, t_st):
                op(act, lambda e: e.activation(out=xn[:], in_=xt[:], func=AF.Square, accum_out=st[:, 0:1]),
                   rd=[t_xt], wr=[t_xn, t_st])
                op(act, lambda e: e.activation(out=st[:, 1:2], in_=st[:, 0:1], func=AF.Sqrt, bias=EPS, scale=1.0 / D),
                   rd=[t_st], wr=[t_st])
                op(dve, lambda e: e.reciprocal(out=st[:, 2:3], in_=st[:, 1:2]), rd=[t_st], wr=[t_st])

            def norm_scale(xt, t_xt, xn, t_xn, st, t_st):
                op(act, lambda e: e.activation(out=xn[:], in_=xt[:], func=AF.Copy, scale=st[:, 2:3]),
                   rd=[t_xt, t_st], wr=[t_xn])

            def norm_tile(row0, xt, t_xt, xn, t_xn, sq, t_sq, st, t_st, src=None):
                norm_dma(row0, xt, t_xt, src)
                norm_stats(xt, t_xt, xn, t_xn, st, t_st)
                norm_scale(xt, t_xt, xn, t_xn, st, t_st)

            def transpose_mod(xn, t_xn, bank, hT_dst, t_hT, abi):
                pv = psb16(bank)
                for k in range(8):
                    op(pe, lambda e, k=k: e.transpose(out=pv[:, k * 128:(k + 1) * 128], in_=xn[:, k * 128:(k + 1) * 128],
                                                      identity=ident[:]), rd=[t_xn, t_ident], wr=[PB[bank]])
                pv3 = pv.rearrange("p (k t) -> p k t", k=8)
                op(dve, lambda e: e.tensor_tensor(out=hT_dst, in0=pv3,
                                                  in1=ab[:, abi, :].unsqueeze(2).to_broadcast([128, 8, 128]), op=ALU.mult),
                   rd=[PB[bank], t_ab], wr=[t_hT])
                op(pool, lambda e: e.tensor_tensor(out=hT_dst, in0=hT_dst,
                                                   in1=ab[:, abi + 1, :].unsqueeze(2).to_broadcast([128, 8, 128]), op=ALU.add),
                   rd=[t_hT, t_ab], wr=[t_hT])

            def transpose_only(xn, t_xn, bank):
                pv = psb16(bank)
                for k in range(8):
                    op(pe, lambda e, k=k: e.transpose(out=pv[:, k * 128:(k + 1) * 128], in_=xn[:, k * 128:(k + 1) * 128],
                                                      identity=ident[:]), rd=[t_xn, t_ident], wr=[PB[bank]])

            def mod_only(bank, hT_dst, t_hT, abi):
                pv3 = psb16(bank).rearrange("p (k t) -> p k t", k=8)
                op(dve, lambda e: e.tensor_tensor(out=hT_dst, in0=pv3,
                                                  in1=ab[:, abi, :].unsqueeze(2).to_broadcast([128, 8, 128]), op=ALU.mult),
                   rd=[PB[bank], t_ab], wr=[t_hT])
                op(pool, lambda e: e.tensor_tensor(out=hT_dst, in0=hT_dst,
                                                   in1=ab[:, abi + 1, :].unsqueeze(2).to_broadcast([128, 8, 128]), op=ALU.add),
                   rd=[t_hT, t_ab], wr=[t_hT])

            with contextlib.ExitStack() as es2:
                kT = sb("kT", [128, S], BF16); t_kT = T()
                kiT = sb("kiT", [128, S], BF16); t_kiT = T()
                Vaug = sb("Vaug", [128, 64, 2, 65], BF16); t_V = T()
                W1 = sb("W1", [128, 8, 328], BF16); t_W1 = T()
                xts = [sb("xt%d" % i, [128, D], F32) for i in range(2)]; t_xts = [T(), T()]
                xns = [sb("xn%d" % i, [128, D], BF16) for i in range(2)]; t_xns = [T(), T()]
                sqj = None; t_sqj = None
                walloc("b")
                G1 = sb("G1", [128, D], F32)
                dma(sp, G1[:], g1s, rd=[t_G1], wr=[t_G1])
                sts = [sb("st%d" % i, [128, 4], F32) for i in range(2)]; t_sts = [T(), T()]
                hTc = sb("hTc", [128, 8, 512], BF16); t_hTc = [T() for _ in range(4)]
                rtmp = sb("rtmp", [128, 4, 16, 8], F32); t_rtmp = T()
                krot = [sb("krot%d" % i, [128, 256], BF16) for i in range(2)]; t_krot = [T(), T()]

                op(pool, lambda e: e.memset(Vaug[:], 1.0), wr=[t_V])
                dma(pool, W1[:, :, 0:128], wview(w_in, 512, 640), wr=[t_W1])
                dma(pool, W1[:, :, 128:192], wview(w_in, 1280, 1344), wr=[t_W1])
                dma(pool, W1[:, :, 192:320], wview(w_in, 640, 768), wr=[t_W1])
                dma(pool, W1[:, :, 320:328], wview(w_in, 1344, 1352), wr=[t_W1])

                def rope(src3, dst3, nh, ti, tsrc, tdst):
                    cs = cosT[:, ti, :].unsqueeze(1).to_broadcast([128, nh, 8])
                    sn = sinT[:, ti, :].unsqueeze(1).to_broadcast([128, nh, 8])
                    x1, x2 = src3[:, :, 0:8], src3[:, :, 8:16]
                    t1, t2, t3, t4 = (rtmp[:, i, 0:nh, :] for i in range(4))
                    op(dve, lambda e: e.tensor_tensor(out=t1, in0=x1, in1=cs, op=ALU.mult), rd=[tsrc, t_cs], wr=[t_rtmp])
                    op(dve, lambda e: e.tensor_tensor(out=t2, in0=x2, in1=sn, op=ALU.mult), rd=[tsrc, t_cs], wr=[t_rtmp])
                    op(dve, lambda e: e.tensor_tensor(out=t3, in0=x2, in1=cs, op=ALU.mult), rd=[tsrc, t_cs], wr=[t_rtmp])
                    op(dve, lambda e: e.tensor_tensor(out=t4, in0=x1, in1=sn, op=ALU.mult), rd=[tsrc, t_cs], wr=[t_rtmp])
                    op(dve, lambda e: e.tensor_tensor(out=dst3[:, :, 0:8], in0=t1, in1=t2, op=ALU.subtract),
                       rd=[t_rtmp], wr=[tdst])
                    op(dve, lambda e: e.tensor_tensor(out=dst3[:, :, 8:16], in0=t3, in1=t4, op=ALU.add),
                       rd=[t_rtmp], wr=[tdst])
                    op(act, lambda e: e.activation(out=dst3[:, :, 16:64], in_=src3[:, :, 16:64], func=AF.Copy),
                       rd=[tsrc], wr=[tdst])

                def rope4(src4, dst4, ti, tsrc, tdst):
                    cs = cosT[:, ti, :].unsqueeze(1).unsqueeze(1).to_broadcast([128, 2, 4, 8])
                    sn = sinT[:, ti, :].unsqueeze(1).unsqueeze(1).to_broadcast([128, 2, 4, 8])
                    x1, x2 = src4[:, :, :, 0:8], src4[:, :, :, 8:16]
                    t1, t2, t3, t4 = (rtmp[:, i, 0:8, :].rearrange("p (g b) d -> p g b d", g=2) for i in range(4))
                    op(dve, lambda e: e.tensor_tensor(out=t1, in0=x1, in1=cs, op=ALU.mult), rd=[tsrc, t_cs], wr=[t_rtmp])
                    op(dve, lambda e: e.tensor_tensor(out=t2, in0=x2, in1=sn, op=ALU.mult), rd=[tsrc, t_cs], wr=[t_rtmp])
                    op(dve, lambda e: e.tensor_tensor(out=t3, in0=x2, in1=cs, op=ALU.mult), rd=[tsrc, t_cs], wr=[t_rtmp])
                    op(dve, lambda e: e.tensor_tensor(out=t4, in0=x1, in1=sn, op=ALU.mult), rd=[tsrc, t_cs], wr=[t_rtmp])
                    op(dve, lambda e: e.tensor_tensor(out=dst4[:, :, :, 0:8], in0=t1, in1=t2, op=ALU.subtract),
                       rd=[t_rtmp], wr=[tdst])
                    op(dve, lambda e: e.tensor_tensor(out=dst4[:, :, :, 8:16], in0=t3, in1=t4, op=ALU.add),
                       rd=[t_rtmp], wr=[tdst])
                    op(act, lambda e: e.activation(out=dst4[:, :, :, 16:64], in_=src4[:, :, :, 16:64], func=AF.Copy),
                       rd=[tsrc], wr=[tdst])

                def ph1_S1(ti):
                    s2 = ti % 2
                    norm_tile(ti * 128, xts[s2], t_xts[s2], xns[s2], t_xns[s2], sqj, t_sqj, sts[s2], t_sts[s2])
                    hs = ti % 4
                    hdst = hTc[:, :, hs * 128:(hs + 1) * 128]
                    transpose_mod(xns[s2], t_xns[s2], 6 + s2, hdst, t_hTc[hs], 0)

                def ph1_S2(ti):
                    s2 = ti % 2
                    hs = ti % 4
                    bk = s2
                    for k in range(8):
                        op(pe, lambda e, k=k: e.matmul(ps[:, bk, 0:320], lhsT=hTc[:, k, hs * 128:(hs + 1) * 128],
                                                       rhs=W1[:, k, 0:320], start=(k == 0), stop=(k == 7)),
                           rd=[t_hTc[hs], t_W1], wr=[PB[bk]])
                    kr = krot[s2]
                    rope(ps[:, bk, 0:192].rearrange("p (h d) -> p h d", d=64),
                         kr[:, 0:192].rearrange("p (h d) -> p h d", d=64), 3, ti, PB[bk], t_krot[s2])
                    op(pool, lambda e: e.tensor_copy(out=kr[:, 192:256], in_=kr[:, 128:192]), rd=[t_krot[s2]], wr=[t_krot[s2]])
                    op(act, lambda e: e.activation(out=Vaug[:, ti, :, 0:64],
                                                   in_=ps[:, bk, 192:320].rearrange("p (g d) -> p g d", d=64), func=AF.Copy),
                       rd=[PB[bk]], wr=[t_V])
                    tb = 4 + s2
                    pv = psb16(tb)
                    op(pe, lambda e: e.transpose(out=pv[:, 0:128], in_=kr[:, 0:128], identity=ident[:]),
                       rd=[t_krot[s2], t_ident], wr=[PB[tb]])
                    op(pe, lambda e: e.transpose(out=pv[:, 128:256], in_=kr[:, 128:256], identity=ident[:]),
                       rd=[t_krot[s2], t_ident], wr=[PB[tb]])
                    op(act, lambda e: e.activation(out=kT[:, ti * 128:(ti + 1) * 128], in_=pv[:, 0:128], func=AF.Copy),
                       rd=[PB[tb]], wr=[t_kT])
                    op(dve, lambda e: e.tensor_copy(out=kiT[:, ti * 128:(ti + 1) * 128], in_=pv[:, 128:256]),
                       rd=[PB[tb]], wr=[t_kiT])


                ph1_S1(0)
                for ti in range(nt1):
                    if ti + 1 < nt1:
                        ph1_S1(ti + 1)
                    ph1_S2(ti)
                dump("kT", kT[:], [t_kT], kb)
                dump("kiT", kiT[:], [t_kiT], kb)
                dump("Vaug", Vaug[:], [t_V], kb)
                stop_if("p1", kb)
                SC = sb("SC", [128, S], F32); t_SC = T()
                junk = hTc[:].rearrange("p k t -> p (k t)").bitcast(U8)
                RbA = sb("RbA", [128, 8, 512], BF16)
                Rb = [RbA[:, i, :] for i in range(8)]; t_Rb = [T() for _ in range(8)]
                Dg = sb("Dg", [128, 8, 128], BF16); t_Dg = T()
                qT = sb("qT", [128, 2, 4, 512], BF16); t_qT = T()
                qiT = sb("qiT", [128, 4, 2, 512], BF16); t_qiT = T()
                op(pool, lambda e: e.memset(qT[:], 0.0), wr=[t_qT])
                op(pool, lambda e: e.memset(qiT[:], 0.0), wr=[t_qiT])
                qrot = [sb("qrot%d" % i, [128, 512], BF16) for i in range(2)]; t_qrot = [T(), T()]
                wsc = sb("wsc", [128, 4, 8], F32); t_wsc = T()
                PT = [sb("PT%d" % i, [128, 512], BF16) for i in range(4)]; t_PT = [T() for _ in range(4)]
                MB = sb("MB", [128, S], BF16); t_MB = T()
                junkA = sb("junkA", [128, JA], U8); t_junkA = T()
                bsa = sb("bsa", [128, 2], F32); t_bsa = T()
                bst = sb("bst", [128, 8], F32); t_bst = T()
                gluT = sb("gluT", [128, 4, 544], BF16); t_glu = T()
                gluH = sb("gluH", [128, 4, 256], BF16); t_gluH = T()
                SCb = SC[:].bitcast(BF16)
                ybf = SCb[:, 0:2048].rearrange("p (c t) -> p c t", c=4); t_ybf = t_SC
                ysq = SCb[:, 2048:4096].rearrange("p (c t) -> p c t", c=4); t_ysq = t_SC
                lnA = SC[:, 2048:2560]; t_lnA = t_SC
                lnB = SC[:, 2560:3072]; t_lnB = t_SC
                zn = SC[:, 3072:3584]; t_zn = t_SC
                sig = SC[:, 3584:4096]; t_sig = t_SC
                RbF = RbA[:].rearrange("p a b -> p (a b)")
                cdh = [RbF[:, 0:2048].rearrange("p (k c) -> p k c", c=128), RbF[:, 2048:3968].rearrange("p (k c) -> p k c", c=128)]
                t_cdh = [t_Rb[0:4], t_Rb[4:8]]
                mixT = sb("mixT", [128, 8, 512], BF16); t_mixT = T()
                attn = sb("attn", [128, 512], BF16); t_attn = T()
                rs4 = sb("rs4", [128, 8], F32); t_rs4 = T()
                x1t = SC[:, 4096:5120]; t_x1t = t_SC
                hmB = sb("hmB", [128, 256], BF16)
                hm = sb("hm", [128, 256], F32)
                dma(sp, hm[:], hmask, wr=[t_small])
                op(pool, lambda e: e.tensor_copy(out=hmB[:], in_=hm[:]), rd=[t_small], wr=[t_small])

                def conv_glu_mm(ws_a, tw_a, ws_g, tw_g, ncols, ct, hcols, t_h):
                    b0 = (ct % 2) * 2
                    for k in range(8):
                        op(pe, lambda e, k=k: e.matmul(ps[:, b0, 0:ncols], lhsT=ws_a[:, k, ct * 128:(ct + 1) * 128],
                                                       rhs=hTc[:, k, hcols], start=(k == 0), stop=(k == 7)),
                           rd=t_h + [tw_a], wr=[PB[b0]])
                    for k in range(8):
                        op(pe, lambda e, k=k: e.matmul(ps[:, b0 + 1, 0:ncols], lhsT=ws_g[:, k, ct * 128:(ct + 1) * 128],
                                                       rhs=hTc[:, k, hcols], start=(k == 0), stop=(k == 7)),
                           rd=t_h + [tw_g], wr=[PB[b0 + 1]])

                def conv_glu_ev(ncols, ct, dst, tdst):
                    b0 = (ct % 2) * 2
                    op(act, lambda e: e.activation(out=sig[:, 0:ncols], in_=ps[:, b0 + 1, 0:ncols], func=AF.Sigmoid),
                       rd=[PB[b0 + 1]], wr=[t_sig])
                    op(dve, lambda e: e.tensor_tensor(out=dst, in0=ps[:, b0, 0:ncols], in1=sig[:, 0:ncols], op=ALU.mult),
                       rd=[PB[b0], t_sig], wr=[tdst])

                def conv_glu(ws_a, tw_a, ws_g, tw_g, ncols, ct, dst, tdst, hcols, t_h):
                    conv_glu_mm(ws_a, tw_a, ws_g, tw_g, ncols, ct, hcols, t_h)
                    conv_glu_ev(ncols, ct, dst, tdst)

                for hi in range(2):
                    norm_tile((64 + hi) * 128, xts[hi], t_xts[hi], xns[hi], t_xns[hi], sqj, t_sqj, sts[hi], t_sts[hi])
                    transpose_mod(xns[hi], t_xns[hi], 6 + hi, hTc[:, :, hi * 128:(hi + 1) * 128], t_hTc[hi], 0)
                wa, twa = wload(wview(w_in, 1352, 1864))
                wg, twg = wload(wview(w_in, 1864, 2376))
                for ct in range(4):
                    conv_glu(wa, twa, wg, twg, 256, ct, gluH[:, ct, :], t_gluH, slice(0, 256), [t_hTc[0], t_hTc[1]])
                    op(pool, lambda e, ct=ct: e.tensor_tensor(out=gluH[:, ct, :], in0=gluH[:, ct, :], in1=hmB[:], op=ALU.mult),
                       rd=[t_gluH, t_small], wr=[t_gluH])

                dump("gluH", gluH[:], [t_gluH], kb)
                stop_if("p2h", kb)
                for j in range(nchunks):
                    tile0 = (2 * j + 1) * 4
                    def norm_stages(jn):
                        tl0 = (2 * jn + 1) * 4

                        def nD(t4):
                            s2 = t4 % 2
                            norm_dma((tl0 + t4) * 128, xts[s2], t_xts[s2])

                        def nS(t4):
                            s2 = t4 % 2
                            norm_stats(xts[s2], t_xts[s2], xns[s2], t_xns[s2], sts[s2], t_sts[s2])

                        def nC(t4):
                            s2 = t4 % 2
                            norm_scale(xts[s2], t_xts[s2], xns[s2], t_xns[s2], sts[s2], t_sts[s2])

                        def nT(t4):
                            s2 = t4 % 2
                            transpose_only(xns[s2], t_xns[s2], 6 + s2)

                        def nM(t4):
                            mod_only(6 + t4 % 2, hTc[:, :, t4 * 128:(t4 + 1) * 128], t_hTc[t4], 0)

                        order = [(nD, 0), (nD, 1), (nS, 0), (nS, 1), (nC, 0), (nC, 1), (nT, 0), (nD, 2), (nM, 0), (nT, 1),
                                 (nS, 2), (nD, 3), (nM, 1), (nC, 2), (nS, 3), (nT, 2), (nC, 3), (nM, 2), (nT, 3), (nM, 3)]
                        for fn, t4 in order:
                            fn(t4)
                            yield

                    if j == 0:
                        for _ in norm_stages(0):
                            pass
                    wqi, twqi = wload(wview(w_in, 768, 1280))
                    wq_box = {}

                    def u_mm(grp, t4, bk):
                        wq, twq = wq_box["w"] if grp == 0 else (wqi, twqi)
                        for k in range(8):
                            op(pe, lambda e, k=k: e.matmul(ps[:, bk, :], lhsT=hTc[:, k, t4 * 128:(t4 + 1) * 128],
                                                           rhs=wq[:, k, :], start=(k == 0), stop=(k == 7)),
                               rd=[t_hTc[t4], twq], wr=[PB[bk]])
                        if grp == 1:
                            for k in range(8):
                                op(pe, lambda e, k=k: e.matmul(ps[:, 2, 0:8], lhsT=hTc[:, k, t4 * 128:(t4 + 1) * 128],
                                                               rhs=W1[:, k, 320:328], start=(k == 0), stop=(k == 7)),
                                   rd=[t_hTc[t4], t_W1], wr=[PB[2]])
                            op(dve, lambda e: e.tensor_scalar(
                                out=wsc[:, t4, :].rearrange("p (b g) -> p g b", g=2),
                                in0=ps[:, 2, 0:8].rearrange("p (g b) -> p g b", g=2),
                                scalar1=float(8 ** -0.5 * 64 ** -0.5), scalar2=None, op0=ALU.mult),
                               rd=[PB[2]], wr=[t_wsc])

                    def u_rope(grp, t4, bk):
                        qr = qrot[bk]
                        src4 = ps[:, bk, :].rearrange("p (g b d) -> p g b d", g=2, b=4)
                        dst4 = qr[:].rearrange("p (b g d) -> p g b d", g=2, b=4)
                        rope4(src4, dst4, tile0 + t4, PB[bk], t_qrot[bk])

                    def u_trP(grp, t4, bk):
                        qr = qrot[bk]
                        tb = 4 + bk
                        pv = psb16(tb)
                        for b in range(4):
                            op(pe, lambda e, b=b: e.transpose(out=pv[:, b * 128:(b + 1) * 128],
                                                              in_=qr[:, b * 128:(b + 1) * 128], identity=ident[:]),
                               rd=[t_qrot[bk], t_ident], wr=[PB[tb]])

                    def u_trC(grp, t4, bk):
                        t_dstT = t_qT if grp == 0 else t_qiT
                        tb = 4 + bk
                        pv = psb16(tb)
                        pv4 = pv[:, 0:512].rearrange("p (b t) -> p b t", b=4)
                        tcols = slice(t4 * 128, (t4 + 1) * 128)
                        if grp == 0:
                            d0, d1 = qT[0:64, 0, :, tcols], qT[64:128, 1, :, tcols]
                        else:
                            d0, d1 = qiT[0:64, :, 0, tcols], qiT[64:128, :, 1, tcols]
                        op(act, lambda e: e.activation(out=d0, in_=pv4[0:64], func=AF.Copy), rd=[PB[tb]], wr=[t_dstT])
                        if grp == 0:
                            op(act, lambda e: e.activation(out=d1, in_=pv4[64:128], func=AF.Copy), rd=[PB[tb]], wr=[t_dstT])
                        else:
                            op(dve, lambda e: e.tensor_copy(out=d1, in_=pv4[64:128]), rd=[PB[tb]], wr=[t_dstT])

                    def units_gen(grp):
                        order = [("mm", 0), ("mm", 1), ("rope", 0), ("trP", 0), ("mm", 2), ("rope", 1), ("trC", 0), ("trP", 1),
                                 ("mm", 3), ("rope", 2), ("trC", 1), ("trP", 2), ("rope", 3), ("trC", 2), ("trP", 3), ("trC", 3)]
                        fns = {"mm": u_mm, "rope": u_rope, "trP": u_trP, "trC": u_trC}
                        for kind, t4 in order:
                            fns[kind](grp, t4, t4 % 2)
                            yield

                    for _ in units_gen(1):
                        pass
                    q_units = units_gen(0)
                    wa, twa = wload(wview(w_in, 1352, 1864))
                    wg, twg = wload(wview(w_in, 1864, 2376))
                    op(pool, lambda e: e.tensor_copy(out=gluT[:, :, 0:32], in_=gluH[:, :, j * 32:(j + 1) * 32]),
                       rd=[t_gluH], wr=[t_glu])
                    conv_glu_mm(wa, twa, wg, twg, 512, 0, slice(0, 512), t_hTc)
                    for ct in range(4):
                        if ct + 1 < 4:
                            conv_glu_mm(wa, twa, wg, twg, 512, ct + 1, slice(0, 512), t_hTc)
                        conv_glu_ev(512, ct, gluT[:, ct, 32:544], t_glu)
                    for ct in range(4):
                        cbk = ct
                        for hf, (t_lo, t_hi) in enumerate(((0, 16), (16, 31))):
                            nt_ = t_hi - t_lo
                            op(pool, lambda e, ct=ct: e.tensor_tensor(
                                out=cdh[hf], in0=ident[:].unsqueeze(1).to_broadcast([128, nt_, 128]),
                                in1=cw[:, ct, t_lo:t_hi].unsqueeze(2).to_broadcast([128, nt_, 128]), op=ALU.mult),
                               rd=[t_ident, t_small], wr=t_cdh[hf])
                        for tap in range(31):
                            hf, tl = (0, tap) if tap < 16 else (1, tap - 16)
                            op(pe, lambda e, tap=tap, ct=ct: e.matmul(ps[:, cbk, :], lhsT=cdh[hf][:, tl, :],
                                                                     rhs=gluT[:, ct, tap + 2:tap + 514],
                                                                     start=(tap == 0), stop=(tap == 30)),
                               rd=t_cdh[hf] + [t_glu], wr=[PB[cbk]])
                        op(act, lambda e, ct=ct: e.activation(out=ybf[:, ct, :], in_=ps[:, cbk, :], func=AF.Identity,
                                                              bias=cb[:, ct:ct + 1], scale=1.0), rd=[PB[cbk], t_small], wr=[t_ybf])
                        op(act, lambda e, ct=ct: e.activation(out=ysq[:, ct, :], in_=ps[:, cbk, :], func=AF.Square,
                                                              bias=cb[:, ct:ct + 1], scale=1.0), rd=[PB[cbk], t_small], wr=[t_ysq])
                    for ct in range(4):
                        op(pe, lambda e, ct=ct: e.matmul(ps[:, 4, :], lhsT=onesm[:], rhs=ybf[:, ct, :],
                                                         start=(ct == 0), stop=(ct == 3)), rd=[t_ybf, t_ident], wr=[PB[4]])
                    for ct in range(4):
                        op(pe, lambda e, ct=ct: e.matmul(ps[:, 5, :], lhsT=onesm[:], rhs=ysq[:, ct, :],
                                                         start=(ct == 0), stop=(ct == 3)), rd=[t_ysq, t_ident], wr=[PB[5]])
                    op(act, lambda e: e.activation(out=lnA, in_=ps[:, 4, :], func=AF.Copy), rd=[PB[4]], wr=[t_lnA])
                    op(dve, lambda e: e.tensor_tensor(out=lnB, in0=lnA, in1=lnA, op=ALU.mult), rd=[t_lnA], wr=[t_lnB])
                    op(dve, lambda e: e.tensor_tensor(out=lnB, in0=ps[:, 5, :], in1=lnB, op=ALU.subtract),
                       rd=[PB[5], t_lnB], wr=[t_lnB])
                    op(dve, lambda e: e.tensor_scalar(out=lnB, in0=lnB, scalar1=0.0, scalar2=EPS, op0=ALU.max, op1=ALU.add),
                       rd=[t_lnB], wr=[t_lnB])
                    op(act, lambda e: e.activation(out=lnB, in_=lnB, func=AF.Sqrt), rd=[t_lnB], wr=[t_lnB])
                    op(dve, lambda e: e.reciprocal(out=lnB, in_=lnB), rd=[t_lnB], wr=[t_lnB])
                    for ct in range(4):
                        op(dve, lambda e, ct=ct: e.scalar_tensor_tensor(out=zn, in0=ps[:, ct, :], scalar=cb[:, ct:ct + 1],
                                                                        in1=lnA, op0=ALU.add, op1=ALU.subtract),
                           rd=[PB[ct], t_small, t_lnA], wr=[t_zn])
                        op(dve, lambda e: e.tensor_tensor(out=zn, in0=zn, in1=lnB, op=ALU.mult),
                           rd=[t_zn, t_lnB], wr=[t_zn])
                        op(act, lambda e, ct=ct: e.activation(out=mixT[:, 4 + ct, :], in_=zn, func=AF.Silu,
                                                              bias=cbn[:, ct:ct + 1], scale=cg[:, ct:ct + 1]),
                           rd=[t_zn, t_small], wr=[t_mixT])

                    if j == nchunks - 1:
                        dump("mixTc", mixT[:, 4:8, :], [t_mixT], kb)
                        stop_if("p2b", kb)
                    def qgeom(qi):
                        segs = [(c * 512, 512) for c in range(2 * j + 1)] + [((2 * j + 1) * 512, (qi + 1) * 128)]
                        nkeys = (2 * j + 1) * 512 + (qi + 1) * 128
                        return segs, nkeys, slice(qi * 128, (qi + 1) * 128)

                    def stage_A(qi):
                        segs, nkeys, qcols = qgeom(qi)
                        op(pool, lambda e: e.tensor_tensor(
                            out=Dg[:], in0=ident[:].unsqueeze(1).to_broadcast([128, 8, 128]),
                            in1=wsc[:, qi, :].unsqueeze(2).to_broadcast([128, 8, 128]), op=ALU.mult),
                           rd=[t_ident, t_wsc], wr=[t_Dg])
                        units = [(si, h) for si in range(len(segs)) for h in range(8)]
                        U = len(units)

                        def emit_L(u):
                            si, h = units[u]
                            c0, n = segs[si]
                            b, g = h // 2, h % 2
                            bk = u % 4
                            op(pe, lambda e: e.matmul(ps[:, bk, 0:n], lhsT=qiT[:, b, g, qcols],
                                                      rhs=kiT[:, c0:c0 + n], start=True, stop=True),
                               rd=[t_qiT, t_kiT], wr=[PB[bk]])
                            r = Rb[u % 8]
                            if u % 2 == 0:
                                op(act, lambda e: e.activation(out=r[:, 0:n], in_=ps[:, bk, 0:n], func=AF.Relu),
                                   rd=[PB[bk]], wr=[t_Rb[u % 8]])
                            else:
                                op(dve, lambda e: e.tensor_scalar(out=r[:, 0:n], in0=ps[:, bk, 0:n], scalar1=0.0, scalar2=None,
                                                                  op0=ALU.max), rd=[PB[bk]], wr=[t_Rb[u % 8]])

                        def emit_D(u):
                            si, h = units[u]
                            c0, n = segs[si]
                            sbk = 4 + (si % 2)
                            op(pe, lambda e: e.matmul(ps[:, sbk, 0:n], lhsT=Dg[:, h, :], rhs=Rb[u % 8][:, 0:n],
                                                      start=(h == 0), stop=(h == 7)),
                               rd=[t_Dg, t_Rb[u % 8]], wr=[PB[sbk]])
                            if h == 7:
                                if si == 2 * j:
                                    op(act, lambda e: e.activation(out=SC[:, c0:c0 + n], in_=ps[:, sbk, 0:n], func=AF.Identity,
                                                                   bias=oflg[:, j:j + 1], scale=1.0),
                                       rd=[PB[sbk], t_small], wr=[t_SC])
                                elif si == 2 * j + 1:
                                    if n > 128:
                                        op(act, lambda e: e.activation(out=SC[:, c0:c0 + n - 128], in_=ps[:, sbk, 0:n - 128],
                                                                       func=AF.Copy), rd=[PB[sbk]], wr=[t_SC])
                                    op(dve, lambda e: e.tensor_tensor(out=SC[:, c0 + n - 128:c0 + n], in0=ps[:, sbk, n - 128:n],
                                                                      in1=trim[:], op=ALU.add), rd=[PB[sbk], t_ident], wr=[t_SC])
                                else:
                                    op(act, lambda e: e.activation(out=SC[:, c0:c0 + n], in_=ps[:, sbk, 0:n], func=AF.Copy),
                                       rd=[PB[sbk]], wr=[t_SC])

                        for u in range(U + 4):
                            if u < U:
                                emit_L(u)
                            if u >= 4:
                                emit_D(u - 4)

                    def stage_B(qi, frac, jk=None, jk_t=None):
                        segs, nkeys, qcols = qgeom(qi)
                        na = min(int(frac * nkeys) // 128 * 128, JA)
                        scv = SC[:, 0:nkeys]
                        br = 12.0 if j == 0 else BR
                        nbis = 12 if j == 0 else NBIS
                        op(dve, lambda e: e.tensor_reduce(out=bst[:, 0:1], in_=scv, axis=AX.X, op=ALU.max), rd=[t_SC], wr=[t_bst])
                        op(dve, lambda e: e.tensor_scalar(out=bst[:, 1:2], in0=bst[:, 0:1], scalar1=-br / 2, scalar2=None,
                                                          op0=ALU.add), rd=[t_bst], wr=[t_bst])
                        for it in range(nbis):
                            if na > 0:
                                op(act, lambda e: e.activation(out=junkA[:, 0:na], in_=SC[:, 0:na], func=AF.Sign,
                                                               bias=bst[:, 1:2], scale=-1.0, accum_out=bsa[:, 0:1]),
                                   rd=[t_SC, t_bst], wr=[t_junkA, t_bsa])
                            jk_ = junk if jk is None else jk
                            jkt_ = t_hTc if jk is None else jk_t
                            op(dve, lambda e: e.tensor_scalar(out=jk_[:, na:nkeys], in0=SC[:, na:nkeys], scalar1=bst[:, 1:2],
                                                              scalar2=None, op0=ALU.is_gt, op1=ALU.add, accum_out=bst[:, 2:3]),
                               rd=[t_SC, t_bst], wr=jkt_ + [t_bst])
                            if na > 0:
                                op(dve, lambda e: e.scalar_tensor_tensor(out=bst[:, 2:3], in0=bsa[:, 0:1], scalar=-0.5,
                                                                         in1=bst[:, 2:3], op0=ALU.mult, op1=ALU.add),
                                   rd=[t_bsa, t_bst], wr=[t_bst])
                            last = (it == nbis - 1)
                            cn = (br / 2) / (2 ** it) if last else (br / 2) / (2 ** (it + 1))
                            op(dve, lambda e: e.tensor_scalar(out=bst[:, 3:4], in0=bst[:, 2:3], scalar1=255.5 - na / 2.0,
                                                              scalar2=(cn if last else 2.0 * cn), op0=ALU.is_gt, op1=ALU.mult),
                               rd=[t_bst], wr=[t_bst])
                            op(dve, lambda e: e.scalar_tensor_tensor(out=bst[:, 1:2], in0=bst[:, 3:4], scalar=-cn,
                                                                     in1=bst[:, 1:2], op0=ALU.add, op1=ALU.add),
                               rd=[t_bst], wr=[t_bst])
                            yield
                        if j == nchunks - 1 and qi == 3:
                            dump("SC", SC[:, 0:nkeys], [t_SC], kb)
                            dump("bst", bst[:, 0:4], [t_bst], kb)
                            stop_if("p2c", kb)
                        op(dve, lambda e: e.tensor_scalar(out=MB[:, 0:nkeys], in0=scv, scalar1=bst[:, 1:2], scalar2=NEG,
                                                          op0=ALU.is_le, op1=ALU.mult), rd=[t_SC, t_bst], wr=[t_MB])

                    def stage_C_main(qi):
                        segs, nkeys, qcols = qgeom(qi)
                        nsb = nkeys // 128
                        U = nsb * 2
                        LAG = 2

                        def emit_S(u):
                            sbi, g = u // 2, u % 2
                            bk = u % 4
                            op(pe, lambda e: e.matmul(ps[:, bk, :], lhsT=kT[:, sbi * 128:(sbi + 1) * 128],
                                                      rhs=qT[:, g, :, qcols], start=True, stop=False),
                               rd=[t_kT, t_qT], wr=[PB[bk]])
                            op(pe, lambda e: e.matmul(ps[:, bk, :], lhsT=MB[:, sbi * 128:(sbi + 1) * 128],
                                                      rhs=ident4[:], start=False, stop=True),
                               rd=[t_MB, t_ident], wr=[PB[bk]])
                            op(act, lambda e: e.activation(out=PT[u % 4][:], in_=ps[:, bk, :], func=AF.Exp, scale=0.125),
                               rd=[PB[bk]], wr=[t_PT[u % 4]])

                        def emit_V(u):
                            sbi, g = u // 2, u % 2
                            pt = PT[u % 4]
                            ob = 4 + g
                            for b in range(4):
                                op(pe, lambda e, b=b: e.matmul(ps[:, ob, b * 65:(b + 1) * 65], lhsT=pt[:, b * 128:(b + 1) * 128],
                                                               rhs=Vaug[:, sbi, g, :], start=(sbi == 0 and b == 0),
                                                               stop=(sbi == nsb - 1 and b == 3), skip_group_check=True),
                                   rd=[t_PT[u % 4], t_V], wr=[PB[ob]])

                        for u in range(U + LAG):
                            if u < U:
                                emit_S(u)
                            if u >= LAG:
                                emit_V(u - LAG)
                            yield

                    def stage_C_tail(qi):
                        segs, nkeys, qcols = qgeom(qi)
                        for g in range(2):
                            ov = ps[:, 4 + g, 0:260].rearrange("p (b e) -> p b e", e=65)
                            op(dve, lambda e: e.reciprocal(out=rs4[:, g * 4:(g + 1) * 4].unsqueeze(2), in_=ov[:, :, 64:65]),
                               rd=[PB[4 + g]], wr=[t_rs4])
                            op(dve, lambda e: e.tensor_tensor(
                                out=attn[:, g * 256:(g + 1) * 256].rearrange("p (b d) -> p b d", d=64), in0=ov[:, :, 0:64],
                                in1=rs4[:, g * 4:(g + 1) * 4].unsqueeze(2).to_broadcast([128, 4, 64]), op=ALU.mult),
                               rd=[PB[4 + g], t_rs4], wr=[t_attn])
                        if j == nchunks - 1 and qi == 3:
                            dump("attn", attn[:], [t_attn], kb)
                            stop_if("p2d", kb)
                        pv = psb16(6 + qi % 2)
                        for f in range(4):
                            op(pe, lambda e, f=f: e.transpose(out=pv[:, f * 128:(f + 1) * 128], in_=attn[:, f * 128:(f + 1) * 128],
                                                              identity=ident[:]), rd=[t_attn, t_ident], wr=[PB[6 + qi % 2]])
                        op(act, lambda e: e.activation(out=mixT[:, 0:4, qcols], in_=pv[:, 0:512].rearrange("p (f t) -> p f t", f=4),
                                                       func=AF.Copy), rd=[PB[6 + qi % 2]], wr=[t_mixT])

                    def interleave(gb, gc, nb):
                        csteps = list(range(gc[1]))
                        per = (len(csteps) + nb - 1) // nb if nb else 0
                        gcg, gbg = gc[0], gb
                        for it in range(nb):
                            next(gbg, None)
                            for _ in range(per):
                                next(gcg, None)
                        for _ in gbg:
                            pass
                        for _ in gcg:
                            pass

                    def csteps_of(qi):
                        return (qgeom(qi)[1] // 128) * 2 + 2

                    wq_box["w"] = wload(wview(w_in, 0, 512))
                    stage_A(0)
                    for _ in stage_B(0, 0.55, jk=MB[:].bitcast(U8), jk_t=[t_MB]):
                        next(q_units, None)
                        next(q_units, None)
                    for _ in q_units:
                        pass
                    for qi in range(1, 4):
                        stage_A(qi)
                        interleave(stage_B(qi, 0.12), (stage_C_main(qi - 1), csteps_of(qi - 1)), 12 if j == 0 else NBIS)
                        stage_C_tail(qi - 1)
                    ng = norm_stages(j + 1) if j + 1 < nchunks else iter(())
                    per = max(1, csteps_of(3) // 22)
                    for i_, _ in enumerate(stage_C_main(3)):
                        if i_ % per == per - 1:
                            next(ng, None)
                    for _ in ng:
                        pass
                    stage_C_tail(3)

                    wo0, two0 = wload(wview(w_out, 0, 512))
                    wo1, two1 = wload(wview(w_out, 512, 1024))
                    for t4 in range(4):
                        for half, (wo, two) in enumerate(((wo0, two0), (wo1, two1))):
                            bk = half
                            for f in range(8):
                                op(pe, lambda e, f=f: e.matmul(ps[:, bk, :], lhsT=mixT[:, f, t4 * 128:(t4 + 1) * 128], rhs=wo[:, f, :],
                                                               start=(f == 0), stop=(f == 7)), rd=[t_mixT, two], wr=[PB[bk]])
                        s2 = t4 % 2
                        dma(sp, xts[s2][:], xp[(tile0 + t4) * 128:(tile0 + t4 + 1) * 128, :], wr=[t_xts[s2]])
                        op(dve, lambda e: e.tensor_tensor(out=x1t, in0=ps[:, 0:2, :].rearrange("p a b -> p (a b)"), in1=G1[:],
                                                          op=ALU.mult), rd=[PB[0], PB[1], t_G1], wr=[t_x1t])
                        op(pool, lambda e: e.tensor_tensor(out=x1t, in0=x1t, in1=xts[s2][:], op=ALU.add),
                           rd=[t_x1t, t_xts[s2]], wr=[t_x1t])
                        r0 = (j * 4 + t4) * 128
                        dma(sp, x1s[r0:r0 + 128, :], x1t, rd=[t_x1t])
                kb.barrier()
                stop_if("p2e", kb)

            with contextlib.ExitStack() as es2:
                Wup = sb("Wup", [128, 8, 4096], BF16); t_Wup = T()
                Wdn = sb("Wdn", [128, 32, 1024], BF16); t_Wdn = T()
                GF = sb("GF", [128, D], F32); t_GF = T()
                xts = [sb("m_xt%d" % i, [128, D], F32) for i in range(4)]; t_xts = [T() for _ in range(4)]
                xns = [sb("m_xn%d" % i, [128, D], BF16) for i in range(4)]; t_xns = [T() for _ in range(4)]
                sqj = None; t_sqj = None
                G2 = sb("G2", [128, D], F32)
                dma(sp, G2[:], g2s, rd=[t_G2], wr=[t_G2])
                sts = [sb("m_st%d" % i, [128, 4], F32) for i in range(4)]; t_sts = [T() for _ in range(4)]
                h2T = [sb("h2T%d" % i, [128, 8, 256], BF16) for i in range(2)]; t_h2T = [[T(), T()], [T(), T()]]
                rT = [sb("rT%d" % i, [128, 256], BF16) for i in range(2)]; t_rT = [T(), T()]
                uT = sb("uT", [128, 32, 256], BF16); t_uT = T()
                x2 = sb("x2", [128, D], F32); t_x2 = T()
                oo = x2; t_oo = t_x2
                for c4 in range(8):
                    dma(pool, Wup[:, :, c4 * 512:(c4 + 1) * 512], wview(w_up, c4 * 512, (c4 + 1) * 512), wr=[t_Wup])
                for c4 in range(4):
                    dma(pool, Wdn[:, c4 * 8:(c4 + 1) * 8, :],
                        w_down[c4 * 1024:(c4 + 1) * 1024, :].rearrange("(k p) e -> p k e", p=128), wr=[t_Wdn])
                dma(sp, GF[:], gfb, wr=[t_GF])
                def m_pre_norm(gi):
                    for t2 in range(2):
                        sl = (gi % 2) * 2 + t2
                        r0 = (gi * 2 + t2) * 128
                        norm_tile(r0, xts[sl], t_xts[sl], xns[sl], t_xns[sl], sqj, t_sqj, sts[sl], t_sts[sl], src=x1s)

                def m_pre_T(gi):
                    for t2 in range(2):
                        sl = (gi % 2) * 2 + t2
                        transpose_mod(xns[sl], t_xns[sl], 6 + t2, h2T[gi % 2][:, :, t2 * 128:(t2 + 1) * 128], t_h2T[gi % 2][t2], 2)

                def m_up(gi):
                    hh = h2T[gi % 2]
                    for ff in range(32):
                        bk = ff % 4
                        for k in range(8):
                            op(pe, lambda e, k=k: e.matmul(ps[:, bk, 0:256], lhsT=Wup[:, k, ff * 128:(ff + 1) * 128], rhs=hh[:, k, :],
                                                           start=(k == 0), stop=(k == 7)), rd=[t_Wup] + t_h2T[gi % 2], wr=[PB[bk]])
                        r = rT[ff % 2]
                        op(act, lambda e: e.activation(out=r[:], in_=ps[:, bk, 0:256], func=AF.Relu), rd=[PB[bk]], wr=[t_rT[ff % 2]])
                        op(pool, lambda e, ff=ff: e.tensor_tensor(out=uT[:, ff, :], in0=r[:], in1=r[:], op=ALU.mult),
                           rd=[t_rT[ff % 2]], wr=[t_uT])
                        if ff == 8 and gi + 1 < 16:
                            m_pre_norm(gi + 1)

                def m_down(gi):
                    for t2 in range(2):
                        sl = (gi % 2) * 2 + t2
                        for half in range(2):
                            bk = 4 + half
                            for ff in range(32):
                                op(pe, lambda e, ff=ff: e.matmul(ps[:, bk, :], lhsT=uT[:, ff, t2 * 128:(t2 + 1) * 128],
                                                                 rhs=Wdn[:, ff, half * 512:(half + 1) * 512],
                                                                 start=(ff == 0), stop=(ff == 31)), rd=[t_uT, t_Wdn], wr=[PB[bk]])
                        op(dve, lambda e: e.tensor_tensor(out=x2[:], in0=ps[:, 4:6, :].rearrange("p a b -> p (a b)"), in1=G2[:],
                                                          op=ALU.mult), rd=[PB[4], PB[5], t_G2], wr=[t_x2])
                        op(pool, lambda e: e.tensor_tensor(out=x2[:], in0=x2[:], in1=xts[sl][:], op=ALU.add),
                           rd=[t_x2, t_xts[sl]], wr=[t_x2])
                        st = sts[sl]
                        op(act, lambda e: e.activation(out=xns[sl][:], in_=x2[:], func=AF.Square, accum_out=st[:, 0:1]),
                           rd=[t_x2], wr=[t_xns[sl], t_sts[sl]])
                        op(act, lambda e: e.activation(out=st[:, 1:2], in_=st[:, 0:1], func=AF.Sqrt, bias=EPS, scale=1.0 / D),
                           rd=[t_sts[sl]], wr=[t_sts[sl]])
                        op(dve, lambda e: e.reciprocal(out=st[:, 2:3], in_=st[:, 1:2]), rd=[t_sts[sl]], wr=[t_sts[sl]])
                        op(dve, lambda e: e.scalar_tensor_tensor(out=oo[:], in0=x2[:], scalar=st[:, 2:3], in1=GF[:],
                                                                 op0=ALU.mult, op1=ALU.mult), rd=[t_x2, t_sts[sl], t_GF], wr=[t_oo])
                        r0 = (gi * 2 + t2) * 128
                        dma(sp, out[r0:r0 + 128, :], oo[:], rd=[t_oo])

                m_pre_norm(0)
                m_pre_T(0)
                for gi in range(16):
                    m_up(gi)
                    if gi + 1 < 16:
                        m_pre_T(gi + 1)
                    m_down(gi)
                kb.barrier()

    try:
        _body()
    except _Stop:
        pass
    return nc


_NC_CACHE = {}


def _layout_inputs(x, c, positions, w_ada, b_ada, g_mix, w_in, conv_w, conv_b, conv_norm_g, conv_norm_b,
                   w_out, g_mlp, w_up, w_down, g_final):
    f32 = np.float32
    x = np.asarray(x, f32); c = np.asarray(c, f32); positions = np.asarray(positions, np.int32)

    def col(v, n):
        return np.ascontiguousarray(np.asarray(v, f32).reshape(n, 128).T)
    shared = {
        "w_ada": np.ascontiguousarray(np.asarray(w_ada, f32)[0]),
        "badac": col(np.asarray(b_ada)[0], 48),
        "badar": np.ascontiguousarray(np.asarray(b_ada, f32)[0][None, :]),
        "gmixc": col(np.asarray(g_mix)[0], 8),
        "gmlpc": col(np.asarray(g_mlp)[0], 8),
        "w_in": np.ascontiguousarray(np.asarray(w_in, f32)[0]),
        "convw": np.ascontiguousarray(np.asarray(conv_w, f32)[0].T.reshape(4, 128, 31).transpose(1, 0, 2)),
        "convb": col(np.asarray(conv_b)[0], 4),
        "cng": col(np.asarray(conv_norm_g)[0], 4),
        "cnb": col(np.asarray(conv_norm_b)[0], 4),
        "w_out": np.ascontiguousarray(np.asarray(w_out, f32)[0]),
        "w_up": np.ascontiguousarray(np.asarray(w_up, f32)[0]),
        "w_down": np.ascontiguousarray(np.asarray(w_down, f32)[0]),
        "gfb": np.ascontiguousarray(np.broadcast_to(np.asarray(g_final, f32)[None, :], (128, D))),
        "invf": np.ascontiguousarray(np.broadcast_to(
            np.power(f32(500000.0), -np.arange(8, dtype=f32) * f32(2.0) / f32(16.0)).astype(f32)[None, :], (128, 8))),
    }
    in_maps = []
    for core in range(8):
        b, p = core // 2, core % 2
        own, oth = OWN[p], OWN[1 - p]
        rows = []
        for j in range(8):
            rows.append(np.arange(oth[j] * 512, oth[j] * 512 + 512))
            rows.append(np.arange(own[j] * 512, own[j] * 512 + 512))
        rows = np.concatenate(rows)
        xpa = np.zeros((NT * 128, D), f32)
        xpa[:S] = x[b][rows]
        pos = np.zeros((NT * 128,), np.int32)
        pos[:S] = positions[b][rows]
        hm = np.ones((256,), f32)
        for j in range(8):
            if own[j] == 0:
                hm[j * 32:(j + 1) * 32] = 0.0
            else:
                hr = np.arange(own[j] * 512 - 32, own[j] * 512)
                xpa[S + j * 32:S + (j + 1) * 32] = x[b][hr]
                pos[S + j * 32:S + (j + 1) * 32] = positions[b][hr]
        of = np.array([0.0 if oth[j] < own[j] else NEG for j in range(8)], f32)
        m = dict(shared)
        m["xp"] = xpa
        m["posp"] = np.ascontiguousarray(pos.reshape(NT, 128).T)
        m["oflag"] = np.ascontiguousarray(np.broadcast_to(of[None, :], (128, 8)))
        m["hmask"] = np.ascontiguousarray(np.broadcast_to(hm[None, :], (128, 256)))
        m["cT"] = col(c[b], 8)
        in_maps.append(m)
    return in_maps


def kernel(**inputs):
    in_maps = _layout_inputs(**inputs)
    if "nc" not in _NC_CACHE:
        _NC_CACHE["nc"] = build_program()
    nc = _NC_CACHE["nc"]
    res = run_bass_kernel_spmd(nc, in_maps, core_ids=list(range(8)))
    outf = np.zeros((4, S, D), np.float32)
    for core in range(8):
        b, p = core // 2, core % 2
        o = res.results[core]["out"]
        for j, ch in enumerate(OWN[p]):
            outf[b, ch * 512:(ch + 1) * 512] = o[j * 512:(j + 1) * 512]
    if DEBUG:
        kernel.debug = res.results
    return outf
```

```python
import numpy as np
import concourse.bass as bass
import concourse.mybir as mybir
from concourse.bass_utils import run_bass_kernel_spmd

F32 = mybir.dt.float32
BF16 = mybir.dt.bfloat16
I32 = mybir.dt.int32
U8 = mybir.dt.uint8
ALU = mybir.AluOpType
AF = mybir.ActivationFunctionType
AX = mybir.AxisListType

D = 1024
S = 8192
NT = 66
NEG = -30000.0
EPS = 1e-6
NBIS = 9
BR = 6.0
JA = 2560
OWN = ([0, 3, 4, 7, 8, 11, 12, 15], [1, 2, 5, 6, 9, 10, 13, 14])
DEBUG = False


class T:
    __slots__ = ("w", "r")

    def __init__(self):
        self.w = {}
        self.r = {}


class Eng:
    def __init__(self, obj, sem, key):
        self.obj = obj
        self.sem = sem
        self.key = key
        self.cnt = 0
        self.seen = {}


class K:
    def __init__(self, nc, sems):
        self.nc = nc
        it = iter(sems)
        self.pe = Eng(nc.tensor, next(it), "pe")
        self.act = Eng(nc.scalar, next(it), "act")
        self.dve = Eng(nc.vector, next(it), "dve")
        self.pool = Eng(nc.gpsimd, next(it), "pool")
        self.sp = Eng(nc.sync, next(it), "sp")
        self.engs = [self.pe, self.act, self.dve, self.pool, self.sp]
        self.dsems = {"sp": [[s, 0] for s in [next(it) for _ in range(8)]],
                      "pool": [[s, 0] for s in [next(it) for _ in range(8)]]}
        self.dptr = {"sp": 0, "pool": 0}

    def _waits(self, eng, rd, wr):
        need = {}

        def add(d, skip_self):
            for k, (s, v) in d.items():
                if skip_self and k == eng.key:
                    continue
                if k not in need or need[k][1] < v:
                    need[k] = (s, v)
        for t in rd:
            add(t.w, False)
        skip = (eng.key == "pe")
        for t in wr:
            add(t.w, skip)
            add(t.r, skip)
        for k, (s, v) in need.items():
            if eng.seen.get(k, 0) < v:
                eng.obj.wait_ge(s, v)
                eng.seen[k] = v

    def op(self, eng, fn, rd=(), wr=()):
        self._waits(eng, rd, wr)
        inst = fn(eng.obj)
        eng.cnt += 1
        inst.then_inc(eng.sem, 1)
        tok = (eng.sem, eng.cnt)
        for t in rd:
            t.r[eng.key] = tok
        for t in wr:
            t.w = {eng.key: tok}
            t.r = {}

    def dma(self, eng, out, in_, rd=(), wr=()):
        ring = self.dsems[eng.key]
        i = self.dptr[eng.key]
        self.dptr[eng.key] = (i + 1) % len(ring)
        sem, val = ring[i]
        key = "d%s%d" % (eng.key, i)
        self._waits(eng, rd, wr)
        if val > 0 and eng.seen.get(key, 0) < val:
            eng.obj.wait_ge(sem, val)
            eng.seen[key] = val
        eng.obj.dma_start(out=out, in_=in_).then_inc(sem, 16)
        ring[i][1] = val + 16
        tok = (sem, val + 16)
        for t in rd:
            t.r[key] = tok
        for t in wr:
            t.w = {key: tok}
            t.r = {}

    def barrier(self):
        for e in self.engs:
            for f in self.engs:
                if f is not e and f.cnt > 0 and e.seen.get(f.key, 0) < f.cnt:
                    e.obj.wait_ge(f.sem, f.cnt)
                    e.seen[f.key] = f.cnt
            for qk, ring in self.dsems.items():
                for i, (s, v) in enumerate(ring):
                    key = "d%s%d" % (qk, i)
                    if v > 0 and e.seen.get(key, 0) < v:
                        e.obj.wait_ge(s, v)
                        e.seen[key] = v


class _Stop(Exception):
    pass


def build_program(stage=None, dumps=(), nchunks=8, nt1=64):
    nc = bass.Bass("TRN2", target_bir_lowering=False)
    dt = nc.dram_tensor
    xp = dt("xp", [NT * 128, D], F32, kind="ExternalInput").ap()
    posp = dt("posp", [128, NT], I32, kind="ExternalInput").ap()
    oflag = dt("oflag", [128, 8], F32, kind="ExternalInput").ap()
    hmask = dt("hmask", [128, 256], F32, kind="ExternalInput").ap()
    invf = dt("invf", [128, 8], F32, kind="ExternalInput").ap()
    cT = dt("cT", [128, 8], F32, kind="ExternalInput").ap()
    w_ada = dt("w_ada", [D, 6 * D], F32, kind="ExternalInput").ap()
    badac = dt("badac", [128, 48], F32, kind="ExternalInput").ap()
    badar = dt("badar", [1, 6 * D], F32, kind="ExternalInput").ap()
    gmixc = dt("gmixc", [128, 8], F32, kind="ExternalInput").ap()
    gmlpc = dt("gmlpc", [128, 8], F32, kind="ExternalInput").ap()
    w_in = dt("w_in", [D, 2376], F32, kind="ExternalInput").ap()
    convw = dt("convw", [128, 4, 31], F32, kind="ExternalInput").ap()
    convb = dt("convb", [128, 4], F32, kind="ExternalInput").ap()
    cng = dt("cng", [128, 4], F32, kind="ExternalInput").ap()
    cnb = dt("cnb", [128, 4], F32, kind="ExternalInput").ap()
    w_out = dt("w_out", [D, D], F32, kind="ExternalInput").ap()
    w_up = dt("w_up", [D, 4 * D], F32, kind="ExternalInput").ap()
    w_down = dt("w_down", [4 * D, D], F32, kind="ExternalInput").ap()
    gfb = dt("gfb", [128, D], F32, kind="ExternalInput").ap()
    out = dt("out", [4096, D], F32, kind="ExternalOutput").ap()
    x1s = dt("x1s", [4096, D], F32).ap()
    g1s = dt("g1s", [128, D], F32).ap()
    g2s = dt("g2s", [128, D], F32).ap()
    if "x1s" in dumps:
        x1s = dt("dbg_x1s", [4096, D], F32, kind="ExternalOutput").ap()

    def wview(w, c0, c1):
        return w[:, c0:c1].rearrange("(k p) e -> p k e", p=128)

    import contextlib
    dump_aps = {}

    def dump(name, ap, tiles, kbref):
        if name not in dumps:
            return
        shp = [int(v) for v in ap.shape]
        d_ap = dt("dbg_" + name, shp, ap.dtype, kind="ExternalOutput").ap()
        kbref.dma(kbref.sp, d_ap, ap, rd=tiles)

    def stop_if(st, kbref):
        if stage == st:
            kbref.barrier()
            raise _Stop()

    def _body():
        with contextlib.ExitStack() as es:
            sems = [es.enter_context(nc.semaphore("s%d" % i)) for i in range(21)]
            kb = K(nc, sems)
            pe, act, dve, pool, sp = kb.pe, kb.act, kb.dve, kb.pool, kb.sp
            op, dma = kb.op, kb.dma

            def sb(name, shape, dtype=F32):
                return es2.enter_context(nc.sbuf_tensor(name, shape, dtype))

            ps = es.enter_context(nc.psum_tensor("ps", [128, 8, 512], F32))
            PB = [T() for _ in range(8)]

            def psb16(b):
                return ps[:, b, :].bitcast(BF16)

            es2 = es
            ident = sb("ident", [128, 128], BF16); t_ident = T()
            ident4 = sb("ident4", [128, 4, 128], BF16)
            identf = sb("identf", [128, 128], F32)
            trim = sb("trim", [128, 128], F32)
            onesm = sb("onesm", [128, 128], BF16)
            onesr = sb("onesr", [1, 128], F32)
            cosT = sb("cosT", [128, NT, 8], F32)
            sinT = sb("sinT", [128, NT, 8], F32); t_cs = T()
            modc = sb("modc", [128, 48], F32); t_modc = T()
            ab = sb("ab", [128, 4, 8], F32); t_ab = T()
            t_G1 = T(); t_G2 = T()
            oflg = sb("oflg", [128, 8], F32); t_small = T()
            cw = sb("cw", [128, 4, 31], F32)
            cb = sb("cb", [128, 4], F32)
            cg = sb("cg", [128, 4], F32)
            cbn = sb("cbn", [128, 4], F32)
            wst = {"slots": None, "tiles": None, "ptr": 0}

            def walloc(tag):
                wst["slots"] = [sb("wslot%s%d" % (tag, i), [128, 8, 512], BF16) for i in range(2)]
                wst["tiles"] = [T() for _ in range(2)]
                wst["ptr"] = 0

            def wload(src_ap):
                i = wst["ptr"]
                wst["ptr"] = (i + 1) % 2
                dma(pool, wst["slots"][i][:], src_ap, wr=[wst["tiles"][i]])
                return wst["slots"][i], wst["tiles"][i]

            op(pool, lambda e: e.memset(identf[:], 0.0), wr=[t_ident])
            op(pool, lambda e: e.affine_select(out=identf[:], in_=identf[:], pattern=[[-1, 128]],
                                               compare_op=ALU.not_equal, fill=1.0, base=0,
                                               channel_multiplier=1), rd=[t_ident], wr=[t_ident])
            op(pool, lambda e: e.tensor_copy(out=ident[:], in_=identf[:]), rd=[t_ident], wr=[t_ident])
            op(pool, lambda e: e.tensor_copy(out=ident4[:], in_=identf[:].unsqueeze(1).to_broadcast([128, 4, 128])),
               rd=[t_ident], wr=[t_ident])
            op(pool, lambda e: e.memset(trim[:], 0.0), wr=[t_ident])
            op(pool, lambda e: e.affine_select(out=trim[:], in_=trim[:], pattern=[[-1, 128]],
                                               compare_op=ALU.is_ge, fill=NEG, base=0,
                                               channel_multiplier=1), rd=[t_ident], wr=[t_ident])
            op(pool, lambda e: e.memset(onesm[:], 1.0 / 512.0), wr=[t_ident])
            op(pool, lambda e: e.memset(onesr[:], 1.0), wr=[t_ident])
            dma(sp, oflg[:], oflag, wr=[t_small])
            dma(sp, cw[:], convw, wr=[t_small])
            dma(sp, cb[:], convb, wr=[t_small])
            dma(sp, cg[:], cng, wr=[t_small])
            dma(sp, cbn[:], cnb, wr=[t_small])

            with contextlib.ExitStack() as es2:
                posi = sb("posi", [128, NT], I32)
                posf = sb("posf", [128, NT], F32)
                ivf = sb("ivf", [128, 8], F32)
                ang = sb("ang", [128, NT, 8], F32)
                tq = sb("tq", [128, NT, 8], F32)
                kq = sb("kq", [128, NT, 8], I32)
                kf = sb("kf", [128, NT, 8], F32)
                red = sb("red", [128, NT, 8], F32)
                t_p0 = T()
                cTs = sb("cTs", [128, 8], F32)
                cond = sb("cond", [128, 8], F32); t_cond = T()
                badc = sb("badc", [128, 48], F32)
                gmc = sb("gmc", [128, 2, 8], F32)
                rowb = sb("rowb", [1, 6144], F32)
                rows2 = [sb("rows%d" % i, [1, 512], F32) for i in range(2)]; t_rows2 = [T(), T()]
                Gtmp = sb("Gtmp", [128, 512], F32); t_Gtmp = T()
                wfs = [sb("wf%d" % i, [128, 8, 512], F32) for i in range(3)]; t_wfs = [T() for _ in range(3)]
                dma(sp, posi[:], posp, wr=[t_p0])
                dma(sp, ivf[:], invf, wr=[t_p0])
                dma(sp, cTs[:], cT, wr=[t_cond])
                dma(sp, badc[:], badac, wr=[t_cond])
                dma(sp, gmc[:, 0, :], gmixc, wr=[t_cond])
                dma(sp, gmc[:, 1, :], gmlpc, wr=[t_cond])
                dma(sp, rowb[:], badar, wr=[t_cond])
                rw = dict(rd=[t_p0], wr=[t_p0])
                op(dve, lambda e: e.tensor_copy(out=posf[:], in_=posi[:]), **rw)
                op(dve, lambda e: e.tensor_tensor(out=ang[:], in0=posf[:].unsqueeze(2).to_broadcast([128, NT, 8]),
                                                  in1=ivf[:].unsqueeze(1).to_broadcast([128, NT, 8]), op=ALU.mult), **rw)
                TWO_PI = 2.0 * np.pi
                C1 = 6.28125
                C2 = TWO_PI - C1

                def reduce_to(dst, shift):
                    op(dve, lambda e: e.tensor_scalar(out=tq[:], in0=ang[:], scalar1=shift, scalar2=1.0 / TWO_PI,
                                                      op0=ALU.add, op1=ALU.mult), **rw)
                    op(dve, lambda e: e.tensor_copy(out=kq[:], in_=tq[:]), **rw)
                    op(dve, lambda e: e.tensor_copy(out=kf[:], in_=kq[:]), **rw)
                    op(dve, lambda e: e.scalar_tensor_tensor(out=red[:], in0=kf[:], scalar=-C1, in1=ang[:],
                                                             op0=ALU.mult, op1=ALU.add), **rw)
                    op(dve, lambda e: e.scalar_tensor_tensor(out=red[:], in0=kf[:], scalar=-C2, in1=red[:],
                                                             op0=ALU.mult, op1=ALU.add), **rw)
                    op(dve, lambda e: e.tensor_scalar(out=red[:], in0=red[:], scalar1=shift, scalar2=None,
                                                      op0=ALU.add), **rw)
                    op(dve, lambda e: e.tensor_scalar(out=tq[:], in0=red[:], scalar1=np.pi, scalar2=-TWO_PI,
                                                      op0=ALU.is_gt, op1=ALU.mult), **rw)
                    op(dve, lambda e: e.tensor_tensor(out=red[:], in0=red[:], in1=tq[:], op=ALU.add), **rw)
                    op(dve, lambda e: e.tensor_scalar(out=tq[:], in0=red[:], scalar1=-np.pi, scalar2=TWO_PI,
                                                      op0=ALU.is_lt, op1=ALU.mult), **rw)
                    op(dve, lambda e: e.tensor_tensor(out=red[:], in0=red[:], in1=tq[:], op=ALU.add), **rw)
                    op(dve, lambda e: e.tensor_scalar(out=red[:], in0=red[:], scalar1=-3.1415925, scalar2=3.1415925,
                                                      op0=ALU.max, op1=ALU.min), **rw)
                    op(act, lambda e: e.activation(out=dst[:], in_=red[:], func=AF.Sin), rd=[t_p0], wr=[t_cs])

                reduce_to(sinT, 0.0)
                reduce_to(cosT, np.pi / 2.0)

                op(act, lambda e: e.activation(out=cond[:], in_=cTs[:], func=AF.Silu), rd=[t_cond], wr=[t_cond])
                for cc in range(12):
                    ws, tw = wfs[cc % 3], t_wfs[cc % 3]
                    dma(sp, ws[:], wview(w_ada, cc * 512, (cc + 1) * 512), wr=[tw])
                    rb_, trb_ = rows2[cc % 2], t_rows2[cc % 2]
                    pbk = 1 + cc % 2
                    for k in range(8):
                        op(pe, lambda e, k=k: e.matmul(ps[0:1, pbk, :], lhsT=cond[:, k:k + 1], rhs=ws[:, k, :],
                                                       start=(k == 0), stop=(k == 7)),
                           rd=[t_cond, tw], wr=[PB[pbk]])
                    op(dve, lambda e: e.tensor_tensor(out=rb_[:], in0=ps[0:1, pbk, :], in1=rowb[:, cc * 512:(cc + 1) * 512],
                                                      op=ALU.add), rd=[PB[pbk], t_cond], wr=[trb_])
                    if cc in (4, 5, 10, 11):
                        op(pe, lambda e: e.matmul(ps[:, 3, :], lhsT=onesr[:], rhs=rb_[:], start=True, stop=True),
                           rd=[trb_, t_ident], wr=[PB[3]])
                        Gs, tG = (g1s, t_G1) if cc < 6 else (g2s, t_G2)
                        go = (cc - 4) * 512 if cc < 6 else (cc - 10) * 512
                        op(act, lambda e: e.activation(out=Gtmp[:], in_=ps[:, 3, :], func=AF.Copy),
                           rd=[PB[3]], wr=[t_Gtmp])
                        dma(sp, Gs[:, go:go + 512], Gtmp[:], rd=[t_Gtmp], wr=[tG])
                    else:
                        for el in range(4):
                            et = cc * 4 + el
                            op(pe, lambda e, el=el, et=et: e.matmul(ps[:, 0, et:et + 1], lhsT=rb_[0:1, el * 128:(el + 1) * 128],
                                                                   rhs=onesr[0:1, 0:1], start=True, stop=True, skip_group_check=True),
                               rd=[trb_, t_ident], wr=[PB[0]])
                op(dve, lambda e: e.memset(modc[:], 0.0), wr=[t_modc])
                for lo_, hi_ in ((0, 16), (24, 40)):
                    op(dve, lambda e: e.tensor_copy(out=modc[:, lo_:hi_], in_=ps[:, 0, lo_:hi_]),
                       rd=[PB[0]], wr=[t_modc])
                op(dve, lambda e: e.scalar_tensor_tensor(out=ab[:, 0, :], in0=modc[:, 8:16], scalar=1.0, in1=gmc[:, 0, :],
                                                         op0=ALU.add, op1=ALU.mult), rd=[t_modc, t_cond], wr=[t_ab])
                op(dve, lambda e: e.tensor_copy(out=ab[:, 1, :], in_=modc[:, 0:8]), rd=[t_modc], wr=[t_ab])
                op(dve, lambda e: e.scalar_tensor_tensor(out=ab[:, 2, :], in0=modc[:, 32:40], scalar=1.0, in1=gmc[:, 1, :],
                                                         op0=ALU.add, op1=ALU.mult), rd=[t_modc, t_cond], wr=[t_ab])
                op(dve, lambda e: e.tensor_copy(out=ab[:, 3, :], in_=modc[:, 24:32]), rd=[t_modc], wr=[t_ab])
                dump("cosT", cosT[:], [t_cs], kb)
                dump("sinT", sinT[:], [t_cs], kb)
                dump("ab", ab[:], [t_ab], kb)
                dump("modc", modc[:], [t_modc], kb)
                kb.barrier()
                stop_if("p0", kb)

            def norm_dma(row0, xt, t_xt, src=None):
                dma(sp, xt[:], (xp if src is None else src)[row0:row0 + 128, :], wr=[t_xt])

            def norm_stats(xt, t_xt, xn, t_xn, st, t_st):
                op(act, lambda e: e.activation(out=xn[:], in_=xt[:], func=AF.Square, accum_out=st[:, 0:1]),
                   rd=[t_xt], wr=[t_xn, t_st])
                op(act, lambda e: e.activation(out=st[:, 1:2], in_=st[:, 0:1], func=AF.Sqrt, bias=EPS, scale=1.0 / D),
                   rd=[t_st], wr=[t_st])
                op(dve, lambda e: e.reciprocal(out=st[:, 2:3], in_=st[:, 1:2]), rd=[t_st], wr=[t_st])

            def norm_scale(xt, t_xt, xn, t_xn, st, t_st):
                op(act, lambda e: e.activation(out=xn[:], in_=xt[:], func=AF.Copy, scale=st[:, 2:3]),
                   rd=[t_xt, t_st], wr=[t_xn])

            def norm_tile(row0, xt, t_xt, xn, t_xn, sq, t_sq, st, t_st, src=None):
                norm_dma(row0, xt, t_xt, src)
                norm_stats(xt, t_xt, xn, t_xn, st, t_st)
                norm_scale(xt, t_xt, xn, t_xn, st, t_st)

            def transpose_mod(xn, t_xn, bank, hT_dst, t_hT, abi):
                pv = psb16(bank)
                for k in range(8):
                    op(pe, lambda e, k=k: e.transpose(out=pv[:, k * 128:(k + 1) * 128], in_=xn[:, k * 128:(k + 1) * 128],
                                                      identity=ident[:]), rd=[t_xn, t_ident], wr=[PB[bank]])
                pv3 = pv.rearrange("p (k t) -> p k t", k=8)
                op(dve, lambda e: e.tensor_tensor(out=hT_dst, in0=pv3,
                                                  in1=ab[:, abi, :].unsqueeze(2).to_broadcast([128, 8, 128]), op=ALU.mult),
                   rd=[PB[bank], t_ab], wr=[t_hT])
                op(pool, lambda e: e.tensor_tensor(out=hT_dst, in0=hT_dst,
                                                   in1=ab[:, abi + 1, :].unsqueeze(2).to_broadcast([128, 8, 128]), op=ALU.add),
                   rd=[t_hT, t_ab], wr=[t_hT])

            def transpose_only(xn, t_xn, bank):
                pv = psb16(bank)
                for k in range(8):
                    op(pe, lambda e, k=k: e.transpose(out=pv[:, k * 128:(k + 1) * 128], in_=xn[:, k * 128:(k + 1) * 128],
                                                      identity=ident[:]), rd=[t_xn, t_ident], wr=[PB[bank]])

            def mod_only(bank, hT_dst, t_hT, abi):
                pv3 = psb16(bank).rearrange("p (k t) -> p k t", k=8)
                op(dve, lambda e: e.tensor_tensor(out=hT_dst, in0=pv3,
                                                  in1=ab[:, abi, :].unsqueeze(2).to_broadcast([128, 8, 128]), op=ALU.mult),
                   rd=[PB[bank], t_ab], wr=[t_hT])
                op(pool, lambda e: e.tensor_tensor(out=hT_dst, in0=hT_dst,
                                                   in1=ab[:, abi + 1, :].unsqueeze(2).to_broadcast([128, 8, 128]), op=ALU.add),
                   rd=[t_hT, t_ab], wr=[t_hT])

            with contextlib.ExitStack() as es2:
                kT = sb("kT", [128, S], BF16); t_kT = T()
                kiT = sb("kiT", [128, S], BF16); t_kiT = T()
                Vaug = sb("Vaug", [128, 64, 2, 65], BF16); t_V = T()
                W1 = sb("W1", [128, 8, 328], BF16); t_W1 = T()
                xts = [sb("xt%d" % i, [128, D], F32) for i in range(2)]; t_xts = [T(), T()]
                xns = [sb("xn%d" % i, [128, D], BF16) for i in range(2)]; t_xns = [T(), T()]
                sqj = None; t_sqj = None
                walloc("b")
                G1 = sb("G1", [128, D], F32)
                dma(sp, G1[:], g1s, rd=[t_G1], wr=[t_G1])
                sts = [sb("st%d" % i, [128, 4], F32) for i in range(2)]; t_sts = [T(), T()]
                hTc = sb("hTc", [128, 8, 512], BF16); t_hTc = [T() for _ in range(4)]
                rtmp = sb("rtmp", [128, 4, 16, 8], F32); t_rtmp = T()
                krot = [sb("krot%d" % i, [128, 256], BF16) for i in range(2)]; t_krot = [T(), T()]

                op(pool, lambda e: e.memset(Vaug[:], 1.0), wr=[t_V])
                dma(pool, W1[:, :, 0:128], wview(w_in, 512, 640), wr=[t_W1])
                dma(pool, W1[:, :, 128:192], wview(w_in, 1280, 1344), wr=[t_W1])
                dma(pool, W1[:, :, 192:320], wview(w_in, 640, 768), wr=[t_W1])
                dma(pool, W1[:, :, 320:328], wview(w_in, 1344, 1352), wr=[t_W1])

                def rope(src3, dst3, nh, ti, tsrc, tdst):
                    cs = cosT[:, ti, :].unsqueeze(1).to_broadcast([128, nh, 8])
                    sn = sinT[:, ti, :].unsqueeze(1).to_broadcast([128, nh, 8])
                    x1, x2 = src3[:, :, 0:8], src3[:, :, 8:16]
                    t1, t2, t3, t4 = (rtmp[:, i, 0:nh, :] for i in range(4))
                    op(dve, lambda e: e.tensor_tensor(out=t1, in0=x1, in1=cs, op=ALU.mult), rd=[tsrc, t_cs], wr=[t_rtmp])
                    op(dve, lambda e: e.tensor_tensor(out=t2, in0=x2, in1=sn, op=ALU.mult), rd=[tsrc, t_cs], wr=[t_rtmp])
                    op(dve, lambda e: e.tensor_tensor(out=t3, in0=x2, in1=cs, op=ALU.mult), rd=[tsrc, t_cs], wr=[t_rtmp])
                    op(dve, lambda e: e.tensor_tensor(out=t4, in0=x1, in1=sn, op=ALU.mult), rd=[tsrc, t_cs], wr=[t_rtmp])
                    op(dve, lambda e: e.tensor_tensor(out=dst3[:, :, 0:8], in0=t1, in1=t2, op=ALU.subtract),
                       rd=[t_rtmp], wr=[tdst])
                    op(dve, lambda e: e.tensor_tensor(out=dst3[:, :, 8:16], in0=t3, in1=t4, op=ALU.add),
                       rd=[t_rtmp], wr=[tdst])
                    op(act, lambda e: e.activation(out=dst3[:, :, 16:64], in_=src3[:, :, 16:64], func=AF.Copy),
                       rd=[tsrc], wr=[tdst])

                def rope4(src4, dst4, ti, tsrc, tdst):
                    cs = cosT[:, ti, :].unsqueeze(1).unsqueeze(1).to_broadcast([128, 2, 4, 8])
                    sn = sinT[:, ti, :].unsqueeze(1).unsqueeze(1).to_broadcast([128, 2, 4, 8])
                    x1, x2 = src4[:, :, :, 0:8], src4[:, :, :, 8:16]
                    t1, t2, t3, t4 = (rtmp[:, i, 0:8, :].rearrange("p (g b) d -> p g b d", g=2) for i in range(4))
                    op(dve, lambda e: e.tensor_tensor(out=t1, in0=x1, in1=cs, op=ALU.mult), rd=[tsrc, t_cs], wr=[t_rtmp])
                    op(dve, lambda e: e.tensor_tensor(out=t2, in0=x2, in1=sn, op=ALU.mult), rd=[tsrc, t_cs], wr=[t_rtmp])
                    op(dve, lambda e: e.tensor_tensor(out=t3, in0=x2, in1=cs, op=ALU.mult), rd=[tsrc, t_cs], wr=[t_rtmp])
                    op(dve, lambda e: e.tensor_tensor(out=t4, in0=x1, in1=sn, op=ALU.mult), rd=[tsrc, t_cs], wr=[t_rtmp])
                    op(dve, lambda e: e.tensor_tensor(out=dst4[:, :, :, 0:8], in0=t1, in1=t2, op=ALU.subtract),
                       rd=[t_rtmp], wr=[tdst])
                    op(dve, lambda e: e.tensor_tensor(out=dst4[:, :, :, 8:16], in0=t3, in1=t4, op=ALU.add),
                       rd=[t_rtmp], wr=[tdst])
                    op(act, lambda e: e.activation(out=dst4[:, :, :, 16:64], in_=src4[:, :, :, 16:64], func=AF.Copy),
                       rd=[tsrc], wr=[tdst])

                def ph1_S1(ti):
                    s2 = ti % 2
                    norm_tile(ti * 128, xts[s2], t_xts[s2], xns[s2], t_xns[s2], sqj, t_sqj, sts[s2], t_sts[s2])
                    hs = ti % 4
                    hdst = hTc[:, :, hs * 128:(hs + 1) * 128]
                    transpose_mod(xns[s2], t_xns[s2], 6 + s2, hdst, t_hTc[hs], 0)

                def ph1_S2(ti):
                    s2 = ti % 2
                    hs = ti % 4
                    bk = s2
                    for k in range(8):
                        op(pe, lambda e, k=k: e.matmul(ps[:, bk, 0:320], lhsT=hTc[:, k, hs * 128:(hs + 1) * 128],
                                                       rhs=W1[:, k, 0:320], start=(k == 0), stop=(k == 7)),
                           rd=[t_hTc[hs], t_W1], wr=[PB[bk]])
                    kr = krot[s2]
                    rope(ps[:, bk, 0:192].rearrange("p (h d) -> p h d", d=64),
                         kr[:, 0:192].rearrange("p (h d) -> p h d", d=64), 3, ti, PB[bk], t_krot[s2])
                    op(pool, lambda e: e.tensor_copy(out=kr[:, 192:256], in_=kr[:, 128:192]), rd=[t_krot[s2]], wr=[t_krot[s2]])
                    op(act, lambda e: e.activation(out=Vaug[:, ti, :, 0:64],
                                                   in_=ps[:, bk, 192:320].rearrange("p (g d) -> p g d", d=64), func=AF.Copy),
                       rd=[PB[bk]], wr=[t_V])
                    tb = 4 + s2
                    pv = psb16(tb)
                    op(pe, lambda e: e.transpose(out=pv[:, 0:128], in_=kr[:, 0:128], identity=ident[:]),
                       rd=[t_krot[s2], t_ident], wr=[PB[tb]])
                    op(pe, lambda e: e.transpose(out=pv[:, 128:256], in_=kr[:, 128:256], identity=ident[:]),
                       rd=[t_krot[s2], t_ident], wr=[PB[tb]])
                    op(act, lambda e: e.activation(out=kT[:, ti * 128:(ti + 1) * 128], in_=pv[:, 0:128], func=AF.Copy),
                       rd=[PB[tb]], wr=[t_kT])
                    op(dve, lambda e: e.tensor_copy(out=kiT[:, ti * 128:(ti + 1) * 128], in_=pv[:, 128:256]),
                       rd=[PB[tb]], wr=[t_kiT])


                ph1_S1(0)
                for ti in range(nt1):
                    if ti + 1 < nt1:
                        ph1_S1(ti + 1)
                    ph1_S2(ti)
                dump("kT", kT[:], [t_kT], kb)
                dump("kiT", kiT[:], [t_kiT], kb)
                dump("Vaug", Vaug[:], [t_V], kb)
                stop_if("p1", kb)
                SC = sb("SC", [128, S], F32); t_SC = T()
                junk = hTc[:].rearrange("p k t -> p (k t)").bitcast(U8)
                RbA = sb("RbA", [128, 8, 512], BF16)
                Rb = [RbA[:, i, :] for i in range(8)]; t_Rb = [T() for _ in range(8)]
                Dg = sb("Dg", [128, 8, 128], BF16); t_Dg = T()
                qT = sb("qT", [128, 2, 4, 512], BF16); t_qT = T()
                qiT = sb("qiT", [128, 4, 2, 512], BF16); t_qiT = T()
                op(pool, lambda e: e.memset(qT[:], 0.0), wr=[t_qT])
                op(pool, lambda e: e.memset(qiT[:], 0.0), wr=[t_qiT])
                qrot = [sb("qrot%d" % i, [128, 512], BF16) for i in range(2)]; t_qrot = [T(), T()]
                wsc = sb("wsc", [128, 4, 8], F32); t_wsc = T()
                PT = [sb("PT%d" % i, [128, 512], BF16) for i in range(4)]; t_PT = [T() for _ in range(4)]
                MB = sb("MB", [128, S], BF16); t_MB = T()
                junkA = sb("junkA", [128, JA], U8); t_junkA = T()
                bsa = sb("bsa", [128, 2], F32); t_bsa = T()
                bst = sb("bst", [128, 8], F32); t_bst = T()
                gluT = sb("gluT", [128, 4, 544], BF16); t_glu = T()
                gluH = sb("gluH", [128, 4, 256], BF16); t_gluH = T()
                SCb = SC[:].bitcast(BF16)
                ybf = SCb[:, 0:2048].rearrange("p (c t) -> p c t", c=4); t_ybf = t_SC
                ysq = SCb[:, 2048:4096].rearrange("p (c t) -> p c t", c=4); t_ysq = t_SC
                lnA = SC[:, 2048:2560]; t_lnA = t_SC
                lnB = SC[:, 2560:3072]; t_lnB = t_SC
                zn = SC[:, 3072:3584]; t_zn = t_SC
                sig = SC[:, 3584:4096]; t_sig = t_SC
                RbF = RbA[:].rearrange("p a b -> p (a b)")
                cdh = [RbF[:, 0:2048].rearrange("p (k c) -> p k c", c=128), RbF[:, 2048:3968].rearrange("p (k c) -> p k c", c=128)]
                t_cdh = [t_Rb[0:4], t_Rb[4:8]]
                mixT = sb("mixT", [128, 8, 512], BF16); t_mixT = T()
                attn = sb("attn", [128, 512], BF16); t_attn = T()
                rs4 = sb("rs4", [128, 8], F32); t_rs4 = T()
                x1t = SC[:, 4096:5120]; t_x1t = t_SC
                hmB = sb("hmB", [128, 256], BF16)
                hm = sb("hm", [128, 256], F32)
                dma(sp, hm[:], hmask, wr=[t_small])
                op(pool, lambda e: e.tensor_copy(out=hmB[:], in_=hm[:]), rd=[t_small], wr=[t_small])

                def conv_glu_mm(ws_a, tw_a, ws_g, tw_g, ncols, ct, hcols, t_h):
                    b0 = (ct % 2) * 2
                    for k in range(8):
                        op(pe, lambda e, k=k: e.matmul(ps[:, b0, 0:ncols], lhsT=ws_a[:, k, ct * 128:(ct + 1) * 128],
                                                       rhs=hTc[:, k, hcols], start=(k == 0), stop=(k == 7)),
                           rd=t_h + [tw_a], wr=[PB[b0]])
                    for k in range(8):
                        op(pe, lambda e, k=k: e.matmul(ps[:, b0 + 1, 0:ncols], lhsT=ws_g[:, k, ct * 128:(ct + 1) * 128],
                                                       rhs=hTc[:, k, hcols], start=(k == 0), stop=(k == 7)),
                           rd=t_h + [tw_g], wr=[PB[b0 + 1]])

                def conv_glu_ev(ncols, ct, dst, tdst):
                    b0 = (ct % 2) * 2
                    op(act, lambda e: e.activation(out=sig[:, 0:ncols], in_=ps[:, b0 + 1, 0:ncols], func=AF.Sigmoid),
                       rd=[PB[b0 + 1]], wr=[t_sig])
                    op(dve, lambda e: e.tensor_tensor(out=dst, in0=ps[:, b0, 0:ncols], in1=sig[:, 0:ncols], op=ALU.mult),
                       rd=[PB[b0], t_sig], wr=[tdst])

                def conv_glu(ws_a, tw_a, ws_g, tw_g, ncols, ct, dst, tdst, hcols, t_h):
                    conv_glu_mm(ws_a, tw_a, ws_g, tw_g, ncols, ct, hcols, t_h)
                    conv_glu_ev(ncols, ct, dst, tdst)

                for hi in range(2):
                    norm_tile((64 + hi) * 128, xts[hi], t_xts[hi], xns[hi], t_xns[hi], sqj, t_sqj, sts[hi], t_sts[hi])
                    transpose_mod(xns[hi], t_xns[hi], 6 + hi, hTc[:, :, hi * 128:(hi + 1) * 128], t_hTc[hi], 0)
                wa, twa = wload(wview(w_in, 1352, 1864))
                wg, twg = wload(wview(w_in, 1864, 2376))
                for ct in range(4):
                    conv_glu(wa, twa, wg, twg, 256, ct, gluH[:, ct, :], t_gluH, slice(0, 256), [t_hTc[0], t_hTc[1]])
                    op(pool, lambda e, ct=ct: e.tensor_tensor(out=gluH[:, ct, :], in0=gluH[:, ct, :], in1=hmB[:], op=ALU.mult),
                       rd=[t_gluH, t_small], wr=[t_gluH])

                dump("gluH", gluH[:], [t_gluH], kb)
                stop_if("p2h", kb)
                for j in range(nchunks):
                    tile0 = (2 * j + 1) * 4
                    def norm_stages(jn):
                        tl0 = (2 * jn + 1) * 4

                        def nD(t4):
                            s2 = t4 % 2
                            norm_dma((tl0 + t4) * 128, xts[s2], t_xts[s2])

                        def nS(t4):
                            s2 = t4 % 2
                            norm_stats(xts[s2], t_xts[s2], xns[s2], t_xns[s2], sts[s2], t_sts[s2])

                        def nC(t4):
                            s2 = t4 % 2
                            norm_scale(xts[s2], t_xts[s2], xns[s2], t_xns[s2], sts[s2], t_sts[s2])

                        def nT(t4):
                            s2 = t4 % 2
                            transpose_only(xns[s2], t_xns[s2], 6 + s2)

                        def nM(t4):
                            mod_only(6 + t4 % 2, hTc[:, :, t4 * 128:(t4 + 1) * 128], t_hTc[t4], 0)

                        order = [(nD, 0), (nD, 1), (nS, 0), (nS, 1), (nC, 0), (nC, 1), (nT, 0), (nD, 2), (nM, 0), (nT, 1),
                                 (nS, 2), (nD, 3), (nM, 1), (nC, 2), (nS, 3), (nT, 2), (nC, 3), (nM, 2), (nT, 3), (nM, 3)]
                        for fn, t4 in order:
                            fn(t4)
                            yield

                    if j == 0:
                        for _ in norm_stages(0):
                            pass
                    wqi, twqi = wload(wview(w_in, 768, 1280))
                    wq_box = {}

                    def u_mm(grp, t4, bk):
                        wq, twq = wq_box["w"] if grp == 0 else (wqi, twqi)
                        for k in range(8):
                            op(pe, lambda e, k=k: e.matmul(ps[:, bk, :], lhsT=hTc[:, k, t4 * 128:(t4 + 1) * 128],
                                                           rhs=wq[:, k, :], start=(k == 0), stop=(k == 7)),
                               rd=[t_hTc[t4], twq], wr=[PB[bk]])
                        if grp == 1:
                            for k in range(8):
                                op(pe, lambda e, k=k: e.matmul(ps[:, 2, 0:8], lhsT=hTc[:, k, t4 * 128:(t4 + 1) * 128],
                                                               rhs=W1[:, k, 320:328], start=(k == 0), stop=(k == 7)),
                                   rd=[t_hTc[t4], t_W1], wr=[PB[2]])
                            op(dve, lambda e: e.tensor_scalar(
                                out=wsc[:, t4, :].rearrange("p (b g) -> p g b", g=2),
                                in0=ps[:, 2, 0:8].rearrange("p (g b) -> p g b", g=2),
                                scalar1=float(8 ** -0.5 * 64 ** -0.5), scalar2=None, op0=ALU.mult),
                               rd=[PB[2]], wr=[t_wsc])

                    def u_rope(grp, t4, bk):
                        qr = qrot[bk]
                        src4 = ps[:, bk, :].rearrange("p (g b d) -> p g b d", g=2, b=4)
                        dst4 = qr[:].rearrange("p (b g d) -> p g b d", g=2, b=4)
                        rope4(src4, dst4, tile0 + t4, PB[bk], t_qrot[bk])

                    def u_trP(grp, t4, bk):
                        qr = qrot[bk]
                        tb = 4 + bk
                        pv = psb16(tb)
                        for b in range(4):
                            op(pe, lambda e, b=b: e.transpose(out=pv[:, b * 128:(b + 1) * 128],
                                                              in_=qr[:, b * 128:(b + 1) * 128], identity=ident[:]),
                               rd=[t_qrot[bk], t_ident], wr=[PB[tb]])

                    def u_trC(grp, t4, bk):
                        t_dstT = t_qT if grp == 0 else t_qiT
                        tb = 4 + bk
                        pv = psb16(tb)
                        pv4 = pv[:, 0:512].rearrange("p (b t) -> p b t", b=4)
                        tcols = slice(t4 * 128, (t4 + 1) * 128)
                        if grp == 0:
                            d0, d1 = qT[0:64, 0, :, tcols], qT[64:128, 1, :, tcols]
                        else:
                            d0, d1 = qiT[0:64, :, 0, tcols], qiT[64:128, :, 1, tcols]
                        op(act, lambda e: e.activation(out=d0, in_=pv4[0:64], func=AF.Copy), rd=[PB[tb]], wr=[t_dstT])
                        if grp == 0:
                            op(act, lambda e: e.activation(out=d1, in_=pv4[64:128], func=AF.Copy), rd=[PB[tb]], wr=[t_dstT])
                        else:
                            op(dve, lambda e: e.tensor_copy(out=d1, in_=pv4[64:128]), rd=[PB[tb]], wr=[t_dstT])

                    def units_gen(grp):
                        order = [("mm", 0), ("mm", 1), ("rope", 0), ("trP", 0), ("mm", 2), ("rope", 1), ("trC", 0), ("trP", 1),
                                 ("mm", 3), ("rope", 2), ("trC", 1), ("trP", 2), ("rope", 3), ("trC", 2), ("trP", 3), ("trC", 3)]
                        fns = {"mm": u_mm, "rope": u_rope, "trP": u_trP, "trC": u_trC}
                        for kind, t4 in order:
                            fns[kind](grp, t4, t4 % 2)
                            yield

                    for _ in units_gen(1):
                        pass
                    q_units = units_gen(0)
                    wa, twa = wload(wview(w_in, 1352, 1864))
                    wg, twg = wload(wview(w_in, 1864, 2376))
                    op(pool, lambda e: e.tensor_copy(out=gluT[:, :, 0:32], in_=gluH[:, :, j * 32:(j + 1) * 32]),
                       rd=[t_gluH], wr=[t_glu])
                    conv_glu_mm(wa, twa, wg, twg, 512, 0, slice(0, 512), t_hTc)
                    for ct in range(4):
                        if ct + 1 < 4:
                            conv_glu_mm(wa, twa, wg, twg, 512, ct + 1, slice(0, 512), t_hTc)
                        conv_glu_ev(512, ct, gluT[:, ct, 32:544], t_glu)
                    for ct in range(4):
                        cbk = ct
                        for hf, (t_lo, t_hi) in enumerate(((0, 16), (16, 31))):
                            nt_ = t_hi - t_lo
                            op(pool, lambda e, ct=ct: e.tensor_tensor(
                                out=cdh[hf], in0=ident[:].unsqueeze(1).to_broadcast([128, nt_, 128]),
                                in1=cw[:, ct, t_lo:t_hi].unsqueeze(2).to_broadcast([128, nt_, 128]), op=ALU.mult),
                               rd=[t_ident, t_small], wr=t_cdh[hf])
                        for tap in range(31):
                            hf, tl = (0, tap) if tap < 16 else (1, tap - 16)
                            op(pe, lambda e, tap=tap, ct=ct: e.matmul(ps[:, cbk, :], lhsT=cdh[hf][:, tl, :],
                                                                     rhs=gluT[:, ct, tap + 2:tap + 514],
                                                                     start=(tap == 0), stop=(tap == 30)),
                               rd=t_cdh[hf] + [t_glu], wr=[PB[cbk]])
                        op(act, lambda e, ct=ct: e.activation(out=ybf[:, ct, :], in_=ps[:, cbk, :], func=AF.Identity,
                                                              bias=cb[:, ct:ct + 1], scale=1.0), rd=[PB[cbk], t_small], wr=[t_ybf])
                        op(act, lambda e, ct=ct: e.activation(out=ysq[:, ct, :], in_=ps[:, cbk, :], func=AF.Square,
                                                              bias=cb[:, ct:ct + 1], scale=1.0), rd=[PB[cbk], t_small], wr=[t_ysq])
                    for ct in range(4):
                        op(pe, lambda e, ct=ct: e.matmul(ps[:, 4, :], lhsT=onesm[:], rhs=ybf[:, ct, :],
                                                         start=(ct == 0), stop=(ct == 3)), rd=[t_ybf, t_ident], wr=[PB[4]])
                    for ct in range(4):
                        op(pe, lambda e, ct=ct: e.matmul(ps[:, 5, :], lhsT=onesm[:], rhs=ysq[:, ct, :],
                                                         start=(ct == 0), stop=(ct == 3)), rd=[t_ysq, t_ident], wr=[PB[5]])
                    op(act, lambda e: e.activation(out=lnA, in_=ps[:, 4, :], func=AF.Copy), rd=[PB[4]], wr=[t_lnA])
                    op(dve, lambda e: e.tensor_tensor(out=lnB, in0=lnA, in1=lnA, op=ALU.mult), rd=[t_lnA], wr=[t_lnB])
                    op(dve, lambda e: e.tensor_tensor(out=lnB, in0=ps[:, 5, :], in1=lnB, op=ALU.subtract),
                       rd=[PB[5], t_lnB], wr=[t_lnB])
                    op(dve, lambda e: e.tensor_scalar(out=lnB, in0=lnB, scalar1=0.0, scalar2=EPS, op0=ALU.max, op1=ALU.add),
                       rd=[t_lnB], wr=[t_lnB])
                    op(act, lambda e: e.activation(out=lnB, in_=lnB, func=AF.Sqrt), rd=[t_lnB], wr=[t_lnB])
                    op(dve, lambda e: e.reciprocal(out=lnB, in_=lnB), rd=[t_lnB], wr=[t_lnB])
                    for ct in range(4):
                        op(dve, lambda e, ct=ct: e.scalar_tensor_tensor(out=zn, in0=ps[:, ct, :], scalar=cb[:, ct:ct + 1],
                                                                        in1=lnA, op0=ALU.add, op1=ALU.subtract),
                           rd=[PB[ct], t_small, t_lnA], wr=[t_zn])
                        op(dve, lambda e: e.tensor_tensor(out=zn, in0=zn, in1=lnB, op=ALU.mult),
                           rd=[t_zn, t_lnB], wr=[t_zn])
                        op(act, lambda e, ct=ct: e.activation(out=mixT[:, 4 + ct, :], in_=zn, func=AF.Silu,
                                                              bias=cbn[:, ct:ct + 1], scale=cg[:, ct:ct + 1]),
                           rd=[t_zn, t_small], wr=[t_mixT])

                    if j == nchunks - 1:
                        dump("mixTc", mixT[:, 4:8, :], [t_mixT], kb)
                        stop_if("p2b", kb)
                    def qgeom(qi):
                        segs = [(c * 512, 512) for c in range(2 * j + 1)] + [((2 * j + 1) * 512, (qi + 1) * 128)]
                        nkeys = (2 * j + 1) * 512 + (qi + 1) * 128
                        return segs, nkeys, slice(qi * 128, (qi + 1) * 128)

                    def stage_A(qi):
                        segs, nkeys, qcols = qgeom(qi)
                        op(pool, lambda e: e.tensor_tensor(
                            out=Dg[:], in0=ident[:].unsqueeze(1).to_broadcast([128, 8, 128]),
                            in1=wsc[:, qi, :].unsqueeze(2).to_broadcast([128, 8, 128]), op=ALU.mult),
                           rd=[t_ident, t_wsc], wr=[t_Dg])
                        units = [(si, h) for si in range(len(segs)) for h in range(8)]
                        U = len(units)

                        def emit_L(u):
                            si, h = units[u]
                            c0, n = segs[si]
                            b, g = h // 2, h % 2
                            bk = u % 4
                            op(pe, lambda e: e.matmul(ps[:, bk, 0:n], lhsT=qiT[:, b, g, qcols],
                                                      rhs=kiT[:, c0:c0 + n], start=True, stop=True),
                               rd=[t_qiT, t_kiT], wr=[PB[bk]])
                            r = Rb[u % 8]
                            if u % 2 == 0:
                                op(act, lambda e: e.activation(out=r[:, 0:n], in_=ps[:, bk, 0:n], func=AF.Relu),
                                   rd=[PB[bk]], wr=[t_Rb[u % 8]])
                            else:
                                op(dve, lambda e: e.tensor_scalar(out=r[:, 0:n], in0=ps[:, bk, 0:n], scalar1=0.0, scalar2=None,
                                                                  op0=ALU.max), rd=[PB[bk]], wr=[t_Rb[u % 8]])

                        def emit_D(u):
                            si, h = units[u]
                            c0, n = segs[si]
                            sbk = 4 + (si % 2)
                            op(pe, lambda e: e.matmul(ps[:, sbk, 0:n], lhsT=Dg[:, h, :], rhs=Rb[u % 8][:, 0:n],
                                                      start=(h == 0), stop=(h == 7)),
                               rd=[t_Dg, t_Rb[u % 8]], wr=[PB[sbk]])
                            if h == 7:
                                if si == 2 * j:
                                    op(act, lambda e: e.activation(out=SC[:, c0:c0 + n], in_=ps[:, sbk, 0:n], func=AF.Identity,
                                                                   bias=oflg[:, j:j + 1], scale=1.0),
                                       rd=[PB[sbk], t_small], wr=[t_SC])
                                elif si == 2 * j + 1:
                                    if n > 128:
                                        op(act, lambda e: e.activation(out=SC[:, c0:c0 + n - 128], in_=ps[:, sbk, 0:n - 128],
                                                                       func=AF.Copy), rd=[PB[sbk]], wr=[t_SC])
                                    op(dve, lambda e: e.tensor_tensor(out=SC[:, c0 + n - 128:c0 + n], in0=ps[:, sbk, n - 128:n],
                                                                      in1=trim[:], op=ALU.add), rd=[PB[sbk], t_ident], wr=[t_SC])
                                else:
                                    op(act, lambda e: e.activation(out=SC[:, c0:c0 + n], in_=ps[:, sbk, 0:n], func=AF.Copy),
                                       rd=[PB[sbk]], wr=[t_SC])

                        for u in range(U + 4):
                            if u < U:
                                emit_L(u)
                            if u >= 4:
                                emit_D(u - 4)

                    def stage_B(qi, frac, jk=None, jk_t=None):
                        segs, nkeys, qcols = qgeom(qi)
                        na = min(int(frac * nkeys) // 128 * 128, JA)
                        scv = SC[:, 0:nkeys]
                        br = 12.0 if j == 0 else BR
                        nbis = 12 if j == 0 else NBIS
                        op(dve, lambda e: e.tensor_reduce(out=bst[:, 0:1], in_=scv, axis=AX.X, op=ALU.max), rd=[t_SC], wr=[t_bst])
                        op(dve, lambda e: e.tensor_scalar(out=bst[:, 1:2], in0=bst[:, 0:1], scalar1=-br / 2, scalar2=None,
                                                          op0=ALU.add), rd=[t_bst], wr=[t_bst])
                        for it in range(nbis):
                            if na > 0:
                                op(act, lambda e: e.activation(out=junkA[:, 0:na], in_=SC[:, 0:na], func=AF.Sign,
                                                               bias=bst[:, 1:2], scale=-1.0, accum_out=bsa[:, 0:1]),
                                   rd=[t_SC, t_bst], wr=[t_junkA, t_bsa])
                            jk_ = junk if jk is None else jk
                            jkt_ = t_hTc if jk is None else jk_t
                            op(dve, lambda e: e.tensor_scalar(out=jk_[:, na:nkeys], in0=SC[:, na:nkeys], scalar1=bst[:, 1:2],
                                                              scalar2=None, op0=ALU.is_gt, op1=ALU.add, accum_out=bst[:, 2:3]),
                               rd=[t_SC, t_bst], wr=jkt_ + [t_bst])
                            if na > 0:
                                op(dve, lambda e: e.scalar_tensor_tensor(out=bst[:, 2:3], in0=bsa[:, 0:1], scalar=-0.5,
                                                                         in1=bst[:, 2:3], op0=ALU.mult, op1=ALU.add),
                                   rd=[t_bsa, t_bst], wr=[t_bst])
                            last = (it == nbis - 1)
                            cn = (br / 2) / (2 ** it) if last else (br / 2) / (2 ** (it + 1))
                            op(dve, lambda e: e.tensor_scalar(out=bst[:, 3:4], in0=bst[:, 2:3], scalar1=255.5 - na / 2.0,
                                                              scalar2=(cn if last else 2.0 * cn), op0=ALU.is_gt, op1=ALU.mult),
                               rd=[t_bst], wr=[t_bst])
                            op(dve, lambda e: e.scalar_tensor_tensor(out=bst[:, 1:2], in0=bst[:, 3:4], scalar=-cn,
                                                                     in1=bst[:, 1:2], op0=ALU.add, op1=ALU.add),
                               rd=[t_bst], wr=[t_bst])
                            yield
                        if j == nchunks - 1 and qi == 3:
                            dump("SC", SC[:, 0:nkeys], [t_SC], kb)
                            dump("bst", bst[:, 0:4], [t_bst], kb)
                            stop_if("p2c", kb)
                        op(dve, lambda e: e.tensor_scalar(out=MB[:, 0:nkeys], in0=scv, scalar1=bst[:, 1:2], scalar2=NEG,
                                                          op0=ALU.is_le, op1=ALU.mult), rd=[t_SC, t_bst], wr=[t_MB])

                    def stage_C_main(qi):
                        segs, nkeys, qcols = qgeom(qi)
                        nsb = nkeys // 128
                        U = nsb * 2
                        LAG = 3

                        def emit_S(u):
                            sbi, g = u // 2, u % 2
                            bk = u % 4
                            op(pe, lambda e: e.matmul(ps[:, bk, :], lhsT=kT[:, sbi * 128:(sbi + 1) * 128],
                                                      rhs=qT[:, g, :, qcols], start=True, stop=False),
                               rd=[t_kT, t_qT], wr=[PB[bk]])
                            op(pe, lambda e: e.matmul(ps[:, bk, :], lhsT=MB[:, sbi * 128:(sbi + 1) * 128],
                                                      rhs=ident4[:], start=False, stop=True),
                               rd=[t_MB, t_ident], wr=[PB[bk]])
                            op(act, lambda e: e.activation(out=PT[u % 4][:], in_=ps[:, bk, :], func=AF.Exp, scale=0.125),
                               rd=[PB[bk]], wr=[t_PT[u % 4]])

                        def emit_V(u):
                            sbi, g = u // 2, u % 2
                            pt = PT[u % 4]
                            ob = 4 + g
                            for b in range(4):
                                op(pe, lambda e, b=b: e.matmul(ps[:, ob, b * 65:(b + 1) * 65], lhsT=pt[:, b * 128:(b + 1) * 128],
                                                               rhs=Vaug[:, sbi, g, :], start=(sbi == 0 and b == 0),
                                                               stop=(sbi == nsb - 1 and b == 3), skip_group_check=True),
                                   rd=[t_PT[u % 4], t_V], wr=[PB[ob]])

                        for u in range(U + LAG):
                            if u < U:
                                emit_S(u)
                            if u >= LAG:
                                emit_V(u - LAG)
                            yield

                    def stage_C_tail(qi):
                        segs, nkeys, qcols = qgeom(qi)
                        for g in range(2):
                            ov = ps[:, 4 + g, 0:260].rearrange("p (b e) -> p b e", e=65)
                            op(dve, lambda e: e.reciprocal(out=rs4[:, g * 4:(g + 1) * 4].unsqueeze(2), in_=ov[:, :, 64:65]),
                               rd=[PB[4 + g]], wr=[t_rs4])
                            op(dve, lambda e: e.tensor_tensor(
                                out=attn[:, g * 256:(g + 1) * 256].rearrange("p (b d) -> p b d", d=64), in0=ov[:, :, 0:64],
                                in1=rs4[:, g * 4:(g + 1) * 4].unsqueeze(2).to_broadcast([128, 4, 64]), op=ALU.mult),
                               rd=[PB[4 + g], t_rs4], wr=[t_attn])
                        if j == nchunks - 1 and qi == 3:
                            dump("attn", attn[:], [t_attn], kb)
                            stop_if("p2d", kb)
                        pv = psb16(6 + qi % 2)
                        for f in range(4):
                            op(pe, lambda e, f=f: e.transpose(out=pv[:, f * 128:(f + 1) * 128], in_=attn[:, f * 128:(f + 1) * 128],
                                                              identity=ident[:]), rd=[t_attn, t_ident], wr=[PB[6 + qi % 2]])
                        op(act, lambda e: e.activation(out=mixT[:, 0:4, qcols], in_=pv[:, 0:512].rearrange("p (f t) -> p f t", f=4),
                                                       func=AF.Copy), rd=[PB[6 + qi % 2]], wr=[t_mixT])

                    def interleave(gb, gc, nb):
                        csteps = list(range(gc[1]))
                        per = (len(csteps) + nb - 1) // nb if nb else 0
                        gcg, gbg = gc[0], gb
                        for it in range(nb):
                            next(gbg, None)
                            for _ in range(per):
                                next(gcg, None)
                        for _ in gbg:
                            pass
                        for _ in gcg:
                            pass

                    def csteps_of(qi):
                        return (qgeom(qi)[1] // 128) * 2 + 2

                    wq_box["w"] = wload(wview(w_in, 0, 512))
                    stage_A(0)
                    for _ in stage_B(0, 0.55, jk=MB[:].bitcast(U8), jk_t=[t_MB]):
                        next(q_units, None)
                        next(q_units, None)
                    for _ in q_units:
                        pass
                    for qi in range(1, 4):
                        stage_A(qi)
                        interleave(stage_B(qi, 0.06), (stage_C_main(qi - 1), csteps_of(qi - 1)), 12 if j == 0 else NBIS)
                        stage_C_tail(qi - 1)
                    ng = norm_stages(j + 1) if j + 1 < nchunks else iter(())
                    per = max(1, csteps_of(3) // 22)
                    for i_, _ in enumerate(stage_C_main(3)):
                        if i_ % per == per - 1:
                            next(ng, None)
                    for _ in ng:
                        pass
                    stage_C_tail(3)

                    wo0, two0 = wload(wview(w_out, 0, 512))
                    wo1, two1 = wload(wview(w_out, 512, 1024))
                    for t4 in range(4):
                        for half, (wo, two) in enumerate(((wo0, two0), (wo1, two1))):
                            bk = half
                            for f in range(8):
                                op(pe, lambda e, f=f: e.matmul(ps[:, bk, :], lhsT=mixT[:, f, t4 * 128:(t4 + 1) * 128], rhs=wo[:, f, :],
                                                               start=(f == 0), stop=(f == 7)), rd=[t_mixT, two], wr=[PB[bk]])
                        s2 = t4 % 2
                        dma(sp, xts[s2][:], xp[(tile0 + t4) * 128:(tile0 + t4 + 1) * 128, :], wr=[t_xts[s2]])
                        op(dve, lambda e: e.tensor_tensor(out=x1t, in0=ps[:, 0:2, :].rearrange("p a b -> p (a b)"), in1=G1[:],
                                                          op=ALU.mult), rd=[PB[0], PB[1], t_G1], wr=[t_x1t])
                        op(pool, lambda e: e.tensor_tensor(out=x1t, in0=x1t, in1=xts[s2][:], op=ALU.add),
                           rd=[t_x1t, t_xts[s2]], wr=[t_x1t])
                        r0 = (j * 4 + t4) * 128
                        dma(sp, x1s[r0:r0 + 128, :], x1t, rd=[t_x1t])
                kb.barrier()
                stop_if("p2e", kb)

            with contextlib.ExitStack() as es2:
                Wup = sb("Wup", [128, 8, 4096], BF16); t_Wup = T()
                Wdn = sb("Wdn", [128, 32, 1024], BF16); t_Wdn = T()
                GF = sb("GF", [128, D], F32); t_GF = T()
                xts = [sb("m_xt%d" % i, [128, D], F32) for i in range(4)]; t_xts = [T() for _ in range(4)]
                xns = [sb("m_xn%d" % i, [128, D], BF16) for i in range(4)]; t_xns = [T() for _ in range(4)]
                sqj = None; t_sqj = None
                G2 = sb("G2", [128, D], F32)
                dma(sp, G2[:], g2s, rd=[t_G2], wr=[t_G2])
                sts = [sb("m_st%d" % i, [128, 4], F32) for i in range(4)]; t_sts = [T() for _ in range(4)]
                h2T = [sb("h2T%d" % i, [128, 8, 256], BF16) for i in range(2)]; t_h2T = [[T(), T()], [T(), T()]]
                rT = [sb("rT%d" % i, [128, 256], BF16) for i in range(2)]; t_rT = [T(), T()]
                uT = sb("uT", [128, 32, 256], BF16); t_uT = T()
                x2 = sb("x2", [128, D], F32); t_x2 = T()
                oo = x2; t_oo = t_x2
                for c4 in range(8):
                    dma(pool, Wup[:, :, c4 * 512:(c4 + 1) * 512], wview(w_up, c4 * 512, (c4 + 1) * 512), wr=[t_Wup])
                for c4 in range(4):
                    dma(pool, Wdn[:, c4 * 8:(c4 + 1) * 8, :],
                        w_down[c4 * 1024:(c4 + 1) * 1024, :].rearrange("(k p) e -> p k e", p=128), wr=[t_Wdn])
                dma(sp, GF[:], gfb, wr=[t_GF])
                def m_pre_norm(gi):
                    for t2 in range(2):
                        sl = (gi % 2) * 2 + t2
                        r0 = (gi * 2 + t2) * 128
                        norm_tile(r0, xts[sl], t_xts[sl], xns[sl], t_xns[sl], sqj, t_sqj, sts[sl], t_sts[sl], src=x1s)

                def m_pre_T(gi):
                    for t2 in range(2):
                        sl = (gi % 2) * 2 + t2
                        transpose_mod(xns[sl], t_xns[sl], 6 + t2, h2T[gi % 2][:, :, t2 * 128:(t2 + 1) * 128], t_h2T[gi % 2][t2], 2)

                def m_up(gi):
                    hh = h2T[gi % 2]
                    for ff in range(32):
                        bk = ff % 4
                        for k in range(8):
                            op(pe, lambda e, k=k: e.matmul(ps[:, bk, 0:256], lhsT=Wup[:, k, ff * 128:(ff + 1) * 128], rhs=hh[:, k, :],
                                                           start=(k == 0), stop=(k == 7)), rd=[t_Wup] + t_h2T[gi % 2], wr=[PB[bk]])
                        r = rT[ff % 2]
                        op(act, lambda e: e.activation(out=r[:], in_=ps[:, bk, 0:256], func=AF.Relu), rd=[PB[bk]], wr=[t_rT[ff % 2]])
                        op(pool, lambda e, ff=ff: e.tensor_tensor(out=uT[:, ff, :], in0=r[:], in1=r[:], op=ALU.mult),
                           rd=[t_rT[ff % 2]], wr=[t_uT])
                        if ff == 8 and gi + 1 < 16:
                            m_pre_norm(gi + 1)

                def m_down(gi):
                    for t2 in range(2):
                        sl = (gi % 2) * 2 + t2
                        for half in range(2):
                            bk = 4 + half
                            for ff in range(32):
                                op(pe, lambda e, ff=ff: e.matmul(ps[:, bk, :], lhsT=uT[:, ff, t2 * 128:(t2 + 1) * 128],
                                                                 rhs=Wdn[:, ff, half * 512:(half + 1) * 512],
                                                                 start=(ff == 0), stop=(ff == 31)), rd=[t_uT, t_Wdn], wr=[PB[bk]])
                        op(dve, lambda e: e.tensor_tensor(out=x2[:], in0=ps[:, 4:6, :].rearrange("p a b -> p (a b)"), in1=G2[:],
                                                          op=ALU.mult), rd=[PB[4], PB[5], t_G2], wr=[t_x2])
                        op(pool, lambda e: e.tensor_tensor(out=x2[:], in0=x2[:], in1=xts[sl][:], op=ALU.add),
                           rd=[t_x2, t_xts[sl]], wr=[t_x2])
                        st = sts[sl]
                        op(act, lambda e: e.activation(out=xns[sl][:], in_=x2[:], func=AF.Square, accum_out=st[:, 0:1]),
                           rd=[t_x2], wr=[t_xns[sl], t_sts[sl]])
                        op(act, lambda e: e.activation(out=st[:, 1:2], in_=st[:, 0:1], func=AF.Sqrt, bias=EPS, scale=1.0 / D),
                           rd=[t_sts[sl]], wr=[t_sts[sl]])
                        op(dve, lambda e: e.reciprocal(out=st[:, 2:3], in_=st[:, 1:2]), rd=[t_sts[sl]], wr=[t_sts[sl]])
                        op(dve, lambda e: e.scalar_tensor_tensor(out=oo[:], in0=x2[:], scalar=st[:, 2:3], in1=GF[:],
                                                                 op0=ALU.mult, op1=ALU.mult), rd=[t_x2, t_sts[sl], t_GF], wr=[t_oo])
                        r0 = (gi * 2 + t2) * 128
                        dma(sp, out[r0:r0 + 128, :], oo[:], rd=[t_oo])

                m_pre_norm(0)
                m_pre_T(0)
                for gi in range(16):
                    m_up(gi)
                    if gi + 1 < 16:
                        m_pre_T(gi + 1)
                    m_down(gi)
                kb.barrier()

    try:
        _body()
    except _Stop:
        pass
    return nc


_NC_CACHE = {}


def _layout_inputs(x, c, positions, w_ada, b_ada, g_mix, w_in, conv_w, conv_b, conv_norm_g, conv_norm_b,
                   w_out, g_mlp, w_up, w_down, g_final):
    f32 = np.float32
    x = np.asarray(x, f32); c = np.asarray(c, f32); positions = np.asarray(positions, np.int32)

    def col(v, n):
        return np.ascontiguousarray(np.asarray(v, f32).reshape(n, 128).T)
    shared = {
        "w_ada": np.ascontiguousarray(np.asarray(w_ada, f32)[0]),
        "badac": col(np.asarray(b_ada)[0], 48),
        "badar": np.ascontiguousarray(np.asarray(b_ada, f32)[0][None, :]),
        "gmixc": col(np.asarray(g_mix)[0], 8),
        "gmlpc": col(np.asarray(g_mlp)[0], 8),
        "w_in": np.ascontiguousarray(np.asarray(w_in, f32)[0]),
        "convw": np.ascontiguousarray(np.asarray(conv_w, f32)[0].T.reshape(4, 128, 31).transpose(1, 0, 2)),
        "convb": col(np.asarray(conv_b)[0], 4),
        "cng": col(np.asarray(conv_norm_g)[0], 4),
        "cnb": col(np.asarray(conv_norm_b)[0], 4),
        "w_out": np.ascontiguousarray(np.asarray(w_out, f32)[0]),
        "w_up": np.ascontiguousarray(np.asarray(w_up, f32)[0]),
        "w_down": np.ascontiguousarray(np.asarray(w_down, f32)[0]),
        "gfb": np.ascontiguousarray(np.broadcast_to(np.asarray(g_final, f32)[None, :], (128, D))),
        "invf": np.ascontiguousarray(np.broadcast_to(
            np.power(f32(500000.0), -np.arange(8, dtype=f32) * f32(2.0) / f32(16.0)).astype(f32)[None, :], (128, 8))),
    }
    in_maps = []
    for core in range(8):
        b, p = core // 2, core % 2
        own, oth = OWN[p], OWN[1 - p]
        rows = []
        for j in range(8):
            rows.append(np.arange(oth[j] * 512, oth[j] * 512 + 512))
            rows.append(np.arange(own[j] * 512, own[j] * 512 + 512))
        rows = np.concatenate(rows)
        xpa = np.zeros((NT * 128, D), f32)
        xpa[:S] = x[b][rows]
        pos = np.zeros((NT * 128,), np.int32)
        pos[:S] = positions[b][rows]
        hm = np.ones((256,), f32)
        for j in range(8):
            if own[j] == 0:
                hm[j * 32:(j + 1) * 32] = 0.0
            else:
                hr = np.arange(own[j] * 512 - 32, own[j] * 512)
                xpa[S + j * 32:S + (j + 1) * 32] = x[b][hr]
                pos[S + j * 32:S + (j + 1) * 32] = positions[b][hr]
        of = np.array([0.0 if oth[j] < own[j] else NEG for j in range(8)], f32)
        m = dict(shared)
        m["xp"] = xpa
        m["posp"] = np.ascontiguousarray(pos.reshape(NT, 128).T)
        m["oflag"] = np.ascontiguousarray(np.broadcast_to(of[None, :], (128, 8)))
        m["hmask"] = np.ascontiguousarray(np.broadcast_to(hm[None, :], (128, 256)))
        m["cT"] = col(c[b], 8)
        in_maps.append(m)
    return in_maps


def kernel(**inputs):
    in_maps = _layout_inputs(**inputs)
    if "nc" not in _NC_CACHE:
        _NC_CACHE["nc"] = build_program()
    nc = _NC_CACHE["nc"]
    res = run_bass_kernel_spmd(nc, in_maps, core_ids=list(range(8)))
    outf = np.zeros((4, S, D), np.float32)
    for core in range(8):
        b, p = core // 2, core % 2
        o = res.results[core]["out"]
        for j, ch in enumerate(OWN[p]):
            outf[b, ch * 512:(ch + 1) * 512] = o[j * 512:(j + 1) * 512]
    if DEBUG:
        kernel.debug = res.results
    return outf
```

```python
import numpy as np
import concourse.bass as bass
import concourse.mybir as mybir
from concourse.bass_utils import run_bass_kernel_spmd

F32 = mybir.dt.float32
BF16 = mybir.dt.bfloat16
I32 = mybir.dt.int32
U8 = mybir.dt.uint8
ALU = mybir.AluOpType
AF = mybir.ActivationFunctionType
AX = mybir.AxisListType

D = 1024
S = 8192
NT = 66
NEG = -30000.0
EPS = 1e-6
NBIS = 9
BR = 6.0
JA = 2560
OWN = ([0, 3, 4, 7, 8, 11, 12, 15], [1, 2, 5, 6, 9, 10, 13, 14])
DEBUG = False


class T:
    __slots__ = ("w", "r")

    def __init__(self):
        self.w = {}
        self.r = {}


class Eng:
    def __init__(self, obj, sem, key):
        self.obj = obj
        self.sem = sem
        self.key = key
        self.cnt = 0
        self.seen = {}


class K:
    def __init__(self, nc, sems):
        self.nc = nc
        it = iter(sems)
        self.pe = Eng(nc.tensor, next(it), "pe")
        self.act = Eng(nc.scalar, next(it), "act")
        self.dve = Eng(nc.vector, next(it), "dve")
        self.pool = Eng(nc.gpsimd, next(it), "pool")
        self.sp = Eng(nc.sync, next(it), "sp")
        self.engs = [self.pe, self.act, self.dve, self.pool, self.sp]
        self.dsems = {"sp": [[s, 0] for s in [next(it) for _ in range(8)]],
                      "pool": [[s, 0] for s in [next(it) for _ in range(8)]]}
        self.dptr = {"sp": 0, "pool": 0}

    def _waits(self, eng, rd, wr):
        need = {}

        def add(d, skip_self):
            for k, (s, v) in d.items():
                if skip_self and k == eng.key:
                    continue
                if k not in need or need[k][1] < v:
                    need[k] = (s, v)
        for t in rd:
            add(t.w, False)
        skip = (eng.key == "pe")
        for t in wr:
            add(t.w, skip)
            add(t.r, skip)
        for k, (s, v) in need.items():
            if eng.seen.get(k, 0) < v:
                eng.obj.wait_ge(s, v)
                eng.seen[k] = v

    def op(self, eng, fn, rd=(), wr=()):
        self._waits(eng, rd, wr)
        inst = fn(eng.obj)
        eng.cnt += 1
        inst.then_inc(eng.sem, 1)
        tok = (eng.sem, eng.cnt)
        for t in rd:
            t.r[eng.key] = tok
        for t in wr:
            t.w = {eng.key: tok}
            t.r = {}

    def dma(self, eng, out, in_, rd=(), wr=()):
        ring = self.dsems[eng.key]
        i = self.dptr[eng.key]
        self.dptr[eng.key] = (i + 1) % len(ring)
        sem, val = ring[i]
        key = "d%s%d" % (eng.key, i)
        self._waits(eng, rd, wr)
        if val > 0 and eng.seen.get(key, 0) < val:
            eng.obj.wait_ge(sem, val)
            eng.seen[key] = val
        eng.obj.dma_start(out=out, in_=in_).then_inc(sem, 16)
        ring[i][1] = val + 16
        tok = (sem, val + 16)
        for t in rd:
            t.r[key] = tok
        for t in wr:
            t.w = {key: tok}
            t.r = {}

    def barrier(self):
        for e in self.engs:
            for f in self.engs:
                if f is not e and f.cnt > 0 and e.seen.get(f.key, 0) < f.cnt:
                    e.obj.wait_ge(f.sem, f.cnt)
                    e.seen[f.key] = f.cnt
            for qk, ring in self.dsems.items():
                for i, (s, v) in enumerate(ring):
                    key = "d%s%d" % (qk, i)
                    if v > 0 and e.seen.get(key, 0) < v:
                        e.obj.wait_ge(s, v)
                        e.seen[key] = v


class _Stop(Exception):
    pass


def build_program(stage=None, dumps=(), nchunks=8, nt1=64):
    nc = bass.Bass("TRN2", target_bir_lowering=False)
    dt = nc.dram_tensor
    xp = dt("xp", [NT * 128, D], F32, kind="ExternalInput").ap()
    posp = dt("posp", [128, NT], I32, kind="ExternalInput").ap()
    oflag = dt("oflag", [128, 8], F32, kind="ExternalInput").ap()
    hmask = dt("hmask", [128, 256], F32, kind="ExternalInput").ap()
    invf = dt("invf", [128, 8], F32, kind="ExternalInput").ap()
    cT = dt("cT", [128, 8], F32, kind="ExternalInput").ap()
    w_ada = dt("w_ada", [D, 6 * D], F32, kind="ExternalInput").ap()
    badac = dt("badac", [128, 48], F32, kind="ExternalInput").ap()
    badar = dt("badar", [1, 6 * D], F32, kind="ExternalInput").ap()
    gmixc = dt("gmixc", [128, 8], F32, kind="ExternalInput").ap()
    gmlpc = dt("gmlpc", [128, 8], F32, kind="ExternalInput").ap()
    w_in = dt("w_in", [D, 2376], F32, kind="ExternalInput").ap()
    convw = dt("convw", [128, 4, 31], F32, kind="ExternalInput").ap()
    convb = dt("convb", [128, 4], F32, kind="ExternalInput").ap()
    cng = dt("cng", [128, 4], F32, kind="ExternalInput").ap()
    cnb = dt("cnb", [128, 4], F32, kind="ExternalInput").ap()
    w_out = dt("w_out", [D, D], F32, kind="ExternalInput").ap()
    w_up = dt("w_up", [D, 4 * D], F32, kind="ExternalInput").ap()
    w_down = dt("w_down", [4 * D, D], F32, kind="ExternalInput").ap()
    gfb = dt("gfb", [128, D], F32, kind="ExternalInput").ap()
    out = dt("out", [4096, D], F32, kind="ExternalOutput").ap()
    x1s = dt("x1s", [4096, D], F32).ap()
    g1s = dt("g1s", [128, D], F32).ap()
    g2s = dt("g2s", [128, D], F32).ap()
    if "x1s" in dumps:
        x1s = dt("dbg_x1s", [4096, D], F32, kind="ExternalOutput").ap()

    def wview(w, c0, c1):
        return w[:, c0:c1].rearrange("(k p) e -> p k e", p=128)

    import contextlib
    dump_aps = {}

    def dump(name, ap, tiles, kbref):
        if name not in dumps:
            return
        shp = [int(v) for v in ap.shape]
        d_ap = dt("dbg_" + name, shp, ap.dtype, kind="ExternalOutput").ap()
        kbref.dma(kbref.sp, d_ap, ap, rd=tiles)

    def stop_if(st, kbref):
        if stage == st:
            kbref.barrier()
            raise _Stop()

    def _body():
        with contextlib.ExitStack() as es:
            sems = [es.enter_context(nc.semaphore("s%d" % i)) for i in range(21)]
            kb = K(nc, sems)
            pe, act, dve, pool, sp = kb.pe, kb.act, kb.dve, kb.pool, kb.sp
            op, dma = kb.op, kb.dma

            def sb(name, shape, dtype=F32):
                return es2.enter_context(nc.sbuf_tensor(name, shape, dtype))

            ps = es.enter_context(nc.psum_tensor("ps", [128, 8, 512], F32))
            PB = [T() for _ in range(8)]

            def psb16(b):
                return ps[:, b, :].bitcast(BF16)

            es2 = es
            ident = sb("ident", [128, 128], BF16); t_ident = T()
            ident4 = sb("ident4", [128, 4, 128], BF16)
            identf = sb("identf", [128, 128], F32)
            trim = sb("trim", [128, 128], F32)
            onesm = sb("onesm", [128, 128], BF16)
            onesr = sb("onesr", [1, 128], F32)
            cosT = sb("cosT", [128, NT, 8], F32)
            sinT = sb("sinT", [128, NT, 8], F32); t_cs = T()
            modc = sb("modc", [128, 48], F32); t_modc = T()
            ab = sb("ab", [128, 4, 8], F32); t_ab = T()
            t_G1 = T(); t_G2 = T()
            oflg = sb("oflg", [128, 8], F32); t_small = T()
            cw = sb("cw", [128, 4, 31], F32)
            cb = sb("cb", [128, 4], F32)
            cg = sb("cg", [128, 4], F32)
            cbn = sb("cbn", [128, 4], F32)
            wst = {"slots": None, "tiles": None, "ptr": 0}

            def walloc(tag):
                wst["slots"] = [sb("wslot%s%d" % (tag, i), [128, 8, 512], BF16) for i in range(2)]
                wst["tiles"] = [T() for _ in range(2)]
                wst["ptr"] = 0

            def wload(src_ap):
                i = wst["ptr"]
                wst["ptr"] = (i + 1) % 2
                dma(pool, wst["slots"][i][:], src_ap, wr=[wst["tiles"][i]])
                return wst["slots"][i], wst["tiles"][i]

            op(pool, lambda e: e.memset(identf[:], 0.0), wr=[t_ident])
            op(pool, lambda e: e.affine_select(out=identf[:], in_=identf[:], pattern=[[-1, 128]],
                                               compare_op=ALU.not_equal, fill=1.0, base=0,
                                               channel_multiplier=1), rd=[t_ident], wr=[t_ident])
            op(pool, lambda e: e.tensor_copy(out=ident[:], in_=identf[:]), rd=[t_ident], wr=[t_ident])
            op(pool, lambda e: e.tensor_copy(out=ident4[:], in_=identf[:].unsqueeze(1).to_broadcast([128, 4, 128])),
               rd=[t_ident], wr=[t_ident])
            op(pool, lambda e: e.memset(trim[:], 0.0), wr=[t_ident])
            op(pool, lambda e: e.affine_select(out=trim[:], in_=trim[:], pattern=[[-1, 128]],
                                               compare_op=ALU.is_ge, fill=NEG, base=0,
                                               channel_multiplier=1), rd=[t_ident], wr=[t_ident])
            op(pool, lambda e: e.memset(onesm[:], 1.0 / 512.0), wr=[t_ident])
            op(pool, lambda e: e.memset(onesr[:], 1.0), wr=[t_ident])
            dma(sp, oflg[:], oflag, wr=[t_small])
            dma(sp, cw[:], convw, wr=[t_small])
            dma(sp, cb[:], convb, wr=[t_small])
            dma(sp, cg[:], cng, wr=[t_small])
            dma(sp, cbn[:], cnb, wr=[t_small])

            with contextlib.ExitStack() as es2:
                posi = sb("posi", [128, NT], I32)
                posf = sb("posf", [128, NT], F32)
                ivf = sb("ivf", [128, 8], F32)
                ang = sb("ang", [128, NT, 8], F32)
                tq = sb("tq", [128, NT, 8], F32)
                kq = sb("kq", [128, NT, 8], I32)
                kf = sb("kf", [128, NT, 8], F32)
                red = sb("red", [128, NT, 8], F32)
                t_p0 = T()
                cTs = sb("cTs", [128, 8], F32)
                cond = sb("cond", [128, 8], F32); t_cond = T()
                badc = sb("badc", [128, 48], F32)
                gmc = sb("gmc", [128, 2, 8], F32)
                rowb = sb("rowb", [1, 6144], F32)
                rows2 = [sb("rows%d" % i, [1, 512], F32) for i in range(2)]; t_rows2 = [T(), T()]
                Gtmp = sb("Gtmp", [128, 512], F32); t_Gtmp = T()
                wfs = [sb("wf%d" % i, [128, 8, 512], F32) for i in range(3)]; t_wfs = [T() for _ in range(3)]
                dma(sp, posi[:], posp, wr=[t_p0])
                dma(sp, ivf[:], invf, wr=[t_p0])
                dma(sp, cTs[:], cT, wr=[t_cond])
                dma(sp, badc[:], badac, wr=[t_cond])
                dma(sp, gmc[:, 0, :], gmixc, wr=[t_cond])
                dma(sp, gmc[:, 1, :], gmlpc, wr=[t_cond])
                dma(sp, rowb[:], badar, wr=[t_cond])
                rw = dict(rd=[t_p0], wr=[t_p0])
                op(dve, lambda e: e.tensor_copy(out=posf[:], in_=posi[:]), **rw)
                op(dve, lambda e: e.tensor_tensor(out=ang[:], in0=posf[:].unsqueeze(2).to_broadcast([128, NT, 8]),
                                                  in1=ivf[:].unsqueeze(1).to_broadcast([128, NT, 8]), op=ALU.mult), **rw)
                TWO_PI = 2.0 * np.pi
                C1 = 6.28125
                C2 = TWO_PI - C1

                def reduce_to(dst, shift):
                    op(dve, lambda e: e.tensor_scalar(out=tq[:], in0=ang[:], scalar1=shift, scalar2=1.0 / TWO_PI,
                                                      op0=ALU.add, op1=ALU.mult), **rw)
                    op(dve, lambda e: e.tensor_copy(out=kq[:], in_=tq[:]), **rw)
                    op(dve, lambda e: e.tensor_copy(out=kf[:], in_=kq[:]), **rw)
                    op(dve, lambda e: e.scalar_tensor_tensor(out=red[:], in0=kf[:], scalar=-C1, in1=ang[:],
                                                             op0=ALU.mult, op1=ALU.add), **rw)
                    op(dve, lambda e: e.scalar_tensor_tensor(out=red[:], in0=kf[:], scalar=-C2, in1=red[:],
                                                             op0=ALU.mult, op1=ALU.add), **rw)
                    op(dve, lambda e: e.tensor_scalar(out=red[:], in0=red[:], scalar1=shift, scalar2=None,
                                                      op0=ALU.add), **rw)
                    op(dve, lambda e: e.tensor_scalar(out=tq[:], in0=red[:], scalar1=np.pi, scalar2=-TWO_PI,
                                                      op0=ALU.is_gt, op1=ALU.mult), **rw)
                    op(dve, lambda e: e.tensor_tensor(out=red[:], in0=red[:], in1=tq[:], op=ALU.add), **rw)
                    op(dve, lambda e: e.tensor_scalar(out=tq[:], in0=red[:], scalar1=-np.pi, scalar2=TWO_PI,
                                                      op0=ALU.is_lt, op1=ALU.mult), **rw)
                    op(dve, lambda e: e.tensor_tensor(out=red[:], in0=red[:], in1=tq[:], op=ALU.add), **rw)
                    op(dve, lambda e: e.tensor_scalar(out=red[:], in0=red[:], scalar1=-3.1415925, scalar2=3.1415925,
                                                      op0=ALU.max, op1=ALU.min), **rw)
                    op(act, lambda e: e.activation(out=dst[:], in_=red[:], func=AF.Sin), rd=[t_p0], wr=[t_cs])

                reduce_to(sinT, 0.0)
                reduce_to(cosT, np.pi / 2.0)

                op(act, lambda e: e.activation(out=cond[:], in_=cTs[:], func=AF.Silu), rd=[t_cond], wr=[t_cond])
                for cc in range(12):
                    ws, tw = wfs[cc % 3], t_wfs[cc % 3]
                    dma(sp, ws[:], wview(w_ada, cc * 512, (cc + 1) * 512), wr=[tw])
                    rb_, trb_ = rows2[cc % 2], t_rows2[cc % 2]
                    pbk = 1 + cc % 2
                    for k in range(8):
                        op(pe, lambda e, k=k: e.matmul(ps[0:1, pbk, :], lhsT=cond[:, k:k + 1], rhs=ws[:, k, :],
                                                       start=(k == 0), stop=(k == 7)),
                           rd=[t_cond, tw], wr=[PB[pbk]])
                    op(dve, lambda e: e.tensor_tensor(out=rb_[:], in0=ps[0:1, pbk, :], in1=rowb[:, cc * 512:(cc + 1) * 512],
                                                      op=ALU.add), rd=[PB[pbk], t_cond], wr=[trb_])
                    if cc in (4, 5, 10, 11):
                        op(pe, lambda e: e.matmul(ps[:, 3, :], lhsT=onesr[:], rhs=rb_[:], start=True, stop=True),
                           rd=[trb_, t_ident], wr=[PB[3]])
                        Gs, tG = (g1s, t_G1) if cc < 6 else (g2s, t_G2)
                        go = (cc - 4) * 512 if cc < 6 else (cc - 10) * 512
                        op(act, lambda e: e.activation(out=Gtmp[:], in_=ps[:, 3, :], func=AF.Copy),
                           rd=[PB[3]], wr=[t_Gtmp])
                        dma(sp, Gs[:, go:go + 512], Gtmp[:], rd=[t_Gtmp], wr=[tG])
                    else:
                        for el in range(4):
                            et = cc * 4 + el
                            op(pe, lambda e, el=el, et=et: e.matmul(ps[:, 0, et:et + 1], lhsT=rb_[0:1, el * 128:(el + 1) * 128],
                                                                   rhs=onesr[0:1, 0:1], start=True, stop=True, skip_group_check=True),
                               rd=[trb_, t_ident], wr=[PB[0]])
                op(dve, lambda e: e.memset(modc[:], 0.0), wr=[t_modc])
                for lo_, hi_ in ((0, 16), (24, 40)):
                    op(dve, lambda e: e.tensor_copy(out=modc[:, lo_:hi_], in_=ps[:, 0, lo_:hi_]),
                       rd=[PB[0]], wr=[t_modc])
                op(dve, lambda e: e.scalar_tensor_tensor(out=ab[:, 0, :], in0=modc[:, 8:16], scalar=1.0, in1=gmc[:, 0, :],
                                                         op0=ALU.add, op1=ALU.mult), rd=[t_modc, t_cond], wr=[t_ab])
                op(dve, lambda e: e.tensor_copy(out=ab[:, 1, :], in_=modc[:, 0:8]), rd=[t_modc], wr=[t_ab])
                op(dve, lambda e: e.scalar_tensor_tensor(out=ab[:, 2, :], in0=modc[:, 32:40], scalar=1.0, in1=gmc[:, 1, :],
                                                         op0=ALU.add, op1=ALU.mult), rd=[t_modc, t_cond], wr=[t_ab])
                op(dve, lambda e: e.tensor_copy(out=ab[:, 3, :], in_=modc[:, 24:32]), rd=[t_modc], wr=[t_ab])
                dump("cosT", cosT[:], [t_cs], kb)
                dump("sinT", sinT[:], [t_cs], kb)
                dump("ab", ab[:], [t_ab], kb)
                dump("modc", modc[:], [t_modc], kb)
                kb.barrier()
                stop_if("p0", kb)

            def norm_dma(row0, xt, t_xt, src=None):
                dma(sp, xt[:], (xp if src is None else src)[row0:row0 + 128, :], wr=[t_xt])

            def norm_stats(xt, t_xt, xn, t_xn, st, t_st):
                op(act, lambda e: e.activation(out=xn[:], in_=xt[:], func=AF.Square, accum_out=st[:, 0:1]),
                   rd=[t_xt], wr=[t_xn, t_st])
                op(act, lambda e: e.activation(out=st[:, 1:2], in_=st[:, 0:1], func=AF.Sqrt, bias=EPS, scale=1.0 / D),
                   rd=[t_st], wr=[t_st])
                op(dve, lambda e: e.reciprocal(out=st[:, 2:3], in_=st[:, 1:2]), rd=[t_st], wr=[t_st])

            def norm_scale(xt, t_xt, xn, t_xn, st, t_st):
                op(act, lambda e: e.activation(out=xn[:], in_=xt[:], func=AF.Copy, scale=st[:, 2:3]),
                   rd=[t_xt, t_st], wr=[t_xn])

            def norm_tile(row0, xt, t_xt, xn, t_xn, sq, t_sq, st, t_st, src=None):
                norm_dma(row0, xt, t_xt, src)
                norm_stats(xt, t_xt, xn, t_xn, st, t_st)
                norm_scale(xt, t_xt, xn, t_xn, st, t_st)

            def transpose_mod(xn, t_xn, bank, hT_dst, t_hT, abi):
                pv = psb16(bank)
                for k in range(8):
                    op(pe, lambda e, k=k: e.transpose(out=pv[:, k * 128:(k + 1) * 128], in_=xn[:, k * 128:(k + 1) * 128],
                                                      identity=ident[:]), rd=[t_xn, t_ident], wr=[PB[bank]])
                pv3 = pv.rearrange("p (k t) -> p k t", k=8)
                op(dve, lambda e: e.tensor_tensor(out=hT_dst, in0=pv3,
                                                  in1=ab[:, abi, :].unsqueeze(2).to_broadcast([128, 8, 128]), op=ALU.mult),
                   rd=[PB[bank], t_ab], wr=[t_hT])
                op(pool, lambda e: e.tensor_tensor(out=hT_dst, in0=hT_dst,
                                                   in1=ab[:, abi + 1, :].unsqueeze(2).to_broadcast([128, 8, 128]), op=ALU.add),
                   rd=[t_hT, t_ab], wr=[t_hT])

            def transpose_only(xn, t_xn, bank):
                pv = psb16(bank)
                for k in range(8):
                    op(pe, lambda e, k=k: e.transpose(out=pv[:, k * 128:(k + 1) * 128], in_=xn[:, k * 128:(k + 1) * 128],
                                                      identity=ident[:]), rd=[t_xn, t_ident], wr=[PB[bank]])

            def mod_only(bank, hT_dst, t_hT, abi):
                pv3 = psb16(bank).rearrange("p (k t) -> p k t", k=8)
                op(dve, lambda e: e.tensor_tensor(out=hT_dst, in0=pv3,
                                                  in1=ab[:, abi, :].unsqueeze(2).to_broadcast([128, 8, 128]), op=ALU.mult),
                   rd=[PB[bank], t_ab], wr=[t_hT])
                op(pool, lambda e: e.tensor_tensor(out=hT_dst, in0=hT_dst,
                                                   in1=ab[:, abi + 1, :].unsqueeze(2).to_broadcast([128, 8, 128]), op=ALU.add),
                   rd=[t_hT, t_ab], wr=[t_hT])

            with contextlib.ExitStack() as es2:
                kT = sb("kT", [128, S], BF16); t_kT = T()
                kiT = sb("kiT", [128, S], BF16); t_kiT = T()
                Vaug = sb("Vaug", [128, 64, 2, 65], BF16); t_V = T()
                W1 = sb("W1", [128, 8, 328], BF16); t_W1 = T()
                xts = [sb("xt%d" % i, [128, D], F32) for i in range(2)]; t_xts = [T(), T()]
                xns = [sb("xn%d" % i, [128, D], BF16) for i in range(2)]; t_xns = [T(), T()]
                sqj = None; t_sqj = None
                walloc("b")
                G1 = sb("G1", [128, D], F32)
                dma(sp, G1[:], g1s, rd=[t_G1], wr=[t_G1])
                sts = [sb("st%d" % i, [128, 4], F32) for i in range(2)]; t_sts = [T(), T()]
                hTc = sb("hTc", [128, 8, 512], BF16); t_hTc = [T() for _ in range(4)]
                rtmp = sb("rtmp", [128, 4, 16, 8], F32); t_rtmp = T(); t_rt4 = [T() for _ in range(4)]
                krot = [sb("krot%d" % i, [128, 256], BF16) for i in range(2)]; t_krot = [T(), T()]

                op(pool, lambda e: e.memset(Vaug[:], 1.0), wr=[t_V])
                dma(pool, W1[:, :, 0:128], wview(w_in, 512, 640), wr=[t_W1])
                dma(pool, W1[:, :, 128:192], wview(w_in, 1280, 1344), wr=[t_W1])
                dma(pool, W1[:, :, 192:320], wview(w_in, 640, 768), wr=[t_W1])
                dma(pool, W1[:, :, 320:328], wview(w_in, 1344, 1352), wr=[t_W1])

                def rope(src3, dst3, nh, ti, tsrc, tdst):
                    cs = cosT[:, ti, :].unsqueeze(1).to_broadcast([128, nh, 8])
                    sn = sinT[:, ti, :].unsqueeze(1).to_broadcast([128, nh, 8])
                    x1, x2 = src3[:, :, 0:8], src3[:, :, 8:16]
                    t1, t2, t3, t4 = (rtmp[:, i, 0:nh, :] for i in range(4))
                    op(dve, lambda e: e.tensor_tensor(out=t1, in0=x1, in1=cs, op=ALU.mult), rd=[tsrc, t_cs], wr=[t_rt4[0]])
                    op(dve, lambda e: e.tensor_tensor(out=t2, in0=x2, in1=sn, op=ALU.mult), rd=[tsrc, t_cs], wr=[t_rt4[1]])
                    op(dve, lambda e: e.tensor_tensor(out=t3, in0=x2, in1=cs, op=ALU.mult), rd=[tsrc, t_cs], wr=[t_rt4[2]])
                    op(dve, lambda e: e.tensor_tensor(out=t4, in0=x1, in1=sn, op=ALU.mult), rd=[tsrc, t_cs], wr=[t_rt4[3]])
                    op(dve, lambda e: e.tensor_tensor(out=dst3[:, :, 0:8], in0=t1, in1=t2, op=ALU.subtract),
                       rd=[t_rt4[0], t_rt4[1]], wr=[tdst])
                    op(dve, lambda e: e.tensor_tensor(out=dst3[:, :, 8:16], in0=t3, in1=t4, op=ALU.add),
                       rd=[t_rt4[2], t_rt4[3]], wr=[tdst])
                    op(act, lambda e: e.activation(out=dst3[:, :, 16:64], in_=src3[:, :, 16:64], func=AF.Copy),
                       rd=[tsrc], wr=[tdst])

                def rope4(src4, dst4, ti, tsrc, tdst):
                    cs = cosT[:, ti, :].unsqueeze(1).unsqueeze(1).to_broadcast([128, 2, 4, 8])
                    sn = sinT[:, ti, :].unsqueeze(1).unsqueeze(1).to_broadcast([128, 2, 4, 8])
                    x1, x2 = src4[:, :, :, 0:8], src4[:, :, :, 8:16]
                    t1, t2, t3, t4 = (rtmp[:, i, 0:8, :].rearrange("p (g b) d -> p g b d", g=2) for i in range(4))
                    op(dve, lambda e: e.tensor_tensor(out=t1, in0=x1, in1=cs, op=ALU.mult), rd=[tsrc, t_cs], wr=[t_rt4[0]])
                    op(dve, lambda e: e.tensor_tensor(out=t2, in0=x2, in1=sn, op=ALU.mult), rd=[tsrc, t_cs], wr=[t_rt4[1]])
                    op(dve, lambda e: e.tensor_tensor(out=t3, in0=x2, in1=cs, op=ALU.mult), rd=[tsrc, t_cs], wr=[t_rt4[2]])
                    op(dve, lambda e: e.tensor_tensor(out=t4, in0=x1, in1=sn, op=ALU.mult), rd=[tsrc, t_cs], wr=[t_rt4[3]])
                    op(dve, lambda e: e.tensor_tensor(out=dst4[:, :, :, 0:8], in0=t1, in1=t2, op=ALU.subtract),
                       rd=[t_rt4[0], t_rt4[1]], wr=[tdst])
                    op(dve, lambda e: e.tensor_tensor(out=dst4[:, :, :, 8:16], in0=t3, in1=t4, op=ALU.add),
                       rd=[t_rt4[2], t_rt4[3]], wr=[tdst])
                    op(act, lambda e: e.activation(out=dst4[:, :, :, 16:64], in_=src4[:, :, :, 16:64], func=AF.Copy),
                       rd=[tsrc], wr=[tdst])

                def ph1_S1(ti):
                    s2 = ti % 2
                    norm_tile(ti * 128, xts[s2], t_xts[s2], xns[s2], t_xns[s2], sqj, t_sqj, sts[s2], t_sts[s2])
                    hs = ti % 4
                    hdst = hTc[:, :, hs * 128:(hs + 1) * 128]
                    transpose_mod(xns[s2], t_xns[s2], 6 + s2, hdst, t_hTc[hs], 0)

                def ph1_S2(ti):
                    s2 = ti % 2
                    hs = ti % 4
                    bk = s2
                    for k in range(8):
                        op(pe, lambda e, k=k: e.matmul(ps[:, bk, 0:320], lhsT=hTc[:, k, hs * 128:(hs + 1) * 128],
                                                       rhs=W1[:, k, 0:320], start=(k == 0), stop=(k == 7)),
                           rd=[t_hTc[hs], t_W1], wr=[PB[bk]])
                    kr = krot[s2]
                    rope(ps[:, bk, 0:192].rearrange("p (h d) -> p h d", d=64),
                         kr[:, 0:192].rearrange("p (h d) -> p h d", d=64), 3, ti, PB[bk], t_krot[s2])
                    op(pool, lambda e: e.tensor_copy(out=kr[:, 192:256], in_=kr[:, 128:192]), rd=[t_krot[s2]], wr=[t_krot[s2]])
                    op(act, lambda e: e.activation(out=Vaug[:, ti, :, 0:64],
                                                   in_=ps[:, bk, 192:320].rearrange("p (g d) -> p g d", d=64), func=AF.Copy),
                       rd=[PB[bk]], wr=[t_V])
                    tb = 4 + s2
                    pv = psb16(tb)
                    op(pe, lambda e: e.transpose(out=pv[:, 0:128], in_=kr[:, 0:128], identity=ident[:]),
                       rd=[t_krot[s2], t_ident], wr=[PB[tb]])
                    op(pe, lambda e: e.transpose(out=pv[:, 128:256], in_=kr[:, 128:256], identity=ident[:]),
                       rd=[t_krot[s2], t_ident], wr=[PB[tb]])
                    op(act, lambda e: e.activation(out=kT[:, ti * 128:(ti + 1) * 128], in_=pv[:, 0:128], func=AF.Copy),
                       rd=[PB[tb]], wr=[t_kT])
                    op(dve, lambda e: e.tensor_copy(out=kiT[:, ti * 128:(ti + 1) * 128], in_=pv[:, 128:256]),
                       rd=[PB[tb]], wr=[t_kiT])


                ph1_S1(0)
                for ti in range(nt1):
                    if ti + 1 < nt1:
                        ph1_S1(ti + 1)
                    ph1_S2(ti)
                dump("kT", kT[:], [t_kT], kb)
                dump("kiT", kiT[:], [t_kiT], kb)
                dump("Vaug", Vaug[:], [t_V], kb)
                stop_if("p1", kb)
                SC = sb("SC", [128, S], F32); t_SC = T()
                junk = hTc[:].rearrange("p k t -> p (k t)").bitcast(U8)
                RbA = sb("RbA", [128, 8, 512], BF16)
                Rb = [RbA[:, i, :] for i in range(8)]; t_Rb = [T() for _ in range(8)]
                Dg = sb("Dg", [128, 8, 128], BF16); t_Dg = T()
                qT = sb("qT", [128, 2, 4, 512], BF16); t_qT = T()
                qiT = sb("qiT", [128, 4, 2, 512], BF16); t_qiT = T()
                op(pool, lambda e: e.memset(qT[:], 0.0), wr=[t_qT])
                op(pool, lambda e: e.memset(qiT[:], 0.0), wr=[t_qiT])
                qrot = [sb("qrot%d" % i, [128, 512], BF16) for i in range(2)]; t_qrot = [T(), T()]
                wsc = sb("wsc", [128, 4, 8], F32); t_wsc = T()
                PT = [sb("PT%d" % i, [128, 512], BF16) for i in range(4)]; t_PT = [T() for _ in range(4)]
                MB = sb("MB", [128, S], BF16); t_MB = T()
                junkA = sb("junkA", [128, JA], U8); t_junkA = T()
                bsa = sb("bsa", [128, 2], F32); t_bsa = T()
                bst = sb("bst", [128, 8], F32); t_bst = T()
                gluT = sb("gluT", [128, 4, 544], BF16); t_glu = T()
                gluH = sb("gluH", [128, 4, 256], BF16); t_gluH = T()
                SCb = SC[:].bitcast(BF16)
                ybf = SCb[:, 0:2048].rearrange("p (c t) -> p c t", c=4); t_ybf = t_SC
                ysq = SCb[:, 2048:4096].rearrange("p (c t) -> p c t", c=4); t_ysq = t_SC
                lnA = SC[:, 2048:2560]; t_lnA = t_SC
                lnB = SC[:, 2560:3072]; t_lnB = t_SC
                zn = SC[:, 3072:3584]; t_zn = t_SC
                sig = SC[:, 3584:4096]; t_sig = t_SC
                RbF = RbA[:].rearrange("p a b -> p (a b)")
                cdh = [RbF[:, 0:2048].rearrange("p (k c) -> p k c", c=128), RbF[:, 2048:3968].rearrange("p (k c) -> p k c", c=128)]
                t_cdh = [t_Rb[0:4], t_Rb[4:8]]
                mixT = sb("mixT", [128, 8, 512], BF16); t_mixT = T()
                attn = sb("attn", [128, 512], BF16); t_attn = T()
                rs4 = sb("rs4", [128, 8], F32); t_rs4 = T()
                x1t = SC[:, 4096:5120]; t_x1t = t_SC
                hmB = sb("hmB", [128, 256], BF16)
                hm = sb("hm", [128, 256], F32)
                dma(sp, hm[:], hmask, wr=[t_small])
                op(pool, lambda e: e.tensor_copy(out=hmB[:], in_=hm[:]), rd=[t_small], wr=[t_small])

                def conv_glu_mm(ws_a, tw_a, ws_g, tw_g, ncols, ct, hcols, t_h):
                    b0 = (ct % 2) * 2
                    for k in range(8):
                        op(pe, lambda e, k=k: e.matmul(ps[:, b0, 0:ncols], lhsT=ws_a[:, k, ct * 128:(ct + 1) * 128],
                                                       rhs=hTc[:, k, hcols], start=(k == 0), stop=(k == 7)),
                           rd=t_h + [tw_a], wr=[PB[b0]])
                    for k in range(8):
                        op(pe, lambda e, k=k: e.matmul(ps[:, b0 + 1, 0:ncols], lhsT=ws_g[:, k, ct * 128:(ct + 1) * 128],
                                                       rhs=hTc[:, k, hcols], start=(k == 0), stop=(k == 7)),
                           rd=t_h + [tw_g], wr=[PB[b0 + 1]])

                def conv_glu_ev(ncols, ct, dst, tdst):
                    b0 = (ct % 2) * 2
                    op(act, lambda e: e.activation(out=sig[:, 0:ncols], in_=ps[:, b0 + 1, 0:ncols], func=AF.Sigmoid),
                       rd=[PB[b0 + 1]], wr=[t_sig])
                    op(dve, lambda e: e.tensor_tensor(out=dst, in0=ps[:, b0, 0:ncols], in1=sig[:, 0:ncols], op=ALU.mult),
                       rd=[PB[b0], t_sig], wr=[tdst])

                def conv_glu(ws_a, tw_a, ws_g, tw_g, ncols, ct, dst, tdst, hcols, t_h):
                    conv_glu_mm(ws_a, tw_a, ws_g, tw_g, ncols, ct, hcols, t_h)
                    conv_glu_ev(ncols, ct, dst, tdst)

                for hi in range(2):
                    norm_tile((64 + hi) * 128, xts[hi], t_xts[hi], xns[hi], t_xns[hi], sqj, t_sqj, sts[hi], t_sts[hi])
                    transpose_mod(xns[hi], t_xns[hi], 6 + hi, hTc[:, :, hi * 128:(hi + 1) * 128], t_hTc[hi], 0)
                wa, twa = wload(wview(w_in, 1352, 1864))
                wg, twg = wload(wview(w_in, 1864, 2376))
                for ct in range(4):
                    conv_glu(wa, twa, wg, twg, 256, ct, gluH[:, ct, :], t_gluH, slice(0, 256), [t_hTc[0], t_hTc[1]])
                    op(pool, lambda e, ct=ct: e.tensor_tensor(out=gluH[:, ct, :], in0=gluH[:, ct, :], in1=hmB[:], op=ALU.mult),
                       rd=[t_gluH, t_small], wr=[t_gluH])

                dump("gluH", gluH[:], [t_gluH], kb)
                stop_if("p2h", kb)
                for j in range(nchunks):
                    tile0 = (2 * j + 1) * 4
                    def norm_stages(jn):
                        tl0 = (2 * jn + 1) * 4

                        def nD(t4):
                            s2 = t4 % 2
                            norm_dma((tl0 + t4) * 128, xts[s2], t_xts[s2])

                        def nS(t4):
                            s2 = t4 % 2
                            norm_stats(xts[s2], t_xts[s2], xns[s2], t_xns[s2], sts[s2], t_sts[s2])

                        def nC(t4):
                            s2 = t4 % 2
                            norm_scale(xts[s2], t_xts[s2], xns[s2], t_xns[s2], sts[s2], t_sts[s2])

                        def nT(t4):
                            s2 = t4 % 2
                            transpose_only(xns[s2], t_xns[s2], 6 + s2)

                        def nM(t4):
                            mod_only(6 + t4 % 2, hTc[:, :, t4 * 128:(t4 + 1) * 128], t_hTc[t4], 0)

                        order = [(nD, 0), (nD, 1), (nS, 0), (nS, 1), (nC, 0), (nC, 1), (nT, 0), (nD, 2), (nM, 0), (nT, 1),
                                 (nS, 2), (nD, 3), (nM, 1), (nC, 2), (nS, 3), (nT, 2), (nC, 3), (nM, 2), (nT, 3), (nM, 3)]
                        for fn, t4 in order:
                            fn(t4)
                            yield

                    if j == 0:
                        for _ in norm_stages(0):
                            pass
                    wqi, twqi = wload(wview(w_in, 768, 1280))
                    wq_box = {}

                    def u_mm(grp, t4, bk):
                        wq, twq = wq_box["w"] if grp == 0 else (wqi, twqi)
                        for k in range(8):
                            op(pe, lambda e, k=k: e.matmul(ps[:, bk, :], lhsT=hTc[:, k, t4 * 128:(t4 + 1) * 128],
                                                           rhs=wq[:, k, :], start=(k == 0), stop=(k == 7)),
                               rd=[t_hTc[t4], twq], wr=[PB[bk]])
                        if grp == 1:
                            for k in range(8):
                                op(pe, lambda e, k=k: e.matmul(ps[:, 2, 0:8], lhsT=hTc[:, k, t4 * 128:(t4 + 1) * 128],
                                                               rhs=W1[:, k, 320:328], start=(k == 0), stop=(k == 7)),
                                   rd=[t_hTc[t4], t_W1], wr=[PB[2]])
                            op(dve, lambda e: e.tensor_scalar(
                                out=wsc[:, t4, :].rearrange("p (b g) -> p g b", g=2),
                                in0=ps[:, 2, 0:8].rearrange("p (g b) -> p g b", g=2),
                                scalar1=float(8 ** -0.5 * 64 ** -0.5), scalar2=None, op0=ALU.mult),
                               rd=[PB[2]], wr=[t_wsc])

                    def u_rope(grp, t4, bk):
                        qr = qrot[bk]
                        src4 = ps[:, bk, :].rearrange("p (g b d) -> p g b d", g=2, b=4)
                        dst4 = qr[:].rearrange("p (b g d) -> p g b d", g=2, b=4)
                        rope4(src4, dst4, tile0 + t4, PB[bk], t_qrot[bk])

                    def u_trP(grp, t4, bk):
                        qr = qrot[bk]
                        tb = 4 + bk
                        pv = psb16(tb)
                        for b in range(4):
                            op(pe, lambda e, b=b: e.transpose(out=pv[:, b * 128:(b + 1) * 128],
                                                              in_=qr[:, b * 128:(b + 1) * 128], identity=ident[:]),
                               rd=[t_qrot[bk], t_ident], wr=[PB[tb]])

                    def u_trC(grp, t4, bk):
                        t_dstT = t_qT if grp == 0 else t_qiT
                        tb = 4 + bk
                        pv = psb16(tb)
                        pv4 = pv[:, 0:512].rearrange("p (b t) -> p b t", b=4)
                        tcols = slice(t4 * 128, (t4 + 1) * 128)
                        if grp == 0:
                            d0, d1 = qT[0:64, 0, :, tcols], qT[64:128, 1, :, tcols]
                        else:
                            d0, d1 = qiT[0:64, :, 0, tcols], qiT[64:128, :, 1, tcols]
                        op(act, lambda e: e.activation(out=d0, in_=pv4[0:64], func=AF.Copy), rd=[PB[tb]], wr=[t_dstT])
                        if grp == 0:
                            op(act, lambda e: e.activation(out=d1, in_=pv4[64:128], func=AF.Copy), rd=[PB[tb]], wr=[t_dstT])
                        else:
                            op(dve, lambda e: e.tensor_copy(out=d1, in_=pv4[64:128]), rd=[PB[tb]], wr=[t_dstT])

                    def units_gen(grp):
                        order = [("mm", 0), ("mm", 1), ("rope", 0), ("trP", 0), ("mm", 2), ("rope", 1), ("trC", 0), ("trP", 1),
                                 ("mm", 3), ("rope", 2), ("trC", 1), ("trP", 2), ("rope", 3), ("trC", 2), ("trP", 3), ("trC", 3)]
                        fns = {"mm": u_mm, "rope": u_rope, "trP": u_trP, "trC": u_trC}
                        for kind, t4 in order:
                            fns[kind](grp, t4, t4 % 2)
                            yield

                    for _ in units_gen(1):
                        pass
                    q_units = units_gen(0)
                    wa, twa = wload(wview(w_in, 1352, 1864))
                    wg, twg = wload(wview(w_in, 1864, 2376))
                    op(pool, lambda e: e.tensor_copy(out=gluT[:, :, 0:32], in_=gluH[:, :, j * 32:(j + 1) * 32]),
                       rd=[t_gluH], wr=[t_glu])
                    conv_glu_mm(wa, twa, wg, twg, 512, 0, slice(0, 512), t_hTc)
                    for ct in range(4):
                        if ct + 1 < 4:
                            conv_glu_mm(wa, twa, wg, twg, 512, ct + 1, slice(0, 512), t_hTc)
                        conv_glu_ev(512, ct, gluT[:, ct, 32:544], t_glu)
                    for ct in range(4):
                        cbk = ct
                        for hf, (t_lo, t_hi) in enumerate(((0, 16), (16, 31))):
                            nt_ = t_hi - t_lo
                            op(pool, lambda e, ct=ct: e.tensor_tensor(
                                out=cdh[hf], in0=ident[:].unsqueeze(1).to_broadcast([128, nt_, 128]),
                                in1=cw[:, ct, t_lo:t_hi].unsqueeze(2).to_broadcast([128, nt_, 128]), op=ALU.mult),
                               rd=[t_ident, t_small], wr=t_cdh[hf])
                        for tap in range(31):
                            hf, tl = (0, tap) if tap < 16 else (1, tap - 16)
                            op(pe, lambda e, tap=tap, ct=ct: e.matmul(ps[:, cbk, :], lhsT=cdh[hf][:, tl, :],
                                                                     rhs=gluT[:, ct, tap + 2:tap + 514],
                                                                     start=(tap == 0), stop=(tap == 30)),
                               rd=t_cdh[hf] + [t_glu], wr=[PB[cbk]])
                        op(act, lambda e, ct=ct: e.activation(out=ybf[:, ct, :], in_=ps[:, cbk, :], func=AF.Identity,
                                                              bias=cb[:, ct:ct + 1], scale=1.0), rd=[PB[cbk], t_small], wr=[t_ybf])
                        op(act, lambda e, ct=ct: e.activation(out=ysq[:, ct, :], in_=ps[:, cbk, :], func=AF.Square,
                                                              bias=cb[:, ct:ct + 1], scale=1.0), rd=[PB[cbk], t_small], wr=[t_ysq])
                    for ct in range(4):
                        op(pe, lambda e, ct=ct: e.matmul(ps[:, 4, :], lhsT=onesm[:], rhs=ybf[:, ct, :],
                                                         start=(ct == 0), stop=(ct == 3)), rd=[t_ybf, t_ident], wr=[PB[4]])
                    for ct in range(4):
                        op(pe, lambda e, ct=ct: e.matmul(ps[:, 5, :], lhsT=onesm[:], rhs=ysq[:, ct, :],
                                                         start=(ct == 0), stop=(ct == 3)), rd=[t_ysq, t_ident], wr=[PB[5]])
                    op(act, lambda e: e.activation(out=lnA, in_=ps[:, 4, :], func=AF.Copy), rd=[PB[4]], wr=[t_lnA])
                    op(dve, lambda e: e.tensor_tensor(out=lnB, in0=lnA, in1=lnA, op=ALU.mult), rd=[t_lnA], wr=[t_lnB])
                    op(dve, lambda e: e.tensor_tensor(out=lnB, in0=ps[:, 5, :], in1=lnB, op=ALU.subtract),
                       rd=[PB[5], t_lnB], wr=[t_lnB])
                    op(dve, lambda e: e.tensor_scalar(out=lnB, in0=lnB, scalar1=0.0, scalar2=EPS, op0=ALU.max, op1=ALU.add),
                       rd=[t_lnB], wr=[t_lnB])
                    op(act, lambda e: e.activation(out=lnB, in_=lnB, func=AF.Sqrt), rd=[t_lnB], wr=[t_lnB])
                    op(dve, lambda e: e.reciprocal(out=lnB, in_=lnB), rd=[t_lnB], wr=[t_lnB])
                    for ct in range(4):
                        op(dve, lambda e, ct=ct: e.scalar_tensor_tensor(out=zn, in0=ps[:, ct, :], scalar=cb[:, ct:ct + 1],
                                                                        in1=lnA, op0=ALU.add, op1=ALU.subtract),
                           rd=[PB[ct], t_small, t_lnA], wr=[t_zn])
                        op(dve, lambda e: e.tensor_tensor(out=zn, in0=zn, in1=lnB, op=ALU.mult),
                           rd=[t_zn, t_lnB], wr=[t_zn])
                        op(act, lambda e, ct=ct: e.activation(out=mixT[:, 4 + ct, :], in_=zn, func=AF.Silu,
                                                              bias=cbn[:, ct:ct + 1], scale=cg[:, ct:ct + 1]),
                           rd=[t_zn, t_small], wr=[t_mixT])

                    if j == nchunks - 1:
                        dump("mixTc", mixT[:, 4:8, :], [t_mixT], kb)
                        stop_if("p2b", kb)
                    def qgeom(qi):
                        segs = [(c * 512, 512) for c in range(2 * j + 1)] + [((2 * j + 1) * 512, (qi + 1) * 128)]
                        nkeys = (2 * j + 1) * 512 + (qi + 1) * 128
                        return segs, nkeys, slice(qi * 128, (qi + 1) * 128)

                    def stage_A(qi):
                        segs, nkeys, qcols = qgeom(qi)
                        op(pool, lambda e: e.tensor_tensor(
                            out=Dg[:], in0=ident[:].unsqueeze(1).to_broadcast([128, 8, 128]),
                            in1=wsc[:, qi, :].unsqueeze(2).to_broadcast([128, 8, 128]), op=ALU.mult),
                           rd=[t_ident, t_wsc], wr=[t_Dg])
                        units = [(si, h) for si in range(len(segs)) for h in range(8)]
                        U = len(units)

                        def emit_L(u):
                            si, h = units[u]
                            c0, n = segs[si]
                            b, g = h // 2, h % 2
                            bk = u % 4
                            op(pe, lambda e: e.matmul(ps[:, bk, 0:n], lhsT=qiT[:, b, g, qcols],
                                                      rhs=kiT[:, c0:c0 + n], start=True, stop=True),
                               rd=[t_qiT, t_kiT], wr=[PB[bk]])
                            r = Rb[u % 8]
                            if u % 2 == 0:
                                op(act, lambda e: e.activation(out=r[:, 0:n], in_=ps[:, bk, 0:n], func=AF.Relu),
                                   rd=[PB[bk]], wr=[t_Rb[u % 8]])
                            else:
                                op(dve, lambda e: e.tensor_scalar(out=r[:, 0:n], in0=ps[:, bk, 0:n], scalar1=0.0, scalar2=None,
                                                                  op0=ALU.max), rd=[PB[bk]], wr=[t_Rb[u % 8]])

                        def emit_D(u):
                            si, h = units[u]
                            c0, n = segs[si]
                            sbk = 4 + (si % 2)
                            op(pe, lambda e: e.matmul(ps[:, sbk, 0:n], lhsT=Dg[:, h, :], rhs=Rb[u % 8][:, 0:n],
                                                      start=(h == 0), stop=(h == 7)),
                               rd=[t_Dg, t_Rb[u % 8]], wr=[PB[sbk]])
                            if h == 7:
                                if si == 2 * j:
                                    op(act, lambda e: e.activation(out=SC[:, c0:c0 + n], in_=ps[:, sbk, 0:n], func=AF.Identity,
                                                                   bias=oflg[:, j:j + 1], scale=1.0),
                                       rd=[PB[sbk], t_small], wr=[t_SC])
                                elif si == 2 * j + 1:
                                    if n > 128:
                                        op(act, lambda e: e.activation(out=SC[:, c0:c0 + n - 128], in_=ps[:, sbk, 0:n - 128],
                                                                       func=AF.Copy), rd=[PB[sbk]], wr=[t_SC])
                                    op(dve, lambda e: e.tensor_tensor(out=SC[:, c0 + n - 128:c0 + n], in0=ps[:, sbk, n - 128:n],
                                                                      in1=trim[:], op=ALU.add), rd=[PB[sbk], t_ident], wr=[t_SC])
                                else:
                                    op(act, lambda e: e.activation(out=SC[:, c0:c0 + n], in_=ps[:, sbk, 0:n], func=AF.Copy),
                                       rd=[PB[sbk]], wr=[t_SC])

                        for u in range(U + 4):
                            if u < U:
                                emit_L(u)
                            if u >= 4:
                                emit_D(u - 4)

                    def stage_B(qi, frac, jk=None, jk_t=None):
                        segs, nkeys, qcols = qgeom(qi)
                        na = min(int(frac * nkeys) // 128 * 128, JA)
                        scv = SC[:, 0:nkeys]
                        br = 12.0 if j == 0 else BR
                        nbis = 12 if j == 0 else NBIS
                        op(dve, lambda e: e.tensor_reduce(out=bst[:, 0:1], in_=scv, axis=AX.X, op=ALU.max), rd=[t_SC], wr=[t_bst])
                        op(dve, lambda e: e.tensor_scalar(out=bst[:, 1:2], in0=bst[:, 0:1], scalar1=-br / 2, scalar2=None,
                                                          op0=ALU.add), rd=[t_bst], wr=[t_bst])
                        for it in range(nbis):
                            if na > 0:
                                op(act, lambda e: e.activation(out=junkA[:, 0:na], in_=SC[:, 0:na], func=AF.Sign,
                                                               bias=bst[:, 1:2], scale=-1.0, accum_out=bsa[:, 0:1]),
                                   rd=[t_SC, t_bst], wr=[t_junkA, t_bsa])
                            jk_ = junk if jk is None else jk
                            jkt_ = t_hTc if jk is None else jk_t
                            op(dve, lambda e: e.tensor_scalar(out=jk_[:, na:nkeys], in0=SC[:, na:nkeys], scalar1=bst[:, 1:2],
                                                              scalar2=None, op0=ALU.is_gt, op1=ALU.add, accum_out=bst[:, 2:3]),
                               rd=[t_SC, t_bst], wr=jkt_ + [t_bst])
                            if na > 0:
                                op(dve, lambda e: e.scalar_tensor_tensor(out=bst[:, 2:3], in0=bsa[:, 0:1], scalar=-0.5,
                                                                         in1=bst[:, 2:3], op0=ALU.mult, op1=ALU.add),
                                   rd=[t_bsa, t_bst], wr=[t_bst])
                            last = (it == nbis - 1)
                            cn = (br / 2) / (2 ** it) if last else (br / 2) / (2 ** (it + 1))
                            op(dve, lambda e: e.tensor_scalar(out=bst[:, 3:4], in0=bst[:, 2:3], scalar1=255.5 - na / 2.0,
                                                              scalar2=(cn if last else 2.0 * cn), op0=ALU.is_gt, op1=ALU.mult),
                               rd=[t_bst], wr=[t_bst])
                            op(dve, lambda e: e.scalar_tensor_tensor(out=bst[:, 1:2], in0=bst[:, 3:4], scalar=-cn,
                                                                     in1=bst[:, 1:2], op0=ALU.add, op1=ALU.add),
                               rd=[t_bst], wr=[t_bst])
                            yield
                        if j == nchunks - 1 and qi == 3:
                            dump("SC", SC[:, 0:nkeys], [t_SC], kb)
                            dump("bst", bst[:, 0:4], [t_bst], kb)
                            stop_if("p2c", kb)
                        op(dve, lambda e: e.tensor_scalar(out=MB[:, 0:nkeys], in0=scv, scalar1=bst[:, 1:2], scalar2=NEG,
                                                          op0=ALU.is_le, op1=ALU.mult), rd=[t_SC, t_bst], wr=[t_MB])

                    def stage_C_main(qi):
                        segs, nkeys, qcols = qgeom(qi)
                        nsb = nkeys // 128
                        U = nsb * 2
                        LAG = 3

                        def emit_S(u):
                            sbi, g = u // 2, u % 2
                            bk = u % 4
                            op(pe, lambda e: e.matmul(ps[:, bk, :], lhsT=kT[:, sbi * 128:(sbi + 1) * 128],
                                                      rhs=qT[:, g, :, qcols], start=True, stop=False),
                               rd=[t_kT, t_qT], wr=[PB[bk]])
                            op(pe, lambda e: e.matmul(ps[:, bk, :], lhsT=MB[:, sbi * 128:(sbi + 1) * 128],
                                                      rhs=ident4[:], start=False, stop=True),
                               rd=[t_MB, t_ident], wr=[PB[bk]])
                            op(act, lambda e: e.activation(out=PT[u % 4][:], in_=ps[:, bk, :], func=AF.Exp, scale=0.125),
                               rd=[PB[bk]], wr=[t_PT[u % 4]])

                        def emit_V(u):
                            sbi, g = u // 2, u % 2
                            pt = PT[u % 4]
                            ob = 4 + g
                            for b in range(4):
                                op(pe, lambda e, b=b: e.matmul(ps[:, ob, b * 65:(b + 1) * 65], lhsT=pt[:, b * 128:(b + 1) * 128],
                                                               rhs=Vaug[:, sbi, g, :], start=(sbi == 0 and b == 0),
                                                               stop=(sbi == nsb - 1 and b == 3), skip_group_check=True),
                                   rd=[t_PT[u % 4], t_V], wr=[PB[ob]])

                        for u in range(U + LAG):
                            if u < U:
                                emit_S(u)
                            if u >= LAG:
                                emit_V(u - LAG)
                            yield

                    def stage_C_tail(qi):
                        segs, nkeys, qcols = qgeom(qi)
                        for g in range(2):
                            ov = ps[:, 4 + g, 0:260].rearrange("p (b e) -> p b e", e=65)
                            op(dve, lambda e: e.reciprocal(out=rs4[:, g * 4:(g + 1) * 4].unsqueeze(2), in_=ov[:, :, 64:65]),
                               rd=[PB[4 + g]], wr=[t_rs4])
                            op(dve, lambda e: e.tensor_tensor(
                                out=attn[:, g * 256:(g + 1) * 256].rearrange("p (b d) -> p b d", d=64), in0=ov[:, :, 0:64],
                                in1=rs4[:, g * 4:(g + 1) * 4].unsqueeze(2).to_broadcast([128, 4, 64]), op=ALU.mult),
                               rd=[PB[4 + g], t_rs4], wr=[t_attn])
                        if j == nchunks - 1 and qi == 3:
                            dump("attn", attn[:], [t_attn], kb)
                            stop_if("p2d", kb)
                        pv = psb16(6 + qi % 2)
                        for f in range(4):
                            op(pe, lambda e, f=f: e.transpose(out=pv[:, f * 128:(f + 1) * 128], in_=attn[:, f * 128:(f + 1) * 128],
                                                              identity=ident[:]), rd=[t_attn, t_ident], wr=[PB[6 + qi % 2]])
                        op(act, lambda e: e.activation(out=mixT[:, 0:4, qcols], in_=pv[:, 0:512].rearrange("p (f t) -> p f t", f=4),
                                                       func=AF.Copy), rd=[PB[6 + qi % 2]], wr=[t_mixT])

                    def interleave(gb, gc, nb):
                        csteps = list(range(gc[1]))
                        per = (len(csteps) + nb - 1) // nb if nb else 0
                        gcg, gbg = gc[0], gb
                        for it in range(nb):
                            next(gbg, None)
                            for _ in range(per):
                                next(gcg, None)
                        for _ in gbg:
                            pass
                        for _ in gcg:
                            pass

                    def csteps_of(qi):
                        return (qgeom(qi)[1] // 128) * 2 + 2

                    wq_box["w"] = wload(wview(w_in, 0, 512))
                    stage_A(0)
                    for _ in stage_B(0, 0.55, jk=MB[:].bitcast(U8), jk_t=[t_MB]):
                        next(q_units, None)
                        next(q_units, None)
                    for _ in q_units:
                        pass
                    for qi in range(1, 4):
                        stage_A(qi)
                        interleave(stage_B(qi, 0.06), (stage_C_main(qi - 1), csteps_of(qi - 1)), 12 if j == 0 else NBIS)
                        stage_C_tail(qi - 1)
                    ng = norm_stages(j + 1) if j + 1 < nchunks else iter(())
                    per = max(1, csteps_of(3) // 22)
                    for i_, _ in enumerate(stage_C_main(3)):
                        if i_ % per == per - 1:
                            next(ng, None)
                    for _ in ng:
                        pass
                    stage_C_tail(3)

                    wo0, two0 = wload(wview(w_out, 0, 512))
                    wo1, two1 = wload(wview(w_out, 512, 1024))
                    for t4 in range(4):
                        for half, (wo, two) in enumerate(((wo0, two0), (wo1, two1))):
                            bk = half
                            for f in range(8):
                                op(pe, lambda e, f=f: e.matmul(ps[:, bk, :], lhsT=mixT[:, f, t4 * 128:(t4 + 1) * 128], rhs=wo[:, f, :],
                                                               start=(f == 0), stop=(f == 7)), rd=[t_mixT, two], wr=[PB[bk]])
                        s2 = t4 % 2
                        dma(sp, xts[s2][:], xp[(tile0 + t4) * 128:(tile0 + t4 + 1) * 128, :], wr=[t_xts[s2]])
                        op(dve, lambda e: e.tensor_tensor(out=x1t, in0=ps[:, 0:2, :].rearrange("p a b -> p (a b)"), in1=G1[:],
                                                          op=ALU.mult), rd=[PB[0], PB[1], t_G1], wr=[t_x1t])
                        op(pool, lambda e: e.tensor_tensor(out=x1t, in0=x1t, in1=xts[s2][:], op=ALU.add),
                           rd=[t_x1t, t_xts[s2]], wr=[t_x1t])
                        r0 = (j * 4 + t4) * 128
                        dma(sp, x1s[r0:r0 + 128, :], x1t, rd=[t_x1t])
                kb.barrier()
                stop_if("p2e", kb)

            with contextlib.ExitStack() as es2:
                Wup = sb("Wup", [128, 8, 4096], BF16); t_Wup = T()
                Wdn = sb("Wdn", [128, 32, 1024], BF16); t_Wdn = T()
                GF = sb("GF", [128, D], F32); t_GF = T()
                xts = [sb("m_xt%d" % i, [128, D], F32) for i in range(4)]; t_xts = [T() for _ in range(4)]
                xns = [sb("m_xn%d" % i, [128, D], BF16) for i in range(4)]; t_xns = [T() for _ in range(4)]
                sqj = None; t_sqj = None
                G2 = sb("G2", [128, D], F32)
                dma(sp, G2[:], g2s, rd=[t_G2], wr=[t_G2])
                sts = [sb("m_st%d" % i, [128, 4], F32) for i in range(4)]; t_sts = [T() for _ in range(4)]
                h2T = [sb("h2T%d" % i, [128, 8, 256], BF16) for i in range(2)]; t_h2T = [[T(), T()], [T(), T()]]
                rT = [sb("rT%d" % i, [128, 256], BF16) for i in range(2)]; t_rT = [T(), T()]
                uT = sb("uT", [128, 32, 256], BF16); t_uT = T()
                x2 = sb("x2", [128, D], F32); t_x2 = T()
                oo = x2; t_oo = t_x2
                for c4 in range(8):
                    dma(pool, Wup[:, :, c4 * 512:(c4 + 1) * 512], wview(w_up, c4 * 512, (c4 + 1) * 512), wr=[t_Wup])
                for c4 in range(4):
                    dma(pool, Wdn[:, c4 * 8:(c4 + 1) * 8, :],
                        w_down[c4 * 1024:(c4 + 1) * 1024, :].rearrange("(k p) e -> p k e", p=128), wr=[t_Wdn])
                dma(sp, GF[:], gfb, wr=[t_GF])
                def m_pre_norm(gi):
                    for t2 in range(2):
                        sl = (gi % 2) * 2 + t2
                        r0 = (gi * 2 + t2) * 128
                        norm_tile(r0, xts[sl], t_xts[sl], xns[sl], t_xns[sl], sqj, t_sqj, sts[sl], t_sts[sl], src=x1s)

                def m_pre_T(gi):
                    for t2 in range(2):
                        sl = (gi % 2) * 2 + t2
                        transpose_mod(xns[sl], t_xns[sl], 6 + t2, h2T[gi % 2][:, :, t2 * 128:(t2 + 1) * 128], t_h2T[gi % 2][t2], 2)

                def m_up(gi):
                    hh = h2T[gi % 2]
                    for ff in range(32):
                        bk = ff % 4
                        for k in range(8):
                            op(pe, lambda e, k=k: e.matmul(ps[:, bk, 0:256], lhsT=Wup[:, k, ff * 128:(ff + 1) * 128], rhs=hh[:, k, :],
                                                           start=(k == 0), stop=(k == 7)), rd=[t_Wup] + t_h2T[gi % 2], wr=[PB[bk]])
                        r = rT[ff % 2]
                        op(act, lambda e: e.activation(out=r[:], in_=ps[:, bk, 0:256], func=AF.Relu), rd=[PB[bk]], wr=[t_rT[ff % 2]])
                        op(pool, lambda e, ff=ff: e.tensor_tensor(out=uT[:, ff, :], in0=r[:], in1=r[:], op=ALU.mult),
                           rd=[t_rT[ff % 2]], wr=[t_uT])
                        if ff == 8 and gi + 1 < 16:
                            m_pre_norm(gi + 1)

                def m_down(gi):
                    for t2 in range(2):
                        sl = (gi % 2) * 2 + t2
                        for half in range(2):
                            bk = 4 + half
                            for ff in range(32):
                                op(pe, lambda e, ff=ff: e.matmul(ps[:, bk, :], lhsT=uT[:, ff, t2 * 128:(t2 + 1) * 128],
                                                                 rhs=Wdn[:, ff, half * 512:(half + 1) * 512],
                                                                 start=(ff == 0), stop=(ff == 31)), rd=[t_uT, t_Wdn], wr=[PB[bk]])
                        op(dve, lambda e: e.tensor_tensor(out=x2[:], in0=ps[:, 4:6, :].rearrange("p a b -> p (a b)"), in1=G2[:],
                                                          op=ALU.mult), rd=[PB[4], PB[5], t_G2], wr=[t_x2])
                        op(pool, lambda e: e.tensor_tensor(out=x2[:], in0=x2[:], in1=xts[sl][:], op=ALU.add),
                           rd=[t_x2, t_xts[sl]], wr=[t_x2])
                        st = sts[sl]
                        op(act, lambda e: e.activation(out=xns[sl][:], in_=x2[:], func=AF.Square, accum_out=st[:, 0:1]),
                           rd=[t_x2], wr=[t_xns[sl], t_sts[sl]])
                        op(act, lambda e: e.activation(out=st[:, 1:2], in_=st[:, 0:1], func=AF.Sqrt, bias=EPS, scale=1.0 / D),
                           rd=[t_sts[sl]], wr=[t_sts[sl]])
                        op(dve, lambda e: e.reciprocal(out=st[:, 2:3], in_=st[:, 1:2]), rd=[t_sts[sl]], wr=[t_sts[sl]])
                        op(dve, lambda e: e.scalar_tensor_tensor(out=oo[:], in0=x2[:], scalar=st[:, 2:3], in1=GF[:],
                                                                 op0=ALU.mult, op1=ALU.mult), rd=[t_x2, t_sts[sl], t_GF], wr=[t_oo])
                        r0 = (gi * 2 + t2) * 128
                        dma(sp, out[r0:r0 + 128, :], oo[:], rd=[t_oo])

                m_pre_norm(0)
                m_pre_T(0)
                for gi in range(16):
                    m_up(gi)
                    if gi + 1 < 16:
                        m_pre_T(gi + 1)
                    m_down(gi)
                kb.barrier()

    try:
        _body()
    except _Stop:
        pass
    return nc


_NC_CACHE = {}


def _layout_inputs(x, c, positions, w_ada, b_ada, g_mix, w_in, conv_w, conv_b, conv_norm_g, conv_norm_b,
                   w_out, g_mlp, w_up, w_down, g_final):
    f32 = np.float32
    x = np.asarray(x, f32); c = np.asarray(c, f32); positions = np.asarray(positions, np.int32)

    def col(v, n):
        return np.ascontiguousarray(np.asarray(v, f32).reshape(n, 128).T)
    shared = {
        "w_ada": np.ascontiguousarray(np.asarray(w_ada, f32)[0]),
        "badac": col(np.asarray(b_ada)[0], 48),
        "badar": np.ascontiguousarray(np.asarray(b_ada, f32)[0][None, :]),
        "gmixc": col(np.asarray(g_mix)[0], 8),
        "gmlpc": col(np.asarray(g_mlp)[0], 8),
        "w_in": np.ascontiguousarray(np.asarray(w_in, f32)[0]),
        "convw": np.ascontiguousarray(np.asarray(conv_w, f32)[0].T.reshape(4, 128, 31).transpose(1, 0, 2)),
        "convb": col(np.asarray(conv_b)[0], 4),
        "cng": col(np.asarray(conv_norm_g)[0], 4),
        "cnb": col(np.asarray(conv_norm_b)[0], 4),
        "w_out": np.ascontiguousarray(np.asarray(w_out, f32)[0]),
        "w_up": np.ascontiguousarray(np.asarray(w_up, f32)[0]),
        "w_down": np.ascontiguousarray(np.asarray(w_down, f32)[0]),
        "gfb": np.ascontiguousarray(np.broadcast_to(np.asarray(g_final, f32)[None, :], (128, D))),
        "invf": np.ascontiguousarray(np.broadcast_to(
            np.power(f32(500000.0), -np.arange(8, dtype=f32) * f32(2.0) / f32(16.0)).astype(f32)[None, :], (128, 8))),
    }
    in_maps = []
    for core in range(8):
        b, p = core // 2, core % 2
        own, oth = OWN[p], OWN[1 - p]
        rows = []
        for j in range(8):
            rows.append(np.arange(oth[j] * 512, oth[j] * 512 + 512))
            rows.append(np.arange(own[j] * 512, own[j] * 512 + 512))
        rows = np.concatenate(rows)
        xpa = np.zeros((NT * 128, D), f32)
        xpa[:S] = x[b][rows]
        pos = np.zeros((NT * 128,), np.int32)
        pos[:S] = positions[b][rows]
        hm = np.ones((256,), f32)
        for j in range(8):
            if own[j] == 0:
                hm[j * 32:(j + 1) * 32] = 0.0
            else:
                hr = np.arange(own[j] * 512 - 32, own[j] * 512)
                xpa[S + j * 32:S + (j + 1) * 32] = x[b][hr]
                pos[S + j * 32:S + (j + 1) * 32] = positions[b][hr]
        of = np.array([0.0 if oth[j] < own[j] else NEG for j in range(8)], f32)
        m = dict(shared)
        m["xp"] = xpa
        m["posp"] = np.ascontiguousarray(pos.reshape(NT, 128).T)
        m["oflag"] = np.ascontiguousarray(np.broadcast_to(of[None, :], (128, 8)))
        m["hmask"] = np.ascontiguousarray(np.broadcast_to(hm[None, :], (128, 256)))
        m["cT"] = col(c[b], 8)
        in_maps.append(m)
    return in_maps


def kernel(**inputs):
    in_maps = _layout_inputs(**inputs)
    if "nc" not in _NC_CACHE:
        _NC_CACHE["nc"] = build_program()
    nc = _NC_CACHE["nc"]
    res = run_bass_kernel_spmd(nc, in_maps, core_ids=list(range(8)))
    outf = np.zeros((4, S, D), np.float32)
    for core in range(8):
        b, p = core // 2, core % 2
        o = res.results[core]["out"]
        for j, ch in enumerate(OWN[p]):
            outf[b, ch * 512:(ch + 1) * 512] = o[j * 512:(j + 1) * 512]
    if DEBUG:
        kernel.debug = res.results
    return outf
```
